# Optimizing a Trainium2 kernel written in Bass

```python
import jax, jax.numpy as jnp
from jax import lax
import numpy as np

D_MODEL = 1024
BATCH = 1
SEQ = 16384
DEPTH = 2

GRID_W = 64
CTX_LEN = 256
CHUNK = 128
Q_BLOCK = 128
EPS = 1e-6
A_GROUPS = 4
A_WIDTH = D_MODEL // 4
B_BLOCKS = 4
B_WIDTH = D_MODEL // 4
CONV_W = 4
LRU_C = 8.0
C_NOPE = 128
C_ROPE = 64
C_V = 128
C_HEADS = (D_MODEL // 2) // C_V
C_WIDTH = C_HEADS * C_V
Q_LORA = 384
KV_LORA = 256
ROPE_BASE = 10000.0
ATTN_SCALE = (C_NOPE + C_ROPE) ** -0.5
MIX_WIDTH = A_WIDTH + B_WIDTH + C_WIDTH
IN_SPLITS = (2 * A_WIDTH, 2 * A_WIDTH + B_WIDTH, 2 * A_WIDTH + 2 * B_WIDTH, 2 * A_WIDTH + 2 * B_WIDTH + Q_LORA, 2 * A_WIDTH + 2 * B_WIDTH + Q_LORA + KV_LORA)
IN_COLS = IN_SPLITS[-1] + C_ROPE
N_EXPERTS = 16
EXPERT_FF = 1024
EC_CAPACITY = 2

kernel_name = 'hybrid_gmlp_rglru_mla_ecmoe_dit'


def rmsnorm(x, g):
    xf = x.astype(jnp.float32)
    y = xf * lax.rsqrt(jnp.mean(xf * xf, axis=-1, keepdims=True) + EPS)
    return (y * g.astype(jnp.float32)).astype(x.dtype)


def modulate(h, shift, scale):
    return h * (1 + scale) + shift


def rope_tables(n):
    rows = n // GRID_W
    r = jnp.repeat(jnp.arange(rows), GRID_W).astype(jnp.float32)
    col = jnp.tile(jnp.arange(GRID_W), rows).astype(jnp.float32)
    nf = C_ROPE // 4
    inv = ROPE_BASE ** (-jnp.arange(nf, dtype=jnp.float32) / nf)
    ang = jnp.concatenate([r[:, None] * inv, col[:, None] * inv], axis=-1)
    return jnp.cos(ang), jnp.sin(ang)


def apply_rope(x, cos, sin):
    shp = x.shape
    nf = C_ROPE // 4
    xf = x.astype(jnp.float32).reshape(*shp[:-1], 2, 2, nf)
    cs = cos.reshape(cos.shape[0], 1, 2, nf)
    sn = sin.reshape(sin.shape[0], 1, 2, nf)
    x1 = xf[..., 0, :]
    x2 = xf[..., 1, :]
    out = jnp.stack([x1 * cs - x2 * sn, x2 * cs + x1 * sn], axis=-2).reshape(shp)
    return out.astype(x.dtype)


def chunk_sgu(z, w_s, b_s):
    u, v = jnp.split(z, 2, axis=-1)
    vf = v.astype(jnp.float32)
    v = (vf * lax.rsqrt(jnp.mean(vf * vf, axis=-1, keepdims=True) + EPS)).astype(z.dtype)
    b, t, _ = v.shape
    vb = v.reshape(b, t // CHUNK, CHUNK, A_GROUPS, A_WIDTH // A_GROUPS)
    mixed = jnp.einsum('gpq,bnqgc->bnpgc', w_s, vb) + b_s.T[:, :, None]
    return u * mixed.reshape(b, t, A_WIDTH)


def dwconv(x, w, b):
    t = x.shape[1]
    left = CONV_W // 2
    xp = jnp.pad(x, ((0, 0), (left, CONV_W - 1 - left), (0, 0)))
    y = b
    for k in range(CONV_W):
        y = y + xp[:, k:k + t] * w[k]
    return y


def blockdiag(x, w, b):
    xb = x.reshape(*x.shape[:-1], B_BLOCKS, B_WIDTH // B_BLOCKS)
    return jnp.einsum('btnc,ncd->btnd', xb, w).reshape(x.shape) + b


def lru_coeffs(xc, wa, ba, wx, bx, lam):
    r = jax.nn.sigmoid(blockdiag(xc, wa, ba)).astype(jnp.float32)
    i = jax.nn.sigmoid(blockdiag(xc, wx, bx)).astype(jnp.float32)
    log_a = -LRU_C * r * jax.nn.softplus(-lam.astype(jnp.float32))
    a = jnp.exp(log_a)
    mult = jnp.sqrt(jnp.maximum(-jnp.expm1(2 * log_a), 0.0))
    return a, mult * (i * xc.astype(jnp.float32))


def linear_scan(a, b, reverse):
    def comb(e1, e2):
        a1, b1 = e1
        a2, b2 = e2
        return a1 * a2, a2 * b1 + b2
    return lax.associative_scan(comb, (a, b), reverse=reverse, axis=1)


def rglru_mixer(xb_l, gb_l, xb_c, gb_c, lp, need_ctx_out):
    cl = dwconv(xb_l, lp['conv_w'], lp['conv_b'])
    cc = dwconv(xb_c, lp['conv_w'], lp['conv_b'])
    hl_dirs = []
    hc_dirs = []
    for d in range(2):
        rev = d == 1
        a_c, b_c = lru_coeffs(cc, lp['lru_wa'][d], lp['lru_ba'][d], lp['lru_wx'][d], lp['lru_bx'][d], lp['lru_lambda'][d])
        _, hc = linear_scan(a_c, b_c, rev)
        h0 = hc[:, 0] if rev else hc[:, -1]
        a_l, b_l = lru_coeffs(cl, lp['lru_wa'][d], lp['lru_ba'][d], lp['lru_wx'][d], lp['lru_bx'][d], lp['lru_lambda'][d])
        acum, bcum = linear_scan(a_l, b_l, rev)
        hl_dirs.append(acum * h0[:, None] + bcum)
        hc_dirs.append(hc)
    yl = (hl_dirs[0] + hl_dirs[1]).astype(xb_l.dtype) * jax.nn.gelu(gb_l)
    yc = (hc_dirs[0] + hc_dirs[1]).astype(xb_c.dtype) * jax.nn.gelu(gb_c) if need_ctx_out else None
    return yl, yc


def mla_q(cq, g, w_uq, rope):
    b, t, _ = cq.shape
    q = (rmsnorm(cq, g) @ w_uq).reshape(b, t, C_HEADS, C_NOPE + C_ROPE)
    if rope is not None:
        q = jnp.concatenate([q[..., :C_NOPE], apply_rope(q[..., C_NOPE:], rope[0], rope[1])], axis=-1)
    return q


def mla_kv(ckv, kr, g, w_ukv, rope):
    b, t, _ = ckv.shape
    kv = (rmsnorm(ckv, g) @ w_ukv).reshape(b, t, C_HEADS, C_NOPE + C_V)
    k_nope, v = kv[..., :C_NOPE], kv[..., C_NOPE:]
    kr = kr[:, :, None, :]
    if rope is not None:
        kr = apply_rope(kr, rope[0], rope[1])
    k = jnp.concatenate([k_nope, jnp.broadcast_to(kr, (b, t, C_HEADS, C_ROPE))], axis=-1)
    return k, v


def attend(q, k, v):
    s = jnp.einsum('bqhd,bkhd->bhqk', q, k, preferred_element_type=jnp.float32) * ATTN_SCALE
    p = jax.nn.softmax(s, axis=-1).astype(v.dtype)
    return jnp.einsum('bhqk,bkhd->bqhd', p, v)


def latent_attention(q, k, v):
    b, t, h, dk = q.shape
    qb = q.reshape(b, t // Q_BLOCK, Q_BLOCK, h, dk).swapaxes(0, 1)
    ob = lax.map(lambda qq: attend(qq, k, v), qb)
    return ob.swapaxes(0, 1).reshape(b, t, h * v.shape[-1])


def ec_moe(h, w_router, w_gate, w_up, w_down):
    b, t, d = h.shape
    cap = EC_CAPACITY * t // N_EXPERTS
    logits = jnp.einsum('btd,de->bte', h, w_router, preferred_element_type=jnp.float32)
    probs = jax.nn.softmax(logits, axis=-1)
    g, idx = lax.top_k(jnp.swapaxes(probs, 1, 2), cap)
    xs = jax.vmap(lambda hb, ib: hb[ib])(h, idx)
    hid = jax.nn.silu(jnp.einsum('becd,edf->becf', xs, w_gate)) * jnp.einsum('becd,edf->becf', xs, w_up)
    ye = jnp.einsum('becf,efd->becd', hid, w_down) * g[..., None].astype(h.dtype)
    return jax.vmap(lambda yb, ib: jnp.zeros((t, d), yb.dtype).at[ib.reshape(-1)].add(yb.reshape(-1, d)))(ye, idx)


def layer(xl, xc, c, c_ctx, lp, rope, last):
    mod_l = (jax.nn.silu(c) @ lp['w_mod'] + lp['b_mod'])[:, None, :]
    mod_c = (jax.nn.silu(c_ctx) @ lp['w_mod'] + lp['b_mod'])[None, None, :]
    sh1_l, sc1_l, g1_l, sh2_l, sc2_l, g2_l = jnp.split(mod_l, 6, axis=-1)
    sh1_c, sc1_c, g1_c, sh2_c, sc2_c, g2_c = jnp.split(mod_c, 6, axis=-1)
    hl = modulate(rmsnorm(xl, lp['norm1_g']), sh1_l, sc1_l)
    hc = modulate(rmsnorm(xc, lp['norm1_g']), sh1_c, sc1_c)
    za_l, xb_l, gb_l, cq_l, ckv_l, kr_l = jnp.split(hl @ lp['w_in'], IN_SPLITS, axis=-1)
    za_c, xb_c, gb_c, cq_c, ckv_c, kr_c = jnp.split(hc @ lp['w_in'], IN_SPLITS, axis=-1)
    kc, vc = mla_kv(ckv_c, kr_c, lp['kv_norm_g'], lp['w_ukv'], None)
    kl, vl = mla_kv(ckv_l, kr_l, lp['kv_norm_g'], lp['w_ukv'], rope)
    ql = mla_q(cq_l, lp['q_norm_g'], lp['w_uq'], rope)
    yc_l = latent_attention(ql, jnp.concatenate([kc, kl], axis=1), jnp.concatenate([vc, vl], axis=1))
    yb_l, yb_c = rglru_mixer(xb_l, gb_l, xb_c, gb_c, lp, not last)
    ya_l = chunk_sgu(jax.nn.gelu(za_l), lp['sgu_w'], lp['sgu_b'])
    xl = xl + g1_l * (jnp.concatenate([ya_l, yb_l, yc_l], axis=-1) @ lp['w_out'])
    h2l = modulate(rmsnorm(xl, lp['norm2_g']), sh2_l, sc2_l)
    xl = xl + g2_l * ec_moe(h2l, lp['w_router'], lp['w_gate'], lp['w_up'], lp['w_down'])
    if not last:
        b = xc.shape[0]
        qc = mla_q(cq_c, lp['q_norm_g'], lp['w_uq'], None)
        yc_c = attend(qc, kc, vc).reshape(b, xc.shape[1], C_WIDTH)
        ya_c = chunk_sgu(jax.nn.gelu(za_c), lp['sgu_w'], lp['sgu_b'])
        xc = xc + g1_c * (jnp.concatenate([ya_c, yb_c, yc_c], axis=-1) @ lp['w_out'])
        h2c = modulate(rmsnorm(xc, lp['norm2_g']), sh2_c, sc2_c)
        xc = xc + g2_c * ec_moe(h2c, lp['w_router'], lp['w_gate'], lp['w_up'], lp['w_down'])
    return xl, xc


def setup_inputs(seed: int = 0) -> dict:
    key = jax.random.key(seed)
    ks = jax.random.split(key, 32)
    L, D = DEPTH, D_MODEL
    f32 = jnp.float32

    def nrm(k, shape, s):
        return jax.random.normal(k, shape, f32) * s

    def gain(k, shape):
        return 1.0 + nrm(k, shape, 0.01)

    u = jax.random.uniform(ks[16], (L, 2, B_WIDTH), f32, minval=0.9, maxval=0.999)
    base = u ** (1.0 / LRU_C)
    lru_lambda = jnp.log(base) - jnp.log1p(-base)
    bw = B_WIDTH // B_BLOCKS
    return {
        'x': nrm(ks[0], (BATCH, SEQ, D), 1.0),
        'c': nrm(ks[1], (BATCH, D), 1.0),
        'ctx': nrm(ks[2], (BATCH, CTX_LEN, D), 1.0),
        'c_ctx': nrm(ks[3], (D,), 1.0),
        'norm1_g': gain(ks[4], (L, D)),
        'w_mod': nrm(ks[5], (L, D, 6 * D), 0.5 * D ** -0.5),
        'b_mod': nrm(ks[6], (L, 6 * D), 0.01),
        'w_in': nrm(ks[7], (L, D, IN_COLS), D ** -0.5),
        'sgu_w': nrm(ks[8], (L, A_GROUPS, CHUNK, CHUNK), 0.5 * CHUNK ** -0.5),
        'sgu_b': 1.0 + nrm(ks[9], (L, A_GROUPS, CHUNK), 0.01),
        'conv_w': nrm(ks[10], (L, CONV_W, B_WIDTH), CONV_W ** -0.5),
        'conv_b': nrm(ks[11], (L, B_WIDTH), 0.01),
        'lru_wa': nrm(ks[12], (L, 2, B_BLOCKS, bw, bw), bw ** -0.5),
        'lru_ba': nrm(ks[13], (L, 2, B_WIDTH), 0.01),
        'lru_wx': nrm(ks[14], (L, 2, B_BLOCKS, bw, bw), bw ** -0.5),
        'lru_bx': nrm(ks[15], (L, 2, B_WIDTH), 0.01),
        'lru_lambda': lru_lambda,
        'q_norm_g': gain(ks[17], (L, Q_LORA)),
        'w_uq': nrm(ks[18], (L, Q_LORA, C_HEADS * (C_NOPE + C_ROPE)), Q_LORA ** -0.5),
        'kv_norm_g': gain(ks[19], (L, KV_LORA)),
        'w_ukv': nrm(ks[20], (L, KV_LORA, C_HEADS * (C_NOPE + C_V)), KV_LORA ** -0.5),
        'w_out': nrm(ks[21], (L, MIX_WIDTH, D), MIX_WIDTH ** -0.5),
        'norm2_g': gain(ks[22], (L, D)),
        'w_router': nrm(ks[23], (L, D, N_EXPERTS), D ** -0.5),
        'w_gate': nrm(ks[24], (L, N_EXPERTS, D, EXPERT_FF), D ** -0.5),
        'w_up': nrm(ks[25], (L, N_EXPERTS, D, EXPERT_FF), D ** -0.5),
        'w_down': nrm(ks[26], (L, N_EXPERTS, EXPERT_FF, D), EXPERT_FF ** -0.5),
        'final_norm_g': gain(ks[27], (D,)),
    }


def reference(x, c, ctx, c_ctx, norm1_g, w_mod, b_mod, w_in, sgu_w, sgu_b, conv_w, conv_b, lru_wa, lru_ba, lru_wx, lru_bx, lru_lambda, q_norm_g, w_uq, kv_norm_g, w_ukv, w_out, norm2_g, w_router, w_gate, w_up, w_down, final_norm_g):
    rope = rope_tables(x.shape[1])
    xl, xc = x, ctx
    for i in range(DEPTH):
        lp = {
            'norm1_g': norm1_g[i], 'w_mod': w_mod[i], 'b_mod': b_mod[i], 'w_in': w_in[i],
            'sgu_w': sgu_w[i], 'sgu_b': sgu_b[i], 'conv_w': conv_w[i], 'conv_b': conv_b[i],
            'lru_wa': lru_wa[i], 'lru_ba': lru_ba[i], 'lru_wx': lru_wx[i], 'lru_bx': lru_bx[i],
            'lru_lambda': lru_lambda[i], 'q_norm_g': q_norm_g[i], 'w_uq': w_uq[i],
            'kv_norm_g': kv_norm_g[i], 'w_ukv': w_ukv[i], 'w_out': w_out[i], 'norm2_g': norm2_g[i],
            'w_router': w_router[i], 'w_gate': w_gate[i], 'w_up': w_up[i], 'w_down': w_down[i],
        }
        xl, xc = layer(xl, xc, c, c_ctx, lp, rope, i == DEPTH - 1)
    return rmsnorm(xl, final_norm_g)
```

```python
import numpy as np
import ml_dtypes
from contextlib import ExitStack
import concourse.bass as bass
import concourse.mybir as mybir
from concourse.bass_utils import run_bass_kernel_spmd

F32 = mybir.dt.float32
BF16 = mybir.dt.bfloat16
AF = mybir.ActivationFunctionType
ALU = mybir.AluOpType

NCORES = 8
D = 1024
SEQ = 16384
CTX = 256
TALL = SEQ + CTX
NLOC = SEQ // NCORES
NL = NLOC + CTX
EPS = 1e-6
NEXP = 16
ATTN_SCALE = 192.0 ** -0.5
ENGS = ['pe', 'act', 'dve', 'pool', 'sp']


def _prod(s):
    r = 1
    for v in s:
        r *= v
    return r


class Prog:
    def __init__(self, nc, ndma=12):
        self.nc = nc
        self.ops = []
        self.lastw = {}
        self.readers = {}
        self.ndma = ndma

    capture = None

    def begin_capture(self):
        self.capture = []

    def end_capture(self):
        c, self.capture = self.capture, None
        return c

    def replay_interleaved(self, lists, nway=2):
        L = max(len(l) for l in lists)
        stride = (L + nway - 1) // nway
        items = []
        for i, l in enumerate(lists):
            for k, call in enumerate(l):
                items.append((i * stride + k, i, call))
        items.sort(key=lambda t: (t[0], t[1]))
        for _, _, call in items:
            self.op(*call)

    def op(self, eng, fn, reads=(), writes=(), dma=False):
        if self.capture is not None:
            self.capture.append((eng, fn, tuple(reads), tuple(writes), dma))
            return -1
        i = len(self.ops)
        deps = set()
        for k in reads:
            if k in self.lastw:
                deps.add(self.lastw[k])
        for k in writes:
            if k in self.lastw:
                deps.add(self.lastw[k])
            deps.update(self.readers.get(k, ()))
        for k in reads:
            self.readers.setdefault(k, []).append(i)
        for k in writes:
            self.lastw[k] = i
            self.readers[k] = []
        self.ops.append(dict(eng=eng, fn=fn, deps=deps, dma=dma))
        return i

    def fence(self):
        self.ops.append(dict(eng=None, fence=True))
        self.lastw = {}
        self.readers = {}

    def mm(self, out, lhsT, rhs, start, stop, reads, writes):
        self.op('pe', lambda e: e.matmul(out, lhsT, rhs, start=start, stop=stop), reads, writes)

    def tr(self, out, in_, ident, reads, writes):
        self.op('pe', lambda e: e.transpose(out, in_, ident), reads, writes)

    def act(self, out, in_, func, reads, writes, bias=None, scale=None, accum=None):
        kw = {}
        if bias is not None:
            kw['bias'] = bias
        if scale is not None:
            kw['scale'] = scale
        if accum is not None:
            kw['accum_out'] = accum
        self.op('act', lambda e: e.activation(out=out, in_=in_, func=func, **kw), reads, writes)

    def dma(self, out, in_, reads, writes, eng='sp'):
        self.op(eng, lambda e: e.dma_start(out=out, in_=in_), reads, writes, dma=True)

    def finalize(self):
        self.fence()
        nc, ops, ndma = self.nc, self.ops, self.ndma
        need = set()
        last = {}
        for i, o in enumerate(ops):
            if o.get('fence'):
                for e, j in last.items():
                    need.add(j)
                continue
            for d in o['deps']:
                Dd = ops[d]
                if Dd['dma']:
                    continue
                if Dd['eng'] == 'pe' and o['eng'] == 'pe' and not o['dma']:
                    continue
                need.add(d)
            if not o['dma']:
                last[o['eng']] = i
        cnt = {e: 0 for e in ENGS}
        dcnt = [0] * ndma
        di = 0
        for i, o in enumerate(ops):
            if o.get('fence'):
                o['snap_cnt'] = dict(cnt)
                o['snap_d'] = list(dcnt)
                continue
            if o['dma']:
                j = di % ndma
                di += 1
                o['dsem'] = j
                o['dprev'] = dcnt[j]
                dcnt[j] += 16
                o['dval'] = dcnt[j]
            elif i in need:
                cnt[o['eng']] += 1
                o['sig'] = cnt[o['eng']]
        self.n_ops = len(ops)
        with ExitStack() as es:
            esem = {e: es.enter_context(nc.semaphore("s_" + e)) for e in ENGS}
            dsem = [es.enter_context(nc.semaphore("d_%d" % j)) for j in range(ndma)]
            block = es.enter_context(nc.Block())

            def emit(ename):
                def body(eng):
                    waited = {}

                    def wait(key, sem, val):
                        if val > waited.get(key, 0):
                            eng.wait_ge(sem, val)
                            waited[key] = val
                    for i, o in enumerate(ops):
                        if o.get('fence'):
                            for e2 in ENGS:
                                if o['snap_cnt'][e2] > 0:
                                    wait(e2, esem[e2], o['snap_cnt'][e2])
                            for j in range(ndma):
                                if o['snap_d'][j] > 0:
                                    wait(('d', j), dsem[j], o['snap_d'][j])
                            continue
                        if o['eng'] != ename:
                            continue
                        for d in sorted(o['deps']):
                            Dd = ops[d]
                            if Dd['dma']:
                                wait(('d', Dd['dsem']), dsem[Dd['dsem']], Dd['dval'])
                            else:
                                if Dd['eng'] == 'pe' and ename == 'pe' and not o['dma']:
                                    continue
                                wait(Dd['eng'], esem[Dd['eng']], Dd['sig'])
                        if o['dma'] and o['dprev'] > 0:
                            wait(('d', o['dsem']), dsem[o['dsem']], o['dprev'])
                        ins = o['fn'](eng)
                        if o['dma']:
                            ins.then_inc(dsem[o['dsem']], 16)
                        elif 'sig' in o:
                            ins.then_inc(esem[ename], 1)
                return body
            block.tensor(emit('pe'))
            block.scalar(emit('act'))
            block.vector(emit('dve'))
            block.gpsimd(emit('pool'))
            block.sync(emit('sp'))


class Arena:
    def __init__(self, t, width):
        self.t = t
        self.top = 0
        self.width = width
        self.peak = 0

    def _shape(self, v, shape):
        if len(shape) == 1:
            return v
        if len(shape) == 2:
            return v.rearrange("p (a b) -> p a b", a=shape[0])
        if len(shape) == 3:
            return v.rearrange("p (a b c) -> p a b c", a=shape[0], b=shape[1])
        raise ValueError

    def f32(self, *shape):
        n = _prod(shape)
        a = self.top
        self.top += n
        self.peak = max(self.peak, self.top)
        assert self.top <= self.width, ("arena overflow", self.top, self.width)
        return self._shape(self.t[:, a:a + n], shape)

    def bf(self, *shape):
        n = _prod(shape)
        nw = (n + 1) // 2
        a = self.top
        self.top += nw
        self.peak = max(self.peak, self.top)
        assert self.top <= self.width, ("arena overflow", self.top, self.width)
        v = self.t[:, a:a + nw].bitcast(BF16)
        return self._shape(v[:, 0:n], shape)

    def mark(self):
        return self.top

    def release(self, m):
        self.top = m


class Ctx:
    def __init__(self, nc, P, A, ps, psb):
        self.nc, self.P, self.A, self.ps, self.psb = nc, P, A, ps, psb
        self.psi = 0
        self.alt = 0
        self.uid = 0

    def key(self, s):
        self.uid += 1
        return "%s#%d" % (s, self.uid)

    ps_range = (0, 6)

    def next_ps(self, lo=None, hi=None):
        if lo is None:
            lo, hi = self.ps_range
        i = lo + self.psi % (hi - lo)
        self.psi += 1
        return i

    def ew(self):
        self.alt += 1
        return 'pool' if self.alt % 4 == 0 else 'dve'

    def load_cast(self, dst_bf, src_dram, ncols, nk, name, scale_col=None, stage=None):
        P = self.P
        for k in range(nk):
            st, sk = stage[k % 2]
            P.dma(st[:, 0:ncols], src_dram[:, k, :], reads=[], writes=[sk])
            e = self.ew()
            if scale_col is None:
                P.op(e, lambda en, st=st, k=k: en.tensor_copy(out=dst_bf[:, k, :], in_=st[:, 0:ncols]),
                     reads=[sk], writes=[name])
            else:
                P.op(e, lambda en, st=st, k=k: en.tensor_scalar(out=dst_bf[:, k, :], in0=st[:, 0:ncols],
                                                                scalar1=scale_col[:, k:k + 1], scalar2=None,
                                                                op0=ALU.mult),
                     reads=[sk, 'modv'], writes=[name])

    def sumsq_rstd(self, src_f32, nk, n, dim, out_rstd, ones_bf, sq_bf, rk, name):
        P = self.P
        P.act(sq_bf[:, 0:nk, 0:n], src_f32, AF.Square, reads=rk, writes=[name + '_sq'])
        pi = self.next_ps()
        pk = 'ps%d' % pi
        for k in range(nk):
            P.mm(self.ps[pi][:, 0:n], ones_bf, sq_bf[:, k, 0:n], k == 0, k == nk - 1,
                 reads=[name + '_sq', 'consts'], writes=[pk])
        P.op('dve', lambda e: e.tensor_scalar(out=out_rstd[:, 0:n], in0=self.ps[pi][:, 0:n], scalar1=1.0 / dim,
                                              scalar2=EPS, op0=ALU.mult, op1=ALU.add),
             reads=[pk], writes=[name + '_rstd'])
        P.act(out_rstd[:, 0:n], out_rstd[:, 0:n], AF.Sqrt, reads=[name + '_rstd'], writes=[name + '_rstd'])
        P.op('dve', lambda e: e.reciprocal(out=out_rstd[:, 0:n], in_=out_rstd[:, 0:n]),
             reads=[name + '_rstd'], writes=[name + '_rstd'])

    def norm_mod(self, x_f32, n, rstd, s_col, sh_col, out, tmp_f32, rk, wk, name):
        P = self.P
        for k in range(8):
            e = self.ew()
            tk = "%s_tmp%d" % (name, k % 2)
            t = tmp_f32[k % 2]
            P.op(e, lambda en, k=k, t=t: en.tensor_tensor(out=t[:, 0:n], in0=x_f32[:, k, 0:n], in1=rstd[:, 0:n],
                                                          op=ALU.mult),
                 reads=rk + [name + '_rstd'], writes=[tk])
            P.op(e, lambda en, k=k, t=t: en.tensor_scalar(out=out[:, k, 0:n], in0=t[:, 0:n],
                                                          scalar1=s_col[:, k:k + 1], scalar2=sh_col[:, k:k + 1],
                                                          op0=ALU.mult, op1=ALU.add),
                 reads=[tk, 'modv'], writes=wk)

    def compute_mods(self, d_cvec, d_wmod, d_bmod, nblk, modT, cs, stage):
        P = self.P
        P.dma(cs[:, 0:16], d_cvec.rearrange("p k w -> p (k w)"), reads=[], writes=['cs'])
        P.act(cs[:, 16:32], cs[:, 0:16], AF.Sigmoid, reads=['cs'], writes=['cs2'])
        P.op('dve', lambda e: e.tensor_tensor(out=cs[:, 0:16], in0=cs[:, 0:16], in1=cs[:, 16:32], op=ALU.mult),
             reads=['cs2', 'cs'], writes=['cs'])
        P.dma(cs[:, 32:32 + nblk * 8], d_bmod, reads=[], writes=['bmod'])
        csv = cs[:, 0:16].rearrange("p (k w) -> p k w", k=8)
        nj = nblk * 8
        for b in range(nblk):
            P.dma(stage[:, :, :], d_wmod[:, :, b * 1024:(b + 1) * 1024], reads=[], writes=['wm_st'])
            for j in range(8):
                c0 = 2 * (b * 8 + j)
                for k in range(8):
                    P.mm(self.ps[6][:, c0:c0 + 2], stage[:, k, j * 128:(j + 1) * 128],
                         csv[:, k, :], k == 0, k == 7, reads=['wm_st', 'cs'], writes=['ps6'])
        psv = self.ps[6][:, 0:2 * nj].rearrange("p (j w) -> p j w", w=2)
        for w in range(2):
            P.op('dve', lambda e, w=w: e.tensor_tensor(out=modT[:, 0:nj, w], in0=psv[:, :, w],
                                                       in1=cs[:, 32:32 + nj], op=ALU.add),
                 reads=['ps6', 'bmod'], writes=['modv'])


def build_AB():
    nc = bass.Bass("TRN2", target_bir_lowering=False)

    def din(name, shape, dt=F32):
        return nc.dram_tensor(name, list(shape), dt, kind="ExternalInput").ap()
    d_xall = din("xall", [128, 8, TALL])
    d_xloc = din("xloc", [128, 8, NLOC])
    d_cvec = din("cvec", [128, 8, 2])
    d_wmod = din("wmod", [128, 8, 5120])
    d_bmod = din("bmod", [128, 40])
    d_vecs = din("vecs", [128, 64])
    d_wA = din("wA", [128, 8, 640])
    d_wB = din("wB", [128, 8, 1152])
    d_wuq = din("wuq", [128, 3, 1024])
    d_wukv = din("wukv", [128, 2, 1024])
    d_wout = din("wout", [128, 8, 1024])
    d_lruw = din("lruw", [128, 8, 128])
    d_sguw = din("sguw", [128, 4, 128])
    d_sgub = din("sgub", [128, 2, 128])
    d_wr = din("wr", [128, 8, 16])
    d_ropeC = din("ropeC", [64, SEQ])
    d_ropeS = din("ropeS", [64, SEQ])
    d_ropeCl = din("ropeCl", [64, NLOC])
    d_ropeSl = din("ropeSl", [64, NLOC])
    d_ident = din("ident", [128, 128])
    o_x1 = nc.dram_tensor("x1T", [128, 8, NL], F32, kind="ExternalOutput").ap()
    o_probs = nc.dram_tensor("probs", [NL, NEXP], F32, kind="ExternalOutput").ap()
    s_xb = nc.dram_tensor("xb_s", [2, 128, TALL], F32).ap()
    s_KT = nc.dram_tensor("KT_s", [4, 128, TALL], BF16).ap()
    s_kr = nc.dram_tensor("kr_s", [64, TALL], BF16).ap()
    s_V = nc.dram_tensor("V_s", [4, 128, TALL // 128, 128], BF16).ap()

    AW = 51 * 1024
    with ExitStack() as es:
        arena_t = es.enter_context(nc.sbuf_tensor("arena", [128, AW], F32))
        ps = [es.enter_context(nc.psum_tensor("ps%d" % i, [128, 512], F32)) for i in range(7)]
        psb = es.enter_context(nc.psum_tensor("psb", [128, 1024], BF16))
        P = Prog(nc)
        A = Arena(arena_t, AW)
        C = Ctx(nc, P, A, ps, psb)

        vecs = A.f32(64)
        modT = A.f32(40, 2)
        cs = A.f32(80)
        sv = A.f32(6, 8, 2)
        spv = A.f32(4)
        ident = A.bf(128)
        ones = A.bf(128)
        onesf = A.f32(128)
        m0 = A.mark()
        P.dma(vecs, d_vecs, [], ['vecs'])
        P.op('pool', lambda e: e.memset(onesf, 1.0), [], ['onesf'])
        stage8 = A.f32(8, 1024)
        C.compute_mods(d_cvec, d_wmod, d_bmod, 5, modT, cs, stage8)
        P.dma(stage8[:, 0, 0:128], d_ident, [], ['wm_st'])
        P.op('dve', lambda e: e.tensor_copy(out=ident, in_=stage8[:, 0, 0:128]), ['wm_st'], ['consts'])
        P.op('dve', lambda e: e.memset(ones, 1.0), [], ['consts'])
        n1g, n2g = vecs[:, 0:8], vecs[:, 8:16]
        lrub, lam, convw, convb = vecs[:, 16:24], vecs[:, 24:28], vecs[:, 28:36], vecs[:, 36:38]
        qng, kvng, cmask = vecs[:, 38:41], vecs[:, 41:43], vecs[:, 43:51]
        mv = modT.rearrange("p (b k) w -> p b k w", b=5)
        for w in range(2):
            for (dst, scb, g) in ((0, 1, n1g), (3, 4, n2g)):
                P.op('dve', lambda e, w=w, dst=dst, scb=scb, g=g: e.scalar_tensor_tensor(
                    out=sv[:, dst, :, w], in0=mv[:, scb, :, w], scalar=1.0, in1=g, op0=ALU.add, op1=ALU.mult),
                    ['modv', 'vecs'], ['svt'])
            for (dst, src) in ((1, 0), (2, 2), (4, 3)):
                P.op('dve', lambda e, w=w, dst=dst, src=src: e.tensor_copy(out=sv[:, dst, :, w], in_=mv[:, src, :, w]),
                     ['modv'], ['svt'])
        P.act(spv, lam, AF.Exp, ['vecs'], ['spv'], scale=-1.0)
        P.act(spv, spv, AF.Ln, ['spv'], ['spv'], bias=1.0)
        P.op('dve', lambda e: e.tensor_single_scalar(out=spv, in_=spv, scalar=-8.0, op=ALU.mult), ['spv'], ['spv'])
        P.fence()
        A.release(m0)

        def svc(idx, w):
            return sv[:, idx, :, w]

        m1 = A.mark()
        wA_bf = A.bf(8, 640)
        wukv_bf = A.bf(2, 1024)
        wst = [(A.f32(1024), 'wst0'), (A.f32(1024), 'wst1')]

        def p1set():
            return dict(xs=A.f32(8, 512), sq=A.bf(8, 512), rstd=A.f32(512), tmpf=[A.f32(512), A.f32(512)],
                        h=A.bf(8, 512), xo=A.f32(2, 512), ckv=A.f32(2, 512), ckv_sq=A.bf(2, 512), rstd2=A.f32(512),
                        ckvn=A.bf(2, 512), Ko=A.bf(4, 512), Vo=A.bf(4, 4, 128), krf=A.f32(2, 512), rC=A.f32(512),
                        rS=A.f32(512), ko=A.bf(512))
        B1 = [p1set(), p1set()]
        X3 = [B1[0]['xs'], B1[1]['xs'], A.f32(8, 512)]
        C.load_cast(wukv_bf, d_wukv, 1024, 2, 'wukv', stage=wst)
        C.load_cast(wA_bf, d_wA, 640, 8, 'wA', scale_col=None, stage=wst)
        blocks = [(0, CTX, 1)] + [(CTX + 512 * i, 512, 0) for i in range(SEQ // 512)]
        caps = []
        for bi, (t0, n, w) in enumerate(blocks):
            B = B1[bi % 2]
            sx = str(bi % 2)
            C.ps_range = (0, 3) if bi % 2 == 0 else (3, 6)
            if bi == 0:
                for b2 in range(2):
                    t02, n2, _ = blocks[b2]
                    P.dma(X3[b2][:, :, 0:n2], d_xall[:, :, t02:t02 + n2], [], ['xst%d' % b2])
            P.begin_capture()
            xs, xk = X3[bi % 3], 'xst%d' % (bi % 3)
            if bi + 2 < len(blocks):
                t02, n2, _ = blocks[bi + 2]
                P.dma(X3[(bi + 2) % 3][:, :, 0:n2], d_xall[:, :, t02:t02 + n2], [], ['xst%d' % ((bi + 2) % 3)])
            C.sumsq_rstd(xs[:, :, 0:n], 8, n, D, B['rstd'], ones, B['sq'], [xk], 'n1' + sx)
            C.norm_mod(xs, n, B['rstd'], svc(0, w), svc(1, w), B['h'], B['tmpf'], [xk], ['h' + sx], 'n1' + sx)
            h_bf, xo, ckv, krf, ckvn = B['h'], B['xo'], B['ckv'], B['krf'], B['ckvn']
            for ct in range(6):
                M = 128 if ct < 4 else 64
                c0 = ct * 128 if ct < 4 else 512 + (ct - 4) * 64
                pi = C.next_ps()
                pk = 'ps%d' % pi
                for k in range(8):
                    P.mm(ps[pi][0:M, 0:n], wA_bf[:, k, c0:c0 + M], h_bf[:, k, 0:n], k == 0, k == 7,
                         ['wA', 'h' + sx], [pk])
                if ct < 2:
                    P.act(xo[:, ct, 0:n], ps[pi][:, 0:n], AF.Copy, [pk], ['xbo' + sx])
                elif ct < 4:
                    P.op('dve', lambda e, pi=pi, ct=ct, n=n, ckv=ckv: e.tensor_copy(out=ckv[:, ct - 2, 0:n],
                                                                                 in_=ps[pi][:, 0:n]),
                         [pk], ['ckv' + sx])
                else:
                    P.op('dve', lambda e, pi=pi, ct=ct, n=n, krf=krf: e.tensor_copy(out=krf[0:64, ct - 4, 0:n],
                                                                                 in_=ps[pi][0:64, 0:n]),
                         [pk], ['krf' + sx])
            P.dma(s_xb[:, :, t0:t0 + n].rearrange("c p t -> p c t"), xo[:, :, 0:n], ['xbo' + sx], ['s_xb%d' % bi])
            C.sumsq_rstd(ckv[:, :, 0:n], 2, n, 256, B['rstd2'], ones, B['ckv_sq'], ['ckv' + sx], 'kvn' + sx)
            for k in range(2):
                P.op('dve', lambda e, k=k, n=n, ckv=ckv, B=B: e.tensor_tensor(out=ckv[:, k, 0:n], in0=ckv[:, k, 0:n],
                                                                           in1=B['rstd2'][:, 0:n], op=ALU.mult),
                     ['ckv' + sx, 'kvn' + sx + '_rstd'], ['ckv' + sx])
                P.act(ckvn[:, k, 0:n], ckv[:, k, 0:n], AF.Copy, ['ckv' + sx, 'vecs'], ['ckvn' + sx],
                      scale=kvng[:, k:k + 1])
            Ko, Kk = B['Ko'], 'Ko' + sx
            for h in range(4):
                pi = C.next_ps()
                pk = 'ps%d' % pi
                for k in range(2):
                    P.mm(ps[pi][:, 0:n], wukv_bf[:, k, h * 128:(h + 1) * 128], ckvn[:, k, 0:n], k == 0, k == 1,
                         ['wukv', 'ckvn' + sx], [pk])
                P.act(Ko[:, h, 0:n], ps[pi][:, 0:n], AF.Copy, [pk], [Kk])
            P.dma(s_KT[:, :, t0:t0 + n].rearrange("h p t -> p h t"), Ko[:, :, 0:n], [Kk], ['s_KT%d' % bi])
            Vo, Vk = B['Vo'], 'Vo' + sx
            for tt in range(n // 128):
                pi = C.next_ps()
                pk = 'ps%d' % pi
                for k in range(2):
                    P.mm(ps[pi][:, 0:512], ckvn[:, k, tt * 128:(tt + 1) * 128], wukv_bf[:, k, 512:1024], k == 0, k == 1,
                         ['wukv', 'ckvn' + sx], [pk])
                P.op('dve', lambda e, pi=pi, tt=tt, Vo=Vo: e.tensor_copy(
                    out=Vo[:, :, tt, :], in_=ps[pi][:, 0:512].rearrange("p (h c) -> p h c", h=4)), [pk], [Vk])
            nt = n // 128
            for h in range(4):
                P.dma(s_V[h, :, t0 // 128:t0 // 128 + nt, :], Vo[:, h, 0:nt, :], [Vk], ['s_V%d_%d' % (bi, h)])
            ko, kk = B['ko'], 'kro' + sx
            if w == 0:
                l0 = t0 - CTX
                rC, rS = B['rC'], B['rS']
                P.dma(rC[0:64, 0:n], d_ropeC[:, l0:l0 + n], [], ['rC' + sx])
                P.dma(rS[0:64, 0:n], d_ropeS[:, l0:l0 + n], [], ['rS' + sx])
                P.op('pool', lambda e, n=n, krf=krf, rC=rC: e.tensor_tensor(out=krf[0:64, 0, 0:n], in0=krf[0:64, 0, 0:n],
                                                                          in1=rC[0:64, 0:n], op=ALU.mult),
                     ['krf' + sx, 'rC' + sx], ['krf' + sx])
                P.op('pool', lambda e, n=n, krf=krf, rS=rS: e.tensor_tensor(out=krf[0:64, 1, 0:n], in0=krf[0:64, 1, 0:n],
                                                                          in1=rS[0:64, 0:n], op=ALU.mult),
                     ['krf' + sx, 'rS' + sx], ['krf' + sx])
                P.op('pool', lambda e, n=n, ko=ko, krf=krf: e.tensor_tensor(out=ko[0:64, 0:n], in0=krf[0:64, 0, 0:n],
                                                                          in1=krf[0:64, 1, 0:n], op=ALU.add),
                     ['krf' + sx], [kk])
            else:
                P.op('pool', lambda e, n=n, ko=ko, krf=krf: e.tensor_copy(out=ko[0:64, 0:n], in_=krf[0:64, 0, 0:n]),
                     ['krf' + sx], [kk])
            P.dma(s_kr[:, t0:t0 + n], ko[0:64, 0:n], [kk], ['s_kr%d' % bi])
            caps.append(P.end_capture())
        C.ps_range = (0, 6)
        P.replay_interleaved(caps, 2)
        P.fence()
        A.release(m1)

        mixT = A.bf(8, NL)
        qTn = A.bf(4, NL)
        qTr = A.bf(4, NL)
        mP = A.mark()
        ysum = A.f32(2, NL)
        m2 = A.mark()
        lruw_bf = A.bf(8, 128)
        C.load_cast(lruw_bf, d_lruw, 128, 8, 'lruw', stage=[(A.f32(128), 'lst0'), (A.f32(128), 'lst1')])
        NCH = 1024
        NCK = SEQ // NCH

        def p2set():
            return dict(xi=A.f32(NCH + 3), cl=A.f32(NCH), clb=A.bf(NCH), rr=A.f32(NCH), ii=A.f32(NCH), aa=A.f32(NCH),
                        t1=A.f32(NCH), t2=A.f32(NCH), hh=A.f32(NCH))
        B2 = [p2set(), p2set()]
        carry = A.f32(4)
        chunks = [(0, CTX, True, True, -1)] + [(CTX + NCH * j, NCH, j == 0, j == NCK - 1, j) for j in range(NCK)]
        CPC = NLOC // NCH
        ci = 0
        first_lat = {}
        caps2 = []
        for ct in range(2):
            for d in range(2):
                order = chunks if d == 0 else [chunks[0]] + chunks[:0:-1]
                cv = carry[:, ct * 2 + d:ct * 2 + d + 1]
                for qi, (t0, n, lz, rz, j) in enumerate(order):
                    B = B2[ci % 2]
                    sx = str(ci % 2)
                    C.ps_range = (0, 3) if ci % 2 == 0 else (3, 6)
                    ci += 1
                    P.begin_capture()
                    xi, cl, clb, rr, ii, aa, t1, t2, hh = (B['xi'], B['cl'], B['clb'], B['rr'], B['ii'], B['aa'], B['t1'],
                                                           B['t2'], B['hh'])
                    xk = 'xin' + sx
                    lo = 0 if lz else 2
                    ro = 0 if rz else 1
                    P.dma(xi[:, 2 - lo:2 + n + ro], s_xb[ct, :, t0 - lo:t0 + n + ro], [], [xk])
                    if lz:
                        P.op('pool', lambda e, xi=xi: e.memset(xi[:, 0:2], 0.0), [], [xk])
                    if rz:
                        P.op('pool', lambda e, xi=xi, n=n: e.memset(xi[:, n + 2:n + 3], 0.0), [], [xk])
                    P.op('dve', lambda e, xi=xi, n=n, ct=ct, cl=cl: e.tensor_scalar(
                        out=cl[:, 0:n], in0=xi[:, 0:n], scalar1=convw[:, ct * 4:ct * 4 + 1],
                        scalar2=convb[:, ct:ct + 1], op0=ALU.mult, op1=ALU.add), [xk, 'vecs'], ['cl' + sx])
                    for k in range(1, 4):
                        P.op('dve', lambda e, xi=xi, n=n, k=k, ct=ct, cl=cl: e.scalar_tensor_tensor(
                            out=cl[:, 0:n], in0=xi[:, k:k + n], scalar=convw[:, ct * 4 + k:ct * 4 + k + 1],
                            in1=cl[:, 0:n], op0=ALU.mult, op1=ALU.add), [xk, 'vecs', 'cl' + sx], ['cl' + sx])
                    P.act(clb[:, 0:n], cl[:, 0:n], AF.Copy, ['cl' + sx], ['clb' + sx])
                    for g, dst, dk in ((0, rr, 'rr' + sx), (1, ii, 'ii' + sx)):
                        wi = d * 4 + g * 2 + ct
                        for sb in range((n + 511) // 512):
                            nn = min(512, n - sb * 512)
                            pi = C.next_ps()
                            pk = 'ps%d' % pi
                            P.mm(ps[pi][:, 0:nn], lruw_bf[:, wi, :], clb[:, sb * 512:sb * 512 + nn], True, True,
                                 ['lruw', 'clb' + sx], [pk])
                            P.act(dst[:, sb * 512:sb * 512 + nn], ps[pi][:, 0:nn], AF.Sigmoid, [pk, 'vecs'], [dk],
                                  bias=lrub[:, wi:wi + 1])
                    P.act(aa[:, 0:n], rr[:, 0:n], AF.Exp, ['rr' + sx, 'spv'], ['aa' + sx],
                          scale=spv[:, d * 2 + ct:d * 2 + ct + 1])
                    P.op('pool', lambda e, n=n, t1=t1, aa=aa: e.tensor_tensor(out=t1[:, 0:n], in0=aa[:, 0:n],
                                                                            in1=aa[:, 0:n], op=ALU.mult),
                         ['aa' + sx], ['t1' + sx])
                    P.act(t1[:, 0:n], t1[:, 0:n], AF.Sqrt, ['t1' + sx], ['t1' + sx], scale=-1.0, bias=1.0)
                    P.op('pool', lambda e, n=n, t2=t2, ii=ii, cl=cl: e.tensor_tensor(out=t2[:, 0:n], in0=ii[:, 0:n],
                                                                                   in1=cl[:, 0:n], op=ALU.mult),
                         ['ii' + sx, 'cl' + sx], ['t2' + sx])
                    P.op('pool', lambda e, n=n, t2=t2, t1=t1: e.tensor_tensor(out=t2[:, 0:n], in0=t2[:, 0:n],
                                                                            in1=t1[:, 0:n], op=ALU.mult),
                         ['t2' + sx, 't1' + sx], ['t2' + sx])
                    init = 0.0 if qi == 0 else cv
                    if d == 0:
                        P.op('dve', lambda e, n=n, init=init, hh=hh, aa=aa, t2=t2: e.tensor_tensor_scan(
                            out=hh[:, 0:n], data0=aa[:, 0:n], data1=t2[:, 0:n], initial=init, op0=ALU.mult,
                            op1=ALU.add), ['aa' + sx, 't2' + sx, 'carry'], ['hh' + sx])
                        P.op('dve', lambda e, n=n, cv=cv, hh=hh: e.tensor_copy(out=cv, in_=hh[:, n - 1:n]),
                             ['hh' + sx], ['carry'])
                    else:
                        P.op('dve', lambda e, n=n, init=init, hh=hh, aa=aa, t2=t2: e.tensor_tensor_scan(
                            out=hh[:, 0:n][:, ::-1], data0=aa[:, 0:n][:, ::-1], data1=t2[:, 0:n][:, ::-1],
                            initial=init, op0=ALU.mult, op1=ALU.add), ['aa' + sx, 't2' + sx, 'carry'], ['hh' + sx])
                        P.op('dve', lambda e, cv=cv, hh=hh: e.tensor_copy(out=cv, in_=hh[:, 0:1]), ['hh' + sx], ['carry'])
                    if j < 0:
                        if d == 0:
                            P.op('pool', lambda e, n=n, ct=ct, hh=hh: e.tensor_copy(out=ysum[:, ct, 0:n], in_=hh[:, 0:n]),
                                 ['hh' + sx], ['ysum'])
                        else:
                            P.op('pool', lambda e, n=n, ct=ct, hh=hh: e.tensor_tensor(
                                out=ysum[:, ct, 0:n], in0=ysum[:, ct, 0:n], in1=hh[:, 0:n], op=ALU.add),
                                ['hh' + sx, 'ysum'], ['ysum'])
                    else:
                        jc, off = j // CPC, CTX + (j % CPC) * NCH
                        key = (ct, j % CPC)
                        if key not in first_lat:
                            first_lat[key] = True
                            P.op('dve', lambda e, n=n, jc=jc, off=off, ct=ct, hh=hh: e.tensor_scalar(
                                out=ysum[:, ct, off:off + n], in0=hh[:, 0:n], scalar1=cmask[:, jc:jc + 1], scalar2=None,
                                op0=ALU.mult), ['hh' + sx, 'vecs'], ['ysum'])
                        else:
                            P.op('dve', lambda e, n=n, jc=jc, off=off, ct=ct, hh=hh: e.scalar_tensor_tensor(
                                out=ysum[:, ct, off:off + n], in0=hh[:, 0:n], scalar=cmask[:, jc:jc + 1],
                                in1=ysum[:, ct, off:off + n], op0=ALU.mult, op1=ALU.add),
                                ['hh' + sx, 'vecs', 'ysum'], ['ysum'])
                    caps2.append(P.end_capture())
        C.ps_range = (0, 6)
        P.replay_interleaved(caps2, 2)
        P.fence()
        A.release(m2)

        wB_bf = A.bf(8, 1152)
        wuq_bf = A.bf(3, 1024)
        sguw_bf = A.bf(4, 128)
        sgub = A.f32(2, 128)
        m3 = A.mark()
        wst3 = [(A.f32(1152), 'wst3_0'), (A.f32(1152), 'wst3_1')]
        C.load_cast(wB_bf, d_wB, 1152, 8, 'wB', stage=wst3)
        C.load_cast(wuq_bf, d_wuq, 1024, 3, 'wuq', stage=wst3)
        C.load_cast(sguw_bf, d_sguw, 128, 4, 'sguw', stage=wst3)
        P.dma(sgub, d_sgub, [], ['sgub'])
        P.fence()
        A.release(m3)
        xs3 = A.f32(8, 512)
        sq3 = A.bf(8, 512)
        rstd3 = A.f32(512)
        tmp3 = [A.f32(512), A.f32(512)]
        h3 = A.bf(8, 512)
        u_bf = A.bf(2, 512)
        vf = A.f32(2, 512)
        vn_bf = A.bf(2, 512)
        vtok = A.bf(256)
        gbf = A.f32(2, 512)
        cqf = A.f32(3, 512)
        cqn = A.bf(3, 512)
        rstdq = A.f32(512)
        rstdv = A.f32(512)
        sqv = A.bf(2, 512)
        sqq = A.bf(3, 512)
        rCl = A.f32(512)
        rSl = A.f32(512)
        tq1 = A.f32(512)
        tq2 = A.f32(512)
        tg = A.f32(128)
        lblocks = [(d_xall[:, :, 0:CTX], CTX, 1, 0, -1)] + \
                  [(d_xloc[:, :, 512 * i:512 * (i + 1)], 512, 0, CTX + 512 * i, 512 * i) for i in range(NLOC // 512)]
        for (src, n, w, q0, l0) in lblocks:
            P.dma(xs3[:, :, 0:n], src, [], ['xs3'])
            C.sumsq_rstd(xs3[:, :, 0:n], 8, n, D, rstd3, ones, sq3, ['xs3'], 'n3')
            C.norm_mod(xs3, n, rstd3, svc(0, w), svc(1, w), h3, tmp3, ['xs3'], ['h3'], 'n3')
            for ct in range(9):
                pi = C.next_ps()
                pk = 'ps%d' % pi
                for k in range(8):
                    P.mm(ps[pi][:, 0:n], wB_bf[:, k, ct * 128:(ct + 1) * 128], h3[:, k, 0:n], k == 0, k == 7,
                         ['wB', 'h3'], [pk])
                if ct < 2:
                    P.act(u_bf[:, ct, 0:n], ps[pi][:, 0:n], AF.Gelu_apprx_tanh, [pk], ['u'])
                elif ct < 4:
                    P.act(vf[:, ct - 2, 0:n], ps[pi][:, 0:n], AF.Gelu_apprx_tanh, [pk], ['vf'])
                elif ct < 6:
                    P.act(gbf[:, ct - 4, 0:n], ps[pi][:, 0:n], AF.Gelu_apprx_tanh, [pk], ['gbf'])
                    P.op('dve', lambda e, ct=ct, n=n, q0=q0: e.tensor_tensor(
                        out=mixT[:, 2 + ct - 4, q0:q0 + n], in0=gbf[:, ct - 4, 0:n], in1=ysum[:, ct - 4, q0:q0 + n],
                        op=ALU.mult), ['gbf', 'ysum'], ['mix_b'])
                else:
                    P.op('dve', lambda e, ct=ct, n=n, pi=pi: e.tensor_copy(out=cqf[:, ct - 6, 0:n], in_=ps[pi][:, 0:n]),
                         [pk], ['cqf'])
            C.sumsq_rstd(vf[:, :, 0:n], 2, n, 256, rstdv, ones, sqv, ['vf'], 'vn')
            for k in range(2):
                P.op('dve', lambda e, k=k, n=n: e.tensor_tensor(out=vn_bf[:, k, 0:n], in0=vf[:, k, 0:n],
                                                                in1=rstdv[:, 0:n], op=ALU.mult),
                     ['vf', 'vn_rstd'], ['vn'])
            for tt in range(n // 128):
                for k in range(2):
                    P.tr(psb[:, k * 128:(k + 1) * 128], vn_bf[:, k, tt * 128:(tt + 1) * 128], ident,
                         ['vn', 'consts'], ['psb'])
                P.op('dve', lambda e: e.tensor_copy(out=vtok, in_=psb[:, 0:256]), ['psb'], ['vtok'])
                pi = C.next_ps()
                pk = 'ps%d' % pi
                for g in range(4):
                    P.mm(ps[pi][(g % 2) * 64:(g % 2) * 64 + 64, (g // 2) * 128:(g // 2) * 128 + 128],
                         vtok[:, g * 64:(g + 1) * 64], sguw_bf[:, g, :], True, True, ['vtok', 'sguw'], [pk])
                for c2 in range(2):
                    P.op('dve', lambda e, c2=c2, pi=pi: e.tensor_tensor(out=tg, in0=ps[pi][:, c2 * 128:(c2 + 1) * 128],
                                                                        in1=sgub[:, c2, :], op=ALU.add),
                         [pk, 'sgub'], ['tg'])
                    P.op('dve', lambda e, c2=c2, tt=tt, q0=q0: e.tensor_tensor(
                        out=mixT[:, c2, q0 + tt * 128:q0 + (tt + 1) * 128], in0=tg,
                        in1=u_bf[:, c2, tt * 128:(tt + 1) * 128], op=ALU.mult), ['tg', 'u'], ['mix_a'])
            C.sumsq_rstd(cqf[:, :, 0:n], 3, n, 384, rstdq, ones, sqq, ['cqf'], 'qn')
            for k in range(3):
                P.op('dve', lambda e, k=k, n=n: e.tensor_tensor(out=cqf[:, k, 0:n], in0=cqf[:, k, 0:n],
                                                                in1=rstdq[:, 0:n], op=ALU.mult),
                     ['cqf', 'qn_rstd'], ['cqf'])
                P.act(cqn[:, k, 0:n], cqf[:, k, 0:n], AF.Copy, ['cqf', 'vecs'], ['cqn'], scale=qng[:, k:k + 1])
            if w == 0:
                P.dma(rCl[0:64, 0:n], d_ropeCl[:, l0:l0 + n], [], ['rCl'])
                P.dma(rSl[0:64, 0:n], d_ropeSl[:, l0:l0 + n], [], ['rSl'])
            for h in range(4):
                pi = C.next_ps()
                pk = 'ps%d' % pi
                for k in range(3):
                    P.mm(ps[pi][:, 0:n], wuq_bf[:, k, h * 256:h * 256 + 128], cqn[:, k, 0:n], k == 0, k == 2,
                         ['wuq', 'cqn'], [pk])
                P.act(qTn[:, h, q0:q0 + n], ps[pi][:, 0:n], AF.Copy, [pk], ['qTn'])
                pa = C.next_ps()
                pak = 'ps%d' % pa
                for k in range(3):
                    P.mm(ps[pa][0:64, 0:n], wuq_bf[:, k, h * 256 + 128:h * 256 + 192], cqn[:, k, 0:n], k == 0, k == 2,
                         ['wuq', 'cqn'], [pak])
                if w == 1:
                    P.act(qTr[0:64, h, q0:q0 + n], ps[pa][0:64, 0:n], AF.Copy, [pak], ['qTr'])
                else:
                    pb = C.next_ps()
                    pbk = 'ps%d' % pb
                    for k in range(3):
                        P.mm(ps[pb][0:64, 0:n], wuq_bf[:, k, h * 256 + 192:h * 256 + 256], cqn[:, k, 0:n], k == 0,
                             k == 2, ['wuq', 'cqn'], [pbk])
                    P.op('dve', lambda e, pa=pa, n=n: e.tensor_tensor(out=tq1[0:64, 0:n], in0=ps[pa][0:64, 0:n],
                                                                      in1=rCl[0:64, 0:n], op=ALU.mult),
                         [pak, 'rCl'], ['tq1'])
                    P.op('dve', lambda e, pb=pb, n=n: e.tensor_tensor(out=tq2[0:64, 0:n], in0=ps[pb][0:64, 0:n],
                                                                      in1=rSl[0:64, 0:n], op=ALU.mult),
                         [pbk, 'rSl'], ['tq2'])
                    P.op('pool', lambda e, h=h, n=n, q0=q0: e.tensor_tensor(
                        out=qTr[0:64, h, q0:q0 + n], in0=tq1[0:64, 0:n], in1=tq2[0:64, 0:n], op=ALU.add),
                        ['tq1', 'tq2'], ['qTr'])
        P.fence()
        A.release(mP)

        NKT = TALL // 128
        KT = A.bf(TALL)
        Vh = A.bf(NKT, 128)
        krT = A.bf(TALL)
        PT = [A.bf(512) for _ in range(6)]
        rinv = A.f32(512)
        lacc = [[A.f32(512) for _ in range(3)] for _ in range(2)]
        NPC = 5
        KPP = NKT // NPC
        for pc in range(NPC):
            a, b = pc * KPP * 128, (pc + 1) * KPP * 128
            P.dma(krT[0:64, a:b], s_kr[:, a:b], ['s_kr'], ['kr_p%d' % pc])
        qblocks = [(0, CTX, 2)] + [(CTX + 512 * i, 512, NKT) for i in range(NLOC // 512)]

        def load_kv(h, pc):
            a, b = pc * KPP * 128, (pc + 1) * KPP * 128
            P.dma(KT[:, a:b], s_KT[h, :, a:b], ['s_KT'], ['KT_p%d' % pc])
            P.dma(Vh[:, pc * KPP:(pc + 1) * KPP, :], s_V[h, :, pc * KPP:(pc + 1) * KPP, :], ['s_V'], ['V_p%d' % pc])
        tiles = []
        qbi = 0
        for h in range(4):
            for qi, (q0, n, nkt) in enumerate(qblocks):
                po, pl = 3 + qbi % 2, 5 + qbi % 2
                qbi += 1
                for kt in range(nkt):
                    tiles.append((h, qi, q0, n, nkt, kt, po, pl))

        def emit_qk(i):
            h, qi, q0, n, nkt, kt, po, pl = tiles[i]
            sb = i % 3
            sk = 'ps%d' % sb
            pc = kt // KPP
            P.mm(ps[sb][:, 0:n], KT[:, kt * 128:(kt + 1) * 128], qTn[:, h, q0:q0 + n], True, False,
                 ['KT_p%d' % pc, 'qTn'], [sk])
            P.mm(ps[sb][:, 0:n], krT[0:64, kt * 128:(kt + 1) * 128], qTr[0:64, h, q0:q0 + n], False, True,
                 ['kr_p%d' % pc, 'qTr'], [sk])
        LA = 2
        for pc in range(NPC):
            load_kv(0, pc)
        for i in range(LA):
            emit_qk(i)
        for i, (h, qi, q0, n, nkt, kt, po, pl) in enumerate(tiles):
            if i + LA < len(tiles):
                emit_qk(i + LA)
            sb = i % 3
            pc = kt // KPP
            pt = PT[i % 6]
            ptk = 'PT%d' % (i % 6)
            P.act(pt[:, 0:n], ps[sb][:, 0:n], AF.Exp, ['ps%d' % sb], [ptk], scale=ATTN_SCALE)
            P.mm(ps[po][:, 0:n], Vh[:, kt, :], pt[:, 0:n], kt == 0, kt == nkt - 1, ['V_p%d' % pc, ptk], ['ps%d' % po])
            c3 = kt % 3
            la, lak = lacc[po - 3][c3], 'lacc%d_%d' % (po - 3, c3)
            le = 'pool' if c3 == 2 else 'dve'
            if kt < 3:
                P.op(le, lambda e, la=la, pt=pt, n=n: e.tensor_copy(out=la[:, 0:n], in_=pt[:, 0:n]), [ptk], [lak])
            else:
                P.op(le, lambda e, la=la, pt=pt, n=n: e.tensor_tensor(out=la[:, 0:n], in0=la[:, 0:n], in1=pt[:, 0:n],
                                                                     op=ALU.add), [ptk, lak], [lak])
            if kt == nkt - 1:
                nacc = min(3, nkt)
                for c in range(nacc):
                    P.mm(ps[pl][:, 0:n], onesf, lacc[po - 3][c][:, 0:n], c == 0, c == nacc - 1,
                         ['lacc%d_%d' % (po - 3, c), 'onesf'], ['ps%d' % pl])
                P.op('dve', lambda e, pl=pl, n=n: e.reciprocal(out=rinv[:, 0:n], in_=ps[pl][:, 0:n]),
                     ['ps%d' % pl], ['rinv'])
                P.op('dve', lambda e, po=po, n=n, h=h, q0=q0: e.tensor_tensor(
                    out=mixT[:, 4 + h, q0:q0 + n], in0=ps[po][:, 0:n], in1=rinv[:, 0:n], op=ALU.mult),
                    ['ps%d' % po, 'rinv'], ['mix_c'])
            if qi == len(qblocks) - 1 and kt % KPP == KPP - 1 and h < 3:
                load_kv(h + 1, kt // KPP)
        P.fence()
        A.release(mP)

        wout_bf = A.bf(8, 1024)
        C.load_cast(wout_bf, d_wout, 1024, 8, 'wout', stage=[(A.f32(1024), 'wst5_0'), (A.f32(1024), 'wst5_1')])
        wr = A.f32(8, 16)
        P.dma(wr, d_wr, [], ['wr'])
        xr = A.f32(8, 512)
        x1 = A.f32(8, 512)
        sq5 = A.bf(8, 512)
        rstd5 = A.f32(512)
        tmp5 = [A.f32(512), A.f32(512)]
        h2 = A.f32(8, 512)
        sm = A.f32(4, 4)
        pe_ = A.f32(4, 16)
        for (src, n, w, q0, l0) in lblocks:
            P.dma(xr[:, :, 0:n], src, [], ['xr'])
            for j in range(8):
                pi = C.next_ps()
                pk = 'ps%d' % pi
                for k in range(8):
                    P.mm(ps[pi][:, 0:n], wout_bf[:, k, j * 128:(j + 1) * 128], mixT[:, k, q0:q0 + n], k == 0, k == 7,
                         ['wout', 'mix_a', 'mix_b', 'mix_c'], [pk])
                P.op('dve', lambda e, j=j, pi=pi, n=n, w=w: e.scalar_tensor_tensor(
                    out=x1[:, j, 0:n], in0=ps[pi][:, 0:n], scalar=svc(2, w)[:, j:j + 1], in1=xr[:, j, 0:n],
                    op0=ALU.mult, op1=ALU.add), [pk, 'xr', 'svt'], ['x1'])
            P.dma(o_x1[:, :, q0:q0 + n], x1[:, :, 0:n], ['x1'], ['o_x1'])
            C.sumsq_rstd(x1[:, :, 0:n], 8, n, D, rstd5, ones, sq5, ['x1'], 'n5')
            C.norm_mod(x1, n, rstd5, svc(3, w), svc(4, w), h2, tmp5, ['x1'], ['h2'], 'n5')
            for tt in range(n // 128):
                pi = C.next_ps()
                pk = 'ps%d' % pi
                si = tt % 4
                smk = 'sm%d' % si
                for k in range(8):
                    P.mm(ps[pi][:, 0:16], h2[:, k, tt * 128:(tt + 1) * 128], wr[:, k, :], k == 0, k == 7,
                         ['h2', 'wr'], [pk])
                P.op('dve', lambda e, pi=pi, si=si: e.reduce_max(out=sm[:, si, 0:1], in_=ps[pi][:, 0:16],
                                                                 axis=mybir.AxisListType.X), [pk], [smk])
                P.op('dve', lambda e, si=si: e.tensor_single_scalar(out=sm[:, si, 1:2], in_=sm[:, si, 0:1], scalar=-1.0,
                                                                    op=ALU.mult), [smk], [smk])
                P.act(pe_[:, si, :], ps[pi][:, 0:16], AF.Exp, [pk, smk], ['pe%d' % si], bias=sm[:, si, 1:2],
                      accum=sm[:, si, 2:3])
                P.op('dve', lambda e, si=si: e.reciprocal(out=sm[:, si, 3:4], in_=sm[:, si, 2:3]), ['pe%d' % si, smk],
                     [smk])
                P.op('dve', lambda e, si=si: e.tensor_scalar(out=pe_[:, si, :], in0=pe_[:, si, :],
                                                             scalar1=sm[:, si, 3:4], scalar2=None, op0=ALU.mult),
                     [smk, 'pe%d' % si], ['pe%d' % si])
                P.dma(o_probs[q0 + tt * 128:q0 + (tt + 1) * 128, :], pe_[:, si, :], ['pe%d' % si], ['o_probs'])
        P.finalize()
        return nc, P, A


def _fm(a):
    k = a.shape[0] // 128
    return np.ascontiguousarray(a.reshape(k, 128, -1).transpose(1, 0, 2))


def _fmv(v):
    return np.ascontiguousarray(v.reshape(-1, 128).T)


def _rope_perm():
    p = np.arange(64)
    o = ((p % 32) // 16) * 32 + (p // 32) * 16 + (p % 16)
    osw = o[(p + 32) % 64]
    return o, osw


def _rope_tables():
    rows = SEQ // 64
    r = np.repeat(np.arange(rows), 64).astype(np.float32)
    col = np.tile(np.arange(64), rows).astype(np.float32)
    inv = (np.float32(10000.0) ** (-np.arange(16, dtype=np.float32) / np.float32(16))).astype(np.float32)
    ang = np.concatenate([r[:, None] * inv, col[:, None] * inv], axis=-1).astype(np.float32)
    cos, sin = np.cos(ang).astype(np.float32), np.sin(ang).astype(np.float32)
    C_ = np.concatenate([cos, cos], axis=1).T
    S_ = np.concatenate([-sin, sin], axis=1).T
    return np.ascontiguousarray(C_), np.ascontiguousarray(S_)


def prep_AB(inp, l, xl, xc):
    o, osw = _rope_perm()
    xall = _fm(np.ascontiguousarray(np.concatenate([xc, xl], axis=0).T))
    cvec = np.stack([_fmv(inp['c'][0]), _fmv(inp['c_ctx'])], axis=-1)
    w_in = inp['w_in'][l]
    wA = np.concatenate([w_in[:, 512:768], w_in[:, 1408:1664], w_in[:, 1664 + o], w_in[:, 1664 + osw]], axis=1)
    wB = np.concatenate([w_in[:, 0:512], w_in[:, 768:1024], w_in[:, 1024:1408]], axis=1)
    wuq = inp['w_uq'][l]
    cols = []
    for h in range(4):
        cols += [wuq[:, h * 192:h * 192 + 128], wuq[:, h * 192 + 128 + o], wuq[:, h * 192 + 128 + osw]]
    wuq2 = np.concatenate(cols, axis=1)
    wukv = inp['w_ukv'][l]
    wukv2 = np.concatenate([wukv[:, h * 256:h * 256 + 128] for h in range(4)] +
                           [wukv[:, h * 256 + 128:h * 256 + 256] for h in range(4)], axis=1)
    lruw = np.zeros((128, 8, 128), np.float32)
    vecs = np.zeros((128, 64), np.float32)
    vecs[:, 0:8] = _fmv(inp['norm1_g'][l])
    vecs[:, 8:16] = _fmv(inp['norm2_g'][l])
    for d in range(2):
        for g in range(2):
            W = (inp['lru_wa'] if g == 0 else inp['lru_wx'])[l][d]
            bb = (inp['lru_ba'] if g == 0 else inp['lru_bx'])[l][d]
            for ct in range(2):
                i = d * 4 + g * 2 + ct
                lruw[0:64, i, 0:64] = W[2 * ct]
                lruw[64:128, i, 64:128] = W[2 * ct + 1]
                vecs[:, 16 + i] = bb[ct * 128:(ct + 1) * 128]
        for ct in range(2):
            vecs[:, 24 + d * 2 + ct] = inp['lru_lambda'][l][d][ct * 128:(ct + 1) * 128]
    for ct in range(2):
        for k in range(4):
            vecs[:, 28 + ct * 4 + k] = inp['conv_w'][l][k][ct * 128:(ct + 1) * 128]
        vecs[:, 36 + ct] = inp['conv_b'][l][ct * 128:(ct + 1) * 128]
    vecs[:, 38:41] = _fmv(inp['q_norm_g'][l])
    vecs[:, 41:43] = _fmv(inp['kv_norm_g'][l])
    sguw = np.ascontiguousarray(inp['sgu_w'][l].transpose(2, 0, 1))
    sgub = np.zeros((128, 2, 128), np.float32)
    for g in range(4):
        sgub[(g % 2) * 64:(g % 2) * 64 + 64, g // 2, :] = inp['sgu_b'][l][g][None, :]
    rC, rS = _rope_tables()
    common = dict(xall=xall, cvec=np.ascontiguousarray(cvec), wmod=_fm(np.ascontiguousarray(inp['w_mod'][l][:, :5120])),
                  bmod=_fmv(inp['b_mod'][l][:5120]), wA=_fm(wA), wB=_fm(wB), wuq=_fm(wuq2), wukv=_fm(wukv2),
                  wout=_fm(inp['w_out'][l]), lruw=lruw, sguw=sguw, sgub=sgub, wr=_fm(inp['w_router'][l]),
                  ropeC=rC, ropeS=rS, ident=np.eye(128, dtype=np.float32))
    maps = []
    for c in range(NCORES):
        v = vecs.copy()
        v[:, 43 + c] = 1.0
        m = dict(common)
        m['vecs'] = v
        m['xloc'] = np.ascontiguousarray(xall[:, :, CTX + NLOC * c:CTX + NLOC * (c + 1)])
        m['ropeCl'] = np.ascontiguousarray(rC[:, NLOC * c:NLOC * (c + 1)])
        m['ropeSl'] = np.ascontiguousarray(rS[:, NLOC * c:NLOC * (c + 1)])
        maps.append(m)
    return maps


def _unfm(a):
    return np.ascontiguousarray(a.transpose(1, 0, 2).reshape(-1, a.shape[2]).T)


def gather_AB(res):
    xl1 = np.concatenate([_unfm(r['x1T'][:, :, CTX:]) for r in res], axis=0)
    xc1 = _unfm(res[0]['x1T'][:, :, :CTX])
    pl = np.concatenate([r['probs'][CTX:] for r in res], axis=0)
    pc = res[0]['probs'][:CTX]
    return xl1, xc1, pl, pc


NBIS = 30


def build_C():
    nc = bass.Bass("TRN2", target_bir_lowering=False)

    def din(name, shape, dt=F32):
        return nc.dram_tensor(name, list(shape), dt, kind="ExternalInput").ap()
    d_x1 = din("x1T", [128, 8, NL])
    d_pA = din("probsA", [128, NLOC])
    d_pC = din("probsC", [16, CTX])
    d_gm = din("gmT", [16, NL])
    d_cvec = din("cvec", [128, 8, 2])
    d_wmod = din("wmod", [128, 8, 3072])
    d_bmod = din("bmod", [128, 24])
    d_vecs = din("vecs", [128, 16])
    d_G = din("G", [128, 128])
    d_oh = din("oh16", [16, 16])
    d_wg = din("wg", [NEXP, D, D])
    d_wu = din("wu", [NEXP, D, D])
    d_wd = din("wd", [NEXP, D, D])
    o_x2 = nc.dram_tensor("x2T", [128, 8, NL], F32, kind="ExternalOutput").ap()
    o_on = nc.dram_tensor("outN", [128, 8, NL], F32, kind="ExternalOutput").ap()

    AW = 51 * 1024
    with ExitStack() as es:
        arena_t = es.enter_context(nc.sbuf_tensor("arena", [128, AW], F32))
        ps = [es.enter_context(nc.psum_tensor("ps%d" % i, [128, 512], F32)) for i in range(7)]
        P = Prog(nc)
        A = Arena(arena_t, AW)
        C = Ctx(nc, P, A, ps, None)

        vecs = A.f32(16)
        modT = A.f32(24, 2)
        cs = A.f32(64)
        sv = A.f32(3, 8, 2)
        G = A.f32(128)
        oh = A.f32(16)
        ones16 = A.f32(128)
        ones = A.bf(128)
        ts = A.f32(16)
        m0 = A.mark()
        P.dma(vecs, d_vecs, [], ['vecs'])
        P.dma(G, d_G, [], ['consts'])
        P.dma(oh[0:16, :], d_oh, [], ['consts'])
        P.op('dve', lambda e: e.memset(ones, 1.0), [], ['consts'])
        P.op('dve', lambda e: e.memset(ones16, 1.0), [], ['consts'])
        stage8 = A.f32(8, 1024)
        C.compute_mods(d_cvec, d_wmod, d_bmod, 3, modT, cs, stage8)
        n2g, fng = vecs[:, 0:8], vecs[:, 8:16]
        mv = modT.rearrange("p (b k) w -> p b k w", b=3)
        for w in range(2):
            P.op('dve', lambda e, w=w: e.scalar_tensor_tensor(
                out=sv[:, 0, :, w], in0=mv[:, 1, :, w], scalar=1.0, in1=n2g, op0=ALU.add, op1=ALU.mult),
                ['modv', 'vecs'], ['svt'])
            P.op('dve', lambda e, w=w: e.tensor_copy(out=sv[:, 1, :, w], in_=mv[:, 0, :, w]), ['modv'], ['svt'])
            P.op('dve', lambda e, w=w: e.tensor_copy(out=sv[:, 2, :, w], in_=mv[:, 2, :, w]), ['modv'], ['svt'])
        P.fence()
        A.release(m0)

        h2T = A.bf(8, NL)
        acc = A.f32(8, NL)
        gmT = A.f32(NL)
        mL = A.mark()
        lblocks = [(0, CTX, 1)] + [(CTX + 512 * i, 512, 0) for i in range(NLOC // 512)]

        xs = [A.f32(8, 512), A.f32(8, 512)]
        sq = A.bf(8, 512)
        rstd = A.f32(512)
        tmpf = [A.f32(512), A.f32(512)]
        for bi, (q0, n, w) in enumerate(lblocks):
            x = xs[bi % 2]
            xk = 'xs%d' % (bi % 2)
            P.dma(x[:, :, 0:n], d_x1[:, :, q0:q0 + n], [], [xk])
            C.sumsq_rstd(x[:, :, 0:n], 8, n, D, rstd, ones, sq, [xk], 'n2')
            C.norm_mod(x, n, rstd, sv[:, 0, :, w], sv[:, 1, :, w], h2T[:, :, q0:q0 + n], tmpf, [xk], ['h2T'], 'n2')
        P.fence()
        A.release(mL)

        pA = A.f32(NLOC)
        mk = A.f32(NLOC)
        pC = A.f32(CTX)
        mkc = A.f32(CTX)
        P.dma(pA, d_pA, [], ['pA'])
        P.dma(pC[0:16, :], d_pC, [], ['pC'])
        P.dma(gmT[0:16, :], d_gm, [], ['gmT'])
        lo, hi, mid, tot, gt, dd, Kv = (ts[:, 0:2], ts[:, 2:4], ts[:, 4:6], ts[:, 6:8], ts[:, 8:10], ts[:, 10:12],
                                        ts[:, 12:14])
        P.op('dve', lambda e: e.memset(ts, 0.0), [], ['ts'])
        P.op('dve', lambda e: e.memset(hi, 1.0), ['ts'], ['ts'])
        P.op('dve', lambda e: e.memset(mid, 0.5), ['ts'], ['ts'])
        P.op('dve', lambda e: e.memset(ts[:, 12:13], NLOC - 0.5), ['ts'], ['ts'])
        P.op('dve', lambda e: e.memset(ts[:, 13:14], 2 * CTX // NEXP - 0.5), ['ts'], ['ts'])
        for it in range(NBIS):
            P.op('dve', lambda e: e.tensor_scalar(out=mk, in0=pA, scalar1=ts[:, 4:5], scalar2=None, op0=ALU.is_gt),
                 ['pA', 'ts'], ['mk'])
            P.op('dve', lambda e: e.reduce_sum(out=ts[:, 14:15], in_=mk, axis=mybir.AxisListType.X), ['mk'], ['cc'])
            P.op('dve', lambda e: e.tensor_scalar(out=mkc[0:16, :], in0=pC[0:16, :], scalar1=ts[0:16, 5:6], scalar2=None,
                                                  op0=ALU.is_gt), ['pC', 'ts'], ['mkc'])
            P.op('dve', lambda e: e.reduce_sum(out=ts[0:16, 7:8], in_=mkc[0:16, :], axis=mybir.AxisListType.X),
                 ['mkc', 'ts'], ['ts'])
            P.mm(ps[6][:, 0:1], G, ts[:, 14:15], True, True, ['cc', 'consts'], ['ps6'])
            P.op('dve', lambda e: e.tensor_copy(out=ts[:, 6:7], in_=ps[6][:, 0:1]), ['ps6', 'ts'], ['ts'])
            P.op('dve', lambda e: e.tensor_tensor(out=gt, in0=tot, in1=Kv, op=ALU.is_gt), ['ts'], ['ts'])
            P.op('dve', lambda e: e.tensor_tensor(out=dd, in0=mid, in1=lo, op=ALU.subtract), ['ts'], ['ts'])
            P.op('dve', lambda e: e.tensor_tensor(out=dd, in0=dd, in1=gt, op=ALU.mult), ['ts'], ['ts'])
            P.op('dve', lambda e: e.tensor_tensor(out=lo, in0=lo, in1=dd, op=ALU.add), ['ts'], ['ts'])
            P.op('dve', lambda e: e.tensor_tensor(out=dd, in0=hi, in1=mid, op=ALU.subtract), ['ts'], ['ts'])
            P.op('dve', lambda e: e.tensor_tensor(out=dd, in0=dd, in1=gt, op=ALU.mult), ['ts'], ['ts'])
            P.op('dve', lambda e: e.tensor_tensor(out=hi, in0=mid, in1=dd, op=ALU.add), ['ts'], ['ts'])
            P.op('dve', lambda e: e.tensor_tensor(out=dd, in0=lo, in1=hi, op=ALU.add), ['ts'], ['ts'])
            P.op('dve', lambda e: e.tensor_single_scalar(out=mid, in_=dd, scalar=0.5, op=ALU.mult), ['ts'], ['ts'])
        P.op('dve', lambda e: e.tensor_scalar(out=mk[0:16, 0:NLOC], in0=gmT[0:16, CTX:NL], scalar1=ts[0:16, 2:3],
                                              scalar2=None, op0=ALU.is_gt), ['gmT', 'ts'], ['mk'])
        P.op('dve', lambda e: e.tensor_tensor(out=gmT[0:16, CTX:NL], in0=gmT[0:16, CTX:NL], in1=mk[0:16, 0:NLOC],
                                              op=ALU.mult), ['mk', 'gmT'], ['gmT'])
        P.op('dve', lambda e: e.tensor_scalar(out=mkc[0:16, :], in0=gmT[0:16, 0:CTX], scalar1=ts[0:16, 3:4],
                                              scalar2=None, op0=ALU.is_gt), ['gmT', 'ts'], ['mkc'])
        P.op('dve', lambda e: e.tensor_tensor(out=gmT[0:16, 0:CTX], in0=gmT[0:16, 0:CTX], in1=mkc[0:16, :],
                                              op=ALU.mult), ['mkc', 'gmT'], ['gmT'])
        P.fence()
        A.release(mL)

        HF = 512
        wbuf = [dict(g=A.bf(8, HF), u=A.bf(8, HF), d=A.bf(4, 1024)) for _ in range(2)]
        wst = [(A.f32(1024), 'wst0'), (A.f32(1024), 'wst1')]
        hid = A.bf(4, 512)
        sg = [A.f32(512), A.f32(512)]
        tt_ = [A.f32(512), A.f32(512)]
        gmb = A.f32(512)
        gme = A.f32(512)
        units = [(ex, hf) for ex in range(NEXP) for hf in range(2)]
        stc = [0]
        gmbs = [gmb, A.f32(512)]
        gmes = [gme, A.f32(512)]

        def emit_gm(gi):
            ui2, bi2 = gi // len(lblocks), gi % len(lblocks)
            ex2 = units[ui2][0]
            q02, n2, _ = lblocks[bi2]
            ge, gb_ = gmes[gi % 2], gmbs[gi % 2]
            P.op('dve', lambda e: e.tensor_scalar(out=ge[0:16, 0:n2], in0=gmT[0:16, q02:q02 + n2],
                                                  scalar1=oh[0:16, ex2:ex2 + 1], scalar2=None, op0=ALU.mult),
                 ['gmT', 'consts'], ['gme%d' % (gi % 2)])
            P.mm(ps[6][:, 0:n2], ones16[0:16, :], ge[0:16, 0:n2], True, True, ['gme%d' % (gi % 2), 'consts'], ['ps6'])
            P.act(gb_[:, 0:n2], ps[6][:, 0:n2], AF.Copy, ['ps6'], ['gmb%d' % (gi % 2)])

        def load_steps(ui):
            ex, hf = units[ui]
            wb = wbuf[ui % 2]
            tag = 'w%d' % (ui % 2)
            steps = []
            for nm, dsrc in (('g', d_wg), ('u', d_wu)):
                for k in range(8):
                    def f(nm=nm, dsrc=dsrc, k=k):
                        st, sk = wst[stc[0] % 2]
                        stc[0] += 1
                        P.dma(st[:, 0:HF], dsrc[ex, k * 128:(k + 1) * 128, hf * HF:(hf + 1) * HF], [], [sk])
                        P.op(C.ew(), lambda en, st=st: en.tensor_copy(out=wb[nm][:, k, :], in_=st[:, 0:HF]),
                             [sk], [tag + nm])
                    steps.append(f)
            for f4 in range(4):
                def f(f4=f4):
                    st, sk = wst[stc[0] % 2]
                    stc[0] += 1
                    r0 = (hf * 4 + f4) * 128
                    P.dma(st[:, 0:1024], d_wd[ex, r0:r0 + 128, :], [], [sk])
                    P.op(C.ew(), lambda en, st=st: en.tensor_copy(out=wb['d'][:, f4, :], in_=st[:, 0:1024]),
                         [sk], [tag + 'd'])
                steps.append(f)
            return steps
        for f in load_steps(0):
            f()
        first_acc = True
        for ui, (ex, hf) in enumerate(units):
            wb = wbuf[ui % 2]
            tag = 'w%d' % (ui % 2)
            nxt = load_steps(ui + 1) if ui + 1 < len(units) else []
            per = (len(nxt) + len(lblocks) - 1) // len(lblocks)
            for bi, (q0, n, w) in enumerate(lblocks):
                gi = ui * len(lblocks) + bi
                if gi == 0:
                    emit_gm(0)
                gmb, gmk = gmbs[gi % 2], 'gmb%d' % (gi % 2)
                for f in range(4):
                    pg, pu = f % 2, 2 + f % 2
                    for k in range(8):
                        P.mm(ps[pg][:, 0:n], wb['g'][:, k, f * 128:(f + 1) * 128], h2T[:, k, q0:q0 + n], k == 0, k == 7,
                             [tag + 'g', 'h2T'], ['ps%d' % pg])
                    for k in range(8):
                        P.mm(ps[pu][:, 0:n], wb['u'][:, k, f * 128:(f + 1) * 128], h2T[:, k, q0:q0 + n], k == 0, k == 7,
                             [tag + 'u', 'h2T'], ['ps%d' % pu])
                    s_, t_ = sg[f % 2], tt_[f % 2]
                    P.act(s_[:, 0:n], ps[pg][:, 0:n], AF.Silu, ['ps%d' % pg], ['sg%d' % (f % 2)])
                    P.op('pool', lambda e, s_=s_, t_=t_, n=n, gmb=gmb: e.tensor_tensor(out=t_[:, 0:n], in0=s_[:, 0:n],
                                                                                      in1=gmb[:, 0:n], op=ALU.mult),
                         ['sg%d' % (f % 2), gmk], ['tt%d' % (f % 2)])
                    P.op('dve', lambda e, t_=t_, n=n, f=f, pu=pu: e.tensor_tensor(
                        out=hid[:, f, 0:n], in0=t_[:, 0:n], in1=ps[pu][:, 0:n], op=ALU.mult),
                        ['tt%d' % (f % 2), 'ps%d' % pu], ['hid'])
                if gi + 1 < len(units) * len(lblocks):
                    emit_gm(gi + 1)
                for j in range(8):
                    pd = 4 + j % 2
                    for f in range(4):
                        P.mm(ps[pd][:, 0:n], wb['d'][:, f, j * 128:(j + 1) * 128], hid[:, f, 0:n], f == 0, f == 3,
                             [tag + 'd', 'hid'], ['ps%d' % pd])
                    if ui == 0:
                        P.op('dve', lambda e, j=j, pd=pd, n=n, q0=q0: e.tensor_copy(out=acc[:, j, q0:q0 + n],
                                                                                   in_=ps[pd][:, 0:n]),
                             ['ps%d' % pd], ['acc'])
                    else:
                        P.op('dve', lambda e, j=j, pd=pd, n=n, q0=q0: e.tensor_tensor(
                            out=acc[:, j, q0:q0 + n], in0=acc[:, j, q0:q0 + n], in1=ps[pd][:, 0:n], op=ALU.add),
                            ['ps%d' % pd, 'acc'], ['acc'])
                for f in nxt[bi * per:(bi + 1) * per]:
                    f()
        P.fence()
        A.release(mL)

        xr = [A.f32(8, 512)] * 2
        x2 = [A.f32(8, 512)] * 2
        on = A.f32(8, 512)
        sq2 = A.bf(8, 512)
        rstd2 = A.f32(512)
        tmp2 = [A.f32(512), A.f32(512)]
        for bi, (q0, n, w) in enumerate(lblocks):
            x, xk = xr[0], 'xr'
            y, yk = x2[0], 'x2'
            P.dma(x[:, :, 0:n], d_x1[:, :, q0:q0 + n], [], [xk])
            for j in range(8):
                P.op('dve', lambda e, j=j, n=n, w=w, q0=q0, x=x, y=y: e.scalar_tensor_tensor(
                    out=y[:, j, 0:n], in0=acc[:, j, q0:q0 + n], scalar=sv[:, 2, j:j + 1, w], in1=x[:, j, 0:n],
                    op0=ALU.mult, op1=ALU.add), [xk, 'acc', 'svt'], [yk])
            P.dma(o_x2[:, :, q0:q0 + n], y[:, :, 0:n], [yk], ['o_x2'])
            C.sumsq_rstd(y[:, :, 0:n], 8, n, D, rstd2, ones, sq2, [yk], 'fn')
            for k in range(8):
                e_ = C.ew()
                t = tmp2[k % 2]
                tk = 'fn_tmp%d' % (k % 2)
                P.op(e_, lambda e, k=k, t=t, n=n, y=y: e.tensor_tensor(out=t[:, 0:n], in0=y[:, k, 0:n], in1=rstd2[:, 0:n],
                                                                      op=ALU.mult), [yk, 'fn_rstd'], [tk])
                P.op(e_, lambda e, k=k, t=t, n=n: e.tensor_scalar(out=on[:, k, 0:n], in0=t[:, 0:n],
                                                                  scalar1=fng[:, k:k + 1], scalar2=None, op0=ALU.mult),
                     [tk, 'vecs'], ['on'])
            P.dma(o_on[:, :, q0:q0 + n], on[:, :, 0:n], ['on'], ['o_on'])
        P.finalize()
        return nc, P, A


def build_C2():
    nc = bass.Bass("TRN2", target_bir_lowering=False)

    def din(name, shape, dt=F32):
        return nc.dram_tensor(name, list(shape), dt, kind="ExternalInput").ap()
    d_x1 = din("x1T", [128, 8, NL])
    d_pA = din("probsA", [128, NLOC])
    d_pC = din("probsC", [16, CTX])
    d_gm = din("gmT", [16, NL])
    d_cvec = din("cvec", [128, 8, 2])
    d_wmod = din("wmod", [128, 8, 3072])
    d_bmod = din("bmod", [128, 24])
    d_vecs = din("vecs", [128, 16])
    d_G = din("G", [128, 128])
    d_oh = din("oh16", [16, 16])
    d_ident = din("ident", [128, 128])
    d_iotaf = din("iotaf", [128, 512])
    d_iotap = din("iotap", [128, 4])
    d_wg = din("wg", [NEXP, D, D])
    d_wu = din("wu", [NEXP, D, D])
    d_wd = din("wd", [NEXP, D, D])
    o_x2 = nc.dram_tensor("x2T", [128, 8, NL], F32, kind="ExternalOutput").ap()
    o_on = nc.dram_tensor("outN", [128, 8, NL], F32, kind="ExternalOutput").ap()

    AW = 51 * 1024
    with ExitStack() as es:
        arena_t = es.enter_context(nc.sbuf_tensor("arena", [128, AW], F32))
        ps = [es.enter_context(nc.psum_tensor("ps%d" % i, [128, 512], F32)) for i in range(7)]
        psb = es.enter_context(nc.psum_tensor("psb", [128, 1024], BF16))
        P = Prog(nc)
        A = Arena(arena_t, AW)
        C = Ctx(nc, P, A, ps, psb)

        vecs = A.f32(16)
        modT = A.f32(24, 2)
        cs = A.f32(64)
        sv = A.f32(3, 8, 2)
        G = A.f32(128)
        oh = A.f32(16)
        ones16 = A.f32(128)
        ones = A.bf(128)
        identb = A.bf(128)
        iotaf = A.f32(512)
        iotap = A.f32(4)
        ts = A.f32(16)
        m0 = A.mark()
        P.dma(vecs, d_vecs, [], ['vecs'])
        P.dma(G, d_G, [], ['consts'])
        P.dma(oh[0:16, :], d_oh, [], ['consts'])
        P.dma(iotaf, d_iotaf, [], ['consts'])
        P.dma(iotap, d_iotap, [], ['consts'])
        P.op('dve', lambda e: e.memset(ones, 1.0), [], ['consts'])
        P.op('dve', lambda e: e.memset(ones16, 1.0), [], ['consts'])
        stage8 = A.f32(8, 1024)
        C.compute_mods(d_cvec, d_wmod, d_bmod, 3, modT, cs, stage8)
        P.dma(stage8[:, 0, 0:128], d_ident, [], ['wm_st'])
        P.op('dve', lambda e: e.tensor_copy(out=identb, in_=stage8[:, 0, 0:128]), ['wm_st'], ['consts'])
        n2g, fng = vecs[:, 0:8], vecs[:, 8:16]
        mv = modT.rearrange("p (b k) w -> p b k w", b=3)
        for w in range(2):
            P.op('dve', lambda e, w=w: e.scalar_tensor_tensor(
                out=sv[:, 0, :, w], in0=mv[:, 1, :, w], scalar=1.0, in1=n2g, op0=ALU.add, op1=ALU.mult),
                ['modv', 'vecs'], ['svt'])
            P.op('dve', lambda e, w=w: e.tensor_copy(out=sv[:, 1, :, w], in_=mv[:, 0, :, w]), ['modv'], ['svt'])
            P.op('dve', lambda e, w=w: e.tensor_copy(out=sv[:, 2, :, w], in_=mv[:, 2, :, w]), ['modv'], ['svt'])
        P.fence()
        A.release(m0)

        NTT = NL // 128
        h2tok = A.bf(NTT, 1024)
        acc = A.f32(8, NL)
        gmT = A.f32(NL)
        ssel = A.f32(NL)
        slotT = A.f32(NTT, 16)
        mL = A.mark()
        lblocks = [(0, CTX, 1)] + [(CTX + 512 * i, 512, 0) for i in range(NLOC // 512)]

        xs = [A.f32(8, 512), A.f32(8, 512)]
        sq = A.bf(8, 512)
        rstd = A.f32(512)
        tmpf = [A.f32(512), A.f32(512)]
        h2b = [A.bf(8, 512), A.bf(8, 512)]
        for bi, (q0, n, w) in enumerate(lblocks):
            x = xs[bi % 2]
            xk = 'xs%d' % (bi % 2)
            P.dma(x[:, :, 0:n], d_x1[:, :, q0:q0 + n], [], [xk])
            C.sumsq_rstd(x[:, :, 0:n], 8, n, D, rstd, ones, sq, [xk], 'n2')
            hb, hk = h2b[bi % 2], 'h2b%d' % (bi % 2)
            C.norm_mod(x, n, rstd, sv[:, 0, :, w], sv[:, 1, :, w], hb, tmpf, [xk], [hk], 'n2')
            for tt in range(n // 128):
                for k in range(8):
                    P.tr(psb[:, k * 128:(k + 1) * 128], hb[:, k, tt * 128:(tt + 1) * 128], identb, [hk, 'consts'], ['psb'])
                P.act(h2tok[:, q0 // 128 + tt, :], psb[:, 0:1024], AF.Copy, ['psb'], ['h2tok'])
        P.fence()
        A.release(mL)

        pA = A.f32(NLOC)
        mk = A.f32(NLOC)
        pC = A.f32(CTX)
        mkc = A.f32(CTX)
        P.dma(pA, d_pA, [], ['pA'])
        P.dma(pC[0:16, :], d_pC, [], ['pC'])
        P.dma(gmT[0:16, :], d_gm, [], ['gmT'])
        lo, hi, mid, tot, gt, dd, Kv = (ts[:, 0:2], ts[:, 2:4], ts[:, 4:6], ts[:, 6:8], ts[:, 8:10], ts[:, 10:12],
                                        ts[:, 12:14])
        P.op('dve', lambda e: e.memset(ts, 0.0), [], ['ts'])
        P.op('dve', lambda e: e.memset(hi, 1.0), ['ts'], ['ts'])
        P.op('dve', lambda e: e.memset(mid, 0.5), ['ts'], ['ts'])
        P.op('dve', lambda e: e.memset(ts[:, 12:13], NLOC - 0.5), ['ts'], ['ts'])
        P.op('dve', lambda e: e.memset(ts[:, 13:14], 2 * CTX // NEXP - 0.5), ['ts'], ['ts'])
        for it in range(NBIS):
            P.op('dve', lambda e: e.tensor_scalar(out=mk, in0=pA, scalar1=ts[:, 4:5], scalar2=None, op0=ALU.is_gt),
                 ['pA', 'ts'], ['mk'])
            P.op('dve', lambda e: e.reduce_sum(out=ts[:, 14:15], in_=mk, axis=mybir.AxisListType.X), ['mk'], ['cc'])
            P.op('dve', lambda e: e.tensor_scalar(out=mkc[0:16, :], in0=pC[0:16, :], scalar1=ts[0:16, 5:6], scalar2=None,
                                                  op0=ALU.is_gt), ['pC', 'ts'], ['mkc'])
            P.op('dve', lambda e: e.reduce_sum(out=ts[0:16, 7:8], in_=mkc[0:16, :], axis=mybir.AxisListType.X),
                 ['mkc', 'ts'], ['ts'])
            P.mm(ps[6][:, 0:1], G, ts[:, 14:15], True, True, ['cc', 'consts'], ['ps6'])
            P.op('dve', lambda e: e.tensor_copy(out=ts[:, 6:7], in_=ps[6][:, 0:1]), ['ps6', 'ts'], ['ts'])
            P.op('dve', lambda e: e.tensor_tensor(out=gt, in0=tot, in1=Kv, op=ALU.is_gt), ['ts'], ['ts'])
            P.op('dve', lambda e: e.tensor_tensor(out=dd, in0=mid, in1=lo, op=ALU.subtract), ['ts'], ['ts'])
            P.op('dve', lambda e: e.tensor_tensor(out=dd, in0=dd, in1=gt, op=ALU.mult), ['ts'], ['ts'])
            P.op('dve', lambda e: e.tensor_tensor(out=lo, in0=lo, in1=dd, op=ALU.add), ['ts'], ['ts'])
            P.op('dve', lambda e: e.tensor_tensor(out=dd, in0=hi, in1=mid, op=ALU.subtract), ['ts'], ['ts'])
            P.op('dve', lambda e: e.tensor_tensor(out=dd, in0=dd, in1=gt, op=ALU.mult), ['ts'], ['ts'])
            P.op('dve', lambda e: e.tensor_tensor(out=hi, in0=mid, in1=dd, op=ALU.add), ['ts'], ['ts'])
            P.op('dve', lambda e: e.tensor_tensor(out=dd, in0=lo, in1=hi, op=ALU.add), ['ts'], ['ts'])
            P.op('dve', lambda e: e.tensor_single_scalar(out=mid, in_=dd, scalar=0.5, op=ALU.mult), ['ts'], ['ts'])
        P.op('dve', lambda e: e.tensor_scalar(out=mk[0:16, 0:NLOC], in0=gmT[0:16, CTX:NL], scalar1=ts[0:16, 2:3],
                                              scalar2=None, op0=ALU.is_gt), ['gmT', 'ts'], ['mk'])
        P.op('dve', lambda e: e.tensor_tensor(out=gmT[0:16, CTX:NL], in0=gmT[0:16, CTX:NL], in1=mk[0:16, 0:NLOC],
                                              op=ALU.mult), ['mk', 'gmT'], ['gmT'])
        P.op('dve', lambda e: e.tensor_scalar(out=mkc[0:16, :], in0=gmT[0:16, 0:CTX], scalar1=ts[0:16, 3:4],
                                              scalar2=None, op0=ALU.is_gt), ['gmT', 'ts'], ['mkc'])
        P.op('dve', lambda e: e.tensor_tensor(out=gmT[0:16, 0:CTX], in0=gmT[0:16, 0:CTX], in1=mkc[0:16, :],
                                              op=ALU.mult), ['mkc', 'gmT'], ['gmT'])
        mrow = A.f32(NL)
        orow = A.f32(NL)
        P.op('dve', lambda e: e.tensor_copy(out=mrow[0:16, CTX:NL], in_=mk[0:16, 0:NLOC]), ['mk'], ['mrow'])
        P.op('dve', lambda e: e.tensor_copy(out=mrow[0:16, 0:CTX], in_=mkc[0:16, :]), ['mkc'], ['mrow'])
        P.op('dve', lambda e: e.memset(orow[0:16, :], 1.0), [], ['orow'])
        P.op('dve', lambda e: e.tensor_tensor_scan(out=ssel[0:16, :], data0=orow[0:16, :], data1=mrow[0:16, :],
                                                   initial=0.0, op0=ALU.mult, op1=ALU.add), ['orow', 'mrow'], ['ssel'])
        P.op('dve', lambda e: e.tensor_tensor(out=ssel[0:16, :], in0=ssel[0:16, :], in1=mrow[0:16, :], op=ALU.mult),
             ['ssel', 'mrow'], ['ssel'])
        for tt in range(NTT):
            P.tr(ps[6][:, 0:16], ssel[0:16, tt * 128:(tt + 1) * 128], oh[0:16, :], ['ssel', 'consts'], ['ps6'])
            P.op('dve', lambda e, tt=tt: e.tensor_copy(out=slotT[:, tt, :], in_=ps[6][:, 0:16]), ['ps6'], ['slotT'])
        P.fence()
        A.release(mL)

        CS = 512
        QF = 256
        NRING = 3
        wgu = [dict(g=A.bf(8, QF), u=A.bf(8, QF)) for _ in range(NRING)]
        wd = A.bf(8, 1024)
        SS = [A.bf(CS) for _ in range(4)]
        xsT = A.bf(8, CS)
        ye = xsT.rearrange("p (a b) c -> p a (b c)", a=4)
        hid = A.bf(8, CS)
        sg = [A.bf(CS), A.bf(CS)]
        gmb = A.f32(512)
        gme = A.f32(512)
        sse = A.f32(512)

        def load_gu(ex, q, slot):
            wb, tag = wgu[slot], 'wgu%d' % slot
            for nm, dsrc in (('g', d_wg), ('u', d_wu)):
                P.dma(wb[nm], dsrc[ex].rearrange("(k p) f -> p k f", p=128)[:, :, q * QF:(q + 1) * QF], [], [tag],
                      eng='pool')

        def load_d(ex):
            for hh in range(2):
                P.dma(wd[:, :, hh * 512:(hh + 1) * 512],
                      d_wd[ex].rearrange("(k p) f -> p k f", p=128)[:, :, hh * 512:(hh + 1) * 512], [], ['wd'], eng='pool')
        usl = [0]
        for q0_ in range(NRING):
            load_gu(0, q0_, q0_)
        for ex in range(NEXP):
            si = 0
            for dh in range(2):
                for tt in range(NTT):
                    S_, Sk = SS[si % 4], 'SS%d' % (si % 4)
                    si += 1
                    P.op('dve', lambda e, S_=S_, tt=tt, ex=ex: e.tensor_scalar(
                        out=S_, in0=iotaf, scalar1=slotT[:, tt, ex:ex + 1], scalar2=None, op0=ALU.is_equal),
                        ['consts', 'slotT'], [Sk])
                    for i in range(4):
                        dci = dh * 4 + i
                        P.mm(ps[i][:, 0:CS], h2tok[:, tt, dci * 128:(dci + 1) * 128], S_, tt == 0, tt == NTT - 1,
                             ['h2tok', Sk], ['ps%d' % i])
                for i in range(4):
                    P.act(xsT[:, dh * 4 + i, :], ps[i][:, 0:CS], AF.Copy, ['ps%d' % i], ['xsye'])
            if ex == 0:
                load_d(0)
            for q in range(4):
                slot = (ex * 4 + q) % NRING
                wb, tag = wgu[slot], 'wgu%d' % slot
                for f2 in range(2):
                    f = q * 2 + f2
                    pg, pu = f % 2, 2 + f % 2
                    for k in range(8):
                        P.mm(ps[pg][:, 0:CS], wb['g'][:, k, f2 * 128:(f2 + 1) * 128], xsT[:, k, :], k == 0, k == 7,
                             [tag, 'xsye'], ['ps%d' % pg])
                    for k in range(8):
                        P.mm(ps[pu][:, 0:CS], wb['u'][:, k, f2 * 128:(f2 + 1) * 128], xsT[:, k, :], k == 0, k == 7,
                             [tag, 'xsye'], ['ps%d' % pu])
                    s_ = sg[f % 2]
                    P.act(s_, ps[pg][:, 0:CS], AF.Silu, ['ps%d' % pg], ['sg%d' % (f % 2)])
                    P.op('dve', lambda e, s_=s_, f=f, pu=pu: e.tensor_tensor(out=hid[:, f, :], in0=s_, in1=ps[pu][:, 0:CS],
                                                                          op=ALU.mult),
                         ['sg%d' % (f % 2), 'ps%d' % pu], ['hid'])
                nq = ex * 4 + q + NRING
                if nq < NEXP * 4:
                    load_gu(nq // 4, nq % 4, slot)
            for st_ in range(4):
                for hh in range(2):
                    for f in range(8):
                        P.mm(ps[4][:, 0:512], hid[:, f, st_ * 128:(st_ + 1) * 128], wd[:, f, hh * 512:(hh + 1) * 512],
                             f == 0, f == 7, ['hid', 'wd'], ['ps4'])
                    P.act(ye[:, st_, hh * 512:(hh + 1) * 512], ps[4][:, 0:512], AF.Copy, ['ps4'], ['xsye'])
            if ex + 1 < NEXP:
                load_d(ex + 1)
            for (q0, n, w) in lblocks:
                P.op('dve', lambda e, ex=ex, q0=q0, n=n: e.tensor_scalar(
                    out=sse[0:16, 0:n], in0=ssel[0:16, q0:q0 + n], scalar1=oh[0:16, ex:ex + 1], scalar2=None,
                    op0=ALU.mult), ['ssel', 'consts'], ['sse'])
                P.op('dve', lambda e, ex=ex, q0=q0, n=n: e.tensor_scalar(
                    out=gme[0:16, 0:n], in0=gmT[0:16, q0:q0 + n], scalar1=oh[0:16, ex:ex + 1], scalar2=None,
                    op0=ALU.mult), ['gmT', 'consts'], ['gme'])
                P.mm(ps[5][:, 0:n], ones16[0:16, :], sse[0:16, 0:n], True, True, ['sse', 'consts'], ['ps5'])
                P.mm(ps[6][:, 0:n], ones16[0:16, :], gme[0:16, 0:n], True, True, ['gme', 'consts'], ['ps6'])
                P.act(gmb[:, 0:n], ps[6][:, 0:n], AF.Copy, ['ps6'], ['gmb'])
                for st_ in range(4):
                    P.op('dve', lambda e, st_=st_, n=n: e.scalar_tensor_tensor(
                        out=SS[st_][:, 0:n], in0=ps[5][:, 0:n], scalar=iotap[:, st_:st_ + 1], in1=gmb[:, 0:n],
                        op0=ALU.is_equal, op1=ALU.mult), ['ps5', 'gmb', 'consts'], ['SS%d' % st_])
                for j in range(8):
                    pi = j % 4
                    for st_ in range(4):
                        P.mm(ps[pi][:, 0:n], ye[:, st_, j * 128:(j + 1) * 128], SS[st_][:, 0:n], st_ == 0, st_ == 3,
                             ['xsye', 'SS%d' % st_], ['ps%d' % pi])
                    if ex == 0:
                        P.op('dve', lambda e, j=j, pi=pi, n=n, q0=q0: e.tensor_copy(out=acc[:, j, q0:q0 + n],
                                                                                   in_=ps[pi][:, 0:n]),
                             ['ps%d' % pi], ['acc'])
                    else:
                        P.op('dve', lambda e, j=j, pi=pi, n=n, q0=q0: e.tensor_tensor(
                            out=acc[:, j, q0:q0 + n], in0=acc[:, j, q0:q0 + n], in1=ps[pi][:, 0:n], op=ALU.add),
                            ['ps%d' % pi, 'acc'], ['acc'])
        P.fence()
        A.release(mL)

        xr = [A.f32(8, 512)] * 2
        x2 = [A.f32(8, 512)] * 2
        on = A.f32(8, 512)
        sq2 = A.bf(8, 512)
        rstd2 = A.f32(512)
        tmp2 = [A.f32(512), A.f32(512)]
        for bi, (q0, n, w) in enumerate(lblocks):
            x, xk = xr[0], 'xr'
            y, yk = x2[0], 'x2'
            P.dma(x[:, :, 0:n], d_x1[:, :, q0:q0 + n], [], [xk])
            for j in range(8):
                P.op('dve', lambda e, j=j, n=n, w=w, q0=q0, x=x, y=y: e.scalar_tensor_tensor(
                    out=y[:, j, 0:n], in0=acc[:, j, q0:q0 + n], scalar=sv[:, 2, j:j + 1, w], in1=x[:, j, 0:n],
                    op0=ALU.mult, op1=ALU.add), [xk, 'acc', 'svt'], [yk])
            P.dma(o_x2[:, :, q0:q0 + n], y[:, :, 0:n], [yk], ['o_x2'])
            C.sumsq_rstd(y[:, :, 0:n], 8, n, D, rstd2, ones, sq2, [yk], 'fn')
            for k in range(8):
                e_ = C.ew()
                t = tmp2[k % 2]
                tk = 'fn_tmp%d' % (k % 2)
                P.op(e_, lambda e, k=k, t=t, n=n, y=y: e.tensor_tensor(out=t[:, 0:n], in0=y[:, k, 0:n], in1=rstd2[:, 0:n],
                                                                      op=ALU.mult), [yk, 'fn_rstd'], [tk])
                P.op(e_, lambda e, k=k, t=t, n=n: e.tensor_scalar(out=on[:, k, 0:n], in0=t[:, 0:n],
                                                                  scalar1=fng[:, k:k + 1], scalar2=None, op0=ALU.mult),
                     [tk, 'vecs'], ['on'])
            P.dma(o_on[:, :, q0:q0 + n], on[:, :, 0:n], ['on'], ['o_on'])
        P.finalize()
        return nc, P, A


def prep_C(inp, l, resAB):
    G = (np.arange(128)[:, None] % 16 == np.arange(128)[None, :] % 16).astype(np.float32)
    cvec = np.ascontiguousarray(np.stack([_fmv(inp['c'][0]), _fmv(inp['c_ctx'])], axis=-1))
    vecs = np.zeros((128, 16), np.float32)
    vecs[:, 0:8] = _fmv(inp['norm2_g'][l])
    vecs[:, 8:16] = _fmv(inp['final_norm_g'])
    pA = np.ascontiguousarray(np.stack([r['probs'][CTX:].T for r in resAB], axis=0).reshape(128, NLOC))
    pC = np.ascontiguousarray(resAB[0]['probs'][:CTX].T)
    iotaf = np.ascontiguousarray(np.broadcast_to(np.arange(1, 513, dtype=np.float32)[None, :], (128, 512)))
    iotap = (np.arange(128, dtype=np.float32)[:, None] + 1 + 128 * np.arange(4, dtype=np.float32)[None, :]).astype(np.float32)
    common = dict(ident=np.eye(128, dtype=np.float32), iotaf=iotaf, iotap=iotap, cvec=cvec, wmod=_fm(np.ascontiguousarray(inp['w_mod'][l][:, 3072:6144])),
                  bmod=_fmv(inp['b_mod'][l][3072:6144]), vecs=vecs, G=G, oh16=np.eye(16, dtype=np.float32),
                  probsA=pA, probsC=pC, wg=inp['w_gate'][l], wu=inp['w_up'][l], wd=inp['w_down'][l])
    maps = []
    for c in range(NCORES):
        m = dict(common)
        m['x1T'] = resAB[c]['x1T']
        m['gmT'] = np.ascontiguousarray(resAB[c]['probs'].T)
        maps.append(m)
    return maps


def kernel(**inp):
    inp = {k: np.asarray(v) for k, v in inp.items()}
    ncAB = build_AB()[0]
    ncC = build_C2()[0]
    xl, xc = inp['x'][0], inp['ctx'][0]
    out = None
    for l in range(2):
        resAB = run_bass_kernel_spmd(ncAB, prep_AB(inp, l, xl, xc), core_ids=list(range(NCORES))).results
        resC = run_bass_kernel_spmd(ncC, prep_C(inp, l, resAB), core_ids=list(range(NCORES))).results
        xl = np.concatenate([_unfm(r['x2T'][:, :, CTX:]) for r in resC], axis=0)
        xc = _unfm(resC[0]['x2T'][:, :, :CTX])
        out = np.concatenate([_unfm(r['outN'][:, :, CTX:]) for r in resC], axis=0)
    return np.ascontiguousarray(out[None].astype(np.float32))
```

```python
import numpy as np
import ml_dtypes
from contextlib import ExitStack
import concourse.bass as bass
import concourse.mybir as mybir
from concourse.bass_utils import run_bass_kernel_spmd

F32 = mybir.dt.float32
BF16 = mybir.dt.bfloat16
AF = mybir.ActivationFunctionType
ALU = mybir.AluOpType

NCORES = 8
D = 1024
SEQ = 16384
CTX = 256
TALL = SEQ + CTX
NLOC = SEQ // NCORES
NL = NLOC + CTX
EPS = 1e-6
NEXP = 16
ATTN_SCALE = 192.0 ** -0.5
ENGS = ['pe', 'act', 'dve', 'pool', 'sp']


def _prod(s):
    r = 1
    for v in s:
        r *= v
    return r


class Prog:
    def __init__(self, nc, ndma=12):
        self.nc = nc
        self.ops = []
        self.lastw = {}
        self.readers = {}
        self.ndma = ndma

    capture = None

    def begin_capture(self):
        self.capture = []

    def end_capture(self):
        c, self.capture = self.capture, None
        return c

    def replay_interleaved(self, lists, nway=2):
        L = max(len(l) for l in lists)
        stride = (L + nway - 1) // nway
        items = []
        for i, l in enumerate(lists):
            for k, call in enumerate(l):
                items.append((i * stride + k, i, call))
        items.sort(key=lambda t: (t[0], t[1]))
        for _, _, call in items:
            self.op(*call)

    def op(self, eng, fn, reads=(), writes=(), dma=False):
        if self.capture is not None:
            self.capture.append((eng, fn, tuple(reads), tuple(writes), dma))
            return -1
        i = len(self.ops)
        deps = set()
        for k in reads:
            if k in self.lastw:
                deps.add(self.lastw[k])
        for k in writes:
            if k in self.lastw:
                deps.add(self.lastw[k])
            deps.update(self.readers.get(k, ()))
        for k in reads:
            self.readers.setdefault(k, []).append(i)
        for k in writes:
            self.lastw[k] = i
            self.readers[k] = []
        self.ops.append(dict(eng=eng, fn=fn, deps=deps, dma=dma))
        return i

    def fence(self):
        self.ops.append(dict(eng=None, fence=True))
        self.lastw = {}
        self.readers = {}

    def mm(self, out, lhsT, rhs, start, stop, reads, writes):
        self.op('pe', lambda e: e.matmul(out, lhsT, rhs, start=start, stop=stop), reads, writes)

    def tr(self, out, in_, ident, reads, writes):
        self.op('pe', lambda e: e.transpose(out, in_, ident), reads, writes)

    def act(self, out, in_, func, reads, writes, bias=None, scale=None, accum=None):
        kw = {}
        if bias is not None:
            kw['bias'] = bias
        if scale is not None:
            kw['scale'] = scale
        if accum is not None:
            kw['accum_out'] = accum
        self.op('act', lambda e: e.activation(out=out, in_=in_, func=func, **kw), reads, writes)

    def dma(self, out, in_, reads, writes, eng='sp'):
        self.op(eng, lambda e: e.dma_start(out=out, in_=in_), reads, writes, dma=True)

    def finalize(self):
        self.fence()
        nc, ops, ndma = self.nc, self.ops, self.ndma
        need = set()
        last = {}
        for i, o in enumerate(ops):
            if o.get('fence'):
                for e, j in last.items():
                    need.add(j)
                continue
            for d in o['deps']:
                Dd = ops[d]
                if Dd['dma']:
                    continue
                if Dd['eng'] == 'pe' and o['eng'] == 'pe' and not o['dma']:
                    continue
                need.add(d)
            if not o['dma']:
                last[o['eng']] = i
        cnt = {e: 0 for e in ENGS}
        dcnt = [0] * ndma
        di = 0
        for i, o in enumerate(ops):
            if o.get('fence'):
                o['snap_cnt'] = dict(cnt)
                o['snap_d'] = list(dcnt)
                continue
            if o['dma']:
                j = di % ndma
                di += 1
                o['dsem'] = j
                o['dprev'] = dcnt[j]
                dcnt[j] += 16
                o['dval'] = dcnt[j]
            elif i in need:
                cnt[o['eng']] += 1
                o['sig'] = cnt[o['eng']]
        self.n_ops = len(ops)
        with ExitStack() as es:
            esem = {e: es.enter_context(nc.semaphore("s_" + e)) for e in ENGS}
            dsem = [es.enter_context(nc.semaphore("d_%d" % j)) for j in range(ndma)]
            block = es.enter_context(nc.Block())

            def emit(ename):
                def body(eng):
                    waited = {}

                    def wait(key, sem, val):
                        if val > waited.get(key, 0):
                            eng.wait_ge(sem, val)
                            waited[key] = val
                    for i, o in enumerate(ops):
                        if o.get('fence'):
                            for e2 in ENGS:
                                if o['snap_cnt'][e2] > 0:
                                    wait(e2, esem[e2], o['snap_cnt'][e2])
                            for j in range(ndma):
                                if o['snap_d'][j] > 0:
                                    wait(('d', j), dsem[j], o['snap_d'][j])
                            continue
                        if o['eng'] != ename:
                            continue
                        for d in sorted(o['deps']):
                            Dd = ops[d]
                            if Dd['dma']:
                                wait(('d', Dd['dsem']), dsem[Dd['dsem']], Dd['dval'])
                            else:
                                if Dd['eng'] == 'pe' and ename == 'pe' and not o['dma']:
                                    continue
                                wait(Dd['eng'], esem[Dd['eng']], Dd['sig'])
                        if o['dma'] and o['dprev'] > 0:
                            wait(('d', o['dsem']), dsem[o['dsem']], o['dprev'])
                        ins = o['fn'](eng)
                        if o['dma']:
                            ins.then_inc(dsem[o['dsem']], 16)
                        elif 'sig' in o:
                            ins.then_inc(esem[ename], 1)
                return body
            block.tensor(emit('pe'))
            block.scalar(emit('act'))
            block.vector(emit('dve'))
            block.gpsimd(emit('pool'))
            block.sync(emit('sp'))


class Arena:
    def __init__(self, t, width):
        self.t = t
        self.top = 0
        self.width = width
        self.peak = 0

    def _shape(self, v, shape):
        if len(shape) == 1:
            return v
        if len(shape) == 2:
            return v.rearrange("p (a b) -> p a b", a=shape[0])
        if len(shape) == 3:
            return v.rearrange("p (a b c) -> p a b c", a=shape[0], b=shape[1])
        raise ValueError

    def f32(self, *shape):
        n = _prod(shape)
        a = self.top
        self.top += n
        self.peak = max(self.peak, self.top)
        assert self.top <= self.width, ("arena overflow", self.top, self.width)
        return self._shape(self.t[:, a:a + n], shape)

    def bf(self, *shape):
        n = _prod(shape)
        nw = (n + 1) // 2
        a = self.top
        self.top += nw
        self.peak = max(self.peak, self.top)
        assert self.top <= self.width, ("arena overflow", self.top, self.width)
        v = self.t[:, a:a + nw].bitcast(BF16)
        return self._shape(v[:, 0:n], shape)

    def mark(self):
        return self.top

    def release(self, m):
        self.top = m


class Ctx:
    def __init__(self, nc, P, A, ps, psb):
        self.nc, self.P, self.A, self.ps, self.psb = nc, P, A, ps, psb
        self.psi = 0
        self.alt = 0
        self.uid = 0

    def key(self, s):
        self.uid += 1
        return "%s#%d" % (s, self.uid)

    ps_range = (0, 6)

    def next_ps(self, lo=None, hi=None):
        if lo is None:
            lo, hi = self.ps_range
        i = lo + self.psi % (hi - lo)
        self.psi += 1
        return i

    def ew(self):
        self.alt += 1
        return 'pool' if self.alt % 4 == 0 else 'dve'

    def load_cast(self, dst_bf, src_dram, ncols, nk, name, scale_col=None, stage=None):
        P = self.P
        for k in range(nk):
            st, sk = stage[k % 2]
            P.dma(st[:, 0:ncols], src_dram[:, k, :], reads=[], writes=[sk])
            e = self.ew()
            if scale_col is None:
                P.op(e, lambda en, st=st, k=k: en.tensor_copy(out=dst_bf[:, k, :], in_=st[:, 0:ncols]),
                     reads=[sk], writes=[name])
            else:
                P.op(e, lambda en, st=st, k=k: en.tensor_scalar(out=dst_bf[:, k, :], in0=st[:, 0:ncols],
                                                                scalar1=scale_col[:, k:k + 1], scalar2=None,
                                                                op0=ALU.mult),
                     reads=[sk, 'modv'], writes=[name])

    def sumsq_rstd(self, src_f32, nk, n, dim, out_rstd, ones_bf, sq_bf, rk, name):
        P = self.P
        P.act(sq_bf[:, 0:nk, 0:n], src_f32, AF.Square, reads=rk, writes=[name + '_sq'])
        pi = self.next_ps()
        pk = 'ps%d' % pi
        for k in range(nk):
            P.mm(self.ps[pi][:, 0:n], ones_bf, sq_bf[:, k, 0:n], k == 0, k == nk - 1,
                 reads=[name + '_sq', 'consts'], writes=[pk])
        P.act(out_rstd[:, 0:n], self.ps[pi][:, 0:n], AF.Ln, reads=[pk], writes=[name + '_rstd'], scale=1.0 / dim, bias=EPS)
        P.act(out_rstd[:, 0:n], out_rstd[:, 0:n], AF.Exp, reads=[name + '_rstd'], writes=[name + '_rstd'], scale=-0.5)

    def norm_mod(self, x_f32, n, rstd, s_col, sh_col, out, tmp_f32, rk, wk, name):
        P = self.P
        for k in range(8):
            e = self.ew()
            tk = "%s_tmp%d" % (name, k % 2)
            t = tmp_f32[k % 2]
            P.op(e, lambda en, k=k, t=t: en.tensor_tensor(out=t[:, 0:n], in0=x_f32[:, k, 0:n], in1=rstd[:, 0:n],
                                                          op=ALU.mult),
                 reads=rk + [name + '_rstd'], writes=[tk])
            P.op(e, lambda en, k=k, t=t: en.tensor_scalar(out=out[:, k, 0:n], in0=t[:, 0:n],
                                                          scalar1=s_col[:, k:k + 1], scalar2=sh_col[:, k:k + 1],
                                                          op0=ALU.mult, op1=ALU.add),
                 reads=[tk, 'modv'], writes=wk)

    def compute_mods(self, d_cvec, d_wmod, d_bmod, nblk, modT, cs, stage):
        P = self.P
        P.dma(cs[:, 0:16], d_cvec.rearrange("p k w -> p (k w)"), reads=[], writes=['cs'])
        P.act(cs[:, 16:32], cs[:, 0:16], AF.Sigmoid, reads=['cs'], writes=['cs2'])
        P.op('dve', lambda e: e.tensor_tensor(out=cs[:, 0:16], in0=cs[:, 0:16], in1=cs[:, 16:32], op=ALU.mult),
             reads=['cs2', 'cs'], writes=['cs'])
        P.dma(cs[:, 32:32 + nblk * 8], d_bmod, reads=[], writes=['bmod'])
        csv = cs[:, 0:16].rearrange("p (k w) -> p k w", k=8)
        nj = nblk * 8
        for b in range(nblk):
            P.dma(stage[:, :, :], d_wmod[:, :, b * 1024:(b + 1) * 1024], reads=[], writes=['wm_st'])
            for j in range(8):
                c0 = 2 * (b * 8 + j)
                for k in range(8):
                    P.mm(self.ps[6][:, c0:c0 + 2], stage[:, k, j * 128:(j + 1) * 128],
                         csv[:, k, :], k == 0, k == 7, reads=['wm_st', 'cs'], writes=['ps6'])
        psv = self.ps[6][:, 0:2 * nj].rearrange("p (j w) -> p j w", w=2)
        for w in range(2):
            P.op('dve', lambda e, w=w: e.tensor_tensor(out=modT[:, 0:nj, w], in0=psv[:, :, w],
                                                       in1=cs[:, 32:32 + nj], op=ALU.add),
                 reads=['ps6', 'bmod'], writes=['modv'])


def build_AB():
    nc = bass.Bass("TRN2", target_bir_lowering=False)

    def din(name, shape, dt=F32):
        return nc.dram_tensor(name, list(shape), dt, kind="ExternalInput").ap()
    d_xall = din("xall", [128, 8, TALL])
    d_xloc = din("xloc", [128, 8, NLOC])
    d_cvec = din("cvec", [128, 8, 2])
    d_wmod = din("wmod", [128, 8, 5120])
    d_bmod = din("bmod", [128, 40])
    d_vecs = din("vecs", [128, 64])
    d_wA = din("wA", [128, 8, 640])
    d_wB = din("wB", [128, 8, 1152])
    d_wuq = din("wuq", [128, 3, 1024])
    d_wukv = din("wukv", [128, 2, 1024])
    d_wout = din("wout", [128, 8, 1024])
    d_lruw = din("lruw", [128, 8, 128])
    d_sguw = din("sguw", [128, 4, 128])
    d_sgub = din("sgub", [128, 2, 128])
    d_wr = din("wr", [128, 8, 16])
    d_ropeC = din("ropeC", [64, SEQ])
    d_ropeS = din("ropeS", [64, SEQ])
    d_ropeCl = din("ropeCl", [64, NLOC])
    d_ropeSl = din("ropeSl", [64, NLOC])
    d_ident = din("ident", [128, 128])
    o_x1 = nc.dram_tensor("x1T", [128, 8, NL], F32, kind="ExternalOutput").ap()
    o_probs = nc.dram_tensor("probs", [NL, NEXP], F32, kind="ExternalOutput").ap()
    s_xb = nc.dram_tensor("xb_s", [2, 128, TALL], F32).ap()
    s_KT = nc.dram_tensor("KT_s", [4, 128, TALL], BF16).ap()
    s_kr = nc.dram_tensor("kr_s", [64, TALL], BF16).ap()
    s_V = nc.dram_tensor("V_s", [4, 128, TALL // 128, 128], BF16).ap()

    AW = 51 * 1024
    with ExitStack() as es:
        arena_t = es.enter_context(nc.sbuf_tensor("arena", [128, AW], F32))
        ps = [es.enter_context(nc.psum_tensor("ps%d" % i, [128, 512], F32)) for i in range(7)]
        psb = es.enter_context(nc.psum_tensor("psb", [128, 1024], BF16))
        P = Prog(nc)
        A = Arena(arena_t, AW)
        C = Ctx(nc, P, A, ps, psb)

        vecs = A.f32(64)
        modT = A.f32(40, 2)
        cs = A.f32(80)
        sv = A.f32(6, 8, 2)
        spv = A.f32(4)
        ident = A.bf(128)
        ones = A.bf(128)
        onesf = A.f32(128)
        m0 = A.mark()
        P.dma(vecs, d_vecs, [], ['vecs'])
        P.op('pool', lambda e: e.memset(onesf, 1.0), [], ['onesf'])
        stage8 = A.f32(8, 1024)
        C.compute_mods(d_cvec, d_wmod, d_bmod, 5, modT, cs, stage8)
        P.dma(stage8[:, 0, 0:128], d_ident, [], ['wm_st'])
        P.op('dve', lambda e: e.tensor_copy(out=ident, in_=stage8[:, 0, 0:128]), ['wm_st'], ['consts'])
        P.op('dve', lambda e: e.memset(ones, 1.0), [], ['consts'])
        n1g, n2g = vecs[:, 0:8], vecs[:, 8:16]
        lrub, lam, convw, convb = vecs[:, 16:24], vecs[:, 24:28], vecs[:, 28:36], vecs[:, 36:38]
        qng, kvng, cmask = vecs[:, 38:41], vecs[:, 41:43], vecs[:, 43:51]
        mv = modT.rearrange("p (b k) w -> p b k w", b=5)
        for w in range(2):
            for (dst, scb, g) in ((0, 1, n1g), (3, 4, n2g)):
                P.op('dve', lambda e, w=w, dst=dst, scb=scb, g=g: e.scalar_tensor_tensor(
                    out=sv[:, dst, :, w], in0=mv[:, scb, :, w], scalar=1.0, in1=g, op0=ALU.add, op1=ALU.mult),
                    ['modv', 'vecs'], ['svt'])
            for (dst, src) in ((1, 0), (2, 2), (4, 3)):
                P.op('dve', lambda e, w=w, dst=dst, src=src: e.tensor_copy(out=sv[:, dst, :, w], in_=mv[:, src, :, w]),
                     ['modv'], ['svt'])
        P.act(spv, lam, AF.Exp, ['vecs'], ['spv'], scale=-1.0)
        P.act(spv, spv, AF.Ln, ['spv'], ['spv'], bias=1.0)
        P.op('dve', lambda e: e.tensor_single_scalar(out=spv, in_=spv, scalar=-8.0, op=ALU.mult), ['spv'], ['spv'])
        P.fence()
        A.release(m0)

        def svc(idx, w):
            return sv[:, idx, :, w]

        m1 = A.mark()
        wA_bf = A.bf(8, 640)
        wukv_bf = A.bf(2, 1024)
        wst = [(A.f32(1024), 'wst0'), (A.f32(1024), 'wst1')]

        def p1set():
            return dict(xs=A.f32(8, 512), sq=A.bf(8, 512), rstd=A.f32(512), tmpf=[A.f32(512), A.f32(512)],
                        h=A.bf(8, 512), xo=A.f32(2, 512), ckv=A.f32(2, 512), ckv_sq=A.bf(2, 512), rstd2=A.f32(512),
                        ckvn=A.bf(2, 512), Ko=A.bf(4, 512), Vo=A.bf(4, 4, 128), krf=A.f32(2, 512), rC=A.f32(512),
                        rS=A.f32(512), ko=A.bf(512))
        B1 = [p1set(), p1set()]
        X3 = [B1[0]['xs'], B1[1]['xs'], A.f32(8, 512)]
        C.load_cast(wukv_bf, d_wukv, 1024, 2, 'wukv', stage=wst)
        C.load_cast(wA_bf, d_wA, 640, 8, 'wA', scale_col=None, stage=wst)
        blocks = [(0, CTX, 1)] + [(CTX + 512 * i, 512, 0) for i in range(SEQ // 512)]
        caps = []
        for bi, (t0, n, w) in enumerate(blocks):
            B = B1[bi % 2]
            sx = str(bi % 2)
            C.ps_range = (0, 3) if bi % 2 == 0 else (3, 6)
            if bi == 0:
                for b2 in range(2):
                    t02, n2, _ = blocks[b2]
                    P.dma(X3[b2][:, :, 0:n2], d_xall[:, :, t02:t02 + n2], [], ['xst%d' % b2])
            P.begin_capture()
            xs, xk = X3[bi % 3], 'xst%d' % (bi % 3)
            if bi + 2 < len(blocks):
                t02, n2, _ = blocks[bi + 2]
                P.dma(X3[(bi + 2) % 3][:, :, 0:n2], d_xall[:, :, t02:t02 + n2], [], ['xst%d' % ((bi + 2) % 3)])
            C.sumsq_rstd(xs[:, :, 0:n], 8, n, D, B['rstd'], ones, B['sq'], [xk], 'n1' + sx)
            C.norm_mod(xs, n, B['rstd'], svc(0, w), svc(1, w), B['h'], B['tmpf'], [xk], ['h' + sx], 'n1' + sx)
            h_bf, xo, ckv, krf, ckvn = B['h'], B['xo'], B['ckv'], B['krf'], B['ckvn']
            for ct in range(6):
                M = 128 if ct < 4 else 64
                c0 = ct * 128 if ct < 4 else 512 + (ct - 4) * 64
                pi = C.next_ps()
                pk = 'ps%d' % pi
                for k in range(8):
                    P.mm(ps[pi][0:M, 0:n], wA_bf[:, k, c0:c0 + M], h_bf[:, k, 0:n], k == 0, k == 7,
                         ['wA', 'h' + sx], [pk])
                if ct < 2:
                    P.act(xo[:, ct, 0:n], ps[pi][:, 0:n], AF.Copy, [pk], ['xbo' + sx])
                elif ct < 4:
                    P.op('dve', lambda e, pi=pi, ct=ct, n=n, ckv=ckv: e.tensor_copy(out=ckv[:, ct - 2, 0:n],
                                                                                 in_=ps[pi][:, 0:n]),
                         [pk], ['ckv' + sx])
                else:
                    P.op('dve', lambda e, pi=pi, ct=ct, n=n, krf=krf: e.tensor_copy(out=krf[0:64, ct - 4, 0:n],
                                                                                 in_=ps[pi][0:64, 0:n]),
                         [pk], ['krf' + sx])
            P.dma(s_xb[:, :, t0:t0 + n].rearrange("c p t -> p c t"), xo[:, :, 0:n], ['xbo' + sx], ['s_xb%d' % bi])
            C.sumsq_rstd(ckv[:, :, 0:n], 2, n, 256, B['rstd2'], ones, B['ckv_sq'], ['ckv' + sx], 'kvn' + sx)
            for k in range(2):
                P.op('dve', lambda e, k=k, n=n, ckv=ckv, B=B: e.tensor_tensor(out=ckv[:, k, 0:n], in0=ckv[:, k, 0:n],
                                                                           in1=B['rstd2'][:, 0:n], op=ALU.mult),
                     ['ckv' + sx, 'kvn' + sx + '_rstd'], ['ckv' + sx])
                P.act(ckvn[:, k, 0:n], ckv[:, k, 0:n], AF.Copy, ['ckv' + sx, 'vecs'], ['ckvn' + sx],
                      scale=kvng[:, k:k + 1])
            Ko, Kk = B['Ko'], 'Ko' + sx
            for h in range(4):
                pi = C.next_ps()
                pk = 'ps%d' % pi
                for k in range(2):
                    P.mm(ps[pi][:, 0:n], wukv_bf[:, k, h * 128:(h + 1) * 128], ckvn[:, k, 0:n], k == 0, k == 1,
                         ['wukv', 'ckvn' + sx], [pk])
                P.act(Ko[:, h, 0:n], ps[pi][:, 0:n], AF.Copy, [pk], [Kk])
            P.dma(s_KT[:, :, t0:t0 + n].rearrange("h p t -> p h t"), Ko[:, :, 0:n], [Kk], ['s_KT%d' % bi])
            Vo, Vk = B['Vo'], 'Vo' + sx
            for tt in range(n // 128):
                pi = C.next_ps()
                pk = 'ps%d' % pi
                for k in range(2):
                    P.mm(ps[pi][:, 0:512], ckvn[:, k, tt * 128:(tt + 1) * 128], wukv_bf[:, k, 512:1024], k == 0, k == 1,
                         ['wukv', 'ckvn' + sx], [pk])
                P.op('dve', lambda e, pi=pi, tt=tt, Vo=Vo: e.tensor_copy(
                    out=Vo[:, :, tt, :], in_=ps[pi][:, 0:512].rearrange("p (h c) -> p h c", h=4)), [pk], [Vk])
            nt = n // 128
            for h in range(4):
                P.dma(s_V[h, :, t0 // 128:t0 // 128 + nt, :], Vo[:, h, 0:nt, :], [Vk], ['s_V%d_%d' % (bi, h)])
            ko, kk = B['ko'], 'kro' + sx
            if w == 0:
                l0 = t0 - CTX
                rC, rS = B['rC'], B['rS']
                P.dma(rC[0:64, 0:n], d_ropeC[:, l0:l0 + n], [], ['rC' + sx])
                P.dma(rS[0:64, 0:n], d_ropeS[:, l0:l0 + n], [], ['rS' + sx])
                P.op('pool', lambda e, n=n, krf=krf, rC=rC: e.tensor_tensor(out=krf[0:64, 0, 0:n], in0=krf[0:64, 0, 0:n],
                                                                          in1=rC[0:64, 0:n], op=ALU.mult),
                     ['krf' + sx, 'rC' + sx], ['krf' + sx])
                P.op('pool', lambda e, n=n, krf=krf, rS=rS: e.tensor_tensor(out=krf[0:64, 1, 0:n], in0=krf[0:64, 1, 0:n],
                                                                          in1=rS[0:64, 0:n], op=ALU.mult),
                     ['krf' + sx, 'rS' + sx], ['krf' + sx])
                P.op('pool', lambda e, n=n, ko=ko, krf=krf: e.tensor_tensor(out=ko[0:64, 0:n], in0=krf[0:64, 0, 0:n],
                                                                          in1=krf[0:64, 1, 0:n], op=ALU.add),
                     ['krf' + sx], [kk])
            else:
                P.op('pool', lambda e, n=n, ko=ko, krf=krf: e.tensor_copy(out=ko[0:64, 0:n], in_=krf[0:64, 0, 0:n]),
                     ['krf' + sx], [kk])
            P.dma(s_kr[:, t0:t0 + n], ko[0:64, 0:n], [kk], ['s_kr%d' % bi])
            caps.append(P.end_capture())
        C.ps_range = (0, 6)
        P.replay_interleaved(caps, 2)
        P.fence()
        A.release(m1)

        mixT = A.bf(8, NL)
        qTn = A.bf(4, NL)
        qTr = A.bf(4, NL)
        mP = A.mark()
        ysum = A.f32(2, NL)
        m2 = A.mark()
        lruw_bf = A.bf(8, 128)
        C.load_cast(lruw_bf, d_lruw, 128, 8, 'lruw', stage=[(A.f32(128), 'lst0'), (A.f32(128), 'lst1')])
        NCH = 1024
        NCK = SEQ // NCH

        def p2set():
            return dict(xi=A.f32(NCH + 3), cl=A.f32(NCH), clb=A.bf(NCH), rr=A.f32(NCH), ii=A.f32(NCH), aa=A.f32(NCH),
                        t1=A.f32(NCH), t2=A.f32(NCH), hh=A.f32(NCH))
        B2 = [p2set(), p2set()]
        carry = A.f32(4)
        chunks = [(0, CTX, True, True, -1)] + [(CTX + NCH * j, NCH, j == 0, j == NCK - 1, j) for j in range(NCK)]
        CPC = NLOC // NCH
        ci = 0
        first_lat = {}
        caps2 = []
        for ct in range(2):
            for d in range(2):
                order = chunks if d == 0 else [chunks[0]] + chunks[:0:-1]
                cv = carry[:, ct * 2 + d:ct * 2 + d + 1]
                for qi, (t0, n, lz, rz, j) in enumerate(order):
                    B = B2[ci % 2]
                    sx = str(ci % 2)
                    C.ps_range = (0, 3) if ci % 2 == 0 else (3, 6)
                    ci += 1
                    P.begin_capture()
                    xi, cl, clb, rr, ii, aa, t1, t2, hh = (B['xi'], B['cl'], B['clb'], B['rr'], B['ii'], B['aa'], B['t1'],
                                                           B['t2'], B['hh'])
                    xk = 'xin' + sx
                    lo = 0 if lz else 2
                    ro = 0 if rz else 1
                    P.dma(xi[:, 2 - lo:2 + n + ro], s_xb[ct, :, t0 - lo:t0 + n + ro], [], [xk])
                    if lz:
                        P.op('pool', lambda e, xi=xi: e.memset(xi[:, 0:2], 0.0), [], [xk])
                    if rz:
                        P.op('pool', lambda e, xi=xi, n=n: e.memset(xi[:, n + 2:n + 3], 0.0), [], [xk])
                    P.op('dve', lambda e, xi=xi, n=n, ct=ct, cl=cl: e.tensor_scalar(
                        out=cl[:, 0:n], in0=xi[:, 0:n], scalar1=convw[:, ct * 4:ct * 4 + 1],
                        scalar2=convb[:, ct:ct + 1], op0=ALU.mult, op1=ALU.add), [xk, 'vecs'], ['cl' + sx])
                    for k in range(1, 4):
                        P.op('dve', lambda e, xi=xi, n=n, k=k, ct=ct, cl=cl: e.scalar_tensor_tensor(
                            out=cl[:, 0:n], in0=xi[:, k:k + n], scalar=convw[:, ct * 4 + k:ct * 4 + k + 1],
                            in1=cl[:, 0:n], op0=ALU.mult, op1=ALU.add), [xk, 'vecs', 'cl' + sx], ['cl' + sx])
                    P.act(clb[:, 0:n], cl[:, 0:n], AF.Copy, ['cl' + sx], ['clb' + sx])
                    for g, dst, dk in ((0, rr, 'rr' + sx), (1, ii, 'ii' + sx)):
                        wi = d * 4 + g * 2 + ct
                        for sb in range((n + 511) // 512):
                            nn = min(512, n - sb * 512)
                            pi = C.next_ps()
                            pk = 'ps%d' % pi
                            P.mm(ps[pi][:, 0:nn], lruw_bf[:, wi, :], clb[:, sb * 512:sb * 512 + nn], True, True,
                                 ['lruw', 'clb' + sx], [pk])
                            P.act(dst[:, sb * 512:sb * 512 + nn], ps[pi][:, 0:nn], AF.Sigmoid, [pk, 'vecs'], [dk],
                                  bias=lrub[:, wi:wi + 1])
                    P.act(aa[:, 0:n], rr[:, 0:n], AF.Exp, ['rr' + sx, 'spv'], ['aa' + sx],
                          scale=spv[:, d * 2 + ct:d * 2 + ct + 1])
                    P.op('pool', lambda e, n=n, t1=t1, aa=aa: e.tensor_tensor(out=t1[:, 0:n], in0=aa[:, 0:n],
                                                                            in1=aa[:, 0:n], op=ALU.mult),
                         ['aa' + sx], ['t1' + sx])
                    P.act(t1[:, 0:n], t1[:, 0:n], AF.Sqrt, ['t1' + sx], ['t1' + sx], scale=-1.0, bias=1.0)
                    P.op('pool', lambda e, n=n, t2=t2, ii=ii, cl=cl: e.tensor_tensor(out=t2[:, 0:n], in0=ii[:, 0:n],
                                                                                   in1=cl[:, 0:n], op=ALU.mult),
                         ['ii' + sx, 'cl' + sx], ['t2' + sx])
                    P.op('pool', lambda e, n=n, t2=t2, t1=t1: e.tensor_tensor(out=t2[:, 0:n], in0=t2[:, 0:n],
                                                                            in1=t1[:, 0:n], op=ALU.mult),
                         ['t2' + sx, 't1' + sx], ['t2' + sx])
                    init = 0.0 if qi == 0 else cv
                    if d == 0:
                        P.op('dve', lambda e, n=n, init=init, hh=hh, aa=aa, t2=t2: e.tensor_tensor_scan(
                            out=hh[:, 0:n], data0=aa[:, 0:n], data1=t2[:, 0:n], initial=init, op0=ALU.mult,
                            op1=ALU.add), ['aa' + sx, 't2' + sx, 'carry'], ['hh' + sx])
                        P.op('dve', lambda e, n=n, cv=cv, hh=hh: e.tensor_copy(out=cv, in_=hh[:, n - 1:n]),
                             ['hh' + sx], ['carry'])
                    else:
                        P.op('dve', lambda e, n=n, init=init, hh=hh, aa=aa, t2=t2: e.tensor_tensor_scan(
                            out=hh[:, 0:n][:, ::-1], data0=aa[:, 0:n][:, ::-1], data1=t2[:, 0:n][:, ::-1],
                            initial=init, op0=ALU.mult, op1=ALU.add), ['aa' + sx, 't2' + sx, 'carry'], ['hh' + sx])
                        P.op('dve', lambda e, cv=cv, hh=hh: e.tensor_copy(out=cv, in_=hh[:, 0:1]), ['hh' + sx], ['carry'])
                    if j < 0:
                        if d == 0:
                            P.op('pool', lambda e, n=n, ct=ct, hh=hh: e.tensor_copy(out=ysum[:, ct, 0:n], in_=hh[:, 0:n]),
                                 ['hh' + sx], ['ysum'])
                        else:
                            P.op('pool', lambda e, n=n, ct=ct, hh=hh: e.tensor_tensor(
                                out=ysum[:, ct, 0:n], in0=ysum[:, ct, 0:n], in1=hh[:, 0:n], op=ALU.add),
                                ['hh' + sx, 'ysum'], ['ysum'])
                    else:
                        jc, off = j // CPC, CTX + (j % CPC) * NCH
                        key = (ct, j % CPC)
                        if key not in first_lat:
                            first_lat[key] = True
                            P.op('dve', lambda e, n=n, jc=jc, off=off, ct=ct, hh=hh: e.tensor_scalar(
                                out=ysum[:, ct, off:off + n], in0=hh[:, 0:n], scalar1=cmask[:, jc:jc + 1], scalar2=None,
                                op0=ALU.mult), ['hh' + sx, 'vecs'], ['ysum'])
                        else:
                            P.op('dve', lambda e, n=n, jc=jc, off=off, ct=ct, hh=hh: e.scalar_tensor_tensor(
                                out=ysum[:, ct, off:off + n], in0=hh[:, 0:n], scalar=cmask[:, jc:jc + 1],
                                in1=ysum[:, ct, off:off + n], op0=ALU.mult, op1=ALU.add),
                                ['hh' + sx, 'vecs', 'ysum'], ['ysum'])
                    caps2.append(P.end_capture())
        C.ps_range = (0, 6)
        P.replay_interleaved(caps2, 2)
        P.fence()
        A.release(m2)

        wB_bf = A.bf(8, 1152)
        wuq_bf = A.bf(3, 1024)
        sguw_bf = A.bf(4, 128)
        sgub = A.f32(2, 128)
        m3 = A.mark()
        wst3 = [(A.f32(1152), 'wst3_0'), (A.f32(1152), 'wst3_1')]
        C.load_cast(wB_bf, d_wB, 1152, 8, 'wB', stage=wst3)
        C.load_cast(wuq_bf, d_wuq, 1024, 3, 'wuq', stage=wst3)
        C.load_cast(sguw_bf, d_sguw, 128, 4, 'sguw', stage=wst3)
        P.dma(sgub, d_sgub, [], ['sgub'])
        P.fence()
        A.release(m3)
        xs3 = A.f32(8, 512)
        sq3 = A.bf(8, 512)
        rstd3 = A.f32(512)
        tmp3 = [A.f32(512), A.f32(512)]
        h3 = A.bf(8, 512)
        u_bf = A.bf(2, 512)
        vf = A.f32(2, 512)
        vn_bf = A.bf(2, 512)
        vtok = A.bf(256)
        gbf = A.f32(2, 512)
        cqf = A.f32(3, 512)
        cqn = A.bf(3, 512)
        rstdq = A.f32(512)
        rstdv = A.f32(512)
        sqv = A.bf(2, 512)
        sqq = A.bf(3, 512)
        rCl = A.f32(512)
        rSl = A.f32(512)
        tq1 = A.f32(512)
        tq2 = A.f32(512)
        tg = A.f32(128)
        lblocks = [(d_xall[:, :, 0:CTX], CTX, 1, 0, -1)] + \
                  [(d_xloc[:, :, 512 * i:512 * (i + 1)], 512, 0, CTX + 512 * i, 512 * i) for i in range(NLOC // 512)]
        for (src, n, w, q0, l0) in lblocks:
            P.dma(xs3[:, :, 0:n], src, [], ['xs3'])
            C.sumsq_rstd(xs3[:, :, 0:n], 8, n, D, rstd3, ones, sq3, ['xs3'], 'n3')
            C.norm_mod(xs3, n, rstd3, svc(0, w), svc(1, w), h3, tmp3, ['xs3'], ['h3'], 'n3')
            for ct in range(9):
                pi = C.next_ps()
                pk = 'ps%d' % pi
                for k in range(8):
                    P.mm(ps[pi][:, 0:n], wB_bf[:, k, ct * 128:(ct + 1) * 128], h3[:, k, 0:n], k == 0, k == 7,
                         ['wB', 'h3'], [pk])
                if ct < 2:
                    P.act(u_bf[:, ct, 0:n], ps[pi][:, 0:n], AF.Gelu_apprx_tanh, [pk], ['u'])
                elif ct < 4:
                    P.act(vf[:, ct - 2, 0:n], ps[pi][:, 0:n], AF.Gelu_apprx_tanh, [pk], ['vf'])
                elif ct < 6:
                    P.act(gbf[:, ct - 4, 0:n], ps[pi][:, 0:n], AF.Gelu_apprx_tanh, [pk], ['gbf'])
                    P.op('dve', lambda e, ct=ct, n=n, q0=q0: e.tensor_tensor(
                        out=mixT[:, 2 + ct - 4, q0:q0 + n], in0=gbf[:, ct - 4, 0:n], in1=ysum[:, ct - 4, q0:q0 + n],
                        op=ALU.mult), ['gbf', 'ysum'], ['mix_b'])
                else:
                    P.op('dve', lambda e, ct=ct, n=n, pi=pi: e.tensor_copy(out=cqf[:, ct - 6, 0:n], in_=ps[pi][:, 0:n]),
                         [pk], ['cqf'])
            C.sumsq_rstd(vf[:, :, 0:n], 2, n, 256, rstdv, ones, sqv, ['vf'], 'vn')
            for k in range(2):
                P.op('dve', lambda e, k=k, n=n: e.tensor_tensor(out=vn_bf[:, k, 0:n], in0=vf[:, k, 0:n],
                                                                in1=rstdv[:, 0:n], op=ALU.mult),
                     ['vf', 'vn_rstd'], ['vn'])
            for tt in range(n // 128):
                for k in range(2):
                    P.tr(psb[:, k * 128:(k + 1) * 128], vn_bf[:, k, tt * 128:(tt + 1) * 128], ident,
                         ['vn', 'consts'], ['psb'])
                P.op('dve', lambda e: e.tensor_copy(out=vtok, in_=psb[:, 0:256]), ['psb'], ['vtok'])
                pi = C.next_ps()
                pk = 'ps%d' % pi
                for g in range(4):
                    P.mm(ps[pi][(g % 2) * 64:(g % 2) * 64 + 64, (g // 2) * 128:(g // 2) * 128 + 128],
                         vtok[:, g * 64:(g + 1) * 64], sguw_bf[:, g, :], True, True, ['vtok', 'sguw'], [pk])
                for c2 in range(2):
                    P.op('dve', lambda e, c2=c2, pi=pi: e.tensor_tensor(out=tg, in0=ps[pi][:, c2 * 128:(c2 + 1) * 128],
                                                                        in1=sgub[:, c2, :], op=ALU.add),
                         [pk, 'sgub'], ['tg'])
                    P.op('dve', lambda e, c2=c2, tt=tt, q0=q0: e.tensor_tensor(
                        out=mixT[:, c2, q0 + tt * 128:q0 + (tt + 1) * 128], in0=tg,
                        in1=u_bf[:, c2, tt * 128:(tt + 1) * 128], op=ALU.mult), ['tg', 'u'], ['mix_a'])
            C.sumsq_rstd(cqf[:, :, 0:n], 3, n, 384, rstdq, ones, sqq, ['cqf'], 'qn')
            for k in range(3):
                P.op('dve', lambda e, k=k, n=n: e.tensor_tensor(out=cqf[:, k, 0:n], in0=cqf[:, k, 0:n],
                                                                in1=rstdq[:, 0:n], op=ALU.mult),
                     ['cqf', 'qn_rstd'], ['cqf'])
                P.act(cqn[:, k, 0:n], cqf[:, k, 0:n], AF.Copy, ['cqf', 'vecs'], ['cqn'], scale=qng[:, k:k + 1])
            if w == 0:
                P.dma(rCl[0:64, 0:n], d_ropeCl[:, l0:l0 + n], [], ['rCl'])
                P.dma(rSl[0:64, 0:n], d_ropeSl[:, l0:l0 + n], [], ['rSl'])
            for h in range(4):
                pi = C.next_ps()
                pk = 'ps%d' % pi
                for k in range(3):
                    P.mm(ps[pi][:, 0:n], wuq_bf[:, k, h * 256:h * 256 + 128], cqn[:, k, 0:n], k == 0, k == 2,
                         ['wuq', 'cqn'], [pk])
                P.act(qTn[:, h, q0:q0 + n], ps[pi][:, 0:n], AF.Copy, [pk], ['qTn'])
                pa = C.next_ps()
                pak = 'ps%d' % pa
                for k in range(3):
                    P.mm(ps[pa][0:64, 0:n], wuq_bf[:, k, h * 256 + 128:h * 256 + 192], cqn[:, k, 0:n], k == 0, k == 2,
                         ['wuq', 'cqn'], [pak])
                if w == 1:
                    P.act(qTr[0:64, h, q0:q0 + n], ps[pa][0:64, 0:n], AF.Copy, [pak], ['qTr'])
                else:
                    pb = C.next_ps()
                    pbk = 'ps%d' % pb
                    for k in range(3):
                        P.mm(ps[pb][0:64, 0:n], wuq_bf[:, k, h * 256 + 192:h * 256 + 256], cqn[:, k, 0:n], k == 0,
                             k == 2, ['wuq', 'cqn'], [pbk])
                    P.op('dve', lambda e, pa=pa, n=n: e.tensor_tensor(out=tq1[0:64, 0:n], in0=ps[pa][0:64, 0:n],
                                                                      in1=rCl[0:64, 0:n], op=ALU.mult),
                         [pak, 'rCl'], ['tq1'])
                    P.op('dve', lambda e, pb=pb, n=n: e.tensor_tensor(out=tq2[0:64, 0:n], in0=ps[pb][0:64, 0:n],
                                                                      in1=rSl[0:64, 0:n], op=ALU.mult),
                         [pbk, 'rSl'], ['tq2'])
                    P.op('pool', lambda e, h=h, n=n, q0=q0: e.tensor_tensor(
                        out=qTr[0:64, h, q0:q0 + n], in0=tq1[0:64, 0:n], in1=tq2[0:64, 0:n], op=ALU.add),
                        ['tq1', 'tq2'], ['qTr'])
        P.fence()
        A.release(mP)

        NKT = TALL // 128
        KT = A.bf(TALL)
        Vh = A.bf(NKT, 128)
        krT = A.bf(TALL)
        PT = [A.bf(512) for _ in range(6)]
        rinv = A.f32(512)
        lacc = [[A.f32(512) for _ in range(3)] for _ in range(2)]
        NPC = 5
        KPP = NKT // NPC
        for pc in range(NPC):
            a, b = pc * KPP * 128, (pc + 1) * KPP * 128
            P.dma(krT[0:64, a:b], s_kr[:, a:b], ['s_kr'], ['kr_p%d' % pc])
        qblocks = [(0, CTX, 2)] + [(CTX + 512 * i, 512, NKT) for i in range(NLOC // 512)]

        def load_kv(h, pc):
            a, b = pc * KPP * 128, (pc + 1) * KPP * 128
            P.dma(KT[:, a:b], s_KT[h, :, a:b], ['s_KT'], ['KT_p%d' % pc])
            P.dma(Vh[:, pc * KPP:(pc + 1) * KPP, :], s_V[h, :, pc * KPP:(pc + 1) * KPP, :], ['s_V'], ['V_p%d' % pc])
        tiles = []
        qbi = 0
        for h in range(4):
            for qi, (q0, n, nkt) in enumerate(qblocks):
                po, pl = 3 + qbi % 2, 5 + qbi % 2
                qbi += 1
                for kt in range(nkt):
                    tiles.append((h, qi, q0, n, nkt, kt, po, pl))

        def emit_qk(i):
            h, qi, q0, n, nkt, kt, po, pl = tiles[i]
            sb = i % 3
            sk = 'ps%d' % sb
            pc = kt // KPP
            P.mm(ps[sb][:, 0:n], KT[:, kt * 128:(kt + 1) * 128], qTn[:, h, q0:q0 + n], True, False,
                 ['KT_p%d' % pc, 'qTn'], [sk])
            P.mm(ps[sb][:, 0:n], krT[0:64, kt * 128:(kt + 1) * 128], qTr[0:64, h, q0:q0 + n], False, True,
                 ['kr_p%d' % pc, 'qTr'], [sk])
        LA = 2
        for pc in range(NPC):
            load_kv(0, pc)
        for i in range(LA):
            emit_qk(i)
        for i, (h, qi, q0, n, nkt, kt, po, pl) in enumerate(tiles):
            if i + LA < len(tiles):
                emit_qk(i + LA)
            sb = i % 3
            pc = kt // KPP
            pt = PT[i % 6]
            ptk = 'PT%d' % (i % 6)
            P.act(pt[:, 0:n], ps[sb][:, 0:n], AF.Exp, ['ps%d' % sb], [ptk], scale=ATTN_SCALE)
            P.mm(ps[po][:, 0:n], Vh[:, kt, :], pt[:, 0:n], kt == 0, kt == nkt - 1, ['V_p%d' % pc, ptk], ['ps%d' % po])
            c3 = kt % 3
            la, lak = lacc[po - 3][c3], 'lacc%d_%d' % (po - 3, c3)
            le = 'pool' if c3 == 2 else 'dve'
            if kt < 3:
                P.op(le, lambda e, la=la, pt=pt, n=n: e.tensor_copy(out=la[:, 0:n], in_=pt[:, 0:n]), [ptk], [lak])
            else:
                P.op(le, lambda e, la=la, pt=pt, n=n: e.tensor_tensor(out=la[:, 0:n], in0=la[:, 0:n], in1=pt[:, 0:n],
                                                                     op=ALU.add), [ptk, lak], [lak])
            if kt == nkt - 1:
                nacc = min(3, nkt)
                for c in range(nacc):
                    P.mm(ps[pl][:, 0:n], onesf, lacc[po - 3][c][:, 0:n], c == 0, c == nacc - 1,
                         ['lacc%d_%d' % (po - 3, c), 'onesf'], ['ps%d' % pl])
                P.op('dve', lambda e, pl=pl, n=n: e.reciprocal(out=rinv[:, 0:n], in_=ps[pl][:, 0:n]),
                     ['ps%d' % pl], ['rinv'])
                P.op('dve', lambda e, po=po, n=n, h=h, q0=q0: e.tensor_tensor(
                    out=mixT[:, 4 + h, q0:q0 + n], in0=ps[po][:, 0:n], in1=rinv[:, 0:n], op=ALU.mult),
                    ['ps%d' % po, 'rinv'], ['mix_c'])
            if qi == len(qblocks) - 1 and kt % KPP == KPP - 1 and h < 3:
                load_kv(h + 1, kt // KPP)
        P.fence()
        A.release(mP)

        wout_bf = A.bf(8, 1024)
        C.load_cast(wout_bf, d_wout, 1024, 8, 'wout', stage=[(A.f32(1024), 'wst5_0'), (A.f32(1024), 'wst5_1')])
        wr = A.f32(8, 16)
        P.dma(wr, d_wr, [], ['wr'])
        xr = A.f32(8, 512)
        x1 = A.f32(8, 512)
        sq5 = A.bf(8, 512)
        rstd5 = A.f32(512)
        tmp5 = [A.f32(512), A.f32(512)]
        h2 = A.f32(8, 512)
        sm = A.f32(4, 4)
        pe_ = A.f32(4, 16)
        for (src, n, w, q0, l0) in lblocks:
            P.dma(xr[:, :, 0:n], src, [], ['xr'])
            for j in range(8):
                pi = C.next_ps()
                pk = 'ps%d' % pi
                for k in range(8):
                    P.mm(ps[pi][:, 0:n], wout_bf[:, k, j * 128:(j + 1) * 128], mixT[:, k, q0:q0 + n], k == 0, k == 7,
                         ['wout', 'mix_a', 'mix_b', 'mix_c'], [pk])
                P.op('dve', lambda e, j=j, pi=pi, n=n, w=w: e.scalar_tensor_tensor(
                    out=x1[:, j, 0:n], in0=ps[pi][:, 0:n], scalar=svc(2, w)[:, j:j + 1], in1=xr[:, j, 0:n],
                    op0=ALU.mult, op1=ALU.add), [pk, 'xr', 'svt'], ['x1'])
            P.dma(o_x1[:, :, q0:q0 + n], x1[:, :, 0:n], ['x1'], ['o_x1'])
            C.sumsq_rstd(x1[:, :, 0:n], 8, n, D, rstd5, ones, sq5, ['x1'], 'n5')
            C.norm_mod(x1, n, rstd5, svc(3, w), svc(4, w), h2, tmp5, ['x1'], ['h2'], 'n5')
            for tt in range(n // 128):
                pi = C.next_ps()
                pk = 'ps%d' % pi
                si = tt % 4
                smk = 'sm%d' % si
                for k in range(8):
                    P.mm(ps[pi][:, 0:16], h2[:, k, tt * 128:(tt + 1) * 128], wr[:, k, :], k == 0, k == 7,
                         ['h2', 'wr'], [pk])
                P.op('dve', lambda e, pi=pi, si=si: e.reduce_max(out=sm[:, si, 0:1], in_=ps[pi][:, 0:16],
                                                                 axis=mybir.AxisListType.X), [pk], [smk])
                P.op('dve', lambda e, si=si: e.tensor_single_scalar(out=sm[:, si, 1:2], in_=sm[:, si, 0:1], scalar=-1.0,
                                                                    op=ALU.mult), [smk], [smk])
                P.act(pe_[:, si, :], ps[pi][:, 0:16], AF.Exp, [pk, smk], ['pe%d' % si], bias=sm[:, si, 1:2],
                      accum=sm[:, si, 2:3])
                P.op('dve', lambda e, si=si: e.reciprocal(out=sm[:, si, 3:4], in_=sm[:, si, 2:3]), ['pe%d' % si, smk],
                     [smk])
                P.op('dve', lambda e, si=si: e.tensor_scalar(out=pe_[:, si, :], in0=pe_[:, si, :],
                                                             scalar1=sm[:, si, 3:4], scalar2=None, op0=ALU.mult),
                     [smk, 'pe%d' % si], ['pe%d' % si])
                P.dma(o_probs[q0 + tt * 128:q0 + (tt + 1) * 128, :], pe_[:, si, :], ['pe%d' % si], ['o_probs'])
        P.finalize()
        return nc, P, A


def _fm(a):
    k = a.shape[0] // 128
    return np.ascontiguousarray(a.reshape(k, 128, -1).transpose(1, 0, 2))


def _fmv(v):
    return np.ascontiguousarray(v.reshape(-1, 128).T)


def _rope_perm():
    p = np.arange(64)
    o = ((p % 32) // 16) * 32 + (p // 32) * 16 + (p % 16)
    osw = o[(p + 32) % 64]
    return o, osw


def _rope_tables():
    rows = SEQ // 64
    r = np.repeat(np.arange(rows), 64).astype(np.float32)
    col = np.tile(np.arange(64), rows).astype(np.float32)
    inv = (np.float32(10000.0) ** (-np.arange(16, dtype=np.float32) / np.float32(16))).astype(np.float32)
    ang = np.concatenate([r[:, None] * inv, col[:, None] * inv], axis=-1).astype(np.float32)
    cos, sin = np.cos(ang).astype(np.float32), np.sin(ang).astype(np.float32)
    C_ = np.concatenate([cos, cos], axis=1).T
    S_ = np.concatenate([-sin, sin], axis=1).T
    return np.ascontiguousarray(C_), np.ascontiguousarray(S_)


def prep_AB(inp, l, xl, xc):
    o, osw = _rope_perm()
    xall = _fm(np.ascontiguousarray(np.concatenate([xc, xl], axis=0).T))
    cvec = np.stack([_fmv(inp['c'][0]), _fmv(inp['c_ctx'])], axis=-1)
    w_in = inp['w_in'][l]
    wA = np.concatenate([w_in[:, 512:768], w_in[:, 1408:1664], w_in[:, 1664 + o], w_in[:, 1664 + osw]], axis=1)
    wB = np.concatenate([w_in[:, 0:512], w_in[:, 768:1024], w_in[:, 1024:1408]], axis=1)
    wuq = inp['w_uq'][l]
    cols = []
    for h in range(4):
        cols += [wuq[:, h * 192:h * 192 + 128], wuq[:, h * 192 + 128 + o], wuq[:, h * 192 + 128 + osw]]
    wuq2 = np.concatenate(cols, axis=1)
    wukv = inp['w_ukv'][l]
    wukv2 = np.concatenate([wukv[:, h * 256:h * 256 + 128] for h in range(4)] +
                           [wukv[:, h * 256 + 128:h * 256 + 256] for h in range(4)], axis=1)
    lruw = np.zeros((128, 8, 128), np.float32)
    vecs = np.zeros((128, 64), np.float32)
    vecs[:, 0:8] = _fmv(inp['norm1_g'][l])
    vecs[:, 8:16] = _fmv(inp['norm2_g'][l])
    for d in range(2):
        for g in range(2):
            W = (inp['lru_wa'] if g == 0 else inp['lru_wx'])[l][d]
            bb = (inp['lru_ba'] if g == 0 else inp['lru_bx'])[l][d]
            for ct in range(2):
                i = d * 4 + g * 2 + ct
                lruw[0:64, i, 0:64] = W[2 * ct]
                lruw[64:128, i, 64:128] = W[2 * ct + 1]
                vecs[:, 16 + i] = bb[ct * 128:(ct + 1) * 128]
        for ct in range(2):
            vecs[:, 24 + d * 2 + ct] = inp['lru_lambda'][l][d][ct * 128:(ct + 1) * 128]
    for ct in range(2):
        for k in range(4):
            vecs[:, 28 + ct * 4 + k] = inp['conv_w'][l][k][ct * 128:(ct + 1) * 128]
        vecs[:, 36 + ct] = inp['conv_b'][l][ct * 128:(ct + 1) * 128]
    vecs[:, 38:41] = _fmv(inp['q_norm_g'][l])
    vecs[:, 41:43] = _fmv(inp['kv_norm_g'][l])
    sguw = np.ascontiguousarray(inp['sgu_w'][l].transpose(2, 0, 1))
    sgub = np.zeros((128, 2, 128), np.float32)
    for g in range(4):
        sgub[(g % 2) * 64:(g % 2) * 64 + 64, g // 2, :] = inp['sgu_b'][l][g][None, :]
    rC, rS = _rope_tables()
    common = dict(xall=xall, cvec=np.ascontiguousarray(cvec), wmod=_fm(np.ascontiguousarray(inp['w_mod'][l][:, :5120])),
                  bmod=_fmv(inp['b_mod'][l][:5120]), wA=_fm(wA), wB=_fm(wB), wuq=_fm(wuq2), wukv=_fm(wukv2),
                  wout=_fm(inp['w_out'][l]), lruw=lruw, sguw=sguw, sgub=sgub, wr=_fm(inp['w_router'][l]),
                  ropeC=rC, ropeS=rS, ident=np.eye(128, dtype=np.float32))
    maps = []
    for c in range(NCORES):
        v = vecs.copy()
        v[:, 43 + c] = 1.0
        m = dict(common)
        m['vecs'] = v
        m['xloc'] = np.ascontiguousarray(xall[:, :, CTX + NLOC * c:CTX + NLOC * (c + 1)])
        m['ropeCl'] = np.ascontiguousarray(rC[:, NLOC * c:NLOC * (c + 1)])
        m['ropeSl'] = np.ascontiguousarray(rS[:, NLOC * c:NLOC * (c + 1)])
        maps.append(m)
    return maps


def _unfm(a):
    return np.ascontiguousarray(a.transpose(1, 0, 2).reshape(-1, a.shape[2]).T)


def gather_AB(res):
    xl1 = np.concatenate([_unfm(r['x1T'][:, :, CTX:]) for r in res], axis=0)
    xc1 = _unfm(res[0]['x1T'][:, :, :CTX])
    pl = np.concatenate([r['probs'][CTX:] for r in res], axis=0)
    pc = res[0]['probs'][:CTX]
    return xl1, xc1, pl, pc


NBIS = 30


def build_C():
    nc = bass.Bass("TRN2", target_bir_lowering=False)

    def din(name, shape, dt=F32):
        return nc.dram_tensor(name, list(shape), dt, kind="ExternalInput").ap()
    d_x1 = din("x1T", [128, 8, NL])
    d_pA = din("probsA", [128, NLOC])
    d_pC = din("probsC", [16, CTX])
    d_gm = din("gmT", [16, NL])
    d_cvec = din("cvec", [128, 8, 2])
    d_wmod = din("wmod", [128, 8, 3072])
    d_bmod = din("bmod", [128, 24])
    d_vecs = din("vecs", [128, 16])
    d_G = din("G", [128, 128])
    d_oh = din("oh16", [16, 16])
    d_wg = din("wg", [NEXP, D, D])
    d_wu = din("wu", [NEXP, D, D])
    d_wd = din("wd", [NEXP, D, D])
    o_x2 = nc.dram_tensor("x2T", [128, 8, NL], F32, kind="ExternalOutput").ap()
    o_on = nc.dram_tensor("outN", [128, 8, NL], F32, kind="ExternalOutput").ap()

    AW = 51 * 1024
    with ExitStack() as es:
        arena_t = es.enter_context(nc.sbuf_tensor("arena", [128, AW], F32))
        ps = [es.enter_context(nc.psum_tensor("ps%d" % i, [128, 512], F32)) for i in range(7)]
        P = Prog(nc)
        A = Arena(arena_t, AW)
        C = Ctx(nc, P, A, ps, None)

        vecs = A.f32(16)
        modT = A.f32(24, 2)
        cs = A.f32(64)
        sv = A.f32(3, 8, 2)
        G = A.f32(128)
        oh = A.f32(16)
        ones16 = A.f32(128)
        ones = A.bf(128)
        ts = A.f32(16)
        m0 = A.mark()
        P.dma(vecs, d_vecs, [], ['vecs'])
        P.dma(G, d_G, [], ['consts'])
        P.dma(oh[0:16, :], d_oh, [], ['consts'])
        P.op('dve', lambda e: e.memset(ones, 1.0), [], ['consts'])
        P.op('dve', lambda e: e.memset(ones16, 1.0), [], ['consts'])
        stage8 = A.f32(8, 1024)
        C.compute_mods(d_cvec, d_wmod, d_bmod, 3, modT, cs, stage8)
        n2g, fng = vecs[:, 0:8], vecs[:, 8:16]
        mv = modT.rearrange("p (b k) w -> p b k w", b=3)
        for w in range(2):
            P.op('dve', lambda e, w=w: e.scalar_tensor_tensor(
                out=sv[:, 0, :, w], in0=mv[:, 1, :, w], scalar=1.0, in1=n2g, op0=ALU.add, op1=ALU.mult),
                ['modv', 'vecs'], ['svt'])
            P.op('dve', lambda e, w=w: e.tensor_copy(out=sv[:, 1, :, w], in_=mv[:, 0, :, w]), ['modv'], ['svt'])
            P.op('dve', lambda e, w=w: e.tensor_copy(out=sv[:, 2, :, w], in_=mv[:, 2, :, w]), ['modv'], ['svt'])
        P.fence()
        A.release(m0)

        h2T = A.bf(8, NL)
        acc = A.f32(8, NL)
        gmT = A.f32(NL)
        mL = A.mark()
        lblocks = [(0, CTX, 1)] + [(CTX + 512 * i, 512, 0) for i in range(NLOC // 512)]

        xs = [A.f32(8, 512), A.f32(8, 512)]
        sq = A.bf(8, 512)
        rstd = A.f32(512)
        tmpf = [A.f32(512), A.f32(512)]
        for bi, (q0, n, w) in enumerate(lblocks):
            x = xs[bi % 2]
            xk = 'xs%d' % (bi % 2)
            P.dma(x[:, :, 0:n], d_x1[:, :, q0:q0 + n], [], [xk])
            C.sumsq_rstd(x[:, :, 0:n], 8, n, D, rstd, ones, sq, [xk], 'n2')
            C.norm_mod(x, n, rstd, sv[:, 0, :, w], sv[:, 1, :, w], h2T[:, :, q0:q0 + n], tmpf, [xk], ['h2T'], 'n2')
        P.fence()
        A.release(mL)

        pA = A.f32(NLOC)
        mk = A.f32(NLOC)
        pC = A.f32(CTX)
        mkc = A.f32(CTX)
        P.dma(pA, d_pA, [], ['pA'])
        P.dma(pC[0:16, :], d_pC, [], ['pC'])
        P.dma(gmT[0:16, :], d_gm, [], ['gmT'])
        lo, hi, mid, tot, gt, dd, Kv = (ts[:, 0:2], ts[:, 2:4], ts[:, 4:6], ts[:, 6:8], ts[:, 8:10], ts[:, 10:12],
                                        ts[:, 12:14])
        P.op('dve', lambda e: e.memset(ts, 0.0), [], ['ts'])
        P.op('dve', lambda e: e.memset(hi, 1.0), ['ts'], ['ts'])
        P.op('dve', lambda e: e.memset(mid, 0.5), ['ts'], ['ts'])
        P.op('dve', lambda e: e.memset(ts[:, 12:13], NLOC - 0.5), ['ts'], ['ts'])
        P.op('dve', lambda e: e.memset(ts[:, 13:14], 2 * CTX // NEXP - 0.5), ['ts'], ['ts'])
        for it in range(NBIS):
            P.op('dve', lambda e: e.tensor_scalar(out=mk, in0=pA, scalar1=ts[:, 4:5], scalar2=None, op0=ALU.is_gt),
                 ['pA', 'ts'], ['mk'])
            P.op('dve', lambda e: e.reduce_sum(out=ts[:, 14:15], in_=mk, axis=mybir.AxisListType.X), ['mk'], ['cc'])
            P.op('dve', lambda e: e.tensor_scalar(out=mkc[0:16, :], in0=pC[0:16, :], scalar1=ts[0:16, 5:6], scalar2=None,
                                                  op0=ALU.is_gt), ['pC', 'ts'], ['mkc'])
            P.op('dve', lambda e: e.reduce_sum(out=ts[0:16, 7:8], in_=mkc[0:16, :], axis=mybir.AxisListType.X),
                 ['mkc', 'ts'], ['ts'])
            P.mm(ps[6][:, 0:1], G, ts[:, 14:15], True, True, ['cc', 'consts'], ['ps6'])
            P.op('dve', lambda e: e.tensor_copy(out=ts[:, 6:7], in_=ps[6][:, 0:1]), ['ps6', 'ts'], ['ts'])
            P.op('dve', lambda e: e.tensor_tensor(out=gt, in0=tot, in1=Kv, op=ALU.is_gt), ['ts'], ['ts'])
            P.op('dve', lambda e: e.tensor_tensor(out=dd, in0=mid, in1=lo, op=ALU.subtract), ['ts'], ['ts'])
            P.op('dve', lambda e: e.tensor_tensor(out=dd, in0=dd, in1=gt, op=ALU.mult), ['ts'], ['ts'])
            P.op('dve', lambda e: e.tensor_tensor(out=lo, in0=lo, in1=dd, op=ALU.add), ['ts'], ['ts'])
            P.op('dve', lambda e: e.tensor_tensor(out=dd, in0=hi, in1=mid, op=ALU.subtract), ['ts'], ['ts'])
            P.op('dve', lambda e: e.tensor_tensor(out=dd, in0=dd, in1=gt, op=ALU.mult), ['ts'], ['ts'])
            P.op('dve', lambda e: e.tensor_tensor(out=hi, in0=mid, in1=dd, op=ALU.add), ['ts'], ['ts'])
            P.op('dve', lambda e: e.tensor_tensor(out=dd, in0=lo, in1=hi, op=ALU.add), ['ts'], ['ts'])
            P.op('dve', lambda e: e.tensor_single_scalar(out=mid, in_=dd, scalar=0.5, op=ALU.mult), ['ts'], ['ts'])
        P.op('dve', lambda e: e.tensor_scalar(out=mk[0:16, 0:NLOC], in0=gmT[0:16, CTX:NL], scalar1=ts[0:16, 2:3],
                                              scalar2=None, op0=ALU.is_gt), ['gmT', 'ts'], ['mk'])
        P.op('dve', lambda e: e.tensor_tensor(out=gmT[0:16, CTX:NL], in0=gmT[0:16, CTX:NL], in1=mk[0:16, 0:NLOC],
                                              op=ALU.mult), ['mk', 'gmT'], ['gmT'])
        P.op('dve', lambda e: e.tensor_scalar(out=mkc[0:16, :], in0=gmT[0:16, 0:CTX], scalar1=ts[0:16, 3:4],
                                              scalar2=None, op0=ALU.is_gt), ['gmT', 'ts'], ['mkc'])
        P.op('dve', lambda e: e.tensor_tensor(out=gmT[0:16, 0:CTX], in0=gmT[0:16, 0:CTX], in1=mkc[0:16, :],
                                              op=ALU.mult), ['mkc', 'gmT'], ['gmT'])
        P.fence()
        A.release(mL)

        HF = 512
        wbuf = [dict(g=A.bf(8, HF), u=A.bf(8, HF), d=A.bf(4, 1024)) for _ in range(2)]
        wst = [(A.f32(1024), 'wst0'), (A.f32(1024), 'wst1')]
        hid = A.bf(4, 512)
        sg = [A.f32(512), A.f32(512)]
        tt_ = [A.f32(512), A.f32(512)]
        gmb = A.f32(512)
        gme = A.f32(512)
        units = [(ex, hf) for ex in range(NEXP) for hf in range(2)]
        stc = [0]
        gmbs = [gmb, A.f32(512)]
        gmes = [gme, A.f32(512)]

        def emit_gm(gi):
            ui2, bi2 = gi // len(lblocks), gi % len(lblocks)
            ex2 = units[ui2][0]
            q02, n2, _ = lblocks[bi2]
            ge, gb_ = gmes[gi % 2], gmbs[gi % 2]
            P.op('dve', lambda e: e.tensor_scalar(out=ge[0:16, 0:n2], in0=gmT[0:16, q02:q02 + n2],
                                                  scalar1=oh[0:16, ex2:ex2 + 1], scalar2=None, op0=ALU.mult),
                 ['gmT', 'consts'], ['gme%d' % (gi % 2)])
            P.mm(ps[6][:, 0:n2], ones16[0:16, :], ge[0:16, 0:n2], True, True, ['gme%d' % (gi % 2), 'consts'], ['ps6'])
            P.act(gb_[:, 0:n2], ps[6][:, 0:n2], AF.Copy, ['ps6'], ['gmb%d' % (gi % 2)])

        def load_steps(ui):
            ex, hf = units[ui]
            wb = wbuf[ui % 2]
            tag = 'w%d' % (ui % 2)
            steps = []
            for nm, dsrc in (('g', d_wg), ('u', d_wu)):
                for k in range(8):
                    def f(nm=nm, dsrc=dsrc, k=k):
                        st, sk = wst[stc[0] % 2]
                        stc[0] += 1
                        P.dma(st[:, 0:HF], dsrc[ex, k * 128:(k + 1) * 128, hf * HF:(hf + 1) * HF], [], [sk])
                        P.op(C.ew(), lambda en, st=st: en.tensor_copy(out=wb[nm][:, k, :], in_=st[:, 0:HF]),
                             [sk], [tag + nm])
                    steps.append(f)
            for f4 in range(4):
                def f(f4=f4):
                    st, sk = wst[stc[0] % 2]
                    stc[0] += 1
                    r0 = (hf * 4 + f4) * 128
                    P.dma(st[:, 0:1024], d_wd[ex, r0:r0 + 128, :], [], [sk])
                    P.op(C.ew(), lambda en, st=st: en.tensor_copy(out=wb['d'][:, f4, :], in_=st[:, 0:1024]),
                         [sk], [tag + 'd'])
                steps.append(f)
            return steps
        for f in load_steps(0):
            f()
        first_acc = True
        for ui, (ex, hf) in enumerate(units):
            wb = wbuf[ui % 2]
            tag = 'w%d' % (ui % 2)
            nxt = load_steps(ui + 1) if ui + 1 < len(units) else []
            per = (len(nxt) + len(lblocks) - 1) // len(lblocks)
            for bi, (q0, n, w) in enumerate(lblocks):
                gi = ui * len(lblocks) + bi
                if gi == 0:
                    emit_gm(0)
                gmb, gmk = gmbs[gi % 2], 'gmb%d' % (gi % 2)
                for f in range(4):
                    pg, pu = f % 2, 2 + f % 2
                    for k in range(8):
                        P.mm(ps[pg][:, 0:n], wb['g'][:, k, f * 128:(f + 1) * 128], h2T[:, k, q0:q0 + n], k == 0, k == 7,
                             [tag + 'g', 'h2T'], ['ps%d' % pg])
                    for k in range(8):
                        P.mm(ps[pu][:, 0:n], wb['u'][:, k, f * 128:(f + 1) * 128], h2T[:, k, q0:q0 + n], k == 0, k == 7,
                             [tag + 'u', 'h2T'], ['ps%d' % pu])
                    s_, t_ = sg[f % 2], tt_[f % 2]
                    P.act(s_[:, 0:n], ps[pg][:, 0:n], AF.Silu, ['ps%d' % pg], ['sg%d' % (f % 2)])
                    P.op('pool', lambda e, s_=s_, t_=t_, n=n, gmb=gmb: e.tensor_tensor(out=t_[:, 0:n], in0=s_[:, 0:n],
                                                                                      in1=gmb[:, 0:n], op=ALU.mult),
                         ['sg%d' % (f % 2), gmk], ['tt%d' % (f % 2)])
                    P.op('dve', lambda e, t_=t_, n=n, f=f, pu=pu: e.tensor_tensor(
                        out=hid[:, f, 0:n], in0=t_[:, 0:n], in1=ps[pu][:, 0:n], op=ALU.mult),
                        ['tt%d' % (f % 2), 'ps%d' % pu], ['hid'])
                if gi + 1 < len(units) * len(lblocks):
                    emit_gm(gi + 1)
                for j in range(8):
                    pd = 4 + j % 2
                    for f in range(4):
                        P.mm(ps[pd][:, 0:n], wb['d'][:, f, j * 128:(j + 1) * 128], hid[:, f, 0:n], f == 0, f == 3,
                             [tag + 'd', 'hid'], ['ps%d' % pd])
                    if ui == 0:
                        P.op('dve', lambda e, j=j, pd=pd, n=n, q0=q0: e.tensor_copy(out=acc[:, j, q0:q0 + n],
                                                                                   in_=ps[pd][:, 0:n]),
                             ['ps%d' % pd], ['acc'])
                    else:
                        P.op('dve', lambda e, j=j, pd=pd, n=n, q0=q0: e.tensor_tensor(
                            out=acc[:, j, q0:q0 + n], in0=acc[:, j, q0:q0 + n], in1=ps[pd][:, 0:n], op=ALU.add),
                            ['ps%d' % pd, 'acc'], ['acc'])
                for f in nxt[bi * per:(bi + 1) * per]:
                    f()
        P.fence()
        A.release(mL)

        xr = [A.f32(8, 512)] * 2
        x2 = [A.f32(8, 512)] * 2
        on = A.f32(8, 512)
        sq2 = A.bf(8, 512)
        rstd2 = A.f32(512)
        tmp2 = [A.f32(512), A.f32(512)]
        for bi, (q0, n, w) in enumerate(lblocks):
            x, xk = xr[0], 'xr'
            y, yk = x2[0], 'x2'
            P.dma(x[:, :, 0:n], d_x1[:, :, q0:q0 + n], [], [xk])
            for j in range(8):
                P.op('dve', lambda e, j=j, n=n, w=w, q0=q0, x=x, y=y: e.scalar_tensor_tensor(
                    out=y[:, j, 0:n], in0=acc[:, j, q0:q0 + n], scalar=sv[:, 2, j:j + 1, w], in1=x[:, j, 0:n],
                    op0=ALU.mult, op1=ALU.add), [xk, 'acc', 'svt'], [yk])
            P.dma(o_x2[:, :, q0:q0 + n], y[:, :, 0:n], [yk], ['o_x2'])
            C.sumsq_rstd(y[:, :, 0:n], 8, n, D, rstd2, ones, sq2, [yk], 'fn')
            for k in range(8):
                e_ = C.ew()
                t = tmp2[k % 2]
                tk = 'fn_tmp%d' % (k % 2)
                P.op(e_, lambda e, k=k, t=t, n=n, y=y: e.tensor_tensor(out=t[:, 0:n], in0=y[:, k, 0:n], in1=rstd2[:, 0:n],
                                                                      op=ALU.mult), [yk, 'fn_rstd'], [tk])
                P.op(e_, lambda e, k=k, t=t, n=n: e.tensor_scalar(out=on[:, k, 0:n], in0=t[:, 0:n],
                                                                  scalar1=fng[:, k:k + 1], scalar2=None, op0=ALU.mult),
                     [tk, 'vecs'], ['on'])
            P.dma(o_on[:, :, q0:q0 + n], on[:, :, 0:n], ['on'], ['o_on'])
        P.finalize()
        return nc, P, A


def build_C2():
    nc = bass.Bass("TRN2", target_bir_lowering=False)

    def din(name, shape, dt=F32):
        return nc.dram_tensor(name, list(shape), dt, kind="ExternalInput").ap()
    d_x1 = din("x1T", [128, 8, NL])
    d_pA = din("probsA", [128, NLOC])
    d_pC = din("probsC", [16, CTX])
    d_gm = din("gmT", [16, NL])
    d_cvec = din("cvec", [128, 8, 2])
    d_wmod = din("wmod", [128, 8, 3072])
    d_bmod = din("bmod", [128, 24])
    d_vecs = din("vecs", [128, 16])
    d_G = din("G", [128, 128])
    d_oh = din("oh16", [16, 16])
    d_ident = din("ident", [128, 128])
    d_iotaf = din("iotaf", [128, 512])
    d_iotap = din("iotap", [128, 4])
    d_wg = din("wg", [NEXP, D, D])
    d_wu = din("wu", [NEXP, D, D])
    d_wd = din("wd", [NEXP, D, D])
    o_x2 = nc.dram_tensor("x2T", [128, 8, NL], F32, kind="ExternalOutput").ap()
    o_on = nc.dram_tensor("outN", [128, 8, NL], F32, kind="ExternalOutput").ap()

    AW = 51 * 1024
    with ExitStack() as es:
        arena_t = es.enter_context(nc.sbuf_tensor("arena", [128, AW], F32))
        ps = [es.enter_context(nc.psum_tensor("ps%d" % i, [128, 512], F32)) for i in range(7)]
        psb = es.enter_context(nc.psum_tensor("psb", [128, 1024], BF16))
        P = Prog(nc)
        A = Arena(arena_t, AW)
        C = Ctx(nc, P, A, ps, psb)

        vecs = A.f32(16)
        modT = A.f32(24, 2)
        cs = A.f32(64)
        sv = A.f32(3, 8, 2)
        G = A.f32(128)
        oh = A.f32(16)
        ones16 = A.f32(128)
        ones = A.bf(128)
        identb = A.bf(128)
        iotaf = A.f32(512)
        iotap = A.f32(4)
        ts = A.f32(16)
        m0 = A.mark()
        P.dma(vecs, d_vecs, [], ['vecs'])
        P.dma(G, d_G, [], ['consts'])
        P.dma(oh[0:16, :], d_oh, [], ['consts'])
        P.dma(iotaf, d_iotaf, [], ['consts'])
        P.dma(iotap, d_iotap, [], ['consts'])
        P.op('dve', lambda e: e.memset(ones, 1.0), [], ['consts'])
        P.op('dve', lambda e: e.memset(ones16, 1.0), [], ['consts'])
        stage8 = A.f32(8, 1024)
        C.compute_mods(d_cvec, d_wmod, d_bmod, 3, modT, cs, stage8)
        P.dma(stage8[:, 0, 0:128], d_ident, [], ['wm_st'])
        P.op('dve', lambda e: e.tensor_copy(out=identb, in_=stage8[:, 0, 0:128]), ['wm_st'], ['consts'])
        n2g, fng = vecs[:, 0:8], vecs[:, 8:16]
        mv = modT.rearrange("p (b k) w -> p b k w", b=3)
        for w in range(2):
            P.op('dve', lambda e, w=w: e.scalar_tensor_tensor(
                out=sv[:, 0, :, w], in0=mv[:, 1, :, w], scalar=1.0, in1=n2g, op0=ALU.add, op1=ALU.mult),
                ['modv', 'vecs'], ['svt'])
            P.op('dve', lambda e, w=w: e.tensor_copy(out=sv[:, 1, :, w], in_=mv[:, 0, :, w]), ['modv'], ['svt'])
            P.op('dve', lambda e, w=w: e.tensor_copy(out=sv[:, 2, :, w], in_=mv[:, 2, :, w]), ['modv'], ['svt'])
        P.fence()
        A.release(m0)

        NTT = NL // 128
        h2tok = A.bf(NTT, 1024)
        acc = A.f32(8, NL)
        gmT = A.f32(NL)
        ssel = A.f32(NL)
        slotT = A.f32(NTT, 16)
        mL = A.mark()
        lblocks = [(0, CTX, 1)] + [(CTX + 512 * i, 512, 0) for i in range(NLOC // 512)]

        xs = [A.f32(8, 512), A.f32(8, 512)]
        sq = A.bf(8, 512)
        rstd = A.f32(512)
        tmpf = [A.f32(512), A.f32(512)]
        h2b = [A.bf(8, 512), A.bf(8, 512)]
        for bi, (q0, n, w) in enumerate(lblocks):
            x = xs[bi % 2]
            xk = 'xs%d' % (bi % 2)
            P.dma(x[:, :, 0:n], d_x1[:, :, q0:q0 + n], [], [xk])
            C.sumsq_rstd(x[:, :, 0:n], 8, n, D, rstd, ones, sq, [xk], 'n2')
            hb, hk = h2b[bi % 2], 'h2b%d' % (bi % 2)
            C.norm_mod(x, n, rstd, sv[:, 0, :, w], sv[:, 1, :, w], hb, tmpf, [xk], [hk], 'n2')
            for tt in range(n // 128):
                for k in range(8):
                    P.tr(psb[:, k * 128:(k + 1) * 128], hb[:, k, tt * 128:(tt + 1) * 128], identb, [hk, 'consts'], ['psb'])
                P.act(h2tok[:, q0 // 128 + tt, :], psb[:, 0:1024], AF.Copy, ['psb'], ['h2tok'])
        P.fence()
        A.release(mL)

        pA = A.f32(NLOC)
        mk = A.f32(NLOC)
        pC = A.f32(CTX)
        mkc = A.f32(CTX)
        P.dma(pA, d_pA, [], ['pA'])
        P.dma(pC[0:16, :], d_pC, [], ['pC'])
        P.dma(gmT[0:16, :], d_gm, [], ['gmT'])
        lo, hi, mid, tot, gt, dd, Kv = (ts[:, 0:2], ts[:, 2:4], ts[:, 4:6], ts[:, 6:8], ts[:, 8:10], ts[:, 10:12],
                                        ts[:, 12:14])
        P.op('dve', lambda e: e.memset(ts, 0.0), [], ['ts'])
        P.op('dve', lambda e: e.memset(hi, 1.0), ['ts'], ['ts'])
        P.op('dve', lambda e: e.memset(mid, 0.5), ['ts'], ['ts'])
        P.op('dve', lambda e: e.memset(ts[:, 12:13], NLOC - 0.5), ['ts'], ['ts'])
        P.op('dve', lambda e: e.memset(ts[:, 13:14], 2 * CTX // NEXP - 0.5), ['ts'], ['ts'])
        for it in range(NBIS):
            P.op('dve', lambda e: e.tensor_scalar(out=mk, in0=pA, scalar1=ts[:, 4:5], scalar2=None, op0=ALU.is_gt),
                 ['pA', 'ts'], ['mk'])
            P.op('dve', lambda e: e.reduce_sum(out=ts[:, 14:15], in_=mk, axis=mybir.AxisListType.X), ['mk'], ['cc'])
            P.op('dve', lambda e: e.tensor_scalar(out=mkc[0:16, :], in0=pC[0:16, :], scalar1=ts[0:16, 5:6], scalar2=None,
                                                  op0=ALU.is_gt), ['pC', 'ts'], ['mkc'])
            P.op('dve', lambda e: e.reduce_sum(out=ts[0:16, 7:8], in_=mkc[0:16, :], axis=mybir.AxisListType.X),
                 ['mkc', 'ts'], ['ts'])
            P.mm(ps[6][:, 0:1], G, ts[:, 14:15], True, True, ['cc', 'consts'], ['ps6'])
            P.op('dve', lambda e: e.tensor_copy(out=ts[:, 6:7], in_=ps[6][:, 0:1]), ['ps6', 'ts'], ['ts'])
            P.op('dve', lambda e: e.tensor_tensor(out=gt, in0=tot, in1=Kv, op=ALU.is_gt), ['ts'], ['ts'])
            P.op('dve', lambda e: e.tensor_tensor(out=dd, in0=mid, in1=lo, op=ALU.subtract), ['ts'], ['ts'])
            P.op('dve', lambda e: e.tensor_tensor(out=dd, in0=dd, in1=gt, op=ALU.mult), ['ts'], ['ts'])
            P.op('dve', lambda e: e.tensor_tensor(out=lo, in0=lo, in1=dd, op=ALU.add), ['ts'], ['ts'])
            P.op('dve', lambda e: e.tensor_tensor(out=dd, in0=hi, in1=mid, op=ALU.subtract), ['ts'], ['ts'])
            P.op('dve', lambda e: e.tensor_tensor(out=dd, in0=dd, in1=gt, op=ALU.mult), ['ts'], ['ts'])
            P.op('dve', lambda e: e.tensor_tensor(out=hi, in0=mid, in1=dd, op=ALU.add), ['ts'], ['ts'])
            P.op('dve', lambda e: e.tensor_tensor(out=dd, in0=lo, in1=hi, op=ALU.add), ['ts'], ['ts'])
            P.op('dve', lambda e: e.tensor_single_scalar(out=mid, in_=dd, scalar=0.5, op=ALU.mult), ['ts'], ['ts'])
        P.op('dve', lambda e: e.tensor_scalar(out=mk[0:16, 0:NLOC], in0=gmT[0:16, CTX:NL], scalar1=ts[0:16, 2:3],
                                              scalar2=None, op0=ALU.is_gt), ['gmT', 'ts'], ['mk'])
        P.op('dve', lambda e: e.tensor_tensor(out=gmT[0:16, CTX:NL], in0=gmT[0:16, CTX:NL], in1=mk[0:16, 0:NLOC],
                                              op=ALU.mult), ['mk', 'gmT'], ['gmT'])
        P.op('dve', lambda e: e.tensor_scalar(out=mkc[0:16, :], in0=gmT[0:16, 0:CTX], scalar1=ts[0:16, 3:4],
                                              scalar2=None, op0=ALU.is_gt), ['gmT', 'ts'], ['mkc'])
        P.op('dve', lambda e: e.tensor_tensor(out=gmT[0:16, 0:CTX], in0=gmT[0:16, 0:CTX], in1=mkc[0:16, :],
                                              op=ALU.mult), ['mkc', 'gmT'], ['gmT'])
        mrow = A.f32(NL)
        orow = A.f32(NL)
        P.op('dve', lambda e: e.tensor_copy(out=mrow[0:16, CTX:NL], in_=mk[0:16, 0:NLOC]), ['mk'], ['mrow'])
        P.op('dve', lambda e: e.tensor_copy(out=mrow[0:16, 0:CTX], in_=mkc[0:16, :]), ['mkc'], ['mrow'])
        P.op('dve', lambda e: e.memset(orow[0:16, :], 1.0), [], ['orow'])
        P.op('dve', lambda e: e.tensor_tensor_scan(out=ssel[0:16, :], data0=orow[0:16, :], data1=mrow[0:16, :],
                                                   initial=0.0, op0=ALU.mult, op1=ALU.add), ['orow', 'mrow'], ['ssel'])
        P.op('dve', lambda e: e.tensor_tensor(out=ssel[0:16, :], in0=ssel[0:16, :], in1=mrow[0:16, :], op=ALU.mult),
             ['ssel', 'mrow'], ['ssel'])
        for tt in range(NTT):
            P.tr(ps[6][:, 0:16], ssel[0:16, tt * 128:(tt + 1) * 128], oh[0:16, :], ['ssel', 'consts'], ['ps6'])
            P.op('dve', lambda e, tt=tt: e.tensor_copy(out=slotT[:, tt, :], in_=ps[6][:, 0:16]), ['ps6'], ['slotT'])
        P.fence()
        A.release(mL)

        CS = 512
        QF = 256
        NRING = 3
        wgu = [dict(g=A.bf(8, QF), u=A.bf(8, QF)) for _ in range(NRING)]
        wd = A.bf(8, 1024)
        SS = [A.bf(CS) for _ in range(4)]
        xsT = A.bf(8, CS)
        ye = xsT.rearrange("p (a b) c -> p a (b c)", a=4)
        hid = A.bf(8, CS)
        sg = [A.bf(CS), A.bf(CS)]
        gmb = A.f32(512)
        gme = A.f32(512)
        sse = A.f32(512)

        def load_gu(ex, q, slot):
            wb, tag = wgu[slot], 'wgu%d' % slot
            for nm, dsrc in (('g', d_wg), ('u', d_wu)):
                P.dma(wb[nm], dsrc[ex].rearrange("(k p) f -> p k f", p=128)[:, :, q * QF:(q + 1) * QF], [], [tag],
                      eng='pool')

        def load_d(ex):
            for hh in range(2):
                P.dma(wd[:, :, hh * 512:(hh + 1) * 512],
                      d_wd[ex].rearrange("(k p) f -> p k f", p=128)[:, :, hh * 512:(hh + 1) * 512], [], ['wd'], eng='pool')
        usl = [0]
        for q0_ in range(NRING):
            load_gu(0, q0_, q0_)
        for ex in range(NEXP):
            si = 0
            for dh in range(2):
                for tt in range(NTT):
                    S_, Sk = SS[si % 4], 'SS%d' % (si % 4)
                    si += 1
                    P.op('dve', lambda e, S_=S_, tt=tt, ex=ex: e.tensor_scalar(
                        out=S_, in0=iotaf, scalar1=slotT[:, tt, ex:ex + 1], scalar2=None, op0=ALU.is_equal),
                        ['consts', 'slotT'], [Sk])
                    for i in range(4):
                        dci = dh * 4 + i
                        P.mm(ps[i][:, 0:CS], h2tok[:, tt, dci * 128:(dci + 1) * 128], S_, tt == 0, tt == NTT - 1,
                             ['h2tok', Sk], ['ps%d' % i])
                for i in range(4):
                    P.act(xsT[:, dh * 4 + i, :], ps[i][:, 0:CS], AF.Copy, ['ps%d' % i], ['xsye'])
            if ex == 0:
                load_d(0)
            for q in range(4):
                slot = (ex * 4 + q) % NRING
                wb, tag = wgu[slot], 'wgu%d' % slot
                for f2 in range(2):
                    f = q * 2 + f2
                    pg, pu = f % 2, 2 + f % 2
                    for k in range(8):
                        P.mm(ps[pg][:, 0:CS], wb['g'][:, k, f2 * 128:(f2 + 1) * 128], xsT[:, k, :], k == 0, k == 7,
                             [tag, 'xsye'], ['ps%d' % pg])
                    for k in range(8):
                        P.mm(ps[pu][:, 0:CS], wb['u'][:, k, f2 * 128:(f2 + 1) * 128], xsT[:, k, :], k == 0, k == 7,
                             [tag, 'xsye'], ['ps%d' % pu])
                    s_ = sg[f % 2]
                    P.act(s_, ps[pg][:, 0:CS], AF.Silu, ['ps%d' % pg], ['sg%d' % (f % 2)])
                    P.op('dve', lambda e, s_=s_, f=f, pu=pu: e.tensor_tensor(out=hid[:, f, :], in0=s_, in1=ps[pu][:, 0:CS],
                                                                          op=ALU.mult),
                         ['sg%d' % (f % 2), 'ps%d' % pu], ['hid'])
                nq = ex * 4 + q + NRING
                if nq < NEXP * 4:
                    load_gu(nq // 4, nq % 4, slot)
            for st_ in range(4):
                for hh in range(2):
                    for f in range(8):
                        P.mm(ps[4][:, 0:512], hid[:, f, st_ * 128:(st_ + 1) * 128], wd[:, f, hh * 512:(hh + 1) * 512],
                             f == 0, f == 7, ['hid', 'wd'], ['ps4'])
                    P.act(ye[:, st_, hh * 512:(hh + 1) * 512], ps[4][:, 0:512], AF.Copy, ['ps4'], ['xsye'])
            if ex + 1 < NEXP:
                load_d(ex + 1)
            for (q0, n, w) in lblocks:
                P.op('dve', lambda e, ex=ex, q0=q0, n=n: e.tensor_scalar(
                    out=sse[0:16, 0:n], in0=ssel[0:16, q0:q0 + n], scalar1=oh[0:16, ex:ex + 1], scalar2=None,
                    op0=ALU.mult), ['ssel', 'consts'], ['sse'])
                P.op('dve', lambda e, ex=ex, q0=q0, n=n: e.tensor_scalar(
                    out=gme[0:16, 0:n], in0=gmT[0:16, q0:q0 + n], scalar1=oh[0:16, ex:ex + 1], scalar2=None,
                    op0=ALU.mult), ['gmT', 'consts'], ['gme'])
                P.mm(ps[5][:, 0:n], ones16[0:16, :], sse[0:16, 0:n], True, True, ['sse', 'consts'], ['ps5'])
                P.mm(ps[6][:, 0:n], ones16[0:16, :], gme[0:16, 0:n], True, True, ['gme', 'consts'], ['ps6'])
                P.act(gmb[:, 0:n], ps[6][:, 0:n], AF.Copy, ['ps6'], ['gmb'])
                for st_ in range(4):
                    P.op('dve', lambda e, st_=st_, n=n: e.scalar_tensor_tensor(
                        out=SS[st_][:, 0:n], in0=ps[5][:, 0:n], scalar=iotap[:, st_:st_ + 1], in1=gmb[:, 0:n],
                        op0=ALU.is_equal, op1=ALU.mult), ['ps5', 'gmb', 'consts'], ['SS%d' % st_])
                for j in range(8):
                    pi = j % 4
                    for st_ in range(4):
                        P.mm(ps[pi][:, 0:n], ye[:, st_, j * 128:(j + 1) * 128], SS[st_][:, 0:n], st_ == 0, st_ == 3,
                             ['xsye', 'SS%d' % st_], ['ps%d' % pi])
                    if ex == 0:
                        P.op('dve', lambda e, j=j, pi=pi, n=n, q0=q0: e.tensor_copy(out=acc[:, j, q0:q0 + n],
                                                                                   in_=ps[pi][:, 0:n]),
                             ['ps%d' % pi], ['acc'])
                    else:
                        P.op('dve', lambda e, j=j, pi=pi, n=n, q0=q0: e.tensor_tensor(
                            out=acc[:, j, q0:q0 + n], in0=acc[:, j, q0:q0 + n], in1=ps[pi][:, 0:n], op=ALU.add),
                            ['ps%d' % pi, 'acc'], ['acc'])
        P.fence()
        A.release(mL)

        xr = [A.f32(8, 512)] * 2
        x2 = [A.f32(8, 512)] * 2
        on = A.f32(8, 512)
        sq2 = A.bf(8, 512)
        rstd2 = A.f32(512)
        tmp2 = [A.f32(512), A.f32(512)]
        for bi, (q0, n, w) in enumerate(lblocks):
            x, xk = xr[0], 'xr'
            y, yk = x2[0], 'x2'
            P.dma(x[:, :, 0:n], d_x1[:, :, q0:q0 + n], [], [xk])
            for j in range(8):
                P.op('dve', lambda e, j=j, n=n, w=w, q0=q0, x=x, y=y: e.scalar_tensor_tensor(
                    out=y[:, j, 0:n], in0=acc[:, j, q0:q0 + n], scalar=sv[:, 2, j:j + 1, w], in1=x[:, j, 0:n],
                    op0=ALU.mult, op1=ALU.add), [xk, 'acc', 'svt'], [yk])
            P.dma(o_x2[:, :, q0:q0 + n], y[:, :, 0:n], [yk], ['o_x2'])
            C.sumsq_rstd(y[:, :, 0:n], 8, n, D, rstd2, ones, sq2, [yk], 'fn')
            for k in range(8):
                e_ = C.ew()
                t = tmp2[k % 2]
                tk = 'fn_tmp%d' % (k % 2)
                P.op(e_, lambda e, k=k, t=t, n=n, y=y: e.tensor_tensor(out=t[:, 0:n], in0=y[:, k, 0:n], in1=rstd2[:, 0:n],
                                                                      op=ALU.mult), [yk, 'fn_rstd'], [tk])
                P.op(e_, lambda e, k=k, t=t, n=n: e.tensor_scalar(out=on[:, k, 0:n], in0=t[:, 0:n],
                                                                  scalar1=fng[:, k:k + 1], scalar2=None, op0=ALU.mult),
                     [tk, 'vecs'], ['on'])
            P.dma(o_on[:, :, q0:q0 + n], on[:, :, 0:n], ['on'], ['o_on'])
        P.finalize()
        return nc, P, A


def prep_C(inp, l, resAB):
    G = (np.arange(128)[:, None] % 16 == np.arange(128)[None, :] % 16).astype(np.float32)
    cvec = np.ascontiguousarray(np.stack([_fmv(inp['c'][0]), _fmv(inp['c_ctx'])], axis=-1))
    vecs = np.zeros((128, 16), np.float32)
    vecs[:, 0:8] = _fmv(inp['norm2_g'][l])
    vecs[:, 8:16] = _fmv(inp['final_norm_g'])
    pA = np.ascontiguousarray(np.stack([r['probs'][CTX:].T for r in resAB], axis=0).reshape(128, NLOC))
    pC = np.ascontiguousarray(resAB[0]['probs'][:CTX].T)
    iotaf = np.ascontiguousarray(np.broadcast_to(np.arange(1, 513, dtype=np.float32)[None, :], (128, 512)))
    iotap = (np.arange(128, dtype=np.float32)[:, None] + 1 + 128 * np.arange(4, dtype=np.float32)[None, :]).astype(np.float32)
    common = dict(ident=np.eye(128, dtype=np.float32), iotaf=iotaf, iotap=iotap, cvec=cvec, wmod=_fm(np.ascontiguousarray(inp['w_mod'][l][:, 3072:6144])),
                  bmod=_fmv(inp['b_mod'][l][3072:6144]), vecs=vecs, G=G, oh16=np.eye(16, dtype=np.float32),
                  probsA=pA, probsC=pC, wg=inp['w_gate'][l], wu=inp['w_up'][l], wd=inp['w_down'][l])
    maps = []
    for c in range(NCORES):
        m = dict(common)
        m['x1T'] = resAB[c]['x1T']
        m['gmT'] = np.ascontiguousarray(resAB[c]['probs'].T)
        maps.append(m)
    return maps


def kernel(**inp):
    inp = {k: np.asarray(v) for k, v in inp.items()}
    ncAB = build_AB()[0]
    ncC = build_C2()[0]
    xl, xc = inp['x'][0], inp['ctx'][0]
    out = None
    for l in range(2):
        resAB = run_bass_kernel_spmd(ncAB, prep_AB(inp, l, xl, xc), core_ids=list(range(NCORES))).results
        resC = run_bass_kernel_spmd(ncC, prep_C(inp, l, resAB), core_ids=list(range(NCORES))).results
        xl = np.concatenate([_unfm(r['x2T'][:, :, CTX:]) for r in resC], axis=0)
        xc = _unfm(resC[0]['x2T'][:, :, :CTX])
        out = np.concatenate([_unfm(r['outN'][:, :, CTX:]) for r in resC], axis=0)
    return np.ascontiguousarray(out[None].astype(np.float32))
```

```python
import numpy as np
import ml_dtypes
from contextlib import ExitStack
import concourse.bass as bass
import concourse.mybir as mybir
from concourse.bass_utils import run_bass_kernel_spmd

F32 = mybir.dt.float32
BF16 = mybir.dt.bfloat16
AF = mybir.ActivationFunctionType
ALU = mybir.AluOpType

NCORES = 8
D = 1024
SEQ = 16384
CTX = 256
TALL = SEQ + CTX
NLOC = SEQ // NCORES
NL = NLOC + CTX
EPS = 1e-6
NEXP = 16
ATTN_SCALE = 192.0 ** -0.5
ENGS = ['pe', 'act', 'dve', 'pool', 'sp']


def _prod(s):
    r = 1
    for v in s:
        r *= v
    return r


class Prog:
    def __init__(self, nc, ndma=12):
        self.nc = nc
        self.ops = []
        self.lastw = {}
        self.readers = {}
        self.ndma = ndma

    capture = None

    def begin_capture(self):
        self.capture = []

    def end_capture(self):
        c, self.capture = self.capture, None
        return c

    def replay_interleaved(self, lists, nway=2):
        L = max(len(l) for l in lists)
        stride = (L + nway - 1) // nway
        items = []
        for i, l in enumerate(lists):
            for k, call in enumerate(l):
                items.append((i * stride + k, i, call))
        items.sort(key=lambda t: (t[0], t[1]))
        for _, _, call in items:
            self.op(*call)

    def op(self, eng, fn, reads=(), writes=(), dma=False):
        if self.capture is not None:
            self.capture.append((eng, fn, tuple(reads), tuple(writes), dma))
            return -1
        i = len(self.ops)
        deps = set()
        for k in reads:
            if k in self.lastw:
                deps.add(self.lastw[k])
        for k in writes:
            if k in self.lastw:
                deps.add(self.lastw[k])
            deps.update(self.readers.get(k, ()))
        for k in reads:
            self.readers.setdefault(k, []).append(i)
        for k in writes:
            self.lastw[k] = i
            self.readers[k] = []
        self.ops.append(dict(eng=eng, fn=fn, deps=deps, dma=dma))
        return i

    def fence(self):
        self.ops.append(dict(eng=None, fence=True))
        self.lastw = {}
        self.readers = {}

    def mm(self, out, lhsT, rhs, start, stop, reads, writes):
        self.op('pe', lambda e: e.matmul(out, lhsT, rhs, start=start, stop=stop), reads, writes)

    def tr(self, out, in_, ident, reads, writes):
        self.op('pe', lambda e: e.transpose(out, in_, ident), reads, writes)

    def act(self, out, in_, func, reads, writes, bias=None, scale=None, accum=None):
        kw = {}
        if bias is not None:
            kw['bias'] = bias
        if scale is not None:
            kw['scale'] = scale
        if accum is not None:
            kw['accum_out'] = accum
        self.op('act', lambda e: e.activation(out=out, in_=in_, func=func, **kw), reads, writes)

    def dma(self, out, in_, reads, writes, eng='sp'):
        self.op(eng, lambda e: e.dma_start(out=out, in_=in_), reads, writes, dma=True)

    def finalize(self):
        self.fence()
        nc, ops, ndma = self.nc, self.ops, self.ndma
        need = set()
        last = {}
        for i, o in enumerate(ops):
            if o.get('fence'):
                for e, j in last.items():
                    need.add(j)
                continue
            for d in o['deps']:
                Dd = ops[d]
                if Dd['dma']:
                    continue
                if Dd['eng'] == 'pe' and o['eng'] == 'pe' and not o['dma']:
                    continue
                need.add(d)
            if not o['dma']:
                last[o['eng']] = i
        cnt = {e: 0 for e in ENGS}
        dcnt = [0] * ndma
        di = 0
        for i, o in enumerate(ops):
            if o.get('fence'):
                o['snap_cnt'] = dict(cnt)
                o['snap_d'] = list(dcnt)
                continue
            if o['dma']:
                j = di % ndma
                di += 1
                o['dsem'] = j
                o['dprev'] = dcnt[j]
                dcnt[j] += 16
                o['dval'] = dcnt[j]
            elif i in need:
                cnt[o['eng']] += 1
                o['sig'] = cnt[o['eng']]
        self.n_ops = len(ops)
        with ExitStack() as es:
            esem = {e: es.enter_context(nc.semaphore("s_" + e)) for e in ENGS}
            dsem = [es.enter_context(nc.semaphore("d_%d" % j)) for j in range(ndma)]
            block = es.enter_context(nc.Block())

            def emit(ename):
                def body(eng):
                    waited = {}

                    def wait(key, sem, val):
                        if val > waited.get(key, 0):
                            eng.wait_ge(sem, val)
                            waited[key] = val
                    for i, o in enumerate(ops):
                        if o.get('fence'):
                            for e2 in ENGS:
                                if o['snap_cnt'][e2] > 0:
                                    wait(e2, esem[e2], o['snap_cnt'][e2])
                            for j in range(ndma):
                                if o['snap_d'][j] > 0:
                                    wait(('d', j), dsem[j], o['snap_d'][j])
                            continue
                        if o['eng'] != ename:
                            continue
                        for d in sorted(o['deps']):
                            Dd = ops[d]
                            if Dd['dma']:
                                wait(('d', Dd['dsem']), dsem[Dd['dsem']], Dd['dval'])
                            else:
                                if Dd['eng'] == 'pe' and ename == 'pe' and not o['dma']:
                                    continue
                                wait(Dd['eng'], esem[Dd['eng']], Dd['sig'])
                        if o['dma'] and o['dprev'] > 0:
                            wait(('d', o['dsem']), dsem[o['dsem']], o['dprev'])
                        ins = o['fn'](eng)
                        if o['dma']:
                            ins.then_inc(dsem[o['dsem']], 16)
                        elif 'sig' in o:
                            ins.then_inc(esem[ename], 1)
                return body
            block.tensor(emit('pe'))
            block.scalar(emit('act'))
            block.vector(emit('dve'))
            block.gpsimd(emit('pool'))
            block.sync(emit('sp'))


class Arena:
    def __init__(self, t, width):
        self.t = t
        self.top = 0
        self.width = width
        self.peak = 0

    def _shape(self, v, shape):
        if len(shape) == 1:
            return v
        if len(shape) == 2:
            return v.rearrange("p (a b) -> p a b", a=shape[0])
        if len(shape) == 3:
            return v.rearrange("p (a b c) -> p a b c", a=shape[0], b=shape[1])
        raise ValueError

    def f32(self, *shape):
        n = _prod(shape)
        a = self.top
        self.top += n
        self.peak = max(self.peak, self.top)
        assert self.top <= self.width, ("arena overflow", self.top, self.width)
        return self._shape(self.t[:, a:a + n], shape)

    def bf(self, *shape):
        n = _prod(shape)
        nw = (n + 1) // 2
        a = self.top
        self.top += nw
        self.peak = max(self.peak, self.top)
        assert self.top <= self.width, ("arena overflow", self.top, self.width)
        v = self.t[:, a:a + nw].bitcast(BF16)
        return self._shape(v[:, 0:n], shape)

    def mark(self):
        return self.top

    def release(self, m):
        self.top = m


class Ctx:
    def __init__(self, nc, P, A, ps, psb):
        self.nc, self.P, self.A, self.ps, self.psb = nc, P, A, ps, psb
        self.psi = 0
        self.alt = 0
        self.uid = 0

    def key(self, s):
        self.uid += 1
        return "%s#%d" % (s, self.uid)

    ps_range = (0, 6)

    def next_ps(self, lo=None, hi=None):
        if lo is None:
            lo, hi = self.ps_range
        i = lo + self.psi % (hi - lo)
        self.psi += 1
        return i

    def ew(self):
        self.alt += 1
        return 'pool' if self.alt % 4 == 0 else 'dve'

    def load_cast(self, dst_bf, src_dram, ncols, nk, name, scale_col=None, stage=None):
        P = self.P
        for k in range(nk):
            st, sk = stage[k % 2]
            P.dma(st[:, 0:ncols], src_dram[:, k, :], reads=[], writes=[sk])
            e = self.ew()
            if scale_col is None:
                P.op(e, lambda en, st=st, k=k: en.tensor_copy(out=dst_bf[:, k, :], in_=st[:, 0:ncols]),
                     reads=[sk], writes=[name])
            else:
                P.op(e, lambda en, st=st, k=k: en.tensor_scalar(out=dst_bf[:, k, :], in0=st[:, 0:ncols],
                                                                scalar1=scale_col[:, k:k + 1], scalar2=None,
                                                                op0=ALU.mult),
                     reads=[sk, 'modv'], writes=[name])

    def sumsq_rstd(self, src_f32, nk, n, dim, out_rstd, ones_bf, sq_bf, rk, name):
        P = self.P
        P.act(sq_bf[:, 0:nk, 0:n], src_f32, AF.Square, reads=rk, writes=[name + '_sq'])
        pi = self.next_ps()
        pk = 'ps%d' % pi
        for k in range(nk):
            P.mm(self.ps[pi][:, 0:n], ones_bf, sq_bf[:, k, 0:n], k == 0, k == nk - 1,
                 reads=[name + '_sq', 'consts'], writes=[pk])
        P.act(out_rstd[:, 0:n], self.ps[pi][:, 0:n], AF.Ln, reads=[pk], writes=[name + '_rstd'], scale=1.0 / dim, bias=EPS)
        P.act(out_rstd[:, 0:n], out_rstd[:, 0:n], AF.Exp, reads=[name + '_rstd'], writes=[name + '_rstd'], scale=-0.5)

    def norm_mod(self, x_f32, n, rstd, s_col, sh_col, out, tmp_f32, rk, wk, name):
        P = self.P
        for k in range(8):
            e = self.ew()
            tk = "%s_tmp%d" % (name, k % 2)
            t = tmp_f32[k % 2]
            P.op(e, lambda en, k=k, t=t: en.tensor_tensor(out=t[:, 0:n], in0=x_f32[:, k, 0:n], in1=rstd[:, 0:n],
                                                          op=ALU.mult),
                 reads=rk + [name + '_rstd'], writes=[tk])
            P.op(e, lambda en, k=k, t=t: en.tensor_scalar(out=out[:, k, 0:n], in0=t[:, 0:n],
                                                          scalar1=s_col[:, k:k + 1], scalar2=sh_col[:, k:k + 1],
                                                          op0=ALU.mult, op1=ALU.add),
                 reads=[tk, 'modv'], writes=wk)

    def compute_mods(self, d_cvec, d_wmod, d_bmod, nblk, modT, cs, stage, id2):
        P = self.P
        P.dma(cs[:, 0:16], d_cvec.rearrange("p k w -> p (k w)"), reads=[], writes=['cs'])
        P.act(cs[:, 16:32], cs[:, 0:16], AF.Sigmoid, reads=['cs'], writes=['cs2'])
        P.op('dve', lambda e: e.tensor_tensor(out=cs[:, 0:16], in0=cs[:, 0:16], in1=cs[:, 16:32], op=ALU.mult),
             reads=['cs2', 'cs'], writes=['cs'])
        P.dma(cs[:, 32:32 + nblk * 8], d_bmod, reads=[], writes=['bmod'])
        csv = cs[:, 0:16].rearrange("p (k w) -> p k w", k=8)
        nj = nblk * 8
        rb = [self.A.f32(512), self.A.f32(512)]
        ri = 0
        for b in range(nblk):
            stg, sk = stage[b % 2], 'wm_st%d' % (b % 2)
            for hk in range(2):
                P.dma(stg[:, hk * 4:(hk + 1) * 4, :], d_wmod[:, hk * 4:(hk + 1) * 4, b * 1024:(b + 1) * 1024], reads=[],
                      writes=[sk + 'ab'[hk]], eng=('sp' if hk == 0 else 'pool'))
            for hf in range(2):
                for k in range(8):
                    P.mm(self.ps[5][0:2, 0:512], csv[:, k, :], stg[:, k, hf * 512:(hf + 1) * 512], k == 0, k == 7,
                         reads=[sk + 'ab'[k // 4], 'cs'], writes=['ps5'])
                r_, rk = rb[ri % 2], 'mrow%d' % (ri % 2)
                ri += 1
                P.op('dve', lambda e, r_=r_: e.tensor_copy(out=r_[0:2, :], in_=self.ps[5][0:2, 0:512]), ['ps5'], [rk])
                for j4 in range(4):
                    c0 = 2 * (b * 8 + hf * 4 + j4)
                    P.tr(self.ps[6][:, c0:c0 + 2], r_[0:2, j4 * 128:(j4 + 1) * 128], id2, [rk, 'id2', 'consts'], ['ps6'])
        psv = self.ps[6][:, 0:2 * nj].rearrange("p (j w) -> p j w", w=2)
        for w in range(2):
            P.op('dve', lambda e, w=w: e.tensor_tensor(out=modT[:, 0:nj, w], in0=psv[:, :, w],
                                                       in1=cs[:, 32:32 + nj], op=ALU.add),
                 reads=['ps6', 'bmod'], writes=['modv'])


def build_AB():
    nc = bass.Bass("TRN2", target_bir_lowering=False)

    def din(name, shape, dt=F32):
        return nc.dram_tensor(name, list(shape), dt, kind="ExternalInput").ap()
    d_xall = din("xall", [128, 8, TALL])
    d_xloc = din("xloc", [128, 8, NLOC])
    d_cvec = din("cvec", [128, 8, 2])
    d_wmod = din("wmod", [128, 8, 5120])
    d_bmod = din("bmod", [128, 40])
    d_vecs = din("vecs", [128, 64])
    d_wA = din("wA", [128, 8, 640])
    d_wB = din("wB", [128, 8, 1152])
    d_wuq = din("wuq", [128, 3, 1024])
    d_wukv = din("wukv", [128, 2, 1024])
    d_wout = din("wout", [128, 8, 1024])
    d_lruw = din("lruw", [128, 8, 128])
    d_sguw = din("sguw", [128, 4, 128])
    d_sgub = din("sgub", [128, 2, 128])
    d_wr = din("wr", [128, 8, 16])
    d_ropeC = din("ropeC", [64, SEQ])
    d_ropeS = din("ropeS", [64, SEQ])
    d_ropeCl = din("ropeCl", [64, NLOC])
    d_ropeSl = din("ropeSl", [64, NLOC])
    d_ident = din("ident", [128, 128])
    o_x1 = nc.dram_tensor("x1T", [128, 8, NL], F32, kind="ExternalOutput").ap()
    o_probs = nc.dram_tensor("probs", [NL, NEXP], F32, kind="ExternalOutput").ap()
    s_xb = nc.dram_tensor("xb_s", [2, 128, TALL], F32).ap()
    s_KT = nc.dram_tensor("KT_s", [4, 128, TALL], BF16).ap()
    s_kr = nc.dram_tensor("kr_s", [64, TALL], BF16).ap()
    s_V = nc.dram_tensor("V_s", [4, 128, TALL // 128, 128], BF16).ap()

    AW = 51 * 1024
    with ExitStack() as es:
        arena_t = es.enter_context(nc.sbuf_tensor("arena", [128, AW], F32))
        ps = [es.enter_context(nc.psum_tensor("ps%d" % i, [128, 512], F32)) for i in range(7)]
        psb = es.enter_context(nc.psum_tensor("psb", [128, 1024], BF16))
        P = Prog(nc)
        A = Arena(arena_t, AW)
        C = Ctx(nc, P, A, ps, psb)

        vecs = A.f32(64)
        modT = A.f32(40, 2)
        cs = A.f32(80)
        sv = A.f32(6, 8, 2)
        spv = A.f32(4)
        ident = A.bf(128)
        ones = A.bf(128)
        onesf = A.f32(128)
        m0 = A.mark()
        P.dma(vecs, d_vecs, [], ['vecs'])
        P.op('pool', lambda e: e.memset(onesf, 1.0), [], ['onesf'])
        stage8 = A.f32(8, 1024)
        stage8b = A.f32(8, 1024)
        id2 = A.f32(2)
        P.dma(id2[0:2, :], d_ident[0:2, 0:2], [], ['id2'])
        C.compute_mods(d_cvec, d_wmod, d_bmod, 5, modT, cs, [stage8, stage8b], id2[0:2, :])
        P.dma(stage8[:, 0, 0:128], d_ident, [], ['wm_st0a'])
        P.op('dve', lambda e: e.tensor_copy(out=ident, in_=stage8[:, 0, 0:128]), ['wm_st0a'], ['consts'])
        P.op('dve', lambda e: e.memset(ones, 1.0), [], ['consts'])
        n1g, n2g = vecs[:, 0:8], vecs[:, 8:16]
        lrub, lam, convw, convb = vecs[:, 16:24], vecs[:, 24:28], vecs[:, 28:36], vecs[:, 36:38]
        qng, kvng, cmask = vecs[:, 38:41], vecs[:, 41:43], vecs[:, 43:51]
        mv = modT.rearrange("p (b k) w -> p b k w", b=5)
        for w in range(2):
            for (dst, scb, g) in ((0, 1, n1g), (3, 4, n2g)):
                P.op('dve', lambda e, w=w, dst=dst, scb=scb, g=g: e.scalar_tensor_tensor(
                    out=sv[:, dst, :, w], in0=mv[:, scb, :, w], scalar=1.0, in1=g, op0=ALU.add, op1=ALU.mult),
                    ['modv', 'vecs'], ['svt'])
            for (dst, src) in ((1, 0), (2, 2), (4, 3)):
                P.op('dve', lambda e, w=w, dst=dst, src=src: e.tensor_copy(out=sv[:, dst, :, w], in_=mv[:, src, :, w]),
                     ['modv'], ['svt'])
        P.act(spv, lam, AF.Exp, ['vecs'], ['spv'], scale=-1.0)
        P.act(spv, spv, AF.Ln, ['spv'], ['spv'], bias=1.0)
        P.op('dve', lambda e: e.tensor_single_scalar(out=spv, in_=spv, scalar=-8.0, op=ALU.mult), ['spv'], ['spv'])
        P.fence()
        A.release(m0)

        def svc(idx, w):
            return sv[:, idx, :, w]

        m1 = A.mark()
        wA_bf = A.bf(8, 640)
        wukv_bf = A.bf(2, 1024)
        wst = [(A.f32(1024), 'wst0'), (A.f32(1024), 'wst1')]

        def p1set():
            return dict(xs=A.f32(8, 512), sq=A.bf(8, 512), rstd=A.f32(512), tmpf=[A.f32(512), A.f32(512)],
                        h=A.bf(8, 512), xo=A.f32(2, 512), ckv=A.f32(2, 512), ckv_sq=A.bf(2, 512), rstd2=A.f32(512),
                        ckvn=A.bf(2, 512), Ko=A.bf(4, 512), Vo=A.bf(4, 4, 128), krf=A.f32(2, 512), rC=A.f32(512),
                        rS=A.f32(512), ko=A.bf(512))
        B1 = [p1set(), p1set()]
        X3 = [B1[0]['xs'], B1[1]['xs'], A.f32(8, 512)]
        C.load_cast(wukv_bf, d_wukv, 1024, 2, 'wukv', stage=wst)
        C.load_cast(wA_bf, d_wA, 640, 8, 'wA', scale_col=None, stage=wst)
        blocks = [(0, CTX, 1)] + [(CTX + 512 * i, 512, 0) for i in range(SEQ // 512)]
        caps = []
        for bi, (t0, n, w) in enumerate(blocks):
            B = B1[bi % 2]
            sx = str(bi % 2)
            C.ps_range = (0, 3) if bi % 2 == 0 else (3, 6)
            if bi == 0:
                for b2 in range(2):
                    t02, n2, _ = blocks[b2]
                    P.dma(X3[b2][:, :, 0:n2], d_xall[:, :, t02:t02 + n2], [], ['xst%d' % b2])
            P.begin_capture()
            xs, xk = X3[bi % 3], 'xst%d' % (bi % 3)
            if bi + 2 < len(blocks):
                t02, n2, _ = blocks[bi + 2]
                P.dma(X3[(bi + 2) % 3][:, :, 0:n2], d_xall[:, :, t02:t02 + n2], [], ['xst%d' % ((bi + 2) % 3)])
            C.sumsq_rstd(xs[:, :, 0:n], 8, n, D, B['rstd'], ones, B['sq'], [xk], 'n1' + sx)
            C.norm_mod(xs, n, B['rstd'], svc(0, w), svc(1, w), B['h'], B['tmpf'], [xk], ['h' + sx], 'n1' + sx)
            h_bf, xo, ckv, krf, ckvn = B['h'], B['xo'], B['ckv'], B['krf'], B['ckvn']
            for ct in range(6):
                M = 128 if ct < 4 else 64
                c0 = ct * 128 if ct < 4 else 512 + (ct - 4) * 64
                pi = C.next_ps()
                pk = 'ps%d' % pi
                for k in range(8):
                    P.mm(ps[pi][0:M, 0:n], wA_bf[:, k, c0:c0 + M], h_bf[:, k, 0:n], k == 0, k == 7,
                         ['wA', 'h' + sx], [pk])
                if ct < 2:
                    P.act(xo[:, ct, 0:n], ps[pi][:, 0:n], AF.Copy, [pk], ['xbo' + sx])
                elif ct < 4:
                    P.op('dve', lambda e, pi=pi, ct=ct, n=n, ckv=ckv: e.tensor_copy(out=ckv[:, ct - 2, 0:n],
                                                                                 in_=ps[pi][:, 0:n]),
                         [pk], ['ckv' + sx])
                else:
                    P.op('dve', lambda e, pi=pi, ct=ct, n=n, krf=krf: e.tensor_copy(out=krf[0:64, ct - 4, 0:n],
                                                                                 in_=ps[pi][0:64, 0:n]),
                         [pk], ['krf' + sx])
            P.dma(s_xb[:, :, t0:t0 + n].rearrange("c p t -> p c t"), xo[:, :, 0:n], ['xbo' + sx], ['s_xb%d' % bi])
            C.sumsq_rstd(ckv[:, :, 0:n], 2, n, 256, B['rstd2'], ones, B['ckv_sq'], ['ckv' + sx], 'kvn' + sx)
            for k in range(2):
                P.op('dve', lambda e, k=k, n=n, ckv=ckv, B=B: e.tensor_tensor(out=ckv[:, k, 0:n], in0=ckv[:, k, 0:n],
                                                                           in1=B['rstd2'][:, 0:n], op=ALU.mult),
                     ['ckv' + sx, 'kvn' + sx + '_rstd'], ['ckv' + sx])
                P.act(ckvn[:, k, 0:n], ckv[:, k, 0:n], AF.Copy, ['ckv' + sx, 'vecs'], ['ckvn' + sx],
                      scale=kvng[:, k:k + 1])
            Ko, Kk = B['Ko'], 'Ko' + sx
            for h in range(4):
                pi = C.next_ps()
                pk = 'ps%d' % pi
                for k in range(2):
                    P.mm(ps[pi][:, 0:n], wukv_bf[:, k, h * 128:(h + 1) * 128], ckvn[:, k, 0:n], k == 0, k == 1,
                         ['wukv', 'ckvn' + sx], [pk])
                P.act(Ko[:, h, 0:n], ps[pi][:, 0:n], AF.Copy, [pk], [Kk])
            P.dma(s_KT[:, :, t0:t0 + n].rearrange("h p t -> p h t"), Ko[:, :, 0:n], [Kk], ['s_KT%d' % bi])
            Vo, Vk = B['Vo'], 'Vo' + sx
            for tt in range(n // 128):
                pi = C.next_ps()
                pk = 'ps%d' % pi
                for k in range(2):
                    P.mm(ps[pi][:, 0:512], ckvn[:, k, tt * 128:(tt + 1) * 128], wukv_bf[:, k, 512:1024], k == 0, k == 1,
                         ['wukv', 'ckvn' + sx], [pk])
                P.op('dve', lambda e, pi=pi, tt=tt, Vo=Vo: e.tensor_copy(
                    out=Vo[:, :, tt, :], in_=ps[pi][:, 0:512].rearrange("p (h c) -> p h c", h=4)), [pk], [Vk])
            nt = n // 128
            for h in range(4):
                P.dma(s_V[h, :, t0 // 128:t0 // 128 + nt, :], Vo[:, h, 0:nt, :], [Vk], ['s_V%d_%d' % (bi, h)])
            ko, kk = B['ko'], 'kro' + sx
            if w == 0:
                l0 = t0 - CTX
                rC, rS = B['rC'], B['rS']
                P.dma(rC[0:64, 0:n], d_ropeC[:, l0:l0 + n], [], ['rC' + sx])
                P.dma(rS[0:64, 0:n], d_ropeS[:, l0:l0 + n], [], ['rS' + sx])
                P.op('pool', lambda e, n=n, krf=krf, rC=rC: e.tensor_tensor(out=krf[0:64, 0, 0:n], in0=krf[0:64, 0, 0:n],
                                                                          in1=rC[0:64, 0:n], op=ALU.mult),
                     ['krf' + sx, 'rC' + sx], ['krf' + sx])
                P.op('pool', lambda e, n=n, krf=krf, rS=rS: e.tensor_tensor(out=krf[0:64, 1, 0:n], in0=krf[0:64, 1, 0:n],
                                                                          in1=rS[0:64, 0:n], op=ALU.mult),
                     ['krf' + sx, 'rS' + sx], ['krf' + sx])
                P.op('pool', lambda e, n=n, ko=ko, krf=krf: e.tensor_tensor(out=ko[0:64, 0:n], in0=krf[0:64, 0, 0:n],
                                                                          in1=krf[0:64, 1, 0:n], op=ALU.add),
                     ['krf' + sx], [kk])
            else:
                P.op('pool', lambda e, n=n, ko=ko, krf=krf: e.tensor_copy(out=ko[0:64, 0:n], in_=krf[0:64, 0, 0:n]),
                     ['krf' + sx], [kk])
            P.dma(s_kr[:, t0:t0 + n], ko[0:64, 0:n], [kk], ['s_kr%d' % bi])
            caps.append(P.end_capture())
        C.ps_range = (0, 6)
        P.replay_interleaved(caps, 2)
        P.fence()
        A.release(m1)

        mixT = A.bf(8, NL)
        qTn = A.bf(4, NL)
        qTr = A.bf(4, NL)
        mP = A.mark()
        ysum = A.f32(2, NL)
        m2 = A.mark()
        lruw_bf = A.bf(8, 128)
        C.load_cast(lruw_bf, d_lruw, 128, 8, 'lruw', stage=[(A.f32(128), 'lst0'), (A.f32(128), 'lst1')])
        NCH = 1024
        NCK = SEQ // NCH

        def p2set():
            return dict(xi=A.f32(NCH + 3), cl=A.f32(NCH), clb=A.bf(NCH), rr=A.f32(NCH), ii=A.f32(NCH), aa=A.f32(NCH),
                        t1=A.f32(NCH), t2=A.f32(NCH), hh=A.f32(NCH))
        B2 = [p2set(), p2set()]
        carry = A.f32(4)
        chunks = [(0, CTX, True, True, -1)] + [(CTX + NCH * j, NCH, j == 0, j == NCK - 1, j) for j in range(NCK)]
        CPC = NLOC // NCH
        ci = 0
        first_lat = {}
        caps2 = []
        for ct in range(2):
            for d in range(2):
                order = chunks if d == 0 else [chunks[0]] + chunks[:0:-1]
                cv = carry[:, ct * 2 + d:ct * 2 + d + 1]
                for qi, (t0, n, lz, rz, j) in enumerate(order):
                    B = B2[ci % 2]
                    sx = str(ci % 2)
                    C.ps_range = (0, 3) if ci % 2 == 0 else (3, 6)
                    ci += 1
                    P.begin_capture()
                    xi, cl, clb, rr, ii, aa, t1, t2, hh = (B['xi'], B['cl'], B['clb'], B['rr'], B['ii'], B['aa'], B['t1'],
                                                           B['t2'], B['hh'])
                    xk = 'xin' + sx
                    lo = 0 if lz else 2
                    ro = 0 if rz else 1
                    P.dma(xi[:, 2 - lo:2 + n + ro], s_xb[ct, :, t0 - lo:t0 + n + ro], [], [xk])
                    if lz:
                        P.op('pool', lambda e, xi=xi: e.memset(xi[:, 0:2], 0.0), [], [xk])
                    if rz:
                        P.op('pool', lambda e, xi=xi, n=n: e.memset(xi[:, n + 2:n + 3], 0.0), [], [xk])
                    P.op('dve', lambda e, xi=xi, n=n, ct=ct, cl=cl: e.tensor_scalar(
                        out=cl[:, 0:n], in0=xi[:, 0:n], scalar1=convw[:, ct * 4:ct * 4 + 1],
                        scalar2=convb[:, ct:ct + 1], op0=ALU.mult, op1=ALU.add), [xk, 'vecs'], ['cl' + sx])
                    for k in range(1, 4):
                        P.op('dve', lambda e, xi=xi, n=n, k=k, ct=ct, cl=cl: e.scalar_tensor_tensor(
                            out=cl[:, 0:n], in0=xi[:, k:k + n], scalar=convw[:, ct * 4 + k:ct * 4 + k + 1],
                            in1=cl[:, 0:n], op0=ALU.mult, op1=ALU.add), [xk, 'vecs', 'cl' + sx], ['cl' + sx])
                    P.act(clb[:, 0:n], cl[:, 0:n], AF.Copy, ['cl' + sx], ['clb' + sx])
                    for g, dst, dk in ((0, rr, 'rr' + sx), (1, ii, 'ii' + sx)):
                        wi = d * 4 + g * 2 + ct
                        for sb in range((n + 511) // 512):
                            nn = min(512, n - sb * 512)
                            pi = C.next_ps()
                            pk = 'ps%d' % pi
                            P.mm(ps[pi][:, 0:nn], lruw_bf[:, wi, :], clb[:, sb * 512:sb * 512 + nn], True, True,
                                 ['lruw', 'clb' + sx], [pk])
                            P.act(dst[:, sb * 512:sb * 512 + nn], ps[pi][:, 0:nn], AF.Sigmoid, [pk, 'vecs'], [dk],
                                  bias=lrub[:, wi:wi + 1])
                    P.act(aa[:, 0:n], rr[:, 0:n], AF.Exp, ['rr' + sx, 'spv'], ['aa' + sx],
                          scale=spv[:, d * 2 + ct:d * 2 + ct + 1])
                    P.op('pool', lambda e, n=n, t1=t1, aa=aa: e.tensor_tensor(out=t1[:, 0:n], in0=aa[:, 0:n],
                                                                            in1=aa[:, 0:n], op=ALU.mult),
                         ['aa' + sx], ['t1' + sx])
                    P.act(t1[:, 0:n], t1[:, 0:n], AF.Sqrt, ['t1' + sx], ['t1' + sx], scale=-1.0, bias=1.0)
                    P.op('pool', lambda e, n=n, t2=t2, ii=ii, cl=cl: e.tensor_tensor(out=t2[:, 0:n], in0=ii[:, 0:n],
                                                                                   in1=cl[:, 0:n], op=ALU.mult),
                         ['ii' + sx, 'cl' + sx], ['t2' + sx])
                    P.op('pool', lambda e, n=n, t2=t2, t1=t1: e.tensor_tensor(out=t2[:, 0:n], in0=t2[:, 0:n],
                                                                            in1=t1[:, 0:n], op=ALU.mult),
                         ['t2' + sx, 't1' + sx], ['t2' + sx])
                    init = 0.0 if qi == 0 else cv
                    if d == 0:
                        P.op('dve', lambda e, n=n, init=init, hh=hh, aa=aa, t2=t2: e.tensor_tensor_scan(
                            out=hh[:, 0:n], data0=aa[:, 0:n], data1=t2[:, 0:n], initial=init, op0=ALU.mult,
                            op1=ALU.add), ['aa' + sx, 't2' + sx, 'carry'], ['hh' + sx])
                        P.op('dve', lambda e, n=n, cv=cv, hh=hh: e.tensor_copy(out=cv, in_=hh[:, n - 1:n]),
                             ['hh' + sx], ['carry'])
                    else:
                        P.op('dve', lambda e, n=n, init=init, hh=hh, aa=aa, t2=t2: e.tensor_tensor_scan(
                            out=hh[:, 0:n][:, ::-1], data0=aa[:, 0:n][:, ::-1], data1=t2[:, 0:n][:, ::-1],
                            initial=init, op0=ALU.mult, op1=ALU.add), ['aa' + sx, 't2' + sx, 'carry'], ['hh' + sx])
                        P.op('dve', lambda e, cv=cv, hh=hh: e.tensor_copy(out=cv, in_=hh[:, 0:1]), ['hh' + sx], ['carry'])
                    if j < 0:
                        if d == 0:
                            P.op('pool', lambda e, n=n, ct=ct, hh=hh: e.tensor_copy(out=ysum[:, ct, 0:n], in_=hh[:, 0:n]),
                                 ['hh' + sx], ['ysum'])
                        else:
                            P.op('pool', lambda e, n=n, ct=ct, hh=hh: e.tensor_tensor(
                                out=ysum[:, ct, 0:n], in0=ysum[:, ct, 0:n], in1=hh[:, 0:n], op=ALU.add),
                                ['hh' + sx, 'ysum'], ['ysum'])
                    else:
                        jc, off = j // CPC, CTX + (j % CPC) * NCH
                        key = (ct, j % CPC)
                        if key not in first_lat:
                            first_lat[key] = True
                            P.op('dve', lambda e, n=n, jc=jc, off=off, ct=ct, hh=hh: e.tensor_scalar(
                                out=ysum[:, ct, off:off + n], in0=hh[:, 0:n], scalar1=cmask[:, jc:jc + 1], scalar2=None,
                                op0=ALU.mult), ['hh' + sx, 'vecs'], ['ysum'])
                        else:
                            P.op('dve', lambda e, n=n, jc=jc, off=off, ct=ct, hh=hh: e.scalar_tensor_tensor(
                                out=ysum[:, ct, off:off + n], in0=hh[:, 0:n], scalar=cmask[:, jc:jc + 1],
                                in1=ysum[:, ct, off:off + n], op0=ALU.mult, op1=ALU.add),
                                ['hh' + sx, 'vecs', 'ysum'], ['ysum'])
                    caps2.append(P.end_capture())
        C.ps_range = (0, 6)
        P.replay_interleaved(caps2, 2)
        P.fence()
        A.release(m2)

        wB_bf = A.bf(8, 1152)
        wuq_bf = A.bf(3, 1024)
        sguw_bf = A.bf(4, 128)
        sgub = A.f32(2, 128)
        m3 = A.mark()
        wst3 = [(A.f32(1152), 'wst3_0'), (A.f32(1152), 'wst3_1')]
        C.load_cast(wB_bf, d_wB, 1152, 8, 'wB', stage=wst3)
        C.load_cast(wuq_bf, d_wuq, 1024, 3, 'wuq', stage=wst3)
        C.load_cast(sguw_bf, d_sguw, 128, 4, 'sguw', stage=wst3)
        P.dma(sgub, d_sgub, [], ['sgub'])
        P.fence()
        A.release(m3)
        xs3 = A.f32(8, 512)
        sq3 = A.bf(8, 512)
        rstd3 = A.f32(512)
        tmp3 = [A.f32(512), A.f32(512)]
        h3 = A.bf(8, 512)
        u_bf = A.bf(2, 512)
        vf = A.f32(2, 512)
        vn_bf = A.bf(2, 512)
        vtok = A.bf(256)
        gbf = A.f32(2, 512)
        cqf = A.f32(3, 512)
        cqn = A.bf(3, 512)
        rstdq = A.f32(512)
        rstdv = A.f32(512)
        sqv = A.bf(2, 512)
        sqq = A.bf(3, 512)
        rCl = A.f32(512)
        rSl = A.f32(512)
        tq1 = A.f32(512)
        tq2 = A.f32(512)
        tg = A.f32(128)
        lblocks = [(d_xall[:, :, 0:CTX], CTX, 1, 0, -1)] + \
                  [(d_xloc[:, :, 512 * i:512 * (i + 1)], 512, 0, CTX + 512 * i, 512 * i) for i in range(NLOC // 512)]
        for (src, n, w, q0, l0) in lblocks:
            P.dma(xs3[:, :, 0:n], src, [], ['xs3'])
            C.sumsq_rstd(xs3[:, :, 0:n], 8, n, D, rstd3, ones, sq3, ['xs3'], 'n3')
            C.norm_mod(xs3, n, rstd3, svc(0, w), svc(1, w), h3, tmp3, ['xs3'], ['h3'], 'n3')
            for ct in range(9):
                pi = C.next_ps()
                pk = 'ps%d' % pi
                for k in range(8):
                    P.mm(ps[pi][:, 0:n], wB_bf[:, k, ct * 128:(ct + 1) * 128], h3[:, k, 0:n], k == 0, k == 7,
                         ['wB', 'h3'], [pk])
                if ct < 2:
                    P.act(u_bf[:, ct, 0:n], ps[pi][:, 0:n], AF.Gelu_apprx_tanh, [pk], ['u'])
                elif ct < 4:
                    P.act(vf[:, ct - 2, 0:n], ps[pi][:, 0:n], AF.Gelu_apprx_tanh, [pk], ['vf'])
                elif ct < 6:
                    P.act(gbf[:, ct - 4, 0:n], ps[pi][:, 0:n], AF.Gelu_apprx_tanh, [pk], ['gbf'])
                    P.op('dve', lambda e, ct=ct, n=n, q0=q0: e.tensor_tensor(
                        out=mixT[:, 2 + ct - 4, q0:q0 + n], in0=gbf[:, ct - 4, 0:n], in1=ysum[:, ct - 4, q0:q0 + n],
                        op=ALU.mult), ['gbf', 'ysum'], ['mix_b'])
                else:
                    P.op('dve', lambda e, ct=ct, n=n, pi=pi: e.tensor_copy(out=cqf[:, ct - 6, 0:n], in_=ps[pi][:, 0:n]),
                         [pk], ['cqf'])
            C.sumsq_rstd(vf[:, :, 0:n], 2, n, 256, rstdv, ones, sqv, ['vf'], 'vn')
            for k in range(2):
                P.op('dve', lambda e, k=k, n=n: e.tensor_tensor(out=vn_bf[:, k, 0:n], in0=vf[:, k, 0:n],
                                                                in1=rstdv[:, 0:n], op=ALU.mult),
                     ['vf', 'vn_rstd'], ['vn'])
            for tt in range(n // 128):
                for k in range(2):
                    P.tr(psb[:, k * 128:(k + 1) * 128], vn_bf[:, k, tt * 128:(tt + 1) * 128], ident,
                         ['vn', 'consts'], ['psb'])
                P.op('dve', lambda e: e.tensor_copy(out=vtok, in_=psb[:, 0:256]), ['psb'], ['vtok'])
                pi = C.next_ps()
                pk = 'ps%d' % pi
                for g in range(4):
                    P.mm(ps[pi][(g % 2) * 64:(g % 2) * 64 + 64, (g // 2) * 128:(g // 2) * 128 + 128],
                         vtok[:, g * 64:(g + 1) * 64], sguw_bf[:, g, :], True, True, ['vtok', 'sguw'], [pk])
                for c2 in range(2):
                    P.op('dve', lambda e, c2=c2, pi=pi: e.tensor_tensor(out=tg, in0=ps[pi][:, c2 * 128:(c2 + 1) * 128],
                                                                        in1=sgub[:, c2, :], op=ALU.add),
                         [pk, 'sgub'], ['tg'])
                    P.op('dve', lambda e, c2=c2, tt=tt, q0=q0: e.tensor_tensor(
                        out=mixT[:, c2, q0 + tt * 128:q0 + (tt + 1) * 128], in0=tg,
                        in1=u_bf[:, c2, tt * 128:(tt + 1) * 128], op=ALU.mult), ['tg', 'u'], ['mix_a'])
            C.sumsq_rstd(cqf[:, :, 0:n], 3, n, 384, rstdq, ones, sqq, ['cqf'], 'qn')
            for k in range(3):
                P.op('dve', lambda e, k=k, n=n: e.tensor_tensor(out=cqf[:, k, 0:n], in0=cqf[:, k, 0:n],
                                                                in1=rstdq[:, 0:n], op=ALU.mult),
                     ['cqf', 'qn_rstd'], ['cqf'])
                P.act(cqn[:, k, 0:n], cqf[:, k, 0:n], AF.Copy, ['cqf', 'vecs'], ['cqn'], scale=qng[:, k:k + 1])
            if w == 0:
                P.dma(rCl[0:64, 0:n], d_ropeCl[:, l0:l0 + n], [], ['rCl'])
                P.dma(rSl[0:64, 0:n], d_ropeSl[:, l0:l0 + n], [], ['rSl'])
            for h in range(4):
                pi = C.next_ps()
                pk = 'ps%d' % pi
                for k in range(3):
                    P.mm(ps[pi][:, 0:n], wuq_bf[:, k, h * 256:h * 256 + 128], cqn[:, k, 0:n], k == 0, k == 2,
                         ['wuq', 'cqn'], [pk])
                P.act(qTn[:, h, q0:q0 + n], ps[pi][:, 0:n], AF.Copy, [pk], ['qTn'])
                pa = C.next_ps()
                pak = 'ps%d' % pa
                for k in range(3):
                    P.mm(ps[pa][0:64, 0:n], wuq_bf[:, k, h * 256 + 128:h * 256 + 192], cqn[:, k, 0:n], k == 0, k == 2,
                         ['wuq', 'cqn'], [pak])
                if w == 1:
                    P.act(qTr[0:64, h, q0:q0 + n], ps[pa][0:64, 0:n], AF.Copy, [pak], ['qTr'])
                else:
                    pb = C.next_ps()
                    pbk = 'ps%d' % pb
                    for k in range(3):
                        P.mm(ps[pb][0:64, 0:n], wuq_bf[:, k, h * 256 + 192:h * 256 + 256], cqn[:, k, 0:n], k == 0,
                             k == 2, ['wuq', 'cqn'], [pbk])
                    P.op('dve', lambda e, pa=pa, n=n: e.tensor_tensor(out=tq1[0:64, 0:n], in0=ps[pa][0:64, 0:n],
                                                                      in1=rCl[0:64, 0:n], op=ALU.mult),
                         [pak, 'rCl'], ['tq1'])
                    P.op('dve', lambda e, pb=pb, n=n: e.tensor_tensor(out=tq2[0:64, 0:n], in0=ps[pb][0:64, 0:n],
                                                                      in1=rSl[0:64, 0:n], op=ALU.mult),
                         [pbk, 'rSl'], ['tq2'])
                    P.op('pool', lambda e, h=h, n=n, q0=q0: e.tensor_tensor(
                        out=qTr[0:64, h, q0:q0 + n], in0=tq1[0:64, 0:n], in1=tq2[0:64, 0:n], op=ALU.add),
                        ['tq1', 'tq2'], ['qTr'])
        P.fence()
        A.release(mP)

        NKT = TALL // 128
        KT = A.bf(TALL)
        Vh = A.bf(NKT, 128)
        krT = A.bf(TALL)
        PT = [A.bf(512) for _ in range(6)]
        rinv = A.f32(512)
        lacc = [[A.f32(512) for _ in range(3)] for _ in range(2)]
        NPC = 5
        KPP = NKT // NPC
        for pc in range(NPC):
            a, b = pc * KPP * 128, (pc + 1) * KPP * 128
            P.dma(krT[0:64, a:b], s_kr[:, a:b], ['s_kr'], ['kr_p%d' % pc])
        qblocks = [(0, CTX, 2)] + [(CTX + 512 * i, 512, NKT) for i in range(NLOC // 512)]

        def load_kv(h, pc):
            a, b = pc * KPP * 128, (pc + 1) * KPP * 128
            P.dma(KT[:, a:b], s_KT[h, :, a:b], ['s_KT'], ['KT_p%d' % pc])
            P.dma(Vh[:, pc * KPP:(pc + 1) * KPP, :], s_V[h, :, pc * KPP:(pc + 1) * KPP, :], ['s_V'], ['V_p%d' % pc])
        tiles = []
        qbi = 0
        for h in range(4):
            for qi, (q0, n, nkt) in enumerate(qblocks):
                po, pl = 3 + qbi % 2, 5 + qbi % 2
                qbi += 1
                for kt in range(nkt):
                    tiles.append((h, qi, q0, n, nkt, kt, po, pl))

        def emit_qk(i):
            h, qi, q0, n, nkt, kt, po, pl = tiles[i]
            sb = i % 3
            sk = 'ps%d' % sb
            pc = kt // KPP
            P.mm(ps[sb][:, 0:n], KT[:, kt * 128:(kt + 1) * 128], qTn[:, h, q0:q0 + n], True, False,
                 ['KT_p%d' % pc, 'qTn'], [sk])
            P.mm(ps[sb][:, 0:n], krT[0:64, kt * 128:(kt + 1) * 128], qTr[0:64, h, q0:q0 + n], False, True,
                 ['kr_p%d' % pc, 'qTr'], [sk])
        LA = 2
        for pc in range(NPC):
            load_kv(0, pc)
        for i in range(LA):
            emit_qk(i)
        for i, (h, qi, q0, n, nkt, kt, po, pl) in enumerate(tiles):
            if i + LA < len(tiles):
                emit_qk(i + LA)
            sb = i % 3
            pc = kt // KPP
            pt = PT[i % 6]
            ptk = 'PT%d' % (i % 6)
            P.act(pt[:, 0:n], ps[sb][:, 0:n], AF.Exp, ['ps%d' % sb], [ptk], scale=ATTN_SCALE)
            P.mm(ps[po][:, 0:n], Vh[:, kt, :], pt[:, 0:n], kt == 0, kt == nkt - 1, ['V_p%d' % pc, ptk], ['ps%d' % po])
            c3 = kt % 3
            la, lak = lacc[po - 3][c3], 'lacc%d_%d' % (po - 3, c3)
            le = 'pool' if c3 == 2 else 'dve'
            if kt < 3:
                P.op(le, lambda e, la=la, pt=pt, n=n: e.tensor_copy(out=la[:, 0:n], in_=pt[:, 0:n]), [ptk], [lak])
            else:
                P.op(le, lambda e, la=la, pt=pt, n=n: e.tensor_tensor(out=la[:, 0:n], in0=la[:, 0:n], in1=pt[:, 0:n],
                                                                     op=ALU.add), [ptk, lak], [lak])
            if kt == nkt - 1:
                nacc = min(3, nkt)
                for c in range(nacc):
                    P.mm(ps[pl][:, 0:n], onesf, lacc[po - 3][c][:, 0:n], c == 0, c == nacc - 1,
                         ['lacc%d_%d' % (po - 3, c), 'onesf'], ['ps%d' % pl])
                P.op('dve', lambda e, pl=pl, n=n: e.reciprocal(out=rinv[:, 0:n], in_=ps[pl][:, 0:n]),
                     ['ps%d' % pl], ['rinv'])
                P.op('dve', lambda e, po=po, n=n, h=h, q0=q0: e.tensor_tensor(
                    out=mixT[:, 4 + h, q0:q0 + n], in0=ps[po][:, 0:n], in1=rinv[:, 0:n], op=ALU.mult),
                    ['ps%d' % po, 'rinv'], ['mix_c'])
            if qi == len(qblocks) - 1 and kt % KPP == KPP - 1 and h < 3:
                load_kv(h + 1, kt // KPP)
        P.fence()
        A.release(mP)

        wout_bf = A.bf(8, 1024)
        C.load_cast(wout_bf, d_wout, 1024, 8, 'wout', stage=[(A.f32(1024), 'wst5_0'), (A.f32(1024), 'wst5_1')])
        wr = A.f32(8, 16)
        P.dma(wr, d_wr, [], ['wr'])
        xr = A.f32(8, 512)
        x1 = A.f32(8, 512)
        sq5 = A.bf(8, 512)
        rstd5 = A.f32(512)
        tmp5 = [A.f32(512), A.f32(512)]
        h2 = A.f32(8, 512)
        sm = A.f32(4, 4)
        pe_ = A.f32(4, 16)
        for (src, n, w, q0, l0) in lblocks:
            P.dma(xr[:, :, 0:n], src, [], ['xr'])
            for j in range(8):
                pi = C.next_ps()
                pk = 'ps%d' % pi
                for k in range(8):
                    P.mm(ps[pi][:, 0:n], wout_bf[:, k, j * 128:(j + 1) * 128], mixT[:, k, q0:q0 + n], k == 0, k == 7,
                         ['wout', 'mix_a', 'mix_b', 'mix_c'], [pk])
                P.op('dve', lambda e, j=j, pi=pi, n=n, w=w: e.scalar_tensor_tensor(
                    out=x1[:, j, 0:n], in0=ps[pi][:, 0:n], scalar=svc(2, w)[:, j:j + 1], in1=xr[:, j, 0:n],
                    op0=ALU.mult, op1=ALU.add), [pk, 'xr', 'svt'], ['x1'])
            P.dma(o_x1[:, :, q0:q0 + n], x1[:, :, 0:n], ['x1'], ['o_x1'])
            C.sumsq_rstd(x1[:, :, 0:n], 8, n, D, rstd5, ones, sq5, ['x1'], 'n5')
            C.norm_mod(x1, n, rstd5, svc(3, w), svc(4, w), h2, tmp5, ['x1'], ['h2'], 'n5')
            for tt in range(n // 128):
                pi = C.next_ps()
                pk = 'ps%d' % pi
                si = tt % 4
                smk = 'sm%d' % si
                for k in range(8):
                    P.mm(ps[pi][:, 0:16], h2[:, k, tt * 128:(tt + 1) * 128], wr[:, k, :], k == 0, k == 7,
                         ['h2', 'wr'], [pk])
                P.op('dve', lambda e, pi=pi, si=si: e.reduce_max(out=sm[:, si, 0:1], in_=ps[pi][:, 0:16],
                                                                 axis=mybir.AxisListType.X), [pk], [smk])
                P.op('dve', lambda e, si=si: e.tensor_single_scalar(out=sm[:, si, 1:2], in_=sm[:, si, 0:1], scalar=-1.0,
                                                                    op=ALU.mult), [smk], [smk])
                P.act(pe_[:, si, :], ps[pi][:, 0:16], AF.Exp, [pk, smk], ['pe%d' % si], bias=sm[:, si, 1:2],
                      accum=sm[:, si, 2:3])
                P.op('dve', lambda e, si=si: e.reciprocal(out=sm[:, si, 3:4], in_=sm[:, si, 2:3]), ['pe%d' % si, smk],
                     [smk])
                P.op('dve', lambda e, si=si: e.tensor_scalar(out=pe_[:, si, :], in0=pe_[:, si, :],
                                                             scalar1=sm[:, si, 3:4], scalar2=None, op0=ALU.mult),
                     [smk, 'pe%d' % si], ['pe%d' % si])
                P.dma(o_probs[q0 + tt * 128:q0 + (tt + 1) * 128, :], pe_[:, si, :], ['pe%d' % si], ['o_probs'])
        P.finalize()
        return nc, P, A


def _fm(a):
    k = a.shape[0] // 128
    return np.ascontiguousarray(a.reshape(k, 128, -1).transpose(1, 0, 2))


def _fmv(v):
    return np.ascontiguousarray(v.reshape(-1, 128).T)


def _rope_perm():
    p = np.arange(64)
    o = ((p % 32) // 16) * 32 + (p // 32) * 16 + (p % 16)
    osw = o[(p + 32) % 64]
    return o, osw


def _rope_tables():
    rows = SEQ // 64
    r = np.repeat(np.arange(rows), 64).astype(np.float32)
    col = np.tile(np.arange(64), rows).astype(np.float32)
    inv = (np.float32(10000.0) ** (-np.arange(16, dtype=np.float32) / np.float32(16))).astype(np.float32)
    ang = np.concatenate([r[:, None] * inv, col[:, None] * inv], axis=-1).astype(np.float32)
    cos, sin = np.cos(ang).astype(np.float32), np.sin(ang).astype(np.float32)
    C_ = np.concatenate([cos, cos], axis=1).T
    S_ = np.concatenate([-sin, sin], axis=1).T
    return np.ascontiguousarray(C_), np.ascontiguousarray(S_)


def prep_AB(inp, l, xl, xc):
    o, osw = _rope_perm()
    xall = _fm(np.ascontiguousarray(np.concatenate([xc, xl], axis=0).T))
    cvec = np.stack([_fmv(inp['c'][0]), _fmv(inp['c_ctx'])], axis=-1)
    w_in = inp['w_in'][l]
    wA = np.concatenate([w_in[:, 512:768], w_in[:, 1408:1664], w_in[:, 1664 + o], w_in[:, 1664 + osw]], axis=1)
    wB = np.concatenate([w_in[:, 0:512], w_in[:, 768:1024], w_in[:, 1024:1408]], axis=1)
    wuq = inp['w_uq'][l]
    cols = []
    for h in range(4):
        cols += [wuq[:, h * 192:h * 192 + 128], wuq[:, h * 192 + 128 + o], wuq[:, h * 192 + 128 + osw]]
    wuq2 = np.concatenate(cols, axis=1)
    wukv = inp['w_ukv'][l]
    wukv2 = np.concatenate([wukv[:, h * 256:h * 256 + 128] for h in range(4)] +
                           [wukv[:, h * 256 + 128:h * 256 + 256] for h in range(4)], axis=1)
    lruw = np.zeros((128, 8, 128), np.float32)
    vecs = np.zeros((128, 64), np.float32)
    vecs[:, 0:8] = _fmv(inp['norm1_g'][l])
    vecs[:, 8:16] = _fmv(inp['norm2_g'][l])
    for d in range(2):
        for g in range(2):
            W = (inp['lru_wa'] if g == 0 else inp['lru_wx'])[l][d]
            bb = (inp['lru_ba'] if g == 0 else inp['lru_bx'])[l][d]
            for ct in range(2):
                i = d * 4 + g * 2 + ct
                lruw[0:64, i, 0:64] = W[2 * ct]
                lruw[64:128, i, 64:128] = W[2 * ct + 1]
                vecs[:, 16 + i] = bb[ct * 128:(ct + 1) * 128]
        for ct in range(2):
            vecs[:, 24 + d * 2 + ct] = inp['lru_lambda'][l][d][ct * 128:(ct + 1) * 128]
    for ct in range(2):
        for k in range(4):
            vecs[:, 28 + ct * 4 + k] = inp['conv_w'][l][k][ct * 128:(ct + 1) * 128]
        vecs[:, 36 + ct] = inp['conv_b'][l][ct * 128:(ct + 1) * 128]
    vecs[:, 38:41] = _fmv(inp['q_norm_g'][l])
    vecs[:, 41:43] = _fmv(inp['kv_norm_g'][l])
    sguw = np.ascontiguousarray(inp['sgu_w'][l].transpose(2, 0, 1))
    sgub = np.zeros((128, 2, 128), np.float32)
    for g in range(4):
        sgub[(g % 2) * 64:(g % 2) * 64 + 64, g // 2, :] = inp['sgu_b'][l][g][None, :]
    rC, rS = _rope_tables()
    common = dict(xall=xall, cvec=np.ascontiguousarray(cvec), wmod=_fm(np.ascontiguousarray(inp['w_mod'][l][:, :5120])),
                  bmod=_fmv(inp['b_mod'][l][:5120]), wA=_fm(wA), wB=_fm(wB), wuq=_fm(wuq2), wukv=_fm(wukv2),
                  wout=_fm(inp['w_out'][l]), lruw=lruw, sguw=sguw, sgub=sgub, wr=_fm(inp['w_router'][l]),
                  ropeC=rC, ropeS=rS, ident=np.eye(128, dtype=np.float32))
    maps = []
    for c in range(NCORES):
        v = vecs.copy()
        v[:, 43 + c] = 1.0
        m = dict(common)
        m['vecs'] = v
        m['xloc'] = np.ascontiguousarray(xall[:, :, CTX + NLOC * c:CTX + NLOC * (c + 1)])
        m['ropeCl'] = np.ascontiguousarray(rC[:, NLOC * c:NLOC * (c + 1)])
        m['ropeSl'] = np.ascontiguousarray(rS[:, NLOC * c:NLOC * (c + 1)])
        maps.append(m)
    return maps


def _unfm(a):
    return np.ascontiguousarray(a.transpose(1, 0, 2).reshape(-1, a.shape[2]).T)


def gather_AB(res):
    xl1 = np.concatenate([_unfm(r['x1T'][:, :, CTX:]) for r in res], axis=0)
    xc1 = _unfm(res[0]['x1T'][:, :, :CTX])
    pl = np.concatenate([r['probs'][CTX:] for r in res], axis=0)
    pc = res[0]['probs'][:CTX]
    return xl1, xc1, pl, pc


NBIS = 30


def build_C():
    nc = bass.Bass("TRN2", target_bir_lowering=False)

    def din(name, shape, dt=F32):
        return nc.dram_tensor(name, list(shape), dt, kind="ExternalInput").ap()
    d_x1 = din("x1T", [128, 8, NL])
    d_pA = din("probsA", [128, NLOC])
    d_pC = din("probsC", [16, CTX])
    d_gm = din("gmT", [16, NL])
    d_cvec = din("cvec", [128, 8, 2])
    d_wmod = din("wmod", [128, 8, 3072])
    d_bmod = din("bmod", [128, 24])
    d_vecs = din("vecs", [128, 16])
    d_G = din("G", [128, 128])
    d_oh = din("oh16", [16, 16])
    d_wg = din("wg", [NEXP, D, D])
    d_wu = din("wu", [NEXP, D, D])
    d_wd = din("wd", [NEXP, D, D])
    o_x2 = nc.dram_tensor("x2T", [128, 8, NL], F32, kind="ExternalOutput").ap()
    o_on = nc.dram_tensor("outN", [128, 8, NL], F32, kind="ExternalOutput").ap()

    AW = 51 * 1024
    with ExitStack() as es:
        arena_t = es.enter_context(nc.sbuf_tensor("arena", [128, AW], F32))
        ps = [es.enter_context(nc.psum_tensor("ps%d" % i, [128, 512], F32)) for i in range(7)]
        P = Prog(nc)
        A = Arena(arena_t, AW)
        C = Ctx(nc, P, A, ps, None)

        vecs = A.f32(16)
        modT = A.f32(24, 2)
        cs = A.f32(64)
        sv = A.f32(3, 8, 2)
        G = A.f32(128)
        oh = A.f32(16)
        ones16 = A.f32(128)
        ones = A.bf(128)
        ts = A.f32(16)
        m0 = A.mark()
        P.dma(vecs, d_vecs, [], ['vecs'])
        P.dma(G, d_G, [], ['consts'])
        P.dma(oh[0:16, :], d_oh, [], ['consts'])
        P.op('dve', lambda e: e.memset(ones, 1.0), [], ['consts'])
        P.op('dve', lambda e: e.memset(ones16, 1.0), [], ['consts'])
        stage8 = A.f32(8, 1024)
        stage8b = A.f32(8, 1024)
        C.compute_mods(d_cvec, d_wmod, d_bmod, 3, modT, cs, [stage8, stage8b], oh[0:2, 0:2])
        n2g, fng = vecs[:, 0:8], vecs[:, 8:16]
        mv = modT.rearrange("p (b k) w -> p b k w", b=3)
        for w in range(2):
            P.op('dve', lambda e, w=w: e.scalar_tensor_tensor(
                out=sv[:, 0, :, w], in0=mv[:, 1, :, w], scalar=1.0, in1=n2g, op0=ALU.add, op1=ALU.mult),
                ['modv', 'vecs'], ['svt'])
            P.op('dve', lambda e, w=w: e.tensor_copy(out=sv[:, 1, :, w], in_=mv[:, 0, :, w]), ['modv'], ['svt'])
            P.op('dve', lambda e, w=w: e.tensor_copy(out=sv[:, 2, :, w], in_=mv[:, 2, :, w]), ['modv'], ['svt'])
        P.fence()
        A.release(m0)

        h2T = A.bf(8, NL)
        acc = A.f32(8, NL)
        gmT = A.f32(NL)
        mL = A.mark()
        lblocks = [(0, CTX, 1)] + [(CTX + 512 * i, 512, 0) for i in range(NLOC // 512)]

        xs = [A.f32(8, 512), A.f32(8, 512)]
        sq = A.bf(8, 512)
        rstd = A.f32(512)
        tmpf = [A.f32(512), A.f32(512)]
        for bi, (q0, n, w) in enumerate(lblocks):
            x = xs[bi % 2]
            xk = 'xs%d' % (bi % 2)
            P.dma(x[:, :, 0:n], d_x1[:, :, q0:q0 + n], [], [xk])
            C.sumsq_rstd(x[:, :, 0:n], 8, n, D, rstd, ones, sq, [xk], 'n2')
            C.norm_mod(x, n, rstd, sv[:, 0, :, w], sv[:, 1, :, w], h2T[:, :, q0:q0 + n], tmpf, [xk], ['h2T'], 'n2')
        P.fence()
        A.release(mL)

        pA = A.f32(NLOC)
        mk = A.f32(NLOC)
        pC = A.f32(CTX)
        mkc = A.f32(CTX)
        P.dma(pA, d_pA, [], ['pA'])
        P.dma(pC[0:16, :], d_pC, [], ['pC'])
        P.dma(gmT[0:16, :], d_gm, [], ['gmT'])
        lo, hi, mid, tot, gt, dd, Kv = (ts[:, 0:2], ts[:, 2:4], ts[:, 4:6], ts[:, 6:8], ts[:, 8:10], ts[:, 10:12],
                                        ts[:, 12:14])
        P.op('dve', lambda e: e.memset(ts, 0.0), [], ['ts'])
        P.op('dve', lambda e: e.memset(hi, 1.0), ['ts'], ['ts'])
        P.op('dve', lambda e: e.memset(mid, 0.5), ['ts'], ['ts'])
        P.op('dve', lambda e: e.memset(ts[:, 12:13], NLOC - 0.5), ['ts'], ['ts'])
        P.op('dve', lambda e: e.memset(ts[:, 13:14], 2 * CTX // NEXP - 0.5), ['ts'], ['ts'])
        for it in range(NBIS):
            P.op('dve', lambda e: e.tensor_scalar(out=mk, in0=pA, scalar1=ts[:, 4:5], scalar2=None, op0=ALU.is_gt),
                 ['pA', 'ts'], ['mk'])
            P.op('dve', lambda e: e.reduce_sum(out=ts[:, 14:15], in_=mk, axis=mybir.AxisListType.X), ['mk'], ['cc'])
            P.op('dve', lambda e: e.tensor_scalar(out=mkc[0:16, :], in0=pC[0:16, :], scalar1=ts[0:16, 5:6], scalar2=None,
                                                  op0=ALU.is_gt), ['pC', 'ts'], ['mkc'])
            P.op('dve', lambda e: e.reduce_sum(out=ts[0:16, 7:8], in_=mkc[0:16, :], axis=mybir.AxisListType.X),
                 ['mkc', 'ts'], ['ts'])
            P.mm(ps[6][:, 0:1], G, ts[:, 14:15], True, True, ['cc', 'consts'], ['ps6'])
            P.op('dve', lambda e: e.tensor_copy(out=ts[:, 6:7], in_=ps[6][:, 0:1]), ['ps6', 'ts'], ['ts'])
            P.op('dve', lambda e: e.tensor_tensor(out=gt, in0=tot, in1=Kv, op=ALU.is_gt), ['ts'], ['ts'])
            P.op('dve', lambda e: e.tensor_tensor(out=dd, in0=mid, in1=lo, op=ALU.subtract), ['ts'], ['ts'])
            P.op('dve', lambda e: e.tensor_tensor(out=dd, in0=dd, in1=gt, op=ALU.mult), ['ts'], ['ts'])
            P.op('dve', lambda e: e.tensor_tensor(out=lo, in0=lo, in1=dd, op=ALU.add), ['ts'], ['ts'])
            P.op('dve', lambda e: e.tensor_tensor(out=dd, in0=hi, in1=mid, op=ALU.subtract), ['ts'], ['ts'])
            P.op('dve', lambda e: e.tensor_tensor(out=dd, in0=dd, in1=gt, op=ALU.mult), ['ts'], ['ts'])
            P.op('dve', lambda e: e.tensor_tensor(out=hi, in0=mid, in1=dd, op=ALU.add), ['ts'], ['ts'])
            P.op('dve', lambda e: e.tensor_tensor(out=dd, in0=lo, in1=hi, op=ALU.add), ['ts'], ['ts'])
            P.op('dve', lambda e: e.tensor_single_scalar(out=mid, in_=dd, scalar=0.5, op=ALU.mult), ['ts'], ['ts'])
        P.op('dve', lambda e: e.tensor_scalar(out=mk[0:16, 0:NLOC], in0=gmT[0:16, CTX:NL], scalar1=ts[0:16, 2:3],
                                              scalar2=None, op0=ALU.is_gt), ['gmT', 'ts'], ['mk'])
        P.op('dve', lambda e: e.tensor_tensor(out=gmT[0:16, CTX:NL], in0=gmT[0:16, CTX:NL], in1=mk[0:16, 0:NLOC],
                                              op=ALU.mult), ['mk', 'gmT'], ['gmT'])
        P.op('dve', lambda e: e.tensor_scalar(out=mkc[0:16, :], in0=gmT[0:16, 0:CTX], scalar1=ts[0:16, 3:4],
                                              scalar2=None, op0=ALU.is_gt), ['gmT', 'ts'], ['mkc'])
        P.op('dve', lambda e: e.tensor_tensor(out=gmT[0:16, 0:CTX], in0=gmT[0:16, 0:CTX], in1=mkc[0:16, :],
                                              op=ALU.mult), ['mkc', 'gmT'], ['gmT'])
        P.fence()
        A.release(mL)

        HF = 512
        wbuf = [dict(g=A.bf(8, HF), u=A.bf(8, HF), d=A.bf(4, 1024)) for _ in range(2)]
        wst = [(A.f32(1024), 'wst0'), (A.f32(1024), 'wst1')]
        hid = A.bf(4, 512)
        sg = [A.f32(512), A.f32(512)]
        tt_ = [A.f32(512), A.f32(512)]
        gmb = A.f32(512)
        gme = A.f32(512)
        units = [(ex, hf) for ex in range(NEXP) for hf in range(2)]
        stc = [0]
        gmbs = [gmb, A.f32(512)]
        gmes = [gme, A.f32(512)]

        def emit_gm(gi):
            ui2, bi2 = gi // len(lblocks), gi % len(lblocks)
            ex2 = units[ui2][0]
            q02, n2, _ = lblocks[bi2]
            ge, gb_ = gmes[gi % 2], gmbs[gi % 2]
            P.op('dve', lambda e: e.tensor_scalar(out=ge[0:16, 0:n2], in0=gmT[0:16, q02:q02 + n2],
                                                  scalar1=oh[0:16, ex2:ex2 + 1], scalar2=None, op0=ALU.mult),
                 ['gmT', 'consts'], ['gme%d' % (gi % 2)])
            P.mm(ps[6][:, 0:n2], ones16[0:16, :], ge[0:16, 0:n2], True, True, ['gme%d' % (gi % 2), 'consts'], ['ps6'])
            P.act(gb_[:, 0:n2], ps[6][:, 0:n2], AF.Copy, ['ps6'], ['gmb%d' % (gi % 2)])

        def load_steps(ui):
            ex, hf = units[ui]
            wb = wbuf[ui % 2]
            tag = 'w%d' % (ui % 2)
            steps = []
            for nm, dsrc in (('g', d_wg), ('u', d_wu)):
                for k in range(8):
                    def f(nm=nm, dsrc=dsrc, k=k):
                        st, sk = wst[stc[0] % 2]
                        stc[0] += 1
                        P.dma(st[:, 0:HF], dsrc[ex, k * 128:(k + 1) * 128, hf * HF:(hf + 1) * HF], [], [sk])
                        P.op(C.ew(), lambda en, st=st: en.tensor_copy(out=wb[nm][:, k, :], in_=st[:, 0:HF]),
                             [sk], [tag + nm])
                    steps.append(f)
            for f4 in range(4):
                def f(f4=f4):
                    st, sk = wst[stc[0] % 2]
                    stc[0] += 1
                    r0 = (hf * 4 + f4) * 128
                    P.dma(st[:, 0:1024], d_wd[ex, r0:r0 + 128, :], [], [sk])
                    P.op(C.ew(), lambda en, st=st: en.tensor_copy(out=wb['d'][:, f4, :], in_=st[:, 0:1024]),
                         [sk], [tag + 'd'])
                steps.append(f)
            return steps
        for f in load_steps(0):
            f()
        first_acc = True
        for ui, (ex, hf) in enumerate(units):
            wb = wbuf[ui % 2]
            tag = 'w%d' % (ui % 2)
            nxt = load_steps(ui + 1) if ui + 1 < len(units) else []
            per = (len(nxt) + len(lblocks) - 1) // len(lblocks)
            for bi, (q0, n, w) in enumerate(lblocks):
                gi = ui * len(lblocks) + bi
                if gi == 0:
                    emit_gm(0)
                gmb, gmk = gmbs[gi % 2], 'gmb%d' % (gi % 2)
                for f in range(4):
                    pg, pu = f % 2, 2 + f % 2
                    for k in range(8):
                        P.mm(ps[pg][:, 0:n], wb['g'][:, k, f * 128:(f + 1) * 128], h2T[:, k, q0:q0 + n], k == 0, k == 7,
                             [tag + 'g', 'h2T'], ['ps%d' % pg])
                    for k in range(8):
                        P.mm(ps[pu][:, 0:n], wb['u'][:, k, f * 128:(f + 1) * 128], h2T[:, k, q0:q0 + n], k == 0, k == 7,
                             [tag + 'u', 'h2T'], ['ps%d' % pu])
                    s_, t_ = sg[f % 2], tt_[f % 2]
                    P.act(s_[:, 0:n], ps[pg][:, 0:n], AF.Silu, ['ps%d' % pg], ['sg%d' % (f % 2)])
                    P.op('pool', lambda e, s_=s_, t_=t_, n=n, gmb=gmb: e.tensor_tensor(out=t_[:, 0:n], in0=s_[:, 0:n],
                                                                                      in1=gmb[:, 0:n], op=ALU.mult),
                         ['sg%d' % (f % 2), gmk], ['tt%d' % (f % 2)])
                    P.op('dve', lambda e, t_=t_, n=n, f=f, pu=pu: e.tensor_tensor(
                        out=hid[:, f, 0:n], in0=t_[:, 0:n], in1=ps[pu][:, 0:n], op=ALU.mult),
                        ['tt%d' % (f % 2), 'ps%d' % pu], ['hid'])
                if gi + 1 < len(units) * len(lblocks):
                    emit_gm(gi + 1)
                for j in range(8):
                    pd = 4 + j % 2
                    for f in range(4):
                        P.mm(ps[pd][:, 0:n], wb['d'][:, f, j * 128:(j + 1) * 128], hid[:, f, 0:n], f == 0, f == 3,
                             [tag + 'd', 'hid'], ['ps%d' % pd])
                    if ui == 0:
                        P.op('dve', lambda e, j=j, pd=pd, n=n, q0=q0: e.tensor_copy(out=acc[:, j, q0:q0 + n],
                                                                                   in_=ps[pd][:, 0:n]),
                             ['ps%d' % pd], ['acc'])
                    else:
                        P.op('dve', lambda e, j=j, pd=pd, n=n, q0=q0: e.tensor_tensor(
                            out=acc[:, j, q0:q0 + n], in0=acc[:, j, q0:q0 + n], in1=ps[pd][:, 0:n], op=ALU.add),
                            ['ps%d' % pd, 'acc'], ['acc'])
                for f in nxt[bi * per:(bi + 1) * per]:
                    f()
        P.fence()
        A.release(mL)

        xr = [A.f32(8, 512)] * 2
        x2 = [A.f32(8, 512)] * 2
        on = A.f32(8, 512)
        sq2 = A.bf(8, 512)
        rstd2 = A.f32(512)
        tmp2 = [A.f32(512), A.f32(512)]
        for bi, (q0, n, w) in enumerate(lblocks):
            x, xk = xr[0], 'xr'
            y, yk = x2[0], 'x2'
            P.dma(x[:, :, 0:n], d_x1[:, :, q0:q0 + n], [], [xk])
            for j in range(8):
                P.op('dve', lambda e, j=j, n=n, w=w, q0=q0, x=x, y=y: e.scalar_tensor_tensor(
                    out=y[:, j, 0:n], in0=acc[:, j, q0:q0 + n], scalar=sv[:, 2, j:j + 1, w], in1=x[:, j, 0:n],
                    op0=ALU.mult, op1=ALU.add), [xk, 'acc', 'svt'], [yk])
            P.dma(o_x2[:, :, q0:q0 + n], y[:, :, 0:n], [yk], ['o_x2'])
            C.sumsq_rstd(y[:, :, 0:n], 8, n, D, rstd2, ones, sq2, [yk], 'fn')
            for k in range(8):
                e_ = C.ew()
                t = tmp2[k % 2]
                tk = 'fn_tmp%d' % (k % 2)
                P.op(e_, lambda e, k=k, t=t, n=n, y=y: e.tensor_tensor(out=t[:, 0:n], in0=y[:, k, 0:n], in1=rstd2[:, 0:n],
                                                                      op=ALU.mult), [yk, 'fn_rstd'], [tk])
                P.op(e_, lambda e, k=k, t=t, n=n: e.tensor_scalar(out=on[:, k, 0:n], in0=t[:, 0:n],
                                                                  scalar1=fng[:, k:k + 1], scalar2=None, op0=ALU.mult),
                     [tk, 'vecs'], ['on'])
            P.dma(o_on[:, :, q0:q0 + n], on[:, :, 0:n], ['on'], ['o_on'])
        P.finalize()
        return nc, P, A


def build_C2():
    nc = bass.Bass("TRN2", target_bir_lowering=False)

    def din(name, shape, dt=F32):
        return nc.dram_tensor(name, list(shape), dt, kind="ExternalInput").ap()
    d_x1 = din("x1T", [128, 8, NL])
    d_pA = din("probsA", [128, NLOC])
    d_pC = din("probsC", [16, CTX])
    d_gm = din("gmT", [16, NL])
    d_cvec = din("cvec", [128, 8, 2])
    d_wmod = din("wmod", [128, 8, 3072])
    d_bmod = din("bmod", [128, 24])
    d_vecs = din("vecs", [128, 16])
    d_G = din("G", [128, 128])
    d_oh = din("oh16", [16, 16])
    d_ident = din("ident", [128, 128])
    d_iotaf = din("iotaf", [128, 512])
    d_iotap = din("iotap", [128, 4])
    d_wg = din("wg", [NEXP, D, D])
    d_wu = din("wu", [NEXP, D, D])
    d_wd = din("wd", [NEXP, D, D])
    o_x2 = nc.dram_tensor("x2T", [128, 8, NL], F32, kind="ExternalOutput").ap()
    o_on = nc.dram_tensor("outN", [128, 8, NL], F32, kind="ExternalOutput").ap()

    AW = 51 * 1024
    with ExitStack() as es:
        arena_t = es.enter_context(nc.sbuf_tensor("arena", [128, AW], F32))
        ps = [es.enter_context(nc.psum_tensor("ps%d" % i, [128, 512], F32)) for i in range(7)]
        psb = es.enter_context(nc.psum_tensor("psb", [128, 1024], BF16))
        P = Prog(nc)
        A = Arena(arena_t, AW)
        C = Ctx(nc, P, A, ps, psb)

        vecs = A.f32(16)
        modT = A.f32(24, 2)
        cs = A.f32(64)
        sv = A.f32(3, 8, 2)
        G = A.f32(128)
        oh = A.f32(16)
        ones16 = A.f32(128)
        ones = A.bf(128)
        identb = A.bf(128)
        iotaf = A.f32(512)
        iotap = A.f32(4)
        ts = A.f32(16)
        m0 = A.mark()
        P.dma(vecs, d_vecs, [], ['vecs'])
        P.dma(G, d_G, [], ['consts'])
        P.dma(oh[0:16, :], d_oh, [], ['consts'])
        P.dma(iotaf, d_iotaf, [], ['consts'])
        P.dma(iotap, d_iotap, [], ['consts'])
        P.op('dve', lambda e: e.memset(ones, 1.0), [], ['consts'])
        P.op('dve', lambda e: e.memset(ones16, 1.0), [], ['consts'])
        stage8 = A.f32(8, 1024)
        stage8b = A.f32(8, 1024)
        C.compute_mods(d_cvec, d_wmod, d_bmod, 3, modT, cs, [stage8, stage8b], oh[0:2, 0:2])
        P.dma(stage8[:, 0, 0:128], d_ident, [], ['wm_st0a'])
        P.op('dve', lambda e: e.tensor_copy(out=identb, in_=stage8[:, 0, 0:128]), ['wm_st0a'], ['consts'])
        n2g, fng = vecs[:, 0:8], vecs[:, 8:16]
        mv = modT.rearrange("p (b k) w -> p b k w", b=3)
        for w in range(2):
            P.op('dve', lambda e, w=w: e.scalar_tensor_tensor(
                out=sv[:, 0, :, w], in0=mv[:, 1, :, w], scalar=1.0, in1=n2g, op0=ALU.add, op1=ALU.mult),
                ['modv', 'vecs'], ['svt'])
            P.op('dve', lambda e, w=w: e.tensor_copy(out=sv[:, 1, :, w], in_=mv[:, 0, :, w]), ['modv'], ['svt'])
            P.op('dve', lambda e, w=w: e.tensor_copy(out=sv[:, 2, :, w], in_=mv[:, 2, :, w]), ['modv'], ['svt'])
        P.fence()
        A.release(m0)

        NTT = NL // 128
        h2tok = A.bf(NTT, 1024)
        acc = A.f32(8, NL)
        gmT = A.f32(NL)
        ssel = A.f32(NL)
        slotT = A.f32(NTT, 16)
        mL = A.mark()
        lblocks = [(0, CTX, 1)] + [(CTX + 512 * i, 512, 0) for i in range(NLOC // 512)]

        xs = [A.f32(8, 512), A.f32(8, 512)]
        sq = A.bf(8, 512)
        rstd = A.f32(512)
        tmpf = [A.f32(512), A.f32(512)]
        h2b = [A.bf(8, 512), A.bf(8, 512)]
        for bi, (q0, n, w) in enumerate(lblocks):
            x = xs[bi % 2]
            xk = 'xs%d' % (bi % 2)
            P.dma(x[:, :, 0:n], d_x1[:, :, q0:q0 + n], [], [xk])
            C.sumsq_rstd(x[:, :, 0:n], 8, n, D, rstd, ones, sq, [xk], 'n2')
            hb, hk = h2b[bi % 2], 'h2b%d' % (bi % 2)
            C.norm_mod(x, n, rstd, sv[:, 0, :, w], sv[:, 1, :, w], hb, tmpf, [xk], [hk], 'n2')
            for tt in range(n // 128):
                for k in range(8):
                    P.tr(psb[:, k * 128:(k + 1) * 128], hb[:, k, tt * 128:(tt + 1) * 128], identb, [hk, 'consts'], ['psb'])
                P.act(h2tok[:, q0 // 128 + tt, :], psb[:, 0:1024], AF.Copy, ['psb'], ['h2tok'])
        P.fence()
        A.release(mL)

        pA = A.f32(NLOC)
        mk = A.f32(NLOC)
        pC = A.f32(CTX)
        mkc = A.f32(CTX)
        P.dma(pA, d_pA, [], ['pA'])
        P.dma(pC[0:16, :], d_pC, [], ['pC'])
        P.dma(gmT[0:16, :], d_gm, [], ['gmT'])
        lo, hi, mid, tot, gt, dd, Kv = (ts[:, 0:2], ts[:, 2:4], ts[:, 4:6], ts[:, 6:8], ts[:, 8:10], ts[:, 10:12],
                                        ts[:, 12:14])
        P.op('dve', lambda e: e.memset(ts, 0.0), [], ['ts'])
        P.op('dve', lambda e: e.memset(hi, 1.0), ['ts'], ['ts'])
        P.op('dve', lambda e: e.memset(mid, 0.5), ['ts'], ['ts'])
        P.op('dve', lambda e: e.memset(ts[:, 12:13], NLOC - 0.5), ['ts'], ['ts'])
        P.op('dve', lambda e: e.memset(ts[:, 13:14], 2 * CTX // NEXP - 0.5), ['ts'], ['ts'])
        for it in range(NBIS):
            P.op('dve', lambda e: e.tensor_scalar(out=mk, in0=pA, scalar1=ts[:, 4:5], scalar2=None, op0=ALU.is_gt),
                 ['pA', 'ts'], ['mk'])
            P.op('dve', lambda e: e.reduce_sum(out=ts[:, 14:15], in_=mk, axis=mybir.AxisListType.X), ['mk'], ['cc'])
            P.op('dve', lambda e: e.tensor_scalar(out=mkc[0:16, :], in0=pC[0:16, :], scalar1=ts[0:16, 5:6], scalar2=None,
                                                  op0=ALU.is_gt), ['pC', 'ts'], ['mkc'])
            P.op('dve', lambda e: e.reduce_sum(out=ts[0:16, 7:8], in_=mkc[0:16, :], axis=mybir.AxisListType.X),
                 ['mkc', 'ts'], ['ts'])
            P.mm(ps[6][:, 0:1], G, ts[:, 14:15], True, True, ['cc', 'consts'], ['ps6'])
            P.op('dve', lambda e: e.tensor_copy(out=ts[:, 6:7], in_=ps[6][:, 0:1]), ['ps6', 'ts'], ['ts'])
            P.op('dve', lambda e: e.tensor_tensor(out=gt, in0=tot, in1=Kv, op=ALU.is_gt), ['ts'], ['ts'])
            P.op('dve', lambda e: e.tensor_tensor(out=dd, in0=mid, in1=lo, op=ALU.subtract), ['ts'], ['ts'])
            P.op('dve', lambda e: e.tensor_tensor(out=dd, in0=dd, in1=gt, op=ALU.mult), ['ts'], ['ts'])
            P.op('dve', lambda e: e.tensor_tensor(out=lo, in0=lo, in1=dd, op=ALU.add), ['ts'], ['ts'])
            P.op('dve', lambda e: e.tensor_tensor(out=dd, in0=hi, in1=mid, op=ALU.subtract), ['ts'], ['ts'])
            P.op('dve', lambda e: e.tensor_tensor(out=dd, in0=dd, in1=gt, op=ALU.mult), ['ts'], ['ts'])
            P.op('dve', lambda e: e.tensor_tensor(out=hi, in0=mid, in1=dd, op=ALU.add), ['ts'], ['ts'])
            P.op('dve', lambda e: e.tensor_tensor(out=dd, in0=lo, in1=hi, op=ALU.add), ['ts'], ['ts'])
            P.op('dve', lambda e: e.tensor_single_scalar(out=mid, in_=dd, scalar=0.5, op=ALU.mult), ['ts'], ['ts'])
        P.op('dve', lambda e: e.tensor_scalar(out=mk[0:16, 0:NLOC], in0=gmT[0:16, CTX:NL], scalar1=ts[0:16, 2:3],
                                              scalar2=None, op0=ALU.is_gt), ['gmT', 'ts'], ['mk'])
        P.op('dve', lambda e: e.tensor_tensor(out=gmT[0:16, CTX:NL], in0=gmT[0:16, CTX:NL], in1=mk[0:16, 0:NLOC],
                                              op=ALU.mult), ['mk', 'gmT'], ['gmT'])
        P.op('dve', lambda e: e.tensor_scalar(out=mkc[0:16, :], in0=gmT[0:16, 0:CTX], scalar1=ts[0:16, 3:4],
                                              scalar2=None, op0=ALU.is_gt), ['gmT', 'ts'], ['mkc'])
        P.op('dve', lambda e: e.tensor_tensor(out=gmT[0:16, 0:CTX], in0=gmT[0:16, 0:CTX], in1=mkc[0:16, :],
                                              op=ALU.mult), ['mkc', 'gmT'], ['gmT'])
        mrow = A.f32(NL)
        orow = A.f32(NL)
        P.op('dve', lambda e: e.tensor_copy(out=mrow[0:16, CTX:NL], in_=mk[0:16, 0:NLOC]), ['mk'], ['mrow'])
        P.op('dve', lambda e: e.tensor_copy(out=mrow[0:16, 0:CTX], in_=mkc[0:16, :]), ['mkc'], ['mrow'])
        P.op('dve', lambda e: e.memset(orow[0:16, :], 1.0), [], ['orow'])
        P.op('dve', lambda e: e.tensor_tensor_scan(out=ssel[0:16, :], data0=orow[0:16, :], data1=mrow[0:16, :],
                                                   initial=0.0, op0=ALU.mult, op1=ALU.add), ['orow', 'mrow'], ['ssel'])
        P.op('dve', lambda e: e.tensor_tensor(out=ssel[0:16, :], in0=ssel[0:16, :], in1=mrow[0:16, :], op=ALU.mult),
             ['ssel', 'mrow'], ['ssel'])
        for tt in range(NTT):
            P.tr(ps[6][:, 0:16], ssel[0:16, tt * 128:(tt + 1) * 128], oh[0:16, :], ['ssel', 'consts'], ['ps6'])
            P.op('dve', lambda e, tt=tt: e.tensor_copy(out=slotT[:, tt, :], in_=ps[6][:, 0:16]), ['ps6'], ['slotT'])
        P.fence()
        A.release(mL)

        CS = 512
        QF = 256
        NRING = 3
        wgu = [dict(g=A.bf(8, QF), u=A.bf(8, QF)) for _ in range(NRING)]
        wd = A.bf(8, 1024)
        SS = [A.bf(CS) for _ in range(4)]
        SS2 = [A.bf(CS) for _ in range(4)]
        xsT = A.bf(8, CS)
        ye = xsT.rearrange("p (a b) c -> p a (b c)", a=4)
        hid = A.bf(8, CS)
        sg = [A.bf(CS), A.bf(CS)]
        gmb = A.f32(512)
        gme = A.f32(512)
        sse = A.f32(512)

        def load_gu(ex, q, slot):
            wb, tag = wgu[slot], 'wgu%d' % slot
            for nm, dsrc in (('g', d_wg), ('u', d_wu)):
                P.dma(wb[nm], dsrc[ex].rearrange("(k p) f -> p k f", p=128)[:, :, q * QF:(q + 1) * QF], [], [tag],
                      eng='pool')

        def load_d(ex):
            for hh in range(2):
                P.dma(wd[:, :, hh * 512:(hh + 1) * 512],
                      d_wd[ex].rearrange("(k p) f -> p k f", p=128)[:, :, hh * 512:(hh + 1) * 512], [], ['wd'], eng='pool')
        usl = [0]
        for q0_ in range(NRING):
            load_gu(0, q0_, q0_)
        for ex in range(NEXP):
            si = 0
            for dh in range(2):
                for tt in range(NTT):
                    S_, Sk = SS[si % 4], 'SS%d' % (si % 4)
                    si += 1
                    P.op('dve', lambda e, S_=S_, tt=tt, ex=ex: e.tensor_scalar(
                        out=S_, in0=iotaf, scalar1=slotT[:, tt, ex:ex + 1], scalar2=None, op0=ALU.is_equal),
                        ['consts', 'slotT'], [Sk])
                    for i in range(4):
                        dci = dh * 4 + i
                        P.mm(ps[i][:, 0:CS], h2tok[:, tt, dci * 128:(dci + 1) * 128], S_, tt == 0, tt == NTT - 1,
                             ['h2tok', Sk], ['ps%d' % i])
                for i in range(4):
                    P.act(xsT[:, dh * 4 + i, :], ps[i][:, 0:CS], AF.Copy, ['ps%d' % i], ['xsye'])
            if ex == 0:
                load_d(0)
            for q in range(4):
                slot = (ex * 4 + q) % NRING
                wb, tag = wgu[slot], 'wgu%d' % slot
                for f2 in range(2):
                    f = q * 2 + f2
                    pg, pu = f % 2, 2 + f % 2
                    for k in range(8):
                        P.mm(ps[pg][:, 0:CS], wb['g'][:, k, f2 * 128:(f2 + 1) * 128], xsT[:, k, :], k == 0, k == 7,
                             [tag, 'xsye'], ['ps%d' % pg])
                    for k in range(8):
                        P.mm(ps[pu][:, 0:CS], wb['u'][:, k, f2 * 128:(f2 + 1) * 128], xsT[:, k, :], k == 0, k == 7,
                             [tag, 'xsye'], ['ps%d' % pu])
                    s_ = sg[f % 2]
                    P.act(s_, ps[pg][:, 0:CS], AF.Silu, ['ps%d' % pg], ['sg%d' % (f % 2)])
                    P.op('dve', lambda e, s_=s_, f=f, pu=pu: e.tensor_tensor(out=hid[:, f, :], in0=s_, in1=ps[pu][:, 0:CS],
                                                                          op=ALU.mult),
                         ['sg%d' % (f % 2), 'ps%d' % pu], ['hid'])
                nq = ex * 4 + q + NRING
                if nq < NEXP * 4:
                    load_gu(nq // 4, nq % 4, slot)
            def prep_scatter(bi_, ex=ex):
                q0_, n_, _ = lblocks[bi_]
                gi_ = ex * len(lblocks) + bi_
                par = gi_ % 2
                tiles_ = SS if par == 0 else SS2
                P.op('dve', lambda e: e.tensor_scalar(out=sse[0:16, 0:n_], in0=ssel[0:16, q0_:q0_ + n_],
                                                      scalar1=oh[0:16, ex:ex + 1], scalar2=None, op0=ALU.mult),
                     ['ssel', 'consts'], ['sse'])
                P.op('dve', lambda e: e.tensor_scalar(out=gme[0:16, 0:n_], in0=gmT[0:16, q0_:q0_ + n_],
                                                      scalar1=oh[0:16, ex:ex + 1], scalar2=None, op0=ALU.mult),
                     ['gmT', 'consts'], ['gme'])
                P.mm(ps[5][:, 0:n_], ones16[0:16, :], sse[0:16, 0:n_], True, True, ['sse', 'consts'], ['ps5'])
                P.mm(ps[6][:, 0:n_], ones16[0:16, :], gme[0:16, 0:n_], True, True, ['gme', 'consts'], ['ps6'])
                P.act(gmb[:, 0:n_], ps[6][:, 0:n_], AF.Copy, ['ps6'], ['gmb'])
                for st_ in range(4):
                    P.op('dve', lambda e, st_=st_: e.scalar_tensor_tensor(
                        out=tiles_[st_][:, 0:n_], in0=ps[5][:, 0:n_], scalar=iotap[:, st_:st_ + 1], in1=gmb[:, 0:n_],
                        op0=ALU.is_equal, op1=ALU.mult), ['ps5', 'gmb', 'consts'],
                        ['SS%d' % st_ if par == 0 else 'SSb%d' % st_])
            prep_scatter(0)
            for st_ in range(4):
                for hh in range(2):
                    for f in range(8):
                        P.mm(ps[4][:, 0:512], hid[:, f, st_ * 128:(st_ + 1) * 128], wd[:, f, hh * 512:(hh + 1) * 512],
                             f == 0, f == 7, ['hid', 'wd'], ['ps4'])
                    P.act(ye[:, st_, hh * 512:(hh + 1) * 512], ps[4][:, 0:512], AF.Copy, ['ps4'], ['xsye'])
            if ex + 1 < NEXP:
                load_d(ex + 1)
            for bi_, (q0, n, w) in enumerate(lblocks):
                gi_ = ex * len(lblocks) + bi_
                par = gi_ % 2
                tiles_ = SS if par == 0 else SS2
                if bi_ + 1 < len(lblocks):
                    prep_scatter(bi_ + 1)
                for j in range(8):
                    pi = j % 4
                    for st_ in range(4):
                        P.mm(ps[pi][:, 0:n], ye[:, st_, j * 128:(j + 1) * 128], tiles_[st_][:, 0:n], st_ == 0, st_ == 3,
                             ['xsye', 'SS%d' % st_ if par == 0 else 'SSb%d' % st_], ['ps%d' % pi])
                    if ex == 0:
                        P.op('dve', lambda e, j=j, pi=pi, n=n, q0=q0: e.tensor_copy(out=acc[:, j, q0:q0 + n],
                                                                                   in_=ps[pi][:, 0:n]),
                             ['ps%d' % pi], ['acc'])
                    else:
                        P.op('dve', lambda e, j=j, pi=pi, n=n, q0=q0: e.tensor_tensor(
                            out=acc[:, j, q0:q0 + n], in0=acc[:, j, q0:q0 + n], in1=ps[pi][:, 0:n], op=ALU.add),
                            ['ps%d' % pi, 'acc'], ['acc'])
        P.fence()
        A.release(mL)

        xr = [A.f32(8, 512)] * 2
        x2 = [A.f32(8, 512)] * 2
        on = A.f32(8, 512)
        sq2 = A.bf(8, 512)
        rstd2 = A.f32(512)
        tmp2 = [A.f32(512), A.f32(512)]
        for bi, (q0, n, w) in enumerate(lblocks):
            x, xk = xr[0], 'xr'
            y, yk = x2[0], 'x2'
            P.dma(x[:, :, 0:n], d_x1[:, :, q0:q0 + n], [], [xk])
            for j in range(8):
                P.op('dve', lambda e, j=j, n=n, w=w, q0=q0, x=x, y=y: e.scalar_tensor_tensor(
                    out=y[:, j, 0:n], in0=acc[:, j, q0:q0 + n], scalar=sv[:, 2, j:j + 1, w], in1=x[:, j, 0:n],
                    op0=ALU.mult, op1=ALU.add), [xk, 'acc', 'svt'], [yk])
            P.dma(o_x2[:, :, q0:q0 + n], y[:, :, 0:n], [yk], ['o_x2'])
            C.sumsq_rstd(y[:, :, 0:n], 8, n, D, rstd2, ones, sq2, [yk], 'fn')
            for k in range(8):
                e_ = C.ew()
                t = tmp2[k % 2]
                tk = 'fn_tmp%d' % (k % 2)
                P.op(e_, lambda e, k=k, t=t, n=n, y=y: e.tensor_tensor(out=t[:, 0:n], in0=y[:, k, 0:n], in1=rstd2[:, 0:n],
                                                                      op=ALU.mult), [yk, 'fn_rstd'], [tk])
                P.op(e_, lambda e, k=k, t=t, n=n: e.tensor_scalar(out=on[:, k, 0:n], in0=t[:, 0:n],
                                                                  scalar1=fng[:, k:k + 1], scalar2=None, op0=ALU.mult),
                     [tk, 'vecs'], ['on'])
            P.dma(o_on[:, :, q0:q0 + n], on[:, :, 0:n], ['on'], ['o_on'])
        P.finalize()
        return nc, P, A


def prep_C(inp, l, resAB):
    G = (np.arange(128)[:, None] % 16 == np.arange(128)[None, :] % 16).astype(np.float32)
    cvec = np.ascontiguousarray(np.stack([_fmv(inp['c'][0]), _fmv(inp['c_ctx'])], axis=-1))
    vecs = np.zeros((128, 16), np.float32)
    vecs[:, 0:8] = _fmv(inp['norm2_g'][l])
    vecs[:, 8:16] = _fmv(inp['final_norm_g'])
    pA = np.ascontiguousarray(np.stack([r['probs'][CTX:].T for r in resAB], axis=0).reshape(128, NLOC))
    pC = np.ascontiguousarray(resAB[0]['probs'][:CTX].T)
    iotaf = np.ascontiguousarray(np.broadcast_to(np.arange(1, 513, dtype=np.float32)[None, :], (128, 512)))
    iotap = (np.arange(128, dtype=np.float32)[:, None] + 1 + 128 * np.arange(4, dtype=np.float32)[None, :]).astype(np.float32)
    common = dict(ident=np.eye(128, dtype=np.float32), iotaf=iotaf, iotap=iotap, cvec=cvec, wmod=_fm(np.ascontiguousarray(inp['w_mod'][l][:, 3072:6144])),
                  bmod=_fmv(inp['b_mod'][l][3072:6144]), vecs=vecs, G=G, oh16=np.eye(16, dtype=np.float32),
                  probsA=pA, probsC=pC, wg=inp['w_gate'][l], wu=inp['w_up'][l], wd=inp['w_down'][l])
    maps = []
    for c in range(NCORES):
        m = dict(common)
        m['x1T'] = resAB[c]['x1T']
        m['gmT'] = np.ascontiguousarray(resAB[c]['probs'].T)
        maps.append(m)
    return maps


def kernel(**inp):
    inp = {k: np.asarray(v) for k, v in inp.items()}
    ncAB = build_AB()[0]
    ncC = build_C2()[0]
    xl, xc = inp['x'][0], inp['ctx'][0]
    out = None
    for l in range(2):
        resAB = run_bass_kernel_spmd(ncAB, prep_AB(inp, l, xl, xc), core_ids=list(range(NCORES))).results
        resC = run_bass_kernel_spmd(ncC, prep_C(inp, l, resAB), core_ids=list(range(NCORES))).results
        xl = np.concatenate([_unfm(r['x2T'][:, :, CTX:]) for r in resC], axis=0)
        xc = _unfm(resC[0]['x2T'][:, :, :CTX])
        out = np.concatenate([_unfm(r['outN'][:, :, CTX:]) for r in resC], axis=0)
    return np.ascontiguousarray(out[None].astype(np.float32))
```

```python
import numpy as np
import ml_dtypes
from contextlib import ExitStack
import concourse.bass as bass
import concourse.mybir as mybir
from concourse.bass_utils import run_bass_kernel_spmd

F32 = mybir.dt.float32
BF16 = mybir.dt.bfloat16
AF = mybir.ActivationFunctionType
ALU = mybir.AluOpType

NCORES = 8
D = 1024
SEQ = 16384
CTX = 256
TALL = SEQ + CTX
NLOC = SEQ // NCORES
NL = NLOC + CTX
EPS = 1e-6
NEXP = 16
ATTN_SCALE = 192.0 ** -0.5
ENGS = ['pe', 'act', 'dve', 'pool', 'sp']


def _prod(s):
    r = 1
    for v in s:
        r *= v
    return r


class Prog:
    def __init__(self, nc, ndma=12):
        self.nc = nc
        self.ops = []
        self.lastw = {}
        self.readers = {}
        self.ndma = ndma

    capture = None

    def begin_capture(self):
        self.capture = []

    def end_capture(self):
        c, self.capture = self.capture, None
        return c

    def replay_interleaved(self, lists, nway=2):
        L = max(len(l) for l in lists)
        stride = (L + nway - 1) // nway
        items = []
        for i, l in enumerate(lists):
            for k, call in enumerate(l):
                items.append((i * stride + k, i, call))
        items.sort(key=lambda t: (t[0], t[1]))
        for _, _, call in items:
            self.op(*call)

    def op(self, eng, fn, reads=(), writes=(), dma=False):
        if self.capture is not None:
            self.capture.append((eng, fn, tuple(reads), tuple(writes), dma))
            return -1
        i = len(self.ops)
        deps = set()
        for k in reads:
            if k in self.lastw:
                deps.add(self.lastw[k])
        for k in writes:
            if k in self.lastw:
                deps.add(self.lastw[k])
            deps.update(self.readers.get(k, ()))
        for k in reads:
            self.readers.setdefault(k, []).append(i)
        for k in writes:
            self.lastw[k] = i
            self.readers[k] = []
        self.ops.append(dict(eng=eng, fn=fn, deps=deps, dma=dma))
        return i

    def fence(self):
        self.ops.append(dict(eng=None, fence=True))
        self.lastw = {}
        self.readers = {}

    def mm(self, out, lhsT, rhs, start, stop, reads, writes, tp=None):
        if tp is None:
            self.op('pe', lambda e: e.matmul(out, lhsT, rhs, start=start, stop=stop), reads, writes)
        else:
            self.op('pe', lambda e: e.matmul(out, lhsT, rhs, start=start, stop=stop, tile_position=tp), reads, writes)

    def tr(self, out, in_, ident, reads, writes):
        self.op('pe', lambda e: e.transpose(out, in_, ident), reads, writes)

    def act(self, out, in_, func, reads, writes, bias=None, scale=None, accum=None):
        kw = {}
        if bias is not None:
            kw['bias'] = bias
        if scale is not None:
            kw['scale'] = scale
        if accum is not None:
            kw['accum_out'] = accum
        self.op('act', lambda e: e.activation(out=out, in_=in_, func=func, **kw), reads, writes)

    def dma(self, out, in_, reads, writes, eng='sp'):
        self.op(eng, lambda e: e.dma_start(out=out, in_=in_), reads, writes, dma=True)

    def finalize(self):
        self.fence()
        nc, ops, ndma = self.nc, self.ops, self.ndma
        need = set()
        last = {}
        for i, o in enumerate(ops):
            if o.get('fence'):
                for e, j in last.items():
                    need.add(j)
                continue
            for d in o['deps']:
                Dd = ops[d]
                if Dd['dma']:
                    continue
                if Dd['eng'] == 'pe' and o['eng'] == 'pe' and not o['dma']:
                    continue
                need.add(d)
            if not o['dma']:
                last[o['eng']] = i
        cnt = {e: 0 for e in ENGS}
        dcnt = [0] * ndma
        di = 0
        for i, o in enumerate(ops):
            if o.get('fence'):
                o['snap_cnt'] = dict(cnt)
                o['snap_d'] = list(dcnt)
                continue
            if o['dma']:
                j = di % ndma
                di += 1
                o['dsem'] = j
                o['dprev'] = dcnt[j]
                dcnt[j] += 16
                o['dval'] = dcnt[j]
            elif i in need:
                cnt[o['eng']] += 1
                o['sig'] = cnt[o['eng']]
        self.n_ops = len(ops)
        with ExitStack() as es:
            esem = {e: es.enter_context(nc.semaphore("s_" + e)) for e in ENGS}
            dsem = [es.enter_context(nc.semaphore("d_%d" % j)) for j in range(ndma)]
            block = es.enter_context(nc.Block())

            def emit(ename):
                def body(eng):
                    waited = {}

                    def wait(key, sem, val):
                        if val > waited.get(key, 0):
                            eng.wait_ge(sem, val)
                            waited[key] = val
                    for i, o in enumerate(ops):
                        if o.get('fence'):
                            for e2 in ENGS:
                                if o['snap_cnt'][e2] > 0:
                                    wait(e2, esem[e2], o['snap_cnt'][e2])
                            for j in range(ndma):
                                if o['snap_d'][j] > 0:
                                    wait(('d', j), dsem[j], o['snap_d'][j])
                            continue
                        if o['eng'] != ename:
                            continue
                        for d in sorted(o['deps']):
                            Dd = ops[d]
                            if Dd['dma']:
                                wait(('d', Dd['dsem']), dsem[Dd['dsem']], Dd['dval'])
                            else:
                                if Dd['eng'] == 'pe' and ename == 'pe' and not o['dma']:
                                    continue
                                wait(Dd['eng'], esem[Dd['eng']], Dd['sig'])
                        if o['dma'] and o['dprev'] > 0:
                            wait(('d', o['dsem']), dsem[o['dsem']], o['dprev'])
                        ins = o['fn'](eng)
                        if o['dma']:
                            ins.then_inc(dsem[o['dsem']], 16)
                        elif 'sig' in o:
                            ins.then_inc(esem[ename], 1)
                return body
            block.tensor(emit('pe'))
            block.scalar(emit('act'))
            block.vector(emit('dve'))
            block.gpsimd(emit('pool'))
            block.sync(emit('sp'))


class Arena:
    def __init__(self, t, width):
        self.t = t
        self.top = 0
        self.width = width
        self.peak = 0

    def _shape(self, v, shape):
        if len(shape) == 1:
            return v
        if len(shape) == 2:
            return v.rearrange("p (a b) -> p a b", a=shape[0])
        if len(shape) == 3:
            return v.rearrange("p (a b c) -> p a b c", a=shape[0], b=shape[1])
        raise ValueError

    def f32(self, *shape):
        n = _prod(shape)
        a = self.top
        self.top += n
        self.peak = max(self.peak, self.top)
        assert self.top <= self.width, ("arena overflow", self.top, self.width)
        return self._shape(self.t[:, a:a + n], shape)

    def bf(self, *shape):
        n = _prod(shape)
        nw = (n + 1) // 2
        a = self.top
        self.top += nw
        self.peak = max(self.peak, self.top)
        assert self.top <= self.width, ("arena overflow", self.top, self.width)
        v = self.t[:, a:a + nw].bitcast(BF16)
        return self._shape(v[:, 0:n], shape)

    def mark(self):
        return self.top

    def release(self, m):
        self.top = m


class Ctx:
    def __init__(self, nc, P, A, ps, psb):
        self.nc, self.P, self.A, self.ps, self.psb = nc, P, A, ps, psb
        self.psi = 0
        self.alt = 0
        self.uid = 0

    def key(self, s):
        self.uid += 1
        return "%s#%d" % (s, self.uid)

    ps_range = (0, 6)

    def next_ps(self, lo=None, hi=None):
        if lo is None:
            lo, hi = self.ps_range
        i = lo + self.psi % (hi - lo)
        self.psi += 1
        return i

    def ew(self):
        self.alt += 1
        return 'pool' if self.alt % 4 == 0 else 'dve'

    def load_cast(self, dst_bf, src_dram, ncols, nk, name, scale_col=None, stage=None):
        P = self.P
        for k in range(nk):
            st, sk = stage[k % 2]
            P.dma(st[:, 0:ncols], src_dram[:, k, :], reads=[], writes=[sk])
            e = self.ew()
            if scale_col is None:
                P.op(e, lambda en, st=st, k=k: en.tensor_copy(out=dst_bf[:, k, :], in_=st[:, 0:ncols]),
                     reads=[sk], writes=[name])
            else:
                P.op(e, lambda en, st=st, k=k: en.tensor_scalar(out=dst_bf[:, k, :], in0=st[:, 0:ncols],
                                                                scalar1=scale_col[:, k:k + 1], scalar2=None,
                                                                op0=ALU.mult),
                     reads=[sk, 'modv'], writes=[name])

    def sumsq_rstd(self, src_f32, nk, n, dim, out_rstd, ones_bf, sq_bf, rk, name):
        P = self.P
        P.act(sq_bf[:, 0:nk, 0:n], src_f32, AF.Square, reads=rk, writes=[name + '_sq'])
        pi = self.next_ps()
        pk = 'ps%d' % pi
        for k in range(nk):
            P.mm(self.ps[pi][:, 0:n], ones_bf, sq_bf[:, k, 0:n], k == 0, k == nk - 1,
                 reads=[name + '_sq', 'consts'], writes=[pk])
        P.act(out_rstd[:, 0:n], self.ps[pi][:, 0:n], AF.Ln, reads=[pk], writes=[name + '_rstd'], scale=1.0 / dim, bias=EPS)
        P.act(out_rstd[:, 0:n], out_rstd[:, 0:n], AF.Exp, reads=[name + '_rstd'], writes=[name + '_rstd'], scale=-0.5)

    def norm_mod(self, x_f32, n, rstd, s_col, sh_col, out, tmp_f32, rk, wk, name):
        P = self.P
        for k in range(8):
            e = self.ew()
            tk = "%s_tmp%d" % (name, k % 2)
            t = tmp_f32[k % 2]
            P.op(e, lambda en, k=k, t=t: en.tensor_tensor(out=t[:, 0:n], in0=x_f32[:, k, 0:n], in1=rstd[:, 0:n],
                                                          op=ALU.mult),
                 reads=rk + [name + '_rstd'], writes=[tk])
            P.op(e, lambda en, k=k, t=t: en.tensor_scalar(out=out[:, k, 0:n], in0=t[:, 0:n],
                                                          scalar1=s_col[:, k:k + 1], scalar2=sh_col[:, k:k + 1],
                                                          op0=ALU.mult, op1=ALU.add),
                 reads=[tk, 'modv'], writes=wk)

    def compute_mods(self, d_cvec, d_wmod, d_bmod, nblk, modT, cs, stage, id2):
        P = self.P
        P.dma(cs[:, 0:16], d_cvec.rearrange("p k w -> p (k w)"), reads=[], writes=['cs'])
        P.act(cs[:, 16:32], cs[:, 0:16], AF.Sigmoid, reads=['cs'], writes=['cs2'])
        P.op('dve', lambda e: e.tensor_tensor(out=cs[:, 0:16], in0=cs[:, 0:16], in1=cs[:, 16:32], op=ALU.mult),
             reads=['cs2', 'cs'], writes=['cs'])
        P.dma(cs[:, 32:32 + nblk * 8], d_bmod, reads=[], writes=['bmod'])
        csv = cs[:, 0:16].rearrange("p (k w) -> p k w", k=8)
        nj = nblk * 8
        rb = [self.A.f32(512), self.A.f32(512)]
        ri = 0
        for b in range(nblk):
            stg, sk = stage[b % 2], 'wm_st%d' % (b % 2)
            for hk in range(2):
                P.dma(stg[:, hk * 4:(hk + 1) * 4, :], d_wmod[:, hk * 4:(hk + 1) * 4, b * 1024:(b + 1) * 1024], reads=[],
                      writes=[sk + 'ab'[hk]], eng=('sp' if hk == 0 else 'pool'))
            for hf in range(2):
                for k in range(8):
                    P.mm(self.ps[5][0:2, 0:512], csv[:, k, :], stg[:, k, hf * 512:(hf + 1) * 512], k == 0, k == 7,
                         reads=[sk + 'ab'[k // 4], 'cs'], writes=['ps5'])
                r_, rk = rb[ri % 2], 'mrow%d' % (ri % 2)
                ri += 1
                P.op('dve', lambda e, r_=r_: e.tensor_copy(out=r_[0:2, :], in_=self.ps[5][0:2, 0:512]), ['ps5'], [rk])
                for j4 in range(4):
                    c0 = 2 * (b * 8 + hf * 4 + j4)
                    P.tr(self.ps[6][:, c0:c0 + 2], r_[0:2, j4 * 128:(j4 + 1) * 128], id2, [rk, 'id2', 'consts'], ['ps6'])
        psv = self.ps[6][:, 0:2 * nj].rearrange("p (j w) -> p j w", w=2)
        for w in range(2):
            P.op('dve', lambda e, w=w: e.tensor_tensor(out=modT[:, 0:nj, w], in0=psv[:, :, w],
                                                       in1=cs[:, 32:32 + nj], op=ALU.add),
                 reads=['ps6', 'bmod'], writes=['modv'])


def build_AB():
    nc = bass.Bass("TRN2", target_bir_lowering=False)

    def din(name, shape, dt=F32):
        return nc.dram_tensor(name, list(shape), dt, kind="ExternalInput").ap()
    d_xall = din("xall", [128, 8, TALL])
    d_xloc = din("xloc", [128, 8, NLOC])
    d_cvec = din("cvec", [128, 8, 2])
    d_wmod = din("wmod", [128, 8, 5120])
    d_bmod = din("bmod", [128, 40])
    d_vecs = din("vecs", [128, 64])
    d_wA = din("wA", [128, 8, 640])
    d_wB = din("wB", [128, 8, 1152])
    d_wuq = din("wuq", [128, 3, 1024])
    d_wukv = din("wukv", [128, 2, 1024])
    d_wout = din("wout", [128, 8, 1024])
    d_lruw = din("lruw", [128, 8, 128])
    d_sguw = din("sguw", [128, 4, 128])
    d_sgub = din("sgub", [128, 2, 128])
    d_wr = din("wr", [128, 8, 16])
    d_ropeC = din("ropeC", [64, SEQ])
    d_ropeS = din("ropeS", [64, SEQ])
    d_ropeCl = din("ropeCl", [64, NLOC])
    d_ropeSl = din("ropeSl", [64, NLOC])
    d_ident = din("ident", [128, 128])
    o_x1 = nc.dram_tensor("x1T", [128, 8, NL], F32, kind="ExternalOutput").ap()
    o_probs = nc.dram_tensor("probs", [NL, NEXP], F32, kind="ExternalOutput").ap()
    s_xb = nc.dram_tensor("xb_s", [2, 128, TALL], F32).ap()
    s_KT = nc.dram_tensor("KT_s", [4, 128, TALL], BF16).ap()
    s_kr = nc.dram_tensor("kr_s", [64, TALL], BF16).ap()
    s_V = nc.dram_tensor("V_s", [4, 128, TALL // 128, 128], BF16).ap()

    AW = 51 * 1024
    with ExitStack() as es:
        arena_t = es.enter_context(nc.sbuf_tensor("arena", [128, AW], F32))
        ps = [es.enter_context(nc.psum_tensor("ps%d" % i, [128, 512], F32)) for i in range(7)]
        psb = es.enter_context(nc.psum_tensor("psb", [128, 1024], BF16))
        P = Prog(nc)
        A = Arena(arena_t, AW)
        C = Ctx(nc, P, A, ps, psb)

        vecs = A.f32(64)
        modT = A.f32(40, 2)
        cs = A.f32(80)
        sv = A.f32(6, 8, 2)
        spv = A.f32(4)
        ident = A.bf(128)
        ones = A.bf(128)
        onesf = A.f32(128)
        m0 = A.mark()
        P.dma(vecs, d_vecs, [], ['vecs'])
        P.op('pool', lambda e: e.memset(onesf, 1.0), [], ['onesf'])
        stage8 = A.f32(8, 1024)
        stage8b = A.f32(8, 1024)
        id2 = A.f32(2)
        P.dma(id2[0:2, :], d_ident[0:2, 0:2], [], ['id2'])
        C.compute_mods(d_cvec, d_wmod, d_bmod, 5, modT, cs, [stage8, stage8b], id2[0:2, :])
        P.dma(stage8[:, 0, 0:128], d_ident, [], ['wm_st0a'])
        P.op('dve', lambda e: e.tensor_copy(out=ident, in_=stage8[:, 0, 0:128]), ['wm_st0a'], ['consts'])
        P.op('dve', lambda e: e.memset(ones, 1.0), [], ['consts'])
        n1g, n2g = vecs[:, 0:8], vecs[:, 8:16]
        lrub, lam, convw, convb = vecs[:, 16:24], vecs[:, 24:28], vecs[:, 28:36], vecs[:, 36:38]
        qng, kvng, cmask = vecs[:, 38:41], vecs[:, 41:43], vecs[:, 43:51]
        mv = modT.rearrange("p (b k) w -> p b k w", b=5)
        for w in range(2):
            for (dst, scb, g) in ((0, 1, n1g), (3, 4, n2g)):
                P.op('dve', lambda e, w=w, dst=dst, scb=scb, g=g: e.scalar_tensor_tensor(
                    out=sv[:, dst, :, w], in0=mv[:, scb, :, w], scalar=1.0, in1=g, op0=ALU.add, op1=ALU.mult),
                    ['modv', 'vecs'], ['svt'])
            for (dst, src) in ((1, 0), (2, 2), (4, 3)):
                P.op('dve', lambda e, w=w, dst=dst, src=src: e.tensor_copy(out=sv[:, dst, :, w], in_=mv[:, src, :, w]),
                     ['modv'], ['svt'])
        P.act(spv, lam, AF.Exp, ['vecs'], ['spv'], scale=-1.0)
        P.act(spv, spv, AF.Ln, ['spv'], ['spv'], bias=1.0)
        P.op('dve', lambda e: e.tensor_single_scalar(out=spv, in_=spv, scalar=-8.0, op=ALU.mult), ['spv'], ['spv'])
        P.fence()
        A.release(m0)

        def svc(idx, w):
            return sv[:, idx, :, w]

        m1 = A.mark()
        wA_bf = A.bf(8, 640)
        wukv_bf = A.bf(2, 1024)
        wst = [(A.f32(1024), 'wst0'), (A.f32(1024), 'wst1')]

        def p1set():
            return dict(xs=A.f32(8, 512), sq=A.bf(8, 512), rstd=A.f32(512), tmpf=[A.f32(512), A.f32(512)],
                        h=A.bf(8, 512), xo=A.f32(2, 512), ckv=A.f32(2, 512), ckv_sq=A.bf(2, 512), rstd2=A.f32(512),
                        ckvn=A.bf(2, 512), Ko=A.bf(4, 512), Vo=A.bf(4, 4, 128), krf=A.f32(2, 512), rC=A.f32(512),
                        rS=A.f32(512), ko=A.bf(512))
        B1 = [p1set(), p1set()]
        X3 = [B1[0]['xs'], B1[1]['xs'], A.f32(8, 512)]
        C.load_cast(wukv_bf, d_wukv, 1024, 2, 'wukv', stage=wst)
        C.load_cast(wA_bf, d_wA, 640, 8, 'wA', scale_col=None, stage=wst)
        blocks = [(0, CTX, 1)] + [(CTX + 512 * i, 512, 0) for i in range(SEQ // 512)]
        caps = []
        for bi, (t0, n, w) in enumerate(blocks):
            B = B1[bi % 2]
            sx = str(bi % 2)
            C.ps_range = (0, 3) if bi % 2 == 0 else (3, 6)
            if bi == 0:
                for b2 in range(2):
                    t02, n2, _ = blocks[b2]
                    P.dma(X3[b2][:, :, 0:n2], d_xall[:, :, t02:t02 + n2], [], ['xst%d' % b2])
            P.begin_capture()
            xs, xk = X3[bi % 3], 'xst%d' % (bi % 3)
            if bi + 2 < len(blocks):
                t02, n2, _ = blocks[bi + 2]
                P.dma(X3[(bi + 2) % 3][:, :, 0:n2], d_xall[:, :, t02:t02 + n2], [], ['xst%d' % ((bi + 2) % 3)])
            C.sumsq_rstd(xs[:, :, 0:n], 8, n, D, B['rstd'], ones, B['sq'], [xk], 'n1' + sx)
            C.norm_mod(xs, n, B['rstd'], svc(0, w), svc(1, w), B['h'], B['tmpf'], [xk], ['h' + sx], 'n1' + sx)
            h_bf, xo, ckv, krf, ckvn = B['h'], B['xo'], B['ckv'], B['krf'], B['ckvn']
            for ct in range(6):
                M = 128 if ct < 4 else 64
                c0 = ct * 128 if ct < 4 else 512 + (ct - 4) * 64
                pi = C.next_ps()
                pk = 'ps%d' % pi
                for k in range(8):
                    P.mm(ps[pi][0:M, 0:n], wA_bf[:, k, c0:c0 + M], h_bf[:, k, 0:n], k == 0, k == 7,
                         ['wA', 'h' + sx], [pk])
                if ct < 2:
                    P.act(xo[:, ct, 0:n], ps[pi][:, 0:n], AF.Copy, [pk], ['xbo' + sx])
                elif ct < 4:
                    P.op('dve', lambda e, pi=pi, ct=ct, n=n, ckv=ckv: e.tensor_copy(out=ckv[:, ct - 2, 0:n],
                                                                                 in_=ps[pi][:, 0:n]),
                         [pk], ['ckv' + sx])
                else:
                    P.op('dve', lambda e, pi=pi, ct=ct, n=n, krf=krf: e.tensor_copy(out=krf[0:64, ct - 4, 0:n],
                                                                                 in_=ps[pi][0:64, 0:n]),
                         [pk], ['krf' + sx])
            P.dma(s_xb[:, :, t0:t0 + n].rearrange("c p t -> p c t"), xo[:, :, 0:n], ['xbo' + sx], ['s_xb%d' % bi])
            C.sumsq_rstd(ckv[:, :, 0:n], 2, n, 256, B['rstd2'], ones, B['ckv_sq'], ['ckv' + sx], 'kvn' + sx)
            for k in range(2):
                P.op('dve', lambda e, k=k, n=n, ckv=ckv, B=B: e.tensor_tensor(out=ckv[:, k, 0:n], in0=ckv[:, k, 0:n],
                                                                           in1=B['rstd2'][:, 0:n], op=ALU.mult),
                     ['ckv' + sx, 'kvn' + sx + '_rstd'], ['ckv' + sx])
                P.act(ckvn[:, k, 0:n], ckv[:, k, 0:n], AF.Copy, ['ckv' + sx, 'vecs'], ['ckvn' + sx],
                      scale=kvng[:, k:k + 1])
            Ko, Kk = B['Ko'], 'Ko' + sx
            for h in range(4):
                pi = C.next_ps()
                pk = 'ps%d' % pi
                for k in range(2):
                    P.mm(ps[pi][:, 0:n], wukv_bf[:, k, h * 128:(h + 1) * 128], ckvn[:, k, 0:n], k == 0, k == 1,
                         ['wukv', 'ckvn' + sx], [pk])
                P.act(Ko[:, h, 0:n], ps[pi][:, 0:n], AF.Copy, [pk], [Kk])
            P.dma(s_KT[:, :, t0:t0 + n].rearrange("h p t -> p h t"), Ko[:, :, 0:n], [Kk], ['s_KT%d' % bi])
            Vo, Vk = B['Vo'], 'Vo' + sx
            for tt in range(n // 128):
                pi = C.next_ps()
                pk = 'ps%d' % pi
                for k in range(2):
                    P.mm(ps[pi][:, 0:512], ckvn[:, k, tt * 128:(tt + 1) * 128], wukv_bf[:, k, 512:1024], k == 0, k == 1,
                         ['wukv', 'ckvn' + sx], [pk])
                P.op('dve', lambda e, pi=pi, tt=tt, Vo=Vo: e.tensor_copy(
                    out=Vo[:, :, tt, :], in_=ps[pi][:, 0:512].rearrange("p (h c) -> p h c", h=4)), [pk], [Vk])
            nt = n // 128
            for h in range(4):
                P.dma(s_V[h, :, t0 // 128:t0 // 128 + nt, :], Vo[:, h, 0:nt, :], [Vk], ['s_V%d_%d' % (bi, h)])
            ko, kk = B['ko'], 'kro' + sx
            if w == 0:
                l0 = t0 - CTX
                rC, rS = B['rC'], B['rS']
                P.dma(rC[0:64, 0:n], d_ropeC[:, l0:l0 + n], [], ['rC' + sx])
                P.dma(rS[0:64, 0:n], d_ropeS[:, l0:l0 + n], [], ['rS' + sx])
                P.op('pool', lambda e, n=n, krf=krf, rC=rC: e.tensor_tensor(out=krf[0:64, 0, 0:n], in0=krf[0:64, 0, 0:n],
                                                                          in1=rC[0:64, 0:n], op=ALU.mult),
                     ['krf' + sx, 'rC' + sx], ['krf' + sx])
                P.op('pool', lambda e, n=n, krf=krf, rS=rS: e.tensor_tensor(out=krf[0:64, 1, 0:n], in0=krf[0:64, 1, 0:n],
                                                                          in1=rS[0:64, 0:n], op=ALU.mult),
                     ['krf' + sx, 'rS' + sx], ['krf' + sx])
                P.op('pool', lambda e, n=n, ko=ko, krf=krf: e.tensor_tensor(out=ko[0:64, 0:n], in0=krf[0:64, 0, 0:n],
                                                                          in1=krf[0:64, 1, 0:n], op=ALU.add),
                     ['krf' + sx], [kk])
            else:
                P.op('pool', lambda e, n=n, ko=ko, krf=krf: e.tensor_copy(out=ko[0:64, 0:n], in_=krf[0:64, 0, 0:n]),
                     ['krf' + sx], [kk])
            P.dma(s_kr[:, t0:t0 + n], ko[0:64, 0:n], [kk], ['s_kr%d' % bi])
            caps.append(P.end_capture())
        C.ps_range = (0, 6)
        P.replay_interleaved(caps, 2)
        P.fence()
        A.release(m1)

        mixT = A.bf(8, NL)
        qTn = A.bf(4, NL)
        qTr = A.bf(4, NL)
        mP = A.mark()
        ysum = A.f32(2, NL)
        m2 = A.mark()
        lruw_bf = A.bf(8, 128)
        C.load_cast(lruw_bf, d_lruw, 128, 8, 'lruw', stage=[(A.f32(128), 'lst0'), (A.f32(128), 'lst1')])
        NCH = 1024
        NCK = SEQ // NCH

        def p2set():
            return dict(xi=A.f32(NCH + 3), cl=A.f32(NCH), clb=A.bf(NCH), rr=A.f32(NCH), ii=A.f32(NCH), aa=A.f32(NCH),
                        t1=A.f32(NCH), t2=A.f32(NCH), hh=A.f32(NCH))
        B2 = [p2set(), p2set()]
        carry = A.f32(4)
        chunks = [(0, CTX, True, True, -1)] + [(CTX + NCH * j, NCH, j == 0, j == NCK - 1, j) for j in range(NCK)]
        CPC = NLOC // NCH
        ci = 0
        first_lat = {}
        caps2 = []
        for ct in range(2):
            for d in range(2):
                order = chunks if d == 0 else [chunks[0]] + chunks[:0:-1]
                cv = carry[:, ct * 2 + d:ct * 2 + d + 1]
                for qi, (t0, n, lz, rz, j) in enumerate(order):
                    B = B2[ci % 2]
                    sx = str(ci % 2)
                    C.ps_range = (0, 3) if ci % 2 == 0 else (3, 6)
                    ci += 1
                    P.begin_capture()
                    xi, cl, clb, rr, ii, aa, t1, t2, hh = (B['xi'], B['cl'], B['clb'], B['rr'], B['ii'], B['aa'], B['t1'],
                                                           B['t2'], B['hh'])
                    xk = 'xin' + sx
                    lo = 0 if lz else 2
                    ro = 0 if rz else 1
                    P.dma(xi[:, 2 - lo:2 + n + ro], s_xb[ct, :, t0 - lo:t0 + n + ro], [], [xk])
                    if lz:
                        P.op('pool', lambda e, xi=xi: e.memset(xi[:, 0:2], 0.0), [], [xk])
                    if rz:
                        P.op('pool', lambda e, xi=xi, n=n: e.memset(xi[:, n + 2:n + 3], 0.0), [], [xk])
                    P.op('dve', lambda e, xi=xi, n=n, ct=ct, cl=cl: e.tensor_scalar(
                        out=cl[:, 0:n], in0=xi[:, 0:n], scalar1=convw[:, ct * 4:ct * 4 + 1],
                        scalar2=convb[:, ct:ct + 1], op0=ALU.mult, op1=ALU.add), [xk, 'vecs'], ['cl' + sx])
                    for k in range(1, 4):
                        P.op('dve', lambda e, xi=xi, n=n, k=k, ct=ct, cl=cl: e.scalar_tensor_tensor(
                            out=cl[:, 0:n], in0=xi[:, k:k + n], scalar=convw[:, ct * 4 + k:ct * 4 + k + 1],
                            in1=cl[:, 0:n], op0=ALU.mult, op1=ALU.add), [xk, 'vecs', 'cl' + sx], ['cl' + sx])
                    P.act(clb[:, 0:n], cl[:, 0:n], AF.Copy, ['cl' + sx], ['clb' + sx])
                    for g, dst, dk in ((0, rr, 'rr' + sx), (1, ii, 'ii' + sx)):
                        wi = d * 4 + g * 2 + ct
                        for sb in range((n + 511) // 512):
                            nn = min(512, n - sb * 512)
                            pi = C.next_ps()
                            pk = 'ps%d' % pi
                            P.mm(ps[pi][:, 0:nn], lruw_bf[:, wi, :], clb[:, sb * 512:sb * 512 + nn], True, True,
                                 ['lruw', 'clb' + sx], [pk])
                            P.act(dst[:, sb * 512:sb * 512 + nn], ps[pi][:, 0:nn], AF.Sigmoid, [pk, 'vecs'], [dk],
                                  bias=lrub[:, wi:wi + 1])
                    P.act(aa[:, 0:n], rr[:, 0:n], AF.Exp, ['rr' + sx, 'spv'], ['aa' + sx],
                          scale=spv[:, d * 2 + ct:d * 2 + ct + 1])
                    P.op('pool', lambda e, n=n, t1=t1, aa=aa: e.tensor_tensor(out=t1[:, 0:n], in0=aa[:, 0:n],
                                                                            in1=aa[:, 0:n], op=ALU.mult),
                         ['aa' + sx], ['t1' + sx])
                    P.act(t1[:, 0:n], t1[:, 0:n], AF.Sqrt, ['t1' + sx], ['t1' + sx], scale=-1.0, bias=1.0)
                    P.op('pool', lambda e, n=n, t2=t2, ii=ii, cl=cl: e.tensor_tensor(out=t2[:, 0:n], in0=ii[:, 0:n],
                                                                                   in1=cl[:, 0:n], op=ALU.mult),
                         ['ii' + sx, 'cl' + sx], ['t2' + sx])
                    P.op('pool', lambda e, n=n, t2=t2, t1=t1: e.tensor_tensor(out=t2[:, 0:n], in0=t2[:, 0:n],
                                                                            in1=t1[:, 0:n], op=ALU.mult),
                         ['t2' + sx, 't1' + sx], ['t2' + sx])
                    init = 0.0 if qi == 0 else cv
                    if d == 0:
                        P.op('dve', lambda e, n=n, init=init, hh=hh, aa=aa, t2=t2: e.tensor_tensor_scan(
                            out=hh[:, 0:n], data0=aa[:, 0:n], data1=t2[:, 0:n], initial=init, op0=ALU.mult,
                            op1=ALU.add), ['aa' + sx, 't2' + sx, 'carry'], ['hh' + sx])
                        P.op('dve', lambda e, n=n, cv=cv, hh=hh: e.tensor_copy(out=cv, in_=hh[:, n - 1:n]),
                             ['hh' + sx], ['carry'])
                    else:
                        P.op('dve', lambda e, n=n, init=init, hh=hh, aa=aa, t2=t2: e.tensor_tensor_scan(
                            out=hh[:, 0:n][:, ::-1], data0=aa[:, 0:n][:, ::-1], data1=t2[:, 0:n][:, ::-1],
                            initial=init, op0=ALU.mult, op1=ALU.add), ['aa' + sx, 't2' + sx, 'carry'], ['hh' + sx])
                        P.op('dve', lambda e, cv=cv, hh=hh: e.tensor_copy(out=cv, in_=hh[:, 0:1]), ['hh' + sx], ['carry'])
                    if j < 0:
                        if d == 0:
                            P.op('pool', lambda e, n=n, ct=ct, hh=hh: e.tensor_copy(out=ysum[:, ct, 0:n], in_=hh[:, 0:n]),
                                 ['hh' + sx], ['ysum'])
                        else:
                            P.op('pool', lambda e, n=n, ct=ct, hh=hh: e.tensor_tensor(
                                out=ysum[:, ct, 0:n], in0=ysum[:, ct, 0:n], in1=hh[:, 0:n], op=ALU.add),
                                ['hh' + sx, 'ysum'], ['ysum'])
                    else:
                        jc, off = j // CPC, CTX + (j % CPC) * NCH
                        key = (ct, j % CPC)
                        if key not in first_lat:
                            first_lat[key] = True
                            P.op('dve', lambda e, n=n, jc=jc, off=off, ct=ct, hh=hh: e.tensor_scalar(
                                out=ysum[:, ct, off:off + n], in0=hh[:, 0:n], scalar1=cmask[:, jc:jc + 1], scalar2=None,
                                op0=ALU.mult), ['hh' + sx, 'vecs'], ['ysum'])
                        else:
                            P.op('dve', lambda e, n=n, jc=jc, off=off, ct=ct, hh=hh: e.scalar_tensor_tensor(
                                out=ysum[:, ct, off:off + n], in0=hh[:, 0:n], scalar=cmask[:, jc:jc + 1],
                                in1=ysum[:, ct, off:off + n], op0=ALU.mult, op1=ALU.add),
                                ['hh' + sx, 'vecs', 'ysum'], ['ysum'])
                    caps2.append(P.end_capture())
        C.ps_range = (0, 6)
        P.replay_interleaved(caps2, 2)
        P.fence()
        A.release(m2)

        wB_bf = A.bf(8, 1152)
        wuq_bf = A.bf(3, 1024)
        sguw_bf = A.bf(4, 128)
        sgub = A.f32(2, 128)
        m3 = A.mark()
        wst3 = [(A.f32(1152), 'wst3_0'), (A.f32(1152), 'wst3_1')]
        C.load_cast(wB_bf, d_wB, 1152, 8, 'wB', stage=wst3)
        C.load_cast(wuq_bf, d_wuq, 1024, 3, 'wuq', stage=wst3)
        C.load_cast(sguw_bf, d_sguw, 128, 4, 'sguw', stage=wst3)
        P.dma(sgub, d_sgub, [], ['sgub'])
        P.fence()
        A.release(m3)
        xs3 = A.f32(8, 512)
        sq3 = A.bf(8, 512)
        rstd3 = A.f32(512)
        tmp3 = [A.f32(512), A.f32(512)]
        h3 = A.bf(8, 512)
        u_bf = A.bf(2, 512)
        vf = A.f32(2, 512)
        vn_bf = A.bf(2, 512)
        vtok = A.bf(256)
        gbf = A.f32(2, 512)
        cqf = A.f32(3, 512)
        cqn = A.bf(3, 512)
        rstdq = A.f32(512)
        rstdv = A.f32(512)
        sqv = A.bf(2, 512)
        sqq = A.bf(3, 512)
        rCl = A.f32(512)
        rSl = A.f32(512)
        tq1 = A.f32(512)
        tq2 = A.f32(512)
        tg = A.f32(128)
        lblocks = [(d_xall[:, :, 0:CTX], CTX, 1, 0, -1)] + \
                  [(d_xloc[:, :, 512 * i:512 * (i + 1)], 512, 0, CTX + 512 * i, 512 * i) for i in range(NLOC // 512)]
        for (src, n, w, q0, l0) in lblocks:
            P.dma(xs3[:, :, 0:n], src, [], ['xs3'])
            C.sumsq_rstd(xs3[:, :, 0:n], 8, n, D, rstd3, ones, sq3, ['xs3'], 'n3')
            C.norm_mod(xs3, n, rstd3, svc(0, w), svc(1, w), h3, tmp3, ['xs3'], ['h3'], 'n3')
            for ct in range(9):
                pi = C.next_ps()
                pk = 'ps%d' % pi
                for k in range(8):
                    P.mm(ps[pi][:, 0:n], wB_bf[:, k, ct * 128:(ct + 1) * 128], h3[:, k, 0:n], k == 0, k == 7,
                         ['wB', 'h3'], [pk])
                if ct < 2:
                    P.act(u_bf[:, ct, 0:n], ps[pi][:, 0:n], AF.Gelu_apprx_tanh, [pk], ['u'])
                elif ct < 4:
                    P.act(vf[:, ct - 2, 0:n], ps[pi][:, 0:n], AF.Gelu_apprx_tanh, [pk], ['vf'])
                elif ct < 6:
                    P.act(gbf[:, ct - 4, 0:n], ps[pi][:, 0:n], AF.Gelu_apprx_tanh, [pk], ['gbf'])
                    P.op('dve', lambda e, ct=ct, n=n, q0=q0: e.tensor_tensor(
                        out=mixT[:, 2 + ct - 4, q0:q0 + n], in0=gbf[:, ct - 4, 0:n], in1=ysum[:, ct - 4, q0:q0 + n],
                        op=ALU.mult), ['gbf', 'ysum'], ['mix_b'])
                else:
                    P.op('dve', lambda e, ct=ct, n=n, pi=pi: e.tensor_copy(out=cqf[:, ct - 6, 0:n], in_=ps[pi][:, 0:n]),
                         [pk], ['cqf'])
            C.sumsq_rstd(vf[:, :, 0:n], 2, n, 256, rstdv, ones, sqv, ['vf'], 'vn')
            for k in range(2):
                P.op('dve', lambda e, k=k, n=n: e.tensor_tensor(out=vn_bf[:, k, 0:n], in0=vf[:, k, 0:n],
                                                                in1=rstdv[:, 0:n], op=ALU.mult),
                     ['vf', 'vn_rstd'], ['vn'])
            for tt in range(n // 128):
                for k in range(2):
                    P.tr(psb[:, k * 128:(k + 1) * 128], vn_bf[:, k, tt * 128:(tt + 1) * 128], ident,
                         ['vn', 'consts'], ['psb'])
                P.op('dve', lambda e: e.tensor_copy(out=vtok, in_=psb[:, 0:256]), ['psb'], ['vtok'])
                pi = C.next_ps()
                pk = 'ps%d' % pi
                for g in range(4):
                    P.mm(ps[pi][(g % 2) * 64:(g % 2) * 64 + 64, (g // 2) * 128:(g // 2) * 128 + 128],
                         vtok[:, g * 64:(g + 1) * 64], sguw_bf[:, g, :], True, True, ['vtok', 'sguw'], [pk])
                for c2 in range(2):
                    P.op('dve', lambda e, c2=c2, pi=pi: e.tensor_tensor(out=tg, in0=ps[pi][:, c2 * 128:(c2 + 1) * 128],
                                                                        in1=sgub[:, c2, :], op=ALU.add),
                         [pk, 'sgub'], ['tg'])
                    P.op('dve', lambda e, c2=c2, tt=tt, q0=q0: e.tensor_tensor(
                        out=mixT[:, c2, q0 + tt * 128:q0 + (tt + 1) * 128], in0=tg,
                        in1=u_bf[:, c2, tt * 128:(tt + 1) * 128], op=ALU.mult), ['tg', 'u'], ['mix_a'])
            C.sumsq_rstd(cqf[:, :, 0:n], 3, n, 384, rstdq, ones, sqq, ['cqf'], 'qn')
            for k in range(3):
                P.op('dve', lambda e, k=k, n=n: e.tensor_tensor(out=cqf[:, k, 0:n], in0=cqf[:, k, 0:n],
                                                                in1=rstdq[:, 0:n], op=ALU.mult),
                     ['cqf', 'qn_rstd'], ['cqf'])
                P.act(cqn[:, k, 0:n], cqf[:, k, 0:n], AF.Copy, ['cqf', 'vecs'], ['cqn'], scale=qng[:, k:k + 1])
            if w == 0:
                P.dma(rCl[0:64, 0:n], d_ropeCl[:, l0:l0 + n], [], ['rCl'])
                P.dma(rSl[0:64, 0:n], d_ropeSl[:, l0:l0 + n], [], ['rSl'])
            for h in range(4):
                pi = C.next_ps()
                pk = 'ps%d' % pi
                for k in range(3):
                    P.mm(ps[pi][:, 0:n], wuq_bf[:, k, h * 256:h * 256 + 128], cqn[:, k, 0:n], k == 0, k == 2,
                         ['wuq', 'cqn'], [pk])
                P.act(qTn[:, h, q0:q0 + n], ps[pi][:, 0:n], AF.Copy, [pk], ['qTn'])
                pa = C.next_ps()
                pak = 'ps%d' % pa
                for k in range(3):
                    P.mm(ps[pa][0:64, 0:n], wuq_bf[:, k, h * 256 + 128:h * 256 + 192], cqn[:, k, 0:n], k == 0, k == 2,
                         ['wuq', 'cqn'], [pak])
                if w == 1:
                    P.act(qTr[0:64, h, q0:q0 + n], ps[pa][0:64, 0:n], AF.Copy, [pak], ['qTr'])
                else:
                    pb = C.next_ps()
                    pbk = 'ps%d' % pb
                    for k in range(3):
                        P.mm(ps[pb][0:64, 0:n], wuq_bf[:, k, h * 256 + 192:h * 256 + 256], cqn[:, k, 0:n], k == 0,
                             k == 2, ['wuq', 'cqn'], [pbk])
                    P.op('dve', lambda e, pa=pa, n=n: e.tensor_tensor(out=tq1[0:64, 0:n], in0=ps[pa][0:64, 0:n],
                                                                      in1=rCl[0:64, 0:n], op=ALU.mult),
                         [pak, 'rCl'], ['tq1'])
                    P.op('dve', lambda e, pb=pb, n=n: e.tensor_tensor(out=tq2[0:64, 0:n], in0=ps[pb][0:64, 0:n],
                                                                      in1=rSl[0:64, 0:n], op=ALU.mult),
                         [pbk, 'rSl'], ['tq2'])
                    P.op('pool', lambda e, h=h, n=n, q0=q0: e.tensor_tensor(
                        out=qTr[0:64, h, q0:q0 + n], in0=tq1[0:64, 0:n], in1=tq2[0:64, 0:n], op=ALU.add),
                        ['tq1', 'tq2'], ['qTr'])
        P.fence()
        A.release(mP)

        NKT = TALL // 128
        KT = A.bf(TALL)
        Vh = A.bf(NKT, 128)
        krT = A.bf(TALL)
        PT = [A.bf(512) for _ in range(6)]
        rinv = A.f32(512)
        lacc = [[A.f32(512) for _ in range(3)] for _ in range(2)]
        NPC = 5
        KPP = NKT // NPC
        for pc in range(NPC):
            a, b = pc * KPP * 128, (pc + 1) * KPP * 128
            P.dma(krT[0:64, a:b], s_kr[:, a:b], ['s_kr'], ['kr_p%d' % pc])
            P.dma(krT[64:128, a:b], s_kr[:, a:b], ['s_kr'], ['kr_p%d' % pc])
        P.dma(qTr[64:128, :, :], qTr[0:64, :, :], ['qTr'], ['qTr2'])
        qblocks = [(0, CTX, 2)] + [(CTX + 512 * i, 512, NKT) for i in range(NLOC // 512)]

        def load_kv(h, pc):
            a, b = pc * KPP * 128, (pc + 1) * KPP * 128
            P.dma(KT[:, a:b], s_KT[h, :, a:b], ['s_KT'], ['KT_p%d' % pc])
            P.dma(Vh[:, pc * KPP:(pc + 1) * KPP, :], s_V[h, :, pc * KPP:(pc + 1) * KPP, :], ['s_V'], ['V_p%d' % pc])
        tiles = []
        qbi = 0
        for h in range(4):
            for qi, (q0, n, nkt) in enumerate(qblocks):
                po, pl = 4 + qbi % 2, 6
                qbi += 1
                for kt in range(nkt):
                    tiles.append((h, qi, q0, n, nkt, kt, po, pl))

        def emit_qk_pair(i0):
            js = [j for j in (i0, i0 + 1) if j < len(tiles)]
            for j in js:
                h, qi, q0, n, nkt, kt, po, pl = tiles[j]
                sb = j % 4
                P.mm(ps[sb][:, 0:n], KT[:, kt * 128:(kt + 1) * 128], qTn[:, h, q0:q0 + n], True, False,
                     ['KT_p%d' % (kt // KPP), 'qTn'], ['ps%d' % sb])
            for j in js:
                h, qi, q0, n, nkt, kt, po, pl = tiles[j]
                sb = j % 4
                r0 = 0 if j % 2 == 0 else 64
                P.mm(ps[sb][:, 0:n], krT[r0:r0 + 64, kt * 128:(kt + 1) * 128], qTr[r0:r0 + 64, h, q0:q0 + n], False, True,
                     ['kr_p%d' % (kt // KPP), 'qTr', 'qTr2'], ['ps%d' % sb], tp=(r0, 0))
        for pc in range(NPC):
            load_kv(0, pc)
        emit_qk_pair(0)
        for i, (h, qi, q0, n, nkt, kt, po, pl) in enumerate(tiles):
            if i % 2 == 0 and i + 2 < len(tiles):
                emit_qk_pair(i + 2)
            sb = i % 4
            pc = kt // KPP
            pt = PT[i % 6]
            ptk = 'PT%d' % (i % 6)
            P.act(pt[:, 0:n], ps[sb][:, 0:n], AF.Exp, ['ps%d' % sb], [ptk], scale=ATTN_SCALE)
            P.mm(ps[po][:, 0:n], Vh[:, kt, :], pt[:, 0:n], kt == 0, kt == nkt - 1, ['V_p%d' % pc, ptk], ['ps%d' % po])
            c3 = kt % 3
            la, lak = lacc[po - 4][c3], 'lacc%d_%d' % (po - 4, c3)
            le = 'pool' if c3 == 2 else 'dve'
            if kt < 3:
                P.op(le, lambda e, la=la, pt=pt, n=n: e.tensor_copy(out=la[:, 0:n], in_=pt[:, 0:n]), [ptk], [lak])
            else:
                P.op(le, lambda e, la=la, pt=pt, n=n: e.tensor_tensor(out=la[:, 0:n], in0=la[:, 0:n], in1=pt[:, 0:n],
                                                                     op=ALU.add), [ptk, lak], [lak])
            if kt == nkt - 1:
                nacc = min(3, nkt)
                for c in range(nacc):
                    P.mm(ps[pl][:, 0:n], onesf, lacc[po - 4][c][:, 0:n], c == 0, c == nacc - 1,
                         ['lacc%d_%d' % (po - 4, c), 'onesf'], ['ps%d' % pl])
                P.op('dve', lambda e, pl=pl, n=n: e.reciprocal(out=rinv[:, 0:n], in_=ps[pl][:, 0:n]),
                     ['ps%d' % pl], ['rinv'])
                P.op('dve', lambda e, po=po, n=n, h=h, q0=q0: e.tensor_tensor(
                    out=mixT[:, 4 + h, q0:q0 + n], in0=ps[po][:, 0:n], in1=rinv[:, 0:n], op=ALU.mult),
                    ['ps%d' % po, 'rinv'], ['mix_c'])
            if qi == len(qblocks) - 1 and kt % KPP == KPP - 1 and h < 3:
                load_kv(h + 1, kt // KPP)
        P.fence()
        A.release(mP)

        wout_bf = A.bf(8, 1024)
        C.load_cast(wout_bf, d_wout, 1024, 8, 'wout', stage=[(A.f32(1024), 'wst5_0'), (A.f32(1024), 'wst5_1')])
        wr = A.f32(8, 16)
        P.dma(wr, d_wr, [], ['wr'])
        xr = A.f32(8, 512)
        x1 = A.f32(8, 512)
        sq5 = A.bf(8, 512)
        rstd5 = A.f32(512)
        tmp5 = [A.f32(512), A.f32(512)]
        h2 = A.f32(8, 512)
        sm = A.f32(4, 4)
        pe_ = A.f32(4, 16)
        for (src, n, w, q0, l0) in lblocks:
            P.dma(xr[:, :, 0:n], src, [], ['xr'])
            for j in range(8):
                pi = C.next_ps()
                pk = 'ps%d' % pi
                for k in range(8):
                    P.mm(ps[pi][:, 0:n], wout_bf[:, k, j * 128:(j + 1) * 128], mixT[:, k, q0:q0 + n], k == 0, k == 7,
                         ['wout', 'mix_a', 'mix_b', 'mix_c'], [pk])
                P.op('dve', lambda e, j=j, pi=pi, n=n, w=w: e.scalar_tensor_tensor(
                    out=x1[:, j, 0:n], in0=ps[pi][:, 0:n], scalar=svc(2, w)[:, j:j + 1], in1=xr[:, j, 0:n],
                    op0=ALU.mult, op1=ALU.add), [pk, 'xr', 'svt'], ['x1'])
            P.dma(o_x1[:, :, q0:q0 + n], x1[:, :, 0:n], ['x1'], ['o_x1'])
            C.sumsq_rstd(x1[:, :, 0:n], 8, n, D, rstd5, ones, sq5, ['x1'], 'n5')
            C.norm_mod(x1, n, rstd5, svc(3, w), svc(4, w), h2, tmp5, ['x1'], ['h2'], 'n5')
            for tt in range(n // 128):
                pi = C.next_ps()
                pk = 'ps%d' % pi
                si = tt % 4
                smk = 'sm%d' % si
                for k in range(8):
                    P.mm(ps[pi][:, 0:16], h2[:, k, tt * 128:(tt + 1) * 128], wr[:, k, :], k == 0, k == 7,
                         ['h2', 'wr'], [pk])
                P.op('dve', lambda e, pi=pi, si=si: e.reduce_max(out=sm[:, si, 0:1], in_=ps[pi][:, 0:16],
                                                                 axis=mybir.AxisListType.X), [pk], [smk])
                P.op('dve', lambda e, si=si: e.tensor_single_scalar(out=sm[:, si, 1:2], in_=sm[:, si, 0:1], scalar=-1.0,
                                                                    op=ALU.mult), [smk], [smk])
                P.act(pe_[:, si, :], ps[pi][:, 0:16], AF.Exp, [pk, smk], ['pe%d' % si], bias=sm[:, si, 1:2],
                      accum=sm[:, si, 2:3])
                P.op('dve', lambda e, si=si: e.reciprocal(out=sm[:, si, 3:4], in_=sm[:, si, 2:3]), ['pe%d' % si, smk],
                     [smk])
                P.op('dve', lambda e, si=si: e.tensor_scalar(out=pe_[:, si, :], in0=pe_[:, si, :],
                                                             scalar1=sm[:, si, 3:4], scalar2=None, op0=ALU.mult),
                     [smk, 'pe%d' % si], ['pe%d' % si])
                P.dma(o_probs[q0 + tt * 128:q0 + (tt + 1) * 128, :], pe_[:, si, :], ['pe%d' % si], ['o_probs'])
        P.finalize()
        return nc, P, A


def _fm(a):
    k = a.shape[0] // 128
    return np.ascontiguousarray(a.reshape(k, 128, -1).transpose(1, 0, 2))


def _fmv(v):
    return np.ascontiguousarray(v.reshape(-1, 128).T)


def _rope_perm():
    p = np.arange(64)
    o = ((p % 32) // 16) * 32 + (p // 32) * 16 + (p % 16)
    osw = o[(p + 32) % 64]
    return o, osw


def _rope_tables():
    rows = SEQ // 64
    r = np.repeat(np.arange(rows), 64).astype(np.float32)
    col = np.tile(np.arange(64), rows).astype(np.float32)
    inv = (np.float32(10000.0) ** (-np.arange(16, dtype=np.float32) / np.float32(16))).astype(np.float32)
    ang = np.concatenate([r[:, None] * inv, col[:, None] * inv], axis=-1).astype(np.float32)
    cos, sin = np.cos(ang).astype(np.float32), np.sin(ang).astype(np.float32)
    C_ = np.concatenate([cos, cos], axis=1).T
    S_ = np.concatenate([-sin, sin], axis=1).T
    return np.ascontiguousarray(C_), np.ascontiguousarray(S_)


def prep_AB(inp, l, xl, xc):
    o, osw = _rope_perm()
    xall = _fm(np.ascontiguousarray(np.concatenate([xc, xl], axis=0).T))
    cvec = np.stack([_fmv(inp['c'][0]), _fmv(inp['c_ctx'])], axis=-1)
    w_in = inp['w_in'][l]
    wA = np.concatenate([w_in[:, 512:768], w_in[:, 1408:1664], w_in[:, 1664 + o], w_in[:, 1664 + osw]], axis=1)
    wB = np.concatenate([w_in[:, 0:512], w_in[:, 768:1024], w_in[:, 1024:1408]], axis=1)
    wuq = inp['w_uq'][l]
    cols = []
    for h in range(4):
        cols += [wuq[:, h * 192:h * 192 + 128], wuq[:, h * 192 + 128 + o], wuq[:, h * 192 + 128 + osw]]
    wuq2 = np.concatenate(cols, axis=1)
    wukv = inp['w_ukv'][l]
    wukv2 = np.concatenate([wukv[:, h * 256:h * 256 + 128] for h in range(4)] +
                           [wukv[:, h * 256 + 128:h * 256 + 256] for h in range(4)], axis=1)
    lruw = np.zeros((128, 8, 128), np.float32)
    vecs = np.zeros((128, 64), np.float32)
    vecs[:, 0:8] = _fmv(inp['norm1_g'][l])
    vecs[:, 8:16] = _fmv(inp['norm2_g'][l])
    for d in range(2):
        for g in range(2):
            W = (inp['lru_wa'] if g == 0 else inp['lru_wx'])[l][d]
            bb = (inp['lru_ba'] if g == 0 else inp['lru_bx'])[l][d]
            for ct in range(2):
                i = d * 4 + g * 2 + ct
                lruw[0:64, i, 0:64] = W[2 * ct]
                lruw[64:128, i, 64:128] = W[2 * ct + 1]
                vecs[:, 16 + i] = bb[ct * 128:(ct + 1) * 128]
        for ct in range(2):
            vecs[:, 24 + d * 2 + ct] = inp['lru_lambda'][l][d][ct * 128:(ct + 1) * 128]
    for ct in range(2):
        for k in range(4):
            vecs[:, 28 + ct * 4 + k] = inp['conv_w'][l][k][ct * 128:(ct + 1) * 128]
        vecs[:, 36 + ct] = inp['conv_b'][l][ct * 128:(ct + 1) * 128]
    vecs[:, 38:41] = _fmv(inp['q_norm_g'][l])
    vecs[:, 41:43] = _fmv(inp['kv_norm_g'][l])
    sguw = np.ascontiguousarray(inp['sgu_w'][l].transpose(2, 0, 1))
    sgub = np.zeros((128, 2, 128), np.float32)
    for g in range(4):
        sgub[(g % 2) * 64:(g % 2) * 64 + 64, g // 2, :] = inp['sgu_b'][l][g][None, :]
    rC, rS = _rope_tables()
    common = dict(xall=xall, cvec=np.ascontiguousarray(cvec), wmod=_fm(np.ascontiguousarray(inp['w_mod'][l][:, :5120])),
                  bmod=_fmv(inp['b_mod'][l][:5120]), wA=_fm(wA), wB=_fm(wB), wuq=_fm(wuq2), wukv=_fm(wukv2),
                  wout=_fm(inp['w_out'][l]), lruw=lruw, sguw=sguw, sgub=sgub, wr=_fm(inp['w_router'][l]),
                  ropeC=rC, ropeS=rS, ident=np.eye(128, dtype=np.float32))
    maps = []
    for c in range(NCORES):
        v = vecs.copy()
        v[:, 43 + c] = 1.0
        m = dict(common)
        m['vecs'] = v
        m['xloc'] = np.ascontiguousarray(xall[:, :, CTX + NLOC * c:CTX + NLOC * (c + 1)])
        m['ropeCl'] = np.ascontiguousarray(rC[:, NLOC * c:NLOC * (c + 1)])
        m['ropeSl'] = np.ascontiguousarray(rS[:, NLOC * c:NLOC * (c + 1)])
        maps.append(m)
    return maps


def _unfm(a):
    return np.ascontiguousarray(a.transpose(1, 0, 2).reshape(-1, a.shape[2]).T)


def gather_AB(res):
    xl1 = np.concatenate([_unfm(r['x1T'][:, :, CTX:]) for r in res], axis=0)
    xc1 = _unfm(res[0]['x1T'][:, :, :CTX])
    pl = np.concatenate([r['probs'][CTX:] for r in res], axis=0)
    pc = res[0]['probs'][:CTX]
    return xl1, xc1, pl, pc


NBIS = 30


def build_C():
    nc = bass.Bass("TRN2", target_bir_lowering=False)

    def din(name, shape, dt=F32):
        return nc.dram_tensor(name, list(shape), dt, kind="ExternalInput").ap()
    d_x1 = din("x1T", [128, 8, NL])
    d_pA = din("probsA", [128, NLOC])
    d_pC = din("probsC", [16, CTX])
    d_gm = din("gmT", [16, NL])
    d_cvec = din("cvec", [128, 8, 2])
    d_wmod = din("wmod", [128, 8, 3072])
    d_bmod = din("bmod", [128, 24])
    d_vecs = din("vecs", [128, 16])
    d_G = din("G", [128, 128])
    d_oh = din("oh16", [16, 16])
    d_wg = din("wg", [NEXP, D, D])
    d_wu = din("wu", [NEXP, D, D])
    d_wd = din("wd", [NEXP, D, D])
    o_x2 = nc.dram_tensor("x2T", [128, 8, NL], F32, kind="ExternalOutput").ap()
    o_on = nc.dram_tensor("outN", [128, 8, NL], F32, kind="ExternalOutput").ap()

    AW = 51 * 1024
    with ExitStack() as es:
        arena_t = es.enter_context(nc.sbuf_tensor("arena", [128, AW], F32))
        ps = [es.enter_context(nc.psum_tensor("ps%d" % i, [128, 512], F32)) for i in range(7)]
        P = Prog(nc)
        A = Arena(arena_t, AW)
        C = Ctx(nc, P, A, ps, None)

        vecs = A.f32(16)
        modT = A.f32(24, 2)
        cs = A.f32(64)
        sv = A.f32(3, 8, 2)
        G = A.f32(128)
        oh = A.f32(16)
        ones16 = A.f32(128)
        ones = A.bf(128)
        ts = A.f32(16)
        m0 = A.mark()
        P.dma(vecs, d_vecs, [], ['vecs'])
        P.dma(G, d_G, [], ['consts'])
        P.dma(oh[0:16, :], d_oh, [], ['consts'])
        P.op('dve', lambda e: e.memset(ones, 1.0), [], ['consts'])
        P.op('dve', lambda e: e.memset(ones16, 1.0), [], ['consts'])
        stage8 = A.f32(8, 1024)
        stage8b = A.f32(8, 1024)
        C.compute_mods(d_cvec, d_wmod, d_bmod, 3, modT, cs, [stage8, stage8b], oh[0:2, 0:2])
        n2g, fng = vecs[:, 0:8], vecs[:, 8:16]
        mv = modT.rearrange("p (b k) w -> p b k w", b=3)
        for w in range(2):
            P.op('dve', lambda e, w=w: e.scalar_tensor_tensor(
                out=sv[:, 0, :, w], in0=mv[:, 1, :, w], scalar=1.0, in1=n2g, op0=ALU.add, op1=ALU.mult),
                ['modv', 'vecs'], ['svt'])
            P.op('dve', lambda e, w=w: e.tensor_copy(out=sv[:, 1, :, w], in_=mv[:, 0, :, w]), ['modv'], ['svt'])
            P.op('dve', lambda e, w=w: e.tensor_copy(out=sv[:, 2, :, w], in_=mv[:, 2, :, w]), ['modv'], ['svt'])
        P.fence()
        A.release(m0)

        h2T = A.bf(8, NL)
        acc = A.f32(8, NL)
        gmT = A.f32(NL)
        mL = A.mark()
        lblocks = [(0, CTX, 1)] + [(CTX + 512 * i, 512, 0) for i in range(NLOC // 512)]

        xs = [A.f32(8, 512), A.f32(8, 512)]
        sq = A.bf(8, 512)
        rstd = A.f32(512)
        tmpf = [A.f32(512), A.f32(512)]
        for bi, (q0, n, w) in enumerate(lblocks):
            x = xs[bi % 2]
            xk = 'xs%d' % (bi % 2)
            P.dma(x[:, :, 0:n], d_x1[:, :, q0:q0 + n], [], [xk])
            C.sumsq_rstd(x[:, :, 0:n], 8, n, D, rstd, ones, sq, [xk], 'n2')
            C.norm_mod(x, n, rstd, sv[:, 0, :, w], sv[:, 1, :, w], h2T[:, :, q0:q0 + n], tmpf, [xk], ['h2T'], 'n2')
        P.fence()
        A.release(mL)

        pA = A.f32(NLOC)
        mk = A.f32(NLOC)
        pC = A.f32(CTX)
        mkc = A.f32(CTX)
        P.dma(pA, d_pA, [], ['pA'])
        P.dma(pC[0:16, :], d_pC, [], ['pC'])
        P.dma(gmT[0:16, :], d_gm, [], ['gmT'])
        lo, hi, mid, tot, gt, dd, Kv = (ts[:, 0:2], ts[:, 2:4], ts[:, 4:6], ts[:, 6:8], ts[:, 8:10], ts[:, 10:12],
                                        ts[:, 12:14])
        P.op('dve', lambda e: e.memset(ts, 0.0), [], ['ts'])
        P.op('dve', lambda e: e.memset(hi, 1.0), ['ts'], ['ts'])
        P.op('dve', lambda e: e.memset(mid, 0.5), ['ts'], ['ts'])
        P.op('dve', lambda e: e.memset(ts[:, 12:13], NLOC - 0.5), ['ts'], ['ts'])
        P.op('dve', lambda e: e.memset(ts[:, 13:14], 2 * CTX // NEXP - 0.5), ['ts'], ['ts'])
        for it in range(NBIS):
            P.op('dve', lambda e: e.tensor_scalar(out=mk, in0=pA, scalar1=ts[:, 4:5], scalar2=None, op0=ALU.is_gt),
                 ['pA', 'ts'], ['mk'])
            P.op('dve', lambda e: e.reduce_sum(out=ts[:, 14:15], in_=mk, axis=mybir.AxisListType.X), ['mk'], ['cc'])
            P.op('dve', lambda e: e.tensor_scalar(out=mkc[0:16, :], in0=pC[0:16, :], scalar1=ts[0:16, 5:6], scalar2=None,
                                                  op0=ALU.is_gt), ['pC', 'ts'], ['mkc'])
            P.op('dve', lambda e: e.reduce_sum(out=ts[0:16, 7:8], in_=mkc[0:16, :], axis=mybir.AxisListType.X),
                 ['mkc', 'ts'], ['ts'])
            P.mm(ps[6][:, 0:1], G, ts[:, 14:15], True, True, ['cc', 'consts'], ['ps6'])
            P.op('dve', lambda e: e.tensor_copy(out=ts[:, 6:7], in_=ps[6][:, 0:1]), ['ps6', 'ts'], ['ts'])
            P.op('dve', lambda e: e.tensor_tensor(out=gt, in0=tot, in1=Kv, op=ALU.is_gt), ['ts'], ['ts'])
            P.op('dve', lambda e: e.tensor_tensor(out=dd, in0=mid, in1=lo, op=ALU.subtract), ['ts'], ['ts'])
            P.op('dve', lambda e: e.tensor_tensor(out=dd, in0=dd, in1=gt, op=ALU.mult), ['ts'], ['ts'])
            P.op('dve', lambda e: e.tensor_tensor(out=lo, in0=lo, in1=dd, op=ALU.add), ['ts'], ['ts'])
            P.op('dve', lambda e: e.tensor_tensor(out=dd, in0=hi, in1=mid, op=ALU.subtract), ['ts'], ['ts'])
            P.op('dve', lambda e: e.tensor_tensor(out=dd, in0=dd, in1=gt, op=ALU.mult), ['ts'], ['ts'])
            P.op('dve', lambda e: e.tensor_tensor(out=hi, in0=mid, in1=dd, op=ALU.add), ['ts'], ['ts'])
            P.op('dve', lambda e: e.tensor_tensor(out=dd, in0=lo, in1=hi, op=ALU.add), ['ts'], ['ts'])
            P.op('dve', lambda e: e.tensor_single_scalar(out=mid, in_=dd, scalar=0.5, op=ALU.mult), ['ts'], ['ts'])
        P.op('dve', lambda e: e.tensor_scalar(out=mk[0:16, 0:NLOC], in0=gmT[0:16, CTX:NL], scalar1=ts[0:16, 2:3],
                                              scalar2=None, op0=ALU.is_gt), ['gmT', 'ts'], ['mk'])
        P.op('dve', lambda e: e.tensor_tensor(out=gmT[0:16, CTX:NL], in0=gmT[0:16, CTX:NL], in1=mk[0:16, 0:NLOC],
                                              op=ALU.mult), ['mk', 'gmT'], ['gmT'])
        P.op('dve', lambda e: e.tensor_scalar(out=mkc[0:16, :], in0=gmT[0:16, 0:CTX], scalar1=ts[0:16, 3:4],
                                              scalar2=None, op0=ALU.is_gt), ['gmT', 'ts'], ['mkc'])
        P.op('dve', lambda e: e.tensor_tensor(out=gmT[0:16, 0:CTX], in0=gmT[0:16, 0:CTX], in1=mkc[0:16, :],
                                              op=ALU.mult), ['mkc', 'gmT'], ['gmT'])
        P.fence()
        A.release(mL)

        HF = 512
        wbuf = [dict(g=A.bf(8, HF), u=A.bf(8, HF), d=A.bf(4, 1024)) for _ in range(2)]
        wst = [(A.f32(1024), 'wst0'), (A.f32(1024), 'wst1')]
        hid = A.bf(4, 512)
        sg = [A.f32(512), A.f32(512)]
        tt_ = [A.f32(512), A.f32(512)]
        gmb = A.f32(512)
        gme = A.f32(512)
        units = [(ex, hf) for ex in range(NEXP) for hf in range(2)]
        stc = [0]
        gmbs = [gmb, A.f32(512)]
        gmes = [gme, A.f32(512)]

        def emit_gm(gi):
            ui2, bi2 = gi // len(lblocks), gi % len(lblocks)
            ex2 = units[ui2][0]
            q02, n2, _ = lblocks[bi2]
            ge, gb_ = gmes[gi % 2], gmbs[gi % 2]
            P.op('dve', lambda e: e.tensor_scalar(out=ge[0:16, 0:n2], in0=gmT[0:16, q02:q02 + n2],
                                                  scalar1=oh[0:16, ex2:ex2 + 1], scalar2=None, op0=ALU.mult),
                 ['gmT', 'consts'], ['gme%d' % (gi % 2)])
            P.mm(ps[6][:, 0:n2], ones16[0:16, :], ge[0:16, 0:n2], True, True, ['gme%d' % (gi % 2), 'consts'], ['ps6'])
            P.act(gb_[:, 0:n2], ps[6][:, 0:n2], AF.Copy, ['ps6'], ['gmb%d' % (gi % 2)])

        def load_steps(ui):
            ex, hf = units[ui]
            wb = wbuf[ui % 2]
            tag = 'w%d' % (ui % 2)
            steps = []
            for nm, dsrc in (('g', d_wg), ('u', d_wu)):
                for k in range(8):
                    def f(nm=nm, dsrc=dsrc, k=k):
                        st, sk = wst[stc[0] % 2]
                        stc[0] += 1
                        P.dma(st[:, 0:HF], dsrc[ex, k * 128:(k + 1) * 128, hf * HF:(hf + 1) * HF], [], [sk])
                        P.op(C.ew(), lambda en, st=st: en.tensor_copy(out=wb[nm][:, k, :], in_=st[:, 0:HF]),
                             [sk], [tag + nm])
                    steps.append(f)
            for f4 in range(4):
                def f(f4=f4):
                    st, sk = wst[stc[0] % 2]
                    stc[0] += 1
                    r0 = (hf * 4 + f4) * 128
                    P.dma(st[:, 0:1024], d_wd[ex, r0:r0 + 128, :], [], [sk])
                    P.op(C.ew(), lambda en, st=st: en.tensor_copy(out=wb['d'][:, f4, :], in_=st[:, 0:1024]),
                         [sk], [tag + 'd'])
                steps.append(f)
            return steps
        for f in load_steps(0):
            f()
        first_acc = True
        for ui, (ex, hf) in enumerate(units):
            wb = wbuf[ui % 2]
            tag = 'w%d' % (ui % 2)
            nxt = load_steps(ui + 1) if ui + 1 < len(units) else []
            per = (len(nxt) + len(lblocks) - 1) // len(lblocks)
            for bi, (q0, n, w) in enumerate(lblocks):
                gi = ui * len(lblocks) + bi
                if gi == 0:
                    emit_gm(0)
                gmb, gmk = gmbs[gi % 2], 'gmb%d' % (gi % 2)
                for f in range(4):
                    pg, pu = f % 2, 2 + f % 2
                    for k in range(8):
                        P.mm(ps[pg][:, 0:n], wb['g'][:, k, f * 128:(f + 1) * 128], h2T[:, k, q0:q0 + n], k == 0, k == 7,
                             [tag + 'g', 'h2T'], ['ps%d' % pg])
                    for k in range(8):
                        P.mm(ps[pu][:, 0:n], wb['u'][:, k, f * 128:(f + 1) * 128], h2T[:, k, q0:q0 + n], k == 0, k == 7,
                             [tag + 'u', 'h2T'], ['ps%d' % pu])
                    s_, t_ = sg[f % 2], tt_[f % 2]
                    P.act(s_[:, 0:n], ps[pg][:, 0:n], AF.Silu, ['ps%d' % pg], ['sg%d' % (f % 2)])
                    P.op('pool', lambda e, s_=s_, t_=t_, n=n, gmb=gmb: e.tensor_tensor(out=t_[:, 0:n], in0=s_[:, 0:n],
                                                                                      in1=gmb[:, 0:n], op=ALU.mult),
                         ['sg%d' % (f % 2), gmk], ['tt%d' % (f % 2)])
                    P.op('dve', lambda e, t_=t_, n=n, f=f, pu=pu: e.tensor_tensor(
                        out=hid[:, f, 0:n], in0=t_[:, 0:n], in1=ps[pu][:, 0:n], op=ALU.mult),
                        ['tt%d' % (f % 2), 'ps%d' % pu], ['hid'])
                if gi + 1 < len(units) * len(lblocks):
                    emit_gm(gi + 1)
                for j in range(8):
                    pd = 4 + j % 2
                    for f in range(4):
                        P.mm(ps[pd][:, 0:n], wb['d'][:, f, j * 128:(j + 1) * 128], hid[:, f, 0:n], f == 0, f == 3,
                             [tag + 'd', 'hid'], ['ps%d' % pd])
                    if ui == 0:
                        P.op('dve', lambda e, j=j, pd=pd, n=n, q0=q0: e.tensor_copy(out=acc[:, j, q0:q0 + n],
                                                                                   in_=ps[pd][:, 0:n]),
                             ['ps%d' % pd], ['acc'])
                    else:
                        P.op('dve', lambda e, j=j, pd=pd, n=n, q0=q0: e.tensor_tensor(
                            out=acc[:, j, q0:q0 + n], in0=acc[:, j, q0:q0 + n], in1=ps[pd][:, 0:n], op=ALU.add),
                            ['ps%d' % pd, 'acc'], ['acc'])
                for f in nxt[bi * per:(bi + 1) * per]:
                    f()
        P.fence()
        A.release(mL)

        xr = [A.f32(8, 512)] * 2
        x2 = [A.f32(8, 512)] * 2
        on = A.f32(8, 512)
        sq2 = A.bf(8, 512)
        rstd2 = A.f32(512)
        tmp2 = [A.f32(512), A.f32(512)]
        for bi, (q0, n, w) in enumerate(lblocks):
            x, xk = xr[0], 'xr'
            y, yk = x2[0], 'x2'
            P.dma(x[:, :, 0:n], d_x1[:, :, q0:q0 + n], [], [xk])
            for j in range(8):
                P.op('dve', lambda e, j=j, n=n, w=w, q0=q0, x=x, y=y: e.scalar_tensor_tensor(
                    out=y[:, j, 0:n], in0=acc[:, j, q0:q0 + n], scalar=sv[:, 2, j:j + 1, w], in1=x[:, j, 0:n],
                    op0=ALU.mult, op1=ALU.add), [xk, 'acc', 'svt'], [yk])
            P.dma(o_x2[:, :, q0:q0 + n], y[:, :, 0:n], [yk], ['o_x2'])
            C.sumsq_rstd(y[:, :, 0:n], 8, n, D, rstd2, ones, sq2, [yk], 'fn')
            for k in range(8):
                e_ = C.ew()
                t = tmp2[k % 2]
                tk = 'fn_tmp%d' % (k % 2)
                P.op(e_, lambda e, k=k, t=t, n=n, y=y: e.tensor_tensor(out=t[:, 0:n], in0=y[:, k, 0:n], in1=rstd2[:, 0:n],
                                                                      op=ALU.mult), [yk, 'fn_rstd'], [tk])
                P.op(e_, lambda e, k=k, t=t, n=n: e.tensor_scalar(out=on[:, k, 0:n], in0=t[:, 0:n],
                                                                  scalar1=fng[:, k:k + 1], scalar2=None, op0=ALU.mult),
                     [tk, 'vecs'], ['on'])
            P.dma(o_on[:, :, q0:q0 + n], on[:, :, 0:n], ['on'], ['o_on'])
        P.finalize()
        return nc, P, A


def build_C2():
    nc = bass.Bass("TRN2", target_bir_lowering=False)

    def din(name, shape, dt=F32):
        return nc.dram_tensor(name, list(shape), dt, kind="ExternalInput").ap()
    d_x1 = din("x1T", [128, 8, NL])
    d_pA = din("probsA", [128, NLOC])
    d_pC = din("probsC", [16, CTX])
    d_gm = din("gmT", [16, NL])
    d_cvec = din("cvec", [128, 8, 2])
    d_wmod = din("wmod", [128, 8, 3072])
    d_bmod = din("bmod", [128, 24])
    d_vecs = din("vecs", [128, 16])
    d_G = din("G", [128, 128])
    d_oh = din("oh16", [16, 16])
    d_ident = din("ident", [128, 128])
    d_iotaf = din("iotaf", [128, 512])
    d_iotap = din("iotap", [128, 4])
    d_wg = din("wg", [NEXP, D, D])
    d_wu = din("wu", [NEXP, D, D])
    d_wd = din("wd", [NEXP, D, D])
    o_x2 = nc.dram_tensor("x2T", [128, 8, NL], F32, kind="ExternalOutput").ap()
    o_on = nc.dram_tensor("outN", [128, 8, NL], F32, kind="ExternalOutput").ap()

    AW = 51 * 1024
    with ExitStack() as es:
        arena_t = es.enter_context(nc.sbuf_tensor("arena", [128, AW], F32))
        ps = [es.enter_context(nc.psum_tensor("ps%d" % i, [128, 512], F32)) for i in range(7)]
        psb = es.enter_context(nc.psum_tensor("psb", [128, 1024], BF16))
        P = Prog(nc)
        A = Arena(arena_t, AW)
        C = Ctx(nc, P, A, ps, psb)

        vecs = A.f32(16)
        modT = A.f32(24, 2)
        cs = A.f32(64)
        sv = A.f32(3, 8, 2)
        G = A.f32(128)
        oh = A.f32(16)
        ones16 = A.f32(128)
        ones = A.bf(128)
        identb = A.bf(128)
        iotaf = A.f32(512)
        iotap = A.f32(4)
        ts = A.f32(16)
        m0 = A.mark()
        P.dma(vecs, d_vecs, [], ['vecs'])
        P.dma(G, d_G, [], ['consts'])
        P.dma(oh[0:16, :], d_oh, [], ['consts'])
        P.dma(iotaf, d_iotaf, [], ['consts'])
        P.dma(iotap, d_iotap, [], ['consts'])
        P.op('dve', lambda e: e.memset(ones, 1.0), [], ['consts'])
        P.op('dve', lambda e: e.memset(ones16, 1.0), [], ['consts'])
        stage8 = A.f32(8, 1024)
        stage8b = A.f32(8, 1024)
        C.compute_mods(d_cvec, d_wmod, d_bmod, 3, modT, cs, [stage8, stage8b], oh[0:2, 0:2])
        P.dma(stage8[:, 0, 0:128], d_ident, [], ['wm_st0a'])
        P.op('dve', lambda e: e.tensor_copy(out=identb, in_=stage8[:, 0, 0:128]), ['wm_st0a'], ['consts'])
        n2g, fng = vecs[:, 0:8], vecs[:, 8:16]
        mv = modT.rearrange("p (b k) w -> p b k w", b=3)
        for w in range(2):
            P.op('dve', lambda e, w=w: e.scalar_tensor_tensor(
                out=sv[:, 0, :, w], in0=mv[:, 1, :, w], scalar=1.0, in1=n2g, op0=ALU.add, op1=ALU.mult),
                ['modv', 'vecs'], ['svt'])
            P.op('dve', lambda e, w=w: e.tensor_copy(out=sv[:, 1, :, w], in_=mv[:, 0, :, w]), ['modv'], ['svt'])
            P.op('dve', lambda e, w=w: e.tensor_copy(out=sv[:, 2, :, w], in_=mv[:, 2, :, w]), ['modv'], ['svt'])
        P.fence()
        A.release(m0)

        NTT = NL // 128
        h2tok = A.bf(NTT, 1024)
        acc = A.f32(8, NL)
        gmT = A.f32(NL)
        ssel = A.f32(NL)
        slotT = A.f32(NTT, 16)
        mL = A.mark()
        lblocks = [(0, CTX, 1)] + [(CTX + 512 * i, 512, 0) for i in range(NLOC // 512)]

        xs = [A.f32(8, 512), A.f32(8, 512)]
        sq = A.bf(8, 512)
        rstd = A.f32(512)
        tmpf = [A.f32(512), A.f32(512)]
        h2b = [A.bf(8, 512), A.bf(8, 512)]
        for bi, (q0, n, w) in enumerate(lblocks):
            x = xs[bi % 2]
            xk = 'xs%d' % (bi % 2)
            P.dma(x[:, :, 0:n], d_x1[:, :, q0:q0 + n], [], [xk])
            C.sumsq_rstd(x[:, :, 0:n], 8, n, D, rstd, ones, sq, [xk], 'n2')
            hb, hk = h2b[bi % 2], 'h2b%d' % (bi % 2)
            C.norm_mod(x, n, rstd, sv[:, 0, :, w], sv[:, 1, :, w], hb, tmpf, [xk], [hk], 'n2')
            for tt in range(n // 128):
                for k in range(8):
                    P.tr(psb[:, k * 128:(k + 1) * 128], hb[:, k, tt * 128:(tt + 1) * 128], identb, [hk, 'consts'], ['psb'])
                P.act(h2tok[:, q0 // 128 + tt, :], psb[:, 0:1024], AF.Copy, ['psb'], ['h2tok'])
        P.fence()
        A.release(mL)

        pA = A.f32(NLOC)
        mk = A.f32(NLOC)
        pC = A.f32(CTX)
        mkc = A.f32(CTX)
        P.dma(pA, d_pA, [], ['pA'])
        P.dma(pC[0:16, :], d_pC, [], ['pC'])
        P.dma(gmT[0:16, :], d_gm, [], ['gmT'])
        lo, hi, mid, tot, gt, dd, Kv = (ts[:, 0:2], ts[:, 2:4], ts[:, 4:6], ts[:, 6:8], ts[:, 8:10], ts[:, 10:12],
                                        ts[:, 12:14])
        P.op('dve', lambda e: e.memset(ts, 0.0), [], ['ts'])
        P.op('dve', lambda e: e.memset(hi, 1.0), ['ts'], ['ts'])
        P.op('dve', lambda e: e.memset(mid, 0.5), ['ts'], ['ts'])
        P.op('dve', lambda e: e.memset(ts[:, 12:13], NLOC - 0.5), ['ts'], ['ts'])
        P.op('dve', lambda e: e.memset(ts[:, 13:14], 2 * CTX // NEXP - 0.5), ['ts'], ['ts'])
        for it in range(NBIS):
            P.op('dve', lambda e: e.tensor_scalar(out=mk, in0=pA, scalar1=ts[:, 4:5], scalar2=None, op0=ALU.is_gt),
                 ['pA', 'ts'], ['mk'])
            P.op('dve', lambda e: e.reduce_sum(out=ts[:, 14:15], in_=mk, axis=mybir.AxisListType.X), ['mk'], ['cc'])
            P.op('dve', lambda e: e.tensor_scalar(out=mkc[0:16, :], in0=pC[0:16, :], scalar1=ts[0:16, 5:6], scalar2=None,
                                                  op0=ALU.is_gt), ['pC', 'ts'], ['mkc'])
            P.op('dve', lambda e: e.reduce_sum(out=ts[0:16, 7:8], in_=mkc[0:16, :], axis=mybir.AxisListType.X),
                 ['mkc', 'ts'], ['ts'])
            P.mm(ps[6][:, 0:1], G, ts[:, 14:15], True, True, ['cc', 'consts'], ['ps6'])
            P.op('dve', lambda e: e.tensor_copy(out=ts[:, 6:7], in_=ps[6][:, 0:1]), ['ps6', 'ts'], ['ts'])
            P.op('dve', lambda e: e.tensor_tensor(out=gt, in0=tot, in1=Kv, op=ALU.is_gt), ['ts'], ['ts'])
            P.op('dve', lambda e: e.tensor_tensor(out=dd, in0=mid, in1=lo, op=ALU.subtract), ['ts'], ['ts'])
            P.op('dve', lambda e: e.tensor_tensor(out=dd, in0=dd, in1=gt, op=ALU.mult), ['ts'], ['ts'])
            P.op('dve', lambda e: e.tensor_tensor(out=lo, in0=lo, in1=dd, op=ALU.add), ['ts'], ['ts'])
            P.op('dve', lambda e: e.tensor_tensor(out=dd, in0=hi, in1=mid, op=ALU.subtract), ['ts'], ['ts'])
            P.op('dve', lambda e: e.tensor_tensor(out=dd, in0=dd, in1=gt, op=ALU.mult), ['ts'], ['ts'])
            P.op('dve', lambda e: e.tensor_tensor(out=hi, in0=mid, in1=dd, op=ALU.add), ['ts'], ['ts'])
            P.op('dve', lambda e: e.tensor_tensor(out=dd, in0=lo, in1=hi, op=ALU.add), ['ts'], ['ts'])
            P.op('dve', lambda e: e.tensor_single_scalar(out=mid, in_=dd, scalar=0.5, op=ALU.mult), ['ts'], ['ts'])
        P.op('dve', lambda e: e.tensor_scalar(out=mk[0:16, 0:NLOC], in0=gmT[0:16, CTX:NL], scalar1=ts[0:16, 2:3],
                                              scalar2=None, op0=ALU.is_gt), ['gmT', 'ts'], ['mk'])
        P.op('dve', lambda e: e.tensor_tensor(out=gmT[0:16, CTX:NL], in0=gmT[0:16, CTX:NL], in1=mk[0:16, 0:NLOC],
                                              op=ALU.mult), ['mk', 'gmT'], ['gmT'])
        P.op('dve', lambda e: e.tensor_scalar(out=mkc[0:16, :], in0=gmT[0:16, 0:CTX], scalar1=ts[0:16, 3:4],
                                              scalar2=None, op0=ALU.is_gt), ['gmT', 'ts'], ['mkc'])
        P.op('dve', lambda e: e.tensor_tensor(out=gmT[0:16, 0:CTX], in0=gmT[0:16, 0:CTX], in1=mkc[0:16, :],
                                              op=ALU.mult), ['mkc', 'gmT'], ['gmT'])
        mrow = A.f32(NL)
        orow = A.f32(NL)
        P.op('dve', lambda e: e.tensor_copy(out=mrow[0:16, CTX:NL], in_=mk[0:16, 0:NLOC]), ['mk'], ['mrow'])
        P.op('dve', lambda e: e.tensor_copy(out=mrow[0:16, 0:CTX], in_=mkc[0:16, :]), ['mkc'], ['mrow'])
        P.op('dve', lambda e: e.memset(orow[0:16, :], 1.0), [], ['orow'])
        P.op('dve', lambda e: e.tensor_tensor_scan(out=ssel[0:16, :], data0=orow[0:16, :], data1=mrow[0:16, :],
                                                   initial=0.0, op0=ALU.mult, op1=ALU.add), ['orow', 'mrow'], ['ssel'])
        P.op('dve', lambda e: e.tensor_tensor(out=ssel[0:16, :], in0=ssel[0:16, :], in1=mrow[0:16, :], op=ALU.mult),
             ['ssel', 'mrow'], ['ssel'])
        for tt in range(NTT):
            P.tr(ps[6][:, 0:16], ssel[0:16, tt * 128:(tt + 1) * 128], oh[0:16, :], ['ssel', 'consts'], ['ps6'])
            P.op('dve', lambda e, tt=tt: e.tensor_copy(out=slotT[:, tt, :], in_=ps[6][:, 0:16]), ['ps6'], ['slotT'])
        P.fence()
        A.release(mL)

        CS = 512
        QF = 256
        NRING = 3
        wgu = [dict(g=A.bf(8, QF), u=A.bf(8, QF)) for _ in range(NRING)]
        wd = A.bf(8, 1024)
        SS = [A.bf(CS) for _ in range(4)]
        SS2 = [A.bf(CS) for _ in range(4)]
        xsT = A.bf(8, CS)
        ye = xsT.rearrange("p (a b) c -> p a (b c)", a=4)
        hid = A.bf(8, CS)
        sg = [A.bf(CS), A.bf(CS)]
        gmb = A.f32(512)
        gme = A.f32(512)
        sse = A.f32(512)

        def load_gu(ex, q, slot):
            wb, tag = wgu[slot], 'wgu%d' % slot
            for nm, dsrc in (('g', d_wg), ('u', d_wu)):
                P.dma(wb[nm], dsrc[ex].rearrange("(k p) f -> p k f", p=128)[:, :, q * QF:(q + 1) * QF], [], [tag],
                      eng='pool')

        def load_d(ex):
            for hh in range(2):
                P.dma(wd[:, :, hh * 512:(hh + 1) * 512],
                      d_wd[ex].rearrange("(k p) f -> p k f", p=128)[:, :, hh * 512:(hh + 1) * 512], [], ['wd'], eng='pool')
        usl = [0]
        for q0_ in range(NRING):
            load_gu(0, q0_, q0_)
        for ex in range(NEXP):
            si = 0
            for dh in range(2):
                for tt in range(NTT):
                    S_, Sk = SS[si % 4], 'SS%d' % (si % 4)
                    si += 1
                    P.op('dve', lambda e, S_=S_, tt=tt, ex=ex: e.tensor_scalar(
                        out=S_, in0=iotaf, scalar1=slotT[:, tt, ex:ex + 1], scalar2=None, op0=ALU.is_equal),
                        ['consts', 'slotT'], [Sk])
                    for i in range(4):
                        dci = dh * 4 + i
                        P.mm(ps[i][:, 0:CS], h2tok[:, tt, dci * 128:(dci + 1) * 128], S_, tt == 0, tt == NTT - 1,
                             ['h2tok', Sk], ['ps%d' % i])
                for i in range(4):
                    P.act(xsT[:, dh * 4 + i, :], ps[i][:, 0:CS], AF.Copy, ['ps%d' % i], ['xsye'])
            if ex == 0:
                load_d(0)
            for q in range(4):
                slot = (ex * 4 + q) % NRING
                wb, tag = wgu[slot], 'wgu%d' % slot
                for f2 in range(2):
                    f = q * 2 + f2
                    pg, pu = f % 2, 2 + f % 2
                    for k in range(8):
                        P.mm(ps[pg][:, 0:CS], wb['g'][:, k, f2 * 128:(f2 + 1) * 128], xsT[:, k, :], k == 0, k == 7,
                             [tag, 'xsye'], ['ps%d' % pg])
                    for k in range(8):
                        P.mm(ps[pu][:, 0:CS], wb['u'][:, k, f2 * 128:(f2 + 1) * 128], xsT[:, k, :], k == 0, k == 7,
                             [tag, 'xsye'], ['ps%d' % pu])
                    s_ = sg[f % 2]
                    P.act(s_, ps[pg][:, 0:CS], AF.Silu, ['ps%d' % pg], ['sg%d' % (f % 2)])
                    P.op('dve', lambda e, s_=s_, f=f, pu=pu: e.tensor_tensor(out=hid[:, f, :], in0=s_, in1=ps[pu][:, 0:CS],
                                                                          op=ALU.mult),
                         ['sg%d' % (f % 2), 'ps%d' % pu], ['hid'])
                nq = ex * 4 + q + NRING
                if nq < NEXP * 4:
                    load_gu(nq // 4, nq % 4, slot)
            def prep_scatter(bi_, ex=ex):
                q0_, n_, _ = lblocks[bi_]
                gi_ = ex * len(lblocks) + bi_
                par = gi_ % 2
                tiles_ = SS if par == 0 else SS2
                P.op('dve', lambda e: e.tensor_scalar(out=sse[0:16, 0:n_], in0=ssel[0:16, q0_:q0_ + n_],
                                                      scalar1=oh[0:16, ex:ex + 1], scalar2=None, op0=ALU.mult),
                     ['ssel', 'consts'], ['sse'])
                P.op('dve', lambda e: e.tensor_scalar(out=gme[0:16, 0:n_], in0=gmT[0:16, q0_:q0_ + n_],
                                                      scalar1=oh[0:16, ex:ex + 1], scalar2=None, op0=ALU.mult),
                     ['gmT', 'consts'], ['gme'])
                P.mm(ps[5][:, 0:n_], ones16[0:16, :], sse[0:16, 0:n_], True, True, ['sse', 'consts'], ['ps5'])
                P.mm(ps[6][:, 0:n_], ones16[0:16, :], gme[0:16, 0:n_], True, True, ['gme', 'consts'], ['ps6'])
                P.act(gmb[:, 0:n_], ps[6][:, 0:n_], AF.Copy, ['ps6'], ['gmb'])
                for st_ in range(4):
                    P.op('dve', lambda e, st_=st_: e.scalar_tensor_tensor(
                        out=tiles_[st_][:, 0:n_], in0=ps[5][:, 0:n_], scalar=iotap[:, st_:st_ + 1], in1=gmb[:, 0:n_],
                        op0=ALU.is_equal, op1=ALU.mult), ['ps5', 'gmb', 'consts'],
                        ['SS%d' % st_ if par == 0 else 'SSb%d' % st_])
            prep_scatter(0)
            for st_ in range(4):
                for hh in range(2):
                    for f in range(8):
                        P.mm(ps[4][:, 0:512], hid[:, f, st_ * 128:(st_ + 1) * 128], wd[:, f, hh * 512:(hh + 1) * 512],
                             f == 0, f == 7, ['hid', 'wd'], ['ps4'])
                    P.act(ye[:, st_, hh * 512:(hh + 1) * 512], ps[4][:, 0:512], AF.Copy, ['ps4'], ['xsye'])
            if ex + 1 < NEXP:
                load_d(ex + 1)
            for bi_, (q0, n, w) in enumerate(lblocks):
                gi_ = ex * len(lblocks) + bi_
                par = gi_ % 2
                tiles_ = SS if par == 0 else SS2
                if bi_ + 1 < len(lblocks):
                    prep_scatter(bi_ + 1)
                for j in range(8):
                    pi = j % 4
                    for st_ in range(4):
                        P.mm(ps[pi][:, 0:n], ye[:, st_, j * 128:(j + 1) * 128], tiles_[st_][:, 0:n], st_ == 0, st_ == 3,
                             ['xsye', 'SS%d' % st_ if par == 0 else 'SSb%d' % st_], ['ps%d' % pi])
                    if ex == 0:
                        P.op('dve', lambda e, j=j, pi=pi, n=n, q0=q0: e.tensor_copy(out=acc[:, j, q0:q0 + n],
                                                                                   in_=ps[pi][:, 0:n]),
                             ['ps%d' % pi], ['acc'])
                    else:
                        P.op('dve', lambda e, j=j, pi=pi, n=n, q0=q0: e.tensor_tensor(
                            out=acc[:, j, q0:q0 + n], in0=acc[:, j, q0:q0 + n], in1=ps[pi][:, 0:n], op=ALU.add),
                            ['ps%d' % pi, 'acc'], ['acc'])
        P.fence()
        A.release(mL)

        xr = [A.f32(8, 512)] * 2
        x2 = [A.f32(8, 512)] * 2
        on = A.f32(8, 512)
        sq2 = A.bf(8, 512)
        rstd2 = A.f32(512)
        tmp2 = [A.f32(512), A.f32(512)]
        for bi, (q0, n, w) in enumerate(lblocks):
            x, xk = xr[0], 'xr'
            y, yk = x2[0], 'x2'
            P.dma(x[:, :, 0:n], d_x1[:, :, q0:q0 + n], [], [xk])
            for j in range(8):
                P.op('dve', lambda e, j=j, n=n, w=w, q0=q0, x=x, y=y: e.scalar_tensor_tensor(
                    out=y[:, j, 0:n], in0=acc[:, j, q0:q0 + n], scalar=sv[:, 2, j:j + 1, w], in1=x[:, j, 0:n],
                    op0=ALU.mult, op1=ALU.add), [xk, 'acc', 'svt'], [yk])
            P.dma(o_x2[:, :, q0:q0 + n], y[:, :, 0:n], [yk], ['o_x2'])
            C.sumsq_rstd(y[:, :, 0:n], 8, n, D, rstd2, ones, sq2, [yk], 'fn')
            for k in range(8):
                e_ = C.ew()
                t = tmp2[k % 2]
                tk = 'fn_tmp%d' % (k % 2)
                P.op(e_, lambda e, k=k, t=t, n=n, y=y: e.tensor_tensor(out=t[:, 0:n], in0=y[:, k, 0:n], in1=rstd2[:, 0:n],
                                                                      op=ALU.mult), [yk, 'fn_rstd'], [tk])
                P.op(e_, lambda e, k=k, t=t, n=n: e.tensor_scalar(out=on[:, k, 0:n], in0=t[:, 0:n],
                                                                  scalar1=fng[:, k:k + 1], scalar2=None, op0=ALU.mult),
                     [tk, 'vecs'], ['on'])
            P.dma(o_on[:, :, q0:q0 + n], on[:, :, 0:n], ['on'], ['o_on'])
        P.finalize()
        return nc, P, A


def prep_C(inp, l, resAB):
    G = (np.arange(128)[:, None] % 16 == np.arange(128)[None, :] % 16).astype(np.float32)
    cvec = np.ascontiguousarray(np.stack([_fmv(inp['c'][0]), _fmv(inp['c_ctx'])], axis=-1))
    vecs = np.zeros((128, 16), np.float32)
    vecs[:, 0:8] = _fmv(inp['norm2_g'][l])
    vecs[:, 8:16] = _fmv(inp['final_norm_g'])
    pA = np.ascontiguousarray(np.stack([r['probs'][CTX:].T for r in resAB], axis=0).reshape(128, NLOC))
    pC = np.ascontiguousarray(resAB[0]['probs'][:CTX].T)
    iotaf = np.ascontiguousarray(np.broadcast_to(np.arange(1, 513, dtype=np.float32)[None, :], (128, 512)))
    iotap = (np.arange(128, dtype=np.float32)[:, None] + 1 + 128 * np.arange(4, dtype=np.float32)[None, :]).astype(np.float32)
    common = dict(ident=np.eye(128, dtype=np.float32), iotaf=iotaf, iotap=iotap, cvec=cvec, wmod=_fm(np.ascontiguousarray(inp['w_mod'][l][:, 3072:6144])),
                  bmod=_fmv(inp['b_mod'][l][3072:6144]), vecs=vecs, G=G, oh16=np.eye(16, dtype=np.float32),
                  probsA=pA, probsC=pC, wg=inp['w_gate'][l], wu=inp['w_up'][l], wd=inp['w_down'][l])
    maps = []
    for c in range(NCORES):
        m = dict(common)
        m['x1T'] = resAB[c]['x1T']
        m['gmT'] = np.ascontiguousarray(resAB[c]['probs'].T)
        maps.append(m)
    return maps


def kernel(**inp):
    inp = {k: np.asarray(v) for k, v in inp.items()}
    ncAB = build_AB()[0]
    ncC = build_C2()[0]
    xl, xc = inp['x'][0], inp['ctx'][0]
    out = None
    for l in range(2):
        resAB = run_bass_kernel_spmd(ncAB, prep_AB(inp, l, xl, xc), core_ids=list(range(NCORES))).results
        resC = run_bass_kernel_spmd(ncC, prep_C(inp, l, resAB), core_ids=list(range(NCORES))).results
        xl = np.concatenate([_unfm(r['x2T'][:, :, CTX:]) for r in resC], axis=0)
        xc = _unfm(resC[0]['x2T'][:, :, :CTX])
        out = np.concatenate([_unfm(r['outN'][:, :, CTX:]) for r in resC], axis=0)
    return np.ascontiguousarray(out[None].astype(np.float32))
```

```python
import numpy as np
import ml_dtypes
from contextlib import ExitStack
import concourse.bass as bass
import concourse.mybir as mybir
from concourse.bass_utils import run_bass_kernel_spmd

F32 = mybir.dt.float32
BF16 = mybir.dt.bfloat16
AF = mybir.ActivationFunctionType
ALU = mybir.AluOpType

NCORES = 8
D = 1024
SEQ = 16384
CTX = 256
TALL = SEQ + CTX
NLOC = SEQ // NCORES
NL = NLOC + CTX
EPS = 1e-6
NEXP = 16
ATTN_SCALE = 192.0 ** -0.5
ENGS = ['pe', 'act', 'dve', 'pool', 'sp']


def _prod(s):
    r = 1
    for v in s:
        r *= v
    return r


class Prog:
    def __init__(self, nc, ndma=12):
        self.nc = nc
        self.ops = []
        self.lastw = {}
        self.readers = {}
        self.ndma = ndma

    capture = None

    def begin_capture(self):
        self.capture = []

    def end_capture(self):
        c, self.capture = self.capture, None
        return c

    def replay_interleaved(self, lists, nway=2):
        L = max(len(l) for l in lists)
        stride = (L + nway - 1) // nway
        items = []
        for i, l in enumerate(lists):
            for k, call in enumerate(l):
                items.append((i * stride + k, i, call))
        items.sort(key=lambda t: (t[0], t[1]))
        for _, _, call in items:
            self.op(*call)

    def op(self, eng, fn, reads=(), writes=(), dma=False):
        if self.capture is not None:
            self.capture.append((eng, fn, tuple(reads), tuple(writes), dma))
            return -1
        i = len(self.ops)
        deps = set()
        for k in reads:
            if k in self.lastw:
                deps.add(self.lastw[k])
        for k in writes:
            if k in self.lastw:
                deps.add(self.lastw[k])
            deps.update(self.readers.get(k, ()))
        for k in reads:
            self.readers.setdefault(k, []).append(i)
        for k in writes:
            self.lastw[k] = i
            self.readers[k] = []
        self.ops.append(dict(eng=eng, fn=fn, deps=deps, dma=dma))
        return i

    def fence(self):
        self.ops.append(dict(eng=None, fence=True))
        self.lastw = {}
        self.readers = {}

    def mm(self, out, lhsT, rhs, start, stop, reads, writes, tp=None):
        if tp is None:
            self.op('pe', lambda e: e.matmul(out, lhsT, rhs, start=start, stop=stop), reads, writes)
        else:
            self.op('pe', lambda e: e.matmul(out, lhsT, rhs, start=start, stop=stop, tile_position=tp), reads, writes)

    def tr(self, out, in_, ident, reads, writes):
        self.op('pe', lambda e: e.transpose(out, in_, ident), reads, writes)

    def act(self, out, in_, func, reads, writes, bias=None, scale=None, accum=None):
        kw = {}
        if bias is not None:
            kw['bias'] = bias
        if scale is not None:
            kw['scale'] = scale
        if accum is not None:
            kw['accum_out'] = accum
        self.op('act', lambda e: e.activation(out=out, in_=in_, func=func, **kw), reads, writes)

    def dma(self, out, in_, reads, writes, eng='sp'):
        self.op(eng, lambda e: e.dma_start(out=out, in_=in_), reads, writes, dma=True)

    def finalize(self):
        self.fence()
        nc, ops, ndma = self.nc, self.ops, self.ndma
        need = set()
        last = {}
        for i, o in enumerate(ops):
            if o.get('fence'):
                for e, j in last.items():
                    need.add(j)
                continue
            for d in o['deps']:
                Dd = ops[d]
                if Dd['dma']:
                    continue
                if Dd['eng'] == 'pe' and o['eng'] == 'pe' and not o['dma']:
                    continue
                need.add(d)
            if not o['dma']:
                last[o['eng']] = i
        cnt = {e: 0 for e in ENGS}
        dcnt = [0] * ndma
        di = 0
        for i, o in enumerate(ops):
            if o.get('fence'):
                o['snap_cnt'] = dict(cnt)
                o['snap_d'] = list(dcnt)
                continue
            if o['dma']:
                j = di % ndma
                di += 1
                o['dsem'] = j
                o['dprev'] = dcnt[j]
                dcnt[j] += 16
                o['dval'] = dcnt[j]
            elif i in need:
                cnt[o['eng']] += 1
                o['sig'] = cnt[o['eng']]
        self.n_ops = len(ops)
        with ExitStack() as es:
            esem = {e: es.enter_context(nc.semaphore("s_" + e)) for e in ENGS}
            dsem = [es.enter_context(nc.semaphore("d_%d" % j)) for j in range(ndma)]
            block = es.enter_context(nc.Block())

            def emit(ename):
                def body(eng):
                    waited = {}

                    def wait(key, sem, val):
                        if val > waited.get(key, 0):
                            eng.wait_ge(sem, val)
                            waited[key] = val
                    for i, o in enumerate(ops):
                        if o.get('fence'):
                            for e2 in ENGS:
                                if o['snap_cnt'][e2] > 0:
                                    wait(e2, esem[e2], o['snap_cnt'][e2])
                            for j in range(ndma):
                                if o['snap_d'][j] > 0:
                                    wait(('d', j), dsem[j], o['snap_d'][j])
                            continue
                        if o['eng'] != ename:
                            continue
                        for d in sorted(o['deps']):
                            Dd = ops[d]
                            if Dd['dma']:
                                wait(('d', Dd['dsem']), dsem[Dd['dsem']], Dd['dval'])
                            else:
                                if Dd['eng'] == 'pe' and ename == 'pe' and not o['dma']:
                                    continue
                                wait(Dd['eng'], esem[Dd['eng']], Dd['sig'])
                        if o['dma'] and o['dprev'] > 0:
                            wait(('d', o['dsem']), dsem[o['dsem']], o['dprev'])
                        ins = o['fn'](eng)
                        if o['dma']:
                            ins.then_inc(dsem[o['dsem']], 16)
                        elif 'sig' in o:
                            ins.then_inc(esem[ename], 1)
                return body
            block.tensor(emit('pe'))
            block.scalar(emit('act'))
            block.vector(emit('dve'))
            block.gpsimd(emit('pool'))
            block.sync(emit('sp'))


class Arena:
    def __init__(self, t, width):
        self.t = t
        self.top = 0
        self.width = width
        self.peak = 0

    def _shape(self, v, shape):
        if len(shape) == 1:
            return v
        if len(shape) == 2:
            return v.rearrange("p (a b) -> p a b", a=shape[0])
        if len(shape) == 3:
            return v.rearrange("p (a b c) -> p a b c", a=shape[0], b=shape[1])
        raise ValueError

    def f32(self, *shape):
        n = _prod(shape)
        a = self.top
        self.top += n
        self.peak = max(self.peak, self.top)
        assert self.top <= self.width, ("arena overflow", self.top, self.width)
        return self._shape(self.t[:, a:a + n], shape)

    def bf(self, *shape):
        n = _prod(shape)
        nw = (n + 1) // 2
        a = self.top
        self.top += nw
        self.peak = max(self.peak, self.top)
        assert self.top <= self.width, ("arena overflow", self.top, self.width)
        v = self.t[:, a:a + nw].bitcast(BF16)
        return self._shape(v[:, 0:n], shape)

    def mark(self):
        return self.top

    def release(self, m):
        self.top = m


class Ctx:
    def __init__(self, nc, P, A, ps, psb):
        self.nc, self.P, self.A, self.ps, self.psb = nc, P, A, ps, psb
        self.psi = 0
        self.alt = 0
        self.uid = 0

    def key(self, s):
        self.uid += 1
        return "%s#%d" % (s, self.uid)

    ps_range = (0, 6)

    def next_ps(self, lo=None, hi=None):
        if lo is None:
            lo, hi = self.ps_range
        i = lo + self.psi % (hi - lo)
        self.psi += 1
        return i

    def ew(self):
        self.alt += 1
        return 'pool' if self.alt % 4 == 0 else 'dve'

    def load_cast(self, dst_bf, src_dram, ncols, nk, name, scale_col=None, stage=None):
        P = self.P
        for k in range(nk):
            st, sk = stage[k % 2]
            P.dma(st[:, 0:ncols], src_dram[:, k, :], reads=[], writes=[sk])
            e = self.ew()
            if scale_col is None:
                P.op(e, lambda en, st=st, k=k: en.tensor_copy(out=dst_bf[:, k, :], in_=st[:, 0:ncols]),
                     reads=[sk], writes=[name])
            else:
                P.op(e, lambda en, st=st, k=k: en.tensor_scalar(out=dst_bf[:, k, :], in0=st[:, 0:ncols],
                                                                scalar1=scale_col[:, k:k + 1], scalar2=None,
                                                                op0=ALU.mult),
                     reads=[sk, 'modv'], writes=[name])

    def sumsq_rstd(self, src_f32, nk, n, dim, out_rstd, ones_bf, sq_bf, rk, name):
        P = self.P
        P.act(sq_bf[:, 0:nk, 0:n], src_f32, AF.Square, reads=rk, writes=[name + '_sq'])
        pi = self.next_ps()
        pk = 'ps%d' % pi
        for k in range(nk):
            P.mm(self.ps[pi][:, 0:n], ones_bf, sq_bf[:, k, 0:n], k == 0, k == nk - 1,
                 reads=[name + '_sq', 'consts'], writes=[pk])
        P.act(out_rstd[:, 0:n], self.ps[pi][:, 0:n], AF.Ln, reads=[pk], writes=[name + '_rstd'], scale=1.0 / dim, bias=EPS)
        P.act(out_rstd[:, 0:n], out_rstd[:, 0:n], AF.Exp, reads=[name + '_rstd'], writes=[name + '_rstd'], scale=-0.5)

    def norm_mod(self, x_f32, n, rstd, s_col, sh_col, out, tmp_f32, rk, wk, name):
        P = self.P
        for k in range(8):
            e = self.ew()
            tk = "%s_tmp%d" % (name, k % 2)
            t = tmp_f32[k % 2]
            P.op(e, lambda en, k=k, t=t: en.tensor_tensor(out=t[:, 0:n], in0=x_f32[:, k, 0:n], in1=rstd[:, 0:n],
                                                          op=ALU.mult),
                 reads=rk + [name + '_rstd'], writes=[tk])
            P.op(e, lambda en, k=k, t=t: en.tensor_scalar(out=out[:, k, 0:n], in0=t[:, 0:n],
                                                          scalar1=s_col[:, k:k + 1], scalar2=sh_col[:, k:k + 1],
                                                          op0=ALU.mult, op1=ALU.add),
                 reads=[tk, 'modv'], writes=wk)

    def compute_mods(self, d_cvec, d_wmod, d_bmod, nblk, modT, cs, stage, id2):
        P = self.P
        P.dma(cs[:, 0:16], d_cvec.rearrange("p k w -> p (k w)"), reads=[], writes=['cs'])
        P.act(cs[:, 16:32], cs[:, 0:16], AF.Sigmoid, reads=['cs'], writes=['cs2'])
        P.op('dve', lambda e: e.tensor_tensor(out=cs[:, 0:16], in0=cs[:, 0:16], in1=cs[:, 16:32], op=ALU.mult),
             reads=['cs2', 'cs'], writes=['cs'])
        P.dma(cs[:, 32:32 + nblk * 8], d_bmod, reads=[], writes=['bmod'])
        csv = cs[:, 0:16].rearrange("p (k w) -> p k w", k=8)
        nj = nblk * 8
        rb = [self.A.f32(512), self.A.f32(512)]
        ri = 0
        for b in range(nblk):
            stg, sk = stage[b % 2], 'wm_st%d' % (b % 2)
            for hk in range(2):
                P.dma(stg[:, hk * 4:(hk + 1) * 4, :], d_wmod[:, hk * 4:(hk + 1) * 4, b * 1024:(b + 1) * 1024], reads=[],
                      writes=[sk + 'ab'[hk]], eng=('sp' if hk == 0 else 'pool'))
            for hf in range(2):
                for k in range(8):
                    P.mm(self.ps[5][0:2, 0:512], csv[:, k, :], stg[:, k, hf * 512:(hf + 1) * 512], k == 0, k == 7,
                         reads=[sk + 'ab'[k // 4], 'cs'], writes=['ps5'])
                r_, rk = rb[ri % 2], 'mrow%d' % (ri % 2)
                ri += 1
                P.op('dve', lambda e, r_=r_: e.tensor_copy(out=r_[0:2, :], in_=self.ps[5][0:2, 0:512]), ['ps5'], [rk])
                for j4 in range(4):
                    c0 = 2 * (b * 8 + hf * 4 + j4)
                    P.tr(self.ps[6][:, c0:c0 + 2], r_[0:2, j4 * 128:(j4 + 1) * 128], id2, [rk, 'id2', 'consts'], ['ps6'])
        psv = self.ps[6][:, 0:2 * nj].rearrange("p (j w) -> p j w", w=2)
        for w in range(2):
            P.op('dve', lambda e, w=w: e.tensor_tensor(out=modT[:, 0:nj, w], in0=psv[:, :, w],
                                                       in1=cs[:, 32:32 + nj], op=ALU.add),
                 reads=['ps6', 'bmod'], writes=['modv'])


def build_AB():
    nc = bass.Bass("TRN2", target_bir_lowering=False)

    def din(name, shape, dt=F32):
        return nc.dram_tensor(name, list(shape), dt, kind="ExternalInput").ap()
    d_xall = din("xall", [128, 8, TALL])
    d_xloc = din("xloc", [128, 8, NLOC])
    d_cvec = din("cvec", [128, 8, 2])
    d_wmod = din("wmod", [128, 8, 5120])
    d_bmod = din("bmod", [128, 40])
    d_vecs = din("vecs", [128, 64])
    d_wA = din("wA", [128, 8, 640])
    d_wB = din("wB", [128, 8, 1152])
    d_wuq = din("wuq", [128, 3, 1024])
    d_wukv = din("wukv", [128, 2, 1024])
    d_wout = din("wout", [128, 8, 1024])
    d_lruw = din("lruw", [128, 8, 128])
    d_sguw = din("sguw", [128, 4, 128])
    d_sgub = din("sgub", [128, 2, 128])
    d_wr = din("wr", [128, 8, 16])
    d_ropeC = din("ropeC", [64, SEQ])
    d_ropeS = din("ropeS", [64, SEQ])
    d_ropeCl = din("ropeCl", [64, NLOC])
    d_ropeSl = din("ropeSl", [64, NLOC])
    d_ident = din("ident", [128, 128])
    o_x1 = nc.dram_tensor("x1T", [128, 8, NL], F32, kind="ExternalOutput").ap()
    o_probs = nc.dram_tensor("probs", [NL, NEXP], F32, kind="ExternalOutput").ap()
    s_xb = nc.dram_tensor("xb_s", [2, 128, TALL], F32).ap()
    s_KT = nc.dram_tensor("KT_s", [4, 128, TALL], BF16).ap()
    s_kr = nc.dram_tensor("kr_s", [64, TALL], BF16).ap()
    s_V = nc.dram_tensor("V_s", [4, 128, TALL // 128, 128], BF16).ap()

    AW = 51 * 1024
    with ExitStack() as es:
        arena_t = es.enter_context(nc.sbuf_tensor("arena", [128, AW], F32))
        ps = [es.enter_context(nc.psum_tensor("ps%d" % i, [128, 512], F32)) for i in range(7)]
        psb = es.enter_context(nc.psum_tensor("psb", [128, 1024], BF16))
        P = Prog(nc)
        A = Arena(arena_t, AW)
        C = Ctx(nc, P, A, ps, psb)

        vecs = A.f32(64)
        modT = A.f32(40, 2)
        cs = A.f32(80)
        sv = A.f32(6, 8, 2)
        spv = A.f32(4)
        ident = A.bf(128)
        ones = A.bf(128)
        onesf = A.f32(128)
        m0 = A.mark()
        P.dma(vecs, d_vecs, [], ['vecs'])
        P.op('pool', lambda e: e.memset(onesf, 1.0), [], ['onesf'])
        stage8 = A.f32(8, 1024)
        stage8b = A.f32(8, 1024)
        id2 = A.f32(2)
        P.dma(id2[0:2, :], d_ident[0:2, 0:2], [], ['id2'])
        C.compute_mods(d_cvec, d_wmod, d_bmod, 5, modT, cs, [stage8, stage8b], id2[0:2, :])
        P.dma(stage8[:, 0, 0:128], d_ident, [], ['wm_st0a'])
        P.op('dve', lambda e: e.tensor_copy(out=ident, in_=stage8[:, 0, 0:128]), ['wm_st0a'], ['consts'])
        P.op('dve', lambda e: e.memset(ones, 1.0), [], ['consts'])
        n1g, n2g = vecs[:, 0:8], vecs[:, 8:16]
        lrub, lam, convw, convb = vecs[:, 16:24], vecs[:, 24:28], vecs[:, 28:36], vecs[:, 36:38]
        qng, kvng, cmask = vecs[:, 38:41], vecs[:, 41:43], vecs[:, 43:51]
        mv = modT.rearrange("p (b k) w -> p b k w", b=5)
        for w in range(2):
            for (dst, scb, g) in ((0, 1, n1g), (3, 4, n2g)):
                P.op('dve', lambda e, w=w, dst=dst, scb=scb, g=g: e.scalar_tensor_tensor(
                    out=sv[:, dst, :, w], in0=mv[:, scb, :, w], scalar=1.0, in1=g, op0=ALU.add, op1=ALU.mult),
                    ['modv', 'vecs'], ['svt'])
            for (dst, src) in ((1, 0), (2, 2), (4, 3)):
                P.op('dve', lambda e, w=w, dst=dst, src=src: e.tensor_copy(out=sv[:, dst, :, w], in_=mv[:, src, :, w]),
                     ['modv'], ['svt'])
        P.act(spv, lam, AF.Exp, ['vecs'], ['spv'], scale=-1.0)
        P.act(spv, spv, AF.Ln, ['spv'], ['spv'], bias=1.0)
        P.op('dve', lambda e: e.tensor_single_scalar(out=spv, in_=spv, scalar=-8.0, op=ALU.mult), ['spv'], ['spv'])
        P.fence()
        A.release(m0)

        def svc(idx, w):
            return sv[:, idx, :, w]

        m1 = A.mark()
        wA_bf = A.bf(8, 640)
        wukv_bf = A.bf(2, 1024)
        wst = [(A.f32(1024), 'wst0'), (A.f32(1024), 'wst1')]

        def p1set():
            return dict(xs=A.f32(8, 512), sq=A.bf(8, 512), rstd=A.f32(512), tmpf=[A.f32(512), A.f32(512)],
                        h=A.bf(8, 512), xo=A.f32(2, 512), ckv=A.f32(2, 512), ckv_sq=A.bf(2, 512), rstd2=A.f32(512),
                        ckvn=A.bf(2, 512), Ko=A.bf(4, 512), Vo=A.bf(4, 4, 128), krf=A.f32(2, 512), rC=A.f32(512),
                        rS=A.f32(512), ko=A.bf(512))
        B1 = [p1set(), p1set()]
        X3 = [B1[0]['xs'], B1[1]['xs'], A.f32(8, 512)]
        C.load_cast(wukv_bf, d_wukv, 1024, 2, 'wukv', stage=wst)
        C.load_cast(wA_bf, d_wA, 640, 8, 'wA', scale_col=None, stage=wst)
        blocks = [(0, CTX, 1)] + [(CTX + 512 * i, 512, 0) for i in range(SEQ // 512)]
        caps = []
        for bi, (t0, n, w) in enumerate(blocks):
            B = B1[bi % 2]
            sx = str(bi % 2)
            C.ps_range = (0, 3) if bi % 2 == 0 else (3, 6)
            if bi == 0:
                for b2 in range(2):
                    t02, n2, _ = blocks[b2]
                    P.dma(X3[b2][:, :, 0:n2], d_xall[:, :, t02:t02 + n2], [], ['xst%d' % b2])
            P.begin_capture()
            xs, xk = X3[bi % 3], 'xst%d' % (bi % 3)
            if bi + 2 < len(blocks):
                t02, n2, _ = blocks[bi + 2]
                P.dma(X3[(bi + 2) % 3][:, :, 0:n2], d_xall[:, :, t02:t02 + n2], [], ['xst%d' % ((bi + 2) % 3)])
            C.sumsq_rstd(xs[:, :, 0:n], 8, n, D, B['rstd'], ones, B['sq'], [xk], 'n1' + sx)
            C.norm_mod(xs, n, B['rstd'], svc(0, w), svc(1, w), B['h'], B['tmpf'], [xk], ['h' + sx], 'n1' + sx)
            h_bf, xo, ckv, krf, ckvn = B['h'], B['xo'], B['ckv'], B['krf'], B['ckvn']
            for ct in range(6):
                M = 128 if ct < 4 else 64
                c0 = ct * 128 if ct < 4 else 512 + (ct - 4) * 64
                pi = C.next_ps()
                pk = 'ps%d' % pi
                for k in range(8):
                    P.mm(ps[pi][0:M, 0:n], wA_bf[:, k, c0:c0 + M], h_bf[:, k, 0:n], k == 0, k == 7,
                         ['wA', 'h' + sx], [pk])
                if ct < 2:
                    P.act(xo[:, ct, 0:n], ps[pi][:, 0:n], AF.Copy, [pk], ['xbo' + sx])
                elif ct < 4:
                    P.op('dve', lambda e, pi=pi, ct=ct, n=n, ckv=ckv: e.tensor_copy(out=ckv[:, ct - 2, 0:n],
                                                                                 in_=ps[pi][:, 0:n]),
                         [pk], ['ckv' + sx])
                else:
                    P.op('dve', lambda e, pi=pi, ct=ct, n=n, krf=krf: e.tensor_copy(out=krf[0:64, ct - 4, 0:n],
                                                                                 in_=ps[pi][0:64, 0:n]),
                         [pk], ['krf' + sx])
            P.dma(s_xb[:, :, t0:t0 + n].rearrange("c p t -> p c t"), xo[:, :, 0:n], ['xbo' + sx], ['s_xb%d' % bi])
            C.sumsq_rstd(ckv[:, :, 0:n], 2, n, 256, B['rstd2'], ones, B['ckv_sq'], ['ckv' + sx], 'kvn' + sx)
            for k in range(2):
                P.op('dve', lambda e, k=k, n=n, ckv=ckv, B=B: e.tensor_tensor(out=ckv[:, k, 0:n], in0=ckv[:, k, 0:n],
                                                                           in1=B['rstd2'][:, 0:n], op=ALU.mult),
                     ['ckv' + sx, 'kvn' + sx + '_rstd'], ['ckv' + sx])
                P.act(ckvn[:, k, 0:n], ckv[:, k, 0:n], AF.Copy, ['ckv' + sx, 'vecs'], ['ckvn' + sx],
                      scale=kvng[:, k:k + 1])
            Ko, Kk = B['Ko'], 'Ko' + sx
            for h in range(4):
                pi = C.next_ps()
                pk = 'ps%d' % pi
                for k in range(2):
                    P.mm(ps[pi][:, 0:n], wukv_bf[:, k, h * 128:(h + 1) * 128], ckvn[:, k, 0:n], k == 0, k == 1,
                         ['wukv', 'ckvn' + sx], [pk])
                P.act(Ko[:, h, 0:n], ps[pi][:, 0:n], AF.Copy, [pk], [Kk])
            P.dma(s_KT[:, :, t0:t0 + n].rearrange("h p t -> p h t"), Ko[:, :, 0:n], [Kk], ['s_KT%d' % bi])
            Vo, Vk = B['Vo'], 'Vo' + sx
            for tt in range(n // 128):
                pi = C.next_ps()
                pk = 'ps%d' % pi
                for k in range(2):
                    P.mm(ps[pi][:, 0:512], ckvn[:, k, tt * 128:(tt + 1) * 128], wukv_bf[:, k, 512:1024], k == 0, k == 1,
                         ['wukv', 'ckvn' + sx], [pk])
                P.op('dve', lambda e, pi=pi, tt=tt, Vo=Vo: e.tensor_copy(
                    out=Vo[:, :, tt, :], in_=ps[pi][:, 0:512].rearrange("p (h c) -> p h c", h=4)), [pk], [Vk])
            nt = n // 128
            for h in range(4):
                P.dma(s_V[h, :, t0 // 128:t0 // 128 + nt, :], Vo[:, h, 0:nt, :], [Vk], ['s_V%d_%d' % (bi, h)])
            ko, kk = B['ko'], 'kro' + sx
            if w == 0:
                l0 = t0 - CTX
                rC, rS = B['rC'], B['rS']
                P.dma(rC[0:64, 0:n], d_ropeC[:, l0:l0 + n], [], ['rC' + sx])
                P.dma(rS[0:64, 0:n], d_ropeS[:, l0:l0 + n], [], ['rS' + sx])
                P.op('pool', lambda e, n=n, krf=krf, rC=rC: e.tensor_tensor(out=krf[0:64, 0, 0:n], in0=krf[0:64, 0, 0:n],
                                                                          in1=rC[0:64, 0:n], op=ALU.mult),
                     ['krf' + sx, 'rC' + sx], ['krf' + sx])
                P.op('pool', lambda e, n=n, krf=krf, rS=rS: e.tensor_tensor(out=krf[0:64, 1, 0:n], in0=krf[0:64, 1, 0:n],
                                                                          in1=rS[0:64, 0:n], op=ALU.mult),
                     ['krf' + sx, 'rS' + sx], ['krf' + sx])
                P.op('pool', lambda e, n=n, ko=ko, krf=krf: e.tensor_tensor(out=ko[0:64, 0:n], in0=krf[0:64, 0, 0:n],
                                                                          in1=krf[0:64, 1, 0:n], op=ALU.add),
                     ['krf' + sx], [kk])
            else:
                P.op('pool', lambda e, n=n, ko=ko, krf=krf: e.tensor_copy(out=ko[0:64, 0:n], in_=krf[0:64, 0, 0:n]),
                     ['krf' + sx], [kk])
            P.dma(s_kr[:, t0:t0 + n], ko[0:64, 0:n], [kk], ['s_kr%d' % bi])
            caps.append(P.end_capture())
        C.ps_range = (0, 6)
        P.replay_interleaved(caps, 2)
        P.fence()
        A.release(m1)

        mixT = A.bf(8, NL)
        qTn = A.bf(4, NL)
        qTr = A.bf(4, NL)
        mP = A.mark()
        ysum = A.f32(2, NL)
        m2 = A.mark()
        lruw_bf = A.bf(8, 128)
        C.load_cast(lruw_bf, d_lruw, 128, 8, 'lruw', stage=[(A.f32(128), 'lst0'), (A.f32(128), 'lst1')])
        NCH = 1024
        NCK = SEQ // NCH

        def p2set():
            return dict(xi=A.f32(NCH + 3), cl=A.f32(NCH), clb=A.bf(NCH), rr=A.f32(NCH), ii=A.f32(NCH), aa=A.f32(NCH),
                        t1=A.f32(NCH), t2=A.f32(NCH), hh=A.f32(NCH))
        B2 = [p2set(), p2set()]
        carry = A.f32(4)
        chunks = [(0, CTX, True, True, -1)] + [(CTX + NCH * j, NCH, j == 0, j == NCK - 1, j) for j in range(NCK)]
        CPC = NLOC // NCH
        ci = 0
        first_lat = {}
        caps2 = []
        for ct in range(2):
            for d in range(2):
                order = chunks if d == 0 else [chunks[0]] + chunks[:0:-1]
                cv = carry[:, ct * 2 + d:ct * 2 + d + 1]
                for qi, (t0, n, lz, rz, j) in enumerate(order):
                    B = B2[ci % 2]
                    sx = str(ci % 2)
                    C.ps_range = (0, 3) if ci % 2 == 0 else (3, 6)
                    ci += 1
                    P.begin_capture()
                    xi, cl, clb, rr, ii, aa, t1, t2, hh = (B['xi'], B['cl'], B['clb'], B['rr'], B['ii'], B['aa'], B['t1'],
                                                           B['t2'], B['hh'])
                    xk = 'xin' + sx
                    lo = 0 if lz else 2
                    ro = 0 if rz else 1
                    P.dma(xi[:, 2 - lo:2 + n + ro], s_xb[ct, :, t0 - lo:t0 + n + ro], [], [xk])
                    if lz:
                        P.op('pool', lambda e, xi=xi: e.memset(xi[:, 0:2], 0.0), [], [xk])
                    if rz:
                        P.op('pool', lambda e, xi=xi, n=n: e.memset(xi[:, n + 2:n + 3], 0.0), [], [xk])
                    P.op('dve', lambda e, xi=xi, n=n, ct=ct, cl=cl: e.tensor_scalar(
                        out=cl[:, 0:n], in0=xi[:, 0:n], scalar1=convw[:, ct * 4:ct * 4 + 1],
                        scalar2=convb[:, ct:ct + 1], op0=ALU.mult, op1=ALU.add), [xk, 'vecs'], ['cl' + sx])
                    for k in range(1, 4):
                        P.op('dve', lambda e, xi=xi, n=n, k=k, ct=ct, cl=cl: e.scalar_tensor_tensor(
                            out=cl[:, 0:n], in0=xi[:, k:k + n], scalar=convw[:, ct * 4 + k:ct * 4 + k + 1],
                            in1=cl[:, 0:n], op0=ALU.mult, op1=ALU.add), [xk, 'vecs', 'cl' + sx], ['cl' + sx])
                    P.act(clb[:, 0:n], cl[:, 0:n], AF.Copy, ['cl' + sx], ['clb' + sx])
                    for g, dst, dk in ((0, rr, 'rr' + sx), (1, ii, 'ii' + sx)):
                        wi = d * 4 + g * 2 + ct
                        for sb in range((n + 511) // 512):
                            nn = min(512, n - sb * 512)
                            pi = C.next_ps()
                            pk = 'ps%d' % pi
                            P.mm(ps[pi][:, 0:nn], lruw_bf[:, wi, :], clb[:, sb * 512:sb * 512 + nn], True, True,
                                 ['lruw', 'clb' + sx], [pk])
                            P.act(dst[:, sb * 512:sb * 512 + nn], ps[pi][:, 0:nn], AF.Sigmoid, [pk, 'vecs'], [dk],
                                  bias=lrub[:, wi:wi + 1])
                    P.act(aa[:, 0:n], rr[:, 0:n], AF.Exp, ['rr' + sx, 'spv'], ['aa' + sx],
                          scale=spv[:, d * 2 + ct:d * 2 + ct + 1])
                    P.op('pool', lambda e, n=n, t1=t1, aa=aa: e.tensor_tensor(out=t1[:, 0:n], in0=aa[:, 0:n],
                                                                            in1=aa[:, 0:n], op=ALU.mult),
                         ['aa' + sx], ['t1' + sx])
                    P.act(t1[:, 0:n], t1[:, 0:n], AF.Sqrt, ['t1' + sx], ['t1' + sx], scale=-1.0, bias=1.0)
                    P.op('pool', lambda e, n=n, t2=t2, ii=ii, cl=cl: e.tensor_tensor(out=t2[:, 0:n], in0=ii[:, 0:n],
                                                                                   in1=cl[:, 0:n], op=ALU.mult),
                         ['ii' + sx, 'cl' + sx], ['t2' + sx])
                    P.op('pool', lambda e, n=n, t2=t2, t1=t1: e.tensor_tensor(out=t2[:, 0:n], in0=t2[:, 0:n],
                                                                            in1=t1[:, 0:n], op=ALU.mult),
                         ['t2' + sx, 't1' + sx], ['t2' + sx])
                    init = 0.0 if qi == 0 else cv
                    if d == 0:
                        P.op('dve', lambda e, n=n, init=init, hh=hh, aa=aa, t2=t2: e.tensor_tensor_scan(
                            out=hh[:, 0:n], data0=aa[:, 0:n], data1=t2[:, 0:n], initial=init, op0=ALU.mult,
                            op1=ALU.add), ['aa' + sx, 't2' + sx, 'carry'], ['hh' + sx])
                        P.op('dve', lambda e, n=n, cv=cv, hh=hh: e.tensor_copy(out=cv, in_=hh[:, n - 1:n]),
                             ['hh' + sx], ['carry'])
                    else:
                        P.op('dve', lambda e, n=n, init=init, hh=hh, aa=aa, t2=t2: e.tensor_tensor_scan(
                            out=hh[:, 0:n][:, ::-1], data0=aa[:, 0:n][:, ::-1], data1=t2[:, 0:n][:, ::-1],
                            initial=init, op0=ALU.mult, op1=ALU.add), ['aa' + sx, 't2' + sx, 'carry'], ['hh' + sx])
                        P.op('dve', lambda e, cv=cv, hh=hh: e.tensor_copy(out=cv, in_=hh[:, 0:1]), ['hh' + sx], ['carry'])
                    if j < 0:
                        if d == 0:
                            P.op('pool', lambda e, n=n, ct=ct, hh=hh: e.tensor_copy(out=ysum[:, ct, 0:n], in_=hh[:, 0:n]),
                                 ['hh' + sx], ['ysum'])
                        else:
                            P.op('pool', lambda e, n=n, ct=ct, hh=hh: e.tensor_tensor(
                                out=ysum[:, ct, 0:n], in0=ysum[:, ct, 0:n], in1=hh[:, 0:n], op=ALU.add),
                                ['hh' + sx, 'ysum'], ['ysum'])
                    else:
                        jc, off = j // CPC, CTX + (j % CPC) * NCH
                        key = (ct, j % CPC)
                        if key not in first_lat:
                            first_lat[key] = True
                            P.op('dve', lambda e, n=n, jc=jc, off=off, ct=ct, hh=hh: e.tensor_scalar(
                                out=ysum[:, ct, off:off + n], in0=hh[:, 0:n], scalar1=cmask[:, jc:jc + 1], scalar2=None,
                                op0=ALU.mult), ['hh' + sx, 'vecs'], ['ysum'])
                        else:
                            P.op('dve', lambda e, n=n, jc=jc, off=off, ct=ct, hh=hh: e.scalar_tensor_tensor(
                                out=ysum[:, ct, off:off + n], in0=hh[:, 0:n], scalar=cmask[:, jc:jc + 1],
                                in1=ysum[:, ct, off:off + n], op0=ALU.mult, op1=ALU.add),
                                ['hh' + sx, 'vecs', 'ysum'], ['ysum'])
                    caps2.append(P.end_capture())
        C.ps_range = (0, 6)
        P.replay_interleaved(caps2, 2)
        P.fence()
        A.release(m2)

        wB_bf = A.bf(8, 1152)
        wuq_bf = A.bf(3, 1024)
        sguw_bf = A.bf(4, 128)
        sgub = A.f32(2, 128)
        m3 = A.mark()
        wst3 = [(A.f32(1152), 'wst3_0'), (A.f32(1152), 'wst3_1')]
        C.load_cast(wB_bf, d_wB, 1152, 8, 'wB', stage=wst3)
        C.load_cast(wuq_bf, d_wuq, 1024, 3, 'wuq', stage=wst3)
        C.load_cast(sguw_bf, d_sguw, 128, 4, 'sguw', stage=wst3)
        P.dma(sgub, d_sgub, [], ['sgub'])
        P.fence()
        A.release(m3)
        xs3 = A.f32(8, 512)
        sq3 = A.bf(8, 512)
        rstd3 = A.f32(512)
        tmp3 = [A.f32(512), A.f32(512)]
        h3 = A.bf(8, 512)
        u_bf = A.bf(2, 512)
        vf = A.f32(2, 512)
        vn_bf = A.bf(2, 512)
        vtok = A.bf(256)
        gbf = A.f32(2, 512)
        cqf = A.f32(3, 512)
        cqn = A.bf(3, 512)
        rstdq = A.f32(512)
        rstdv = A.f32(512)
        sqv = A.bf(2, 512)
        sqq = A.bf(3, 512)
        rCl = A.f32(512)
        rSl = A.f32(512)
        tq1 = A.f32(512)
        tq2 = A.f32(512)
        tg = A.f32(128)
        lblocks = [(d_xall[:, :, 0:CTX], CTX, 1, 0, -1)] + \
                  [(d_xloc[:, :, 512 * i:512 * (i + 1)], 512, 0, CTX + 512 * i, 512 * i) for i in range(NLOC // 512)]
        for (src, n, w, q0, l0) in lblocks:
            P.dma(xs3[:, :, 0:n], src, [], ['xs3'])
            C.sumsq_rstd(xs3[:, :, 0:n], 8, n, D, rstd3, ones, sq3, ['xs3'], 'n3')
            C.norm_mod(xs3, n, rstd3, svc(0, w), svc(1, w), h3, tmp3, ['xs3'], ['h3'], 'n3')
            for ct in range(9):
                pi = C.next_ps()
                pk = 'ps%d' % pi
                for k in range(8):
                    P.mm(ps[pi][:, 0:n], wB_bf[:, k, ct * 128:(ct + 1) * 128], h3[:, k, 0:n], k == 0, k == 7,
                         ['wB', 'h3'], [pk])
                if ct < 2:
                    P.act(u_bf[:, ct, 0:n], ps[pi][:, 0:n], AF.Gelu_apprx_tanh, [pk], ['u'])
                elif ct < 4:
                    P.act(vf[:, ct - 2, 0:n], ps[pi][:, 0:n], AF.Gelu_apprx_tanh, [pk], ['vf'])
                elif ct < 6:
                    P.act(gbf[:, ct - 4, 0:n], ps[pi][:, 0:n], AF.Gelu_apprx_tanh, [pk], ['gbf'])
                    P.op('dve', lambda e, ct=ct, n=n, q0=q0: e.tensor_tensor(
                        out=mixT[:, 2 + ct - 4, q0:q0 + n], in0=gbf[:, ct - 4, 0:n], in1=ysum[:, ct - 4, q0:q0 + n],
                        op=ALU.mult), ['gbf', 'ysum'], ['mix_b'])
                else:
                    P.op('dve', lambda e, ct=ct, n=n, pi=pi: e.tensor_copy(out=cqf[:, ct - 6, 0:n], in_=ps[pi][:, 0:n]),
                         [pk], ['cqf'])
            C.sumsq_rstd(vf[:, :, 0:n], 2, n, 256, rstdv, ones, sqv, ['vf'], 'vn')
            for k in range(2):
                P.op('dve', lambda e, k=k, n=n: e.tensor_tensor(out=vn_bf[:, k, 0:n], in0=vf[:, k, 0:n],
                                                                in1=rstdv[:, 0:n], op=ALU.mult),
                     ['vf', 'vn_rstd'], ['vn'])
            for tt in range(n // 128):
                for k in range(2):
                    P.tr(psb[:, k * 128:(k + 1) * 128], vn_bf[:, k, tt * 128:(tt + 1) * 128], ident,
                         ['vn', 'consts'], ['psb'])
                P.op('dve', lambda e: e.tensor_copy(out=vtok, in_=psb[:, 0:256]), ['psb'], ['vtok'])
                pi = C.next_ps()
                pk = 'ps%d' % pi
                for g in range(4):
                    P.mm(ps[pi][(g % 2) * 64:(g % 2) * 64 + 64, (g // 2) * 128:(g // 2) * 128 + 128],
                         vtok[:, g * 64:(g + 1) * 64], sguw_bf[:, g, :], True, True, ['vtok', 'sguw'], [pk])
                for c2 in range(2):
                    P.op('dve', lambda e, c2=c2, pi=pi: e.tensor_tensor(out=tg, in0=ps[pi][:, c2 * 128:(c2 + 1) * 128],
                                                                        in1=sgub[:, c2, :], op=ALU.add),
                         [pk, 'sgub'], ['tg'])
                    P.op('dve', lambda e, c2=c2, tt=tt, q0=q0: e.tensor_tensor(
                        out=mixT[:, c2, q0 + tt * 128:q0 + (tt + 1) * 128], in0=tg,
                        in1=u_bf[:, c2, tt * 128:(tt + 1) * 128], op=ALU.mult), ['tg', 'u'], ['mix_a'])
            C.sumsq_rstd(cqf[:, :, 0:n], 3, n, 384, rstdq, ones, sqq, ['cqf'], 'qn')
            for k in range(3):
                P.op('dve', lambda e, k=k, n=n: e.tensor_tensor(out=cqf[:, k, 0:n], in0=cqf[:, k, 0:n],
                                                                in1=rstdq[:, 0:n], op=ALU.mult),
                     ['cqf', 'qn_rstd'], ['cqf'])
                P.act(cqn[:, k, 0:n], cqf[:, k, 0:n], AF.Copy, ['cqf', 'vecs'], ['cqn'], scale=qng[:, k:k + 1])
            if w == 0:
                P.dma(rCl[0:64, 0:n], d_ropeCl[:, l0:l0 + n], [], ['rCl'])
                P.dma(rSl[0:64, 0:n], d_ropeSl[:, l0:l0 + n], [], ['rSl'])
            for h in range(4):
                pi = C.next_ps()
                pk = 'ps%d' % pi
                for k in range(3):
                    P.mm(ps[pi][:, 0:n], wuq_bf[:, k, h * 256:h * 256 + 128], cqn[:, k, 0:n], k == 0, k == 2,
                         ['wuq', 'cqn'], [pk])
                P.act(qTn[:, h, q0:q0 + n], ps[pi][:, 0:n], AF.Copy, [pk], ['qTn'])
                pa = C.next_ps()
                pak = 'ps%d' % pa
                for k in range(3):
                    P.mm(ps[pa][0:64, 0:n], wuq_bf[:, k, h * 256 + 128:h * 256 + 192], cqn[:, k, 0:n], k == 0, k == 2,
                         ['wuq', 'cqn'], [pak])
                if w == 1:
                    P.act(qTr[0:64, h, q0:q0 + n], ps[pa][0:64, 0:n], AF.Copy, [pak], ['qTr'])
                else:
                    pb = C.next_ps()
                    pbk = 'ps%d' % pb
                    for k in range(3):
                        P.mm(ps[pb][0:64, 0:n], wuq_bf[:, k, h * 256 + 192:h * 256 + 256], cqn[:, k, 0:n], k == 0,
                             k == 2, ['wuq', 'cqn'], [pbk])
                    P.op('dve', lambda e, pa=pa, n=n: e.tensor_tensor(out=tq1[0:64, 0:n], in0=ps[pa][0:64, 0:n],
                                                                      in1=rCl[0:64, 0:n], op=ALU.mult),
                         [pak, 'rCl'], ['tq1'])
                    P.op('dve', lambda e, pb=pb, n=n: e.tensor_tensor(out=tq2[0:64, 0:n], in0=ps[pb][0:64, 0:n],
                                                                      in1=rSl[0:64, 0:n], op=ALU.mult),
                         [pbk, 'rSl'], ['tq2'])
                    P.op('pool', lambda e, h=h, n=n, q0=q0: e.tensor_tensor(
                        out=qTr[0:64, h, q0:q0 + n], in0=tq1[0:64, 0:n], in1=tq2[0:64, 0:n], op=ALU.add),
                        ['tq1', 'tq2'], ['qTr'])
        P.fence()
        A.release(mP)

        NKT = TALL // 128
        KT = A.bf(TALL)
        Vh = A.bf(NKT, 128)
        krT = A.bf(TALL)
        PT = [A.bf(512) for _ in range(6)]
        rinv = A.f32(512)
        lacc = [[A.f32(512) for _ in range(3)] for _ in range(2)]
        NPC = 5
        KPP = NKT // NPC
        for pc in range(NPC):
            a, b = pc * KPP * 128, (pc + 1) * KPP * 128
            P.dma(krT[0:64, a:b], s_kr[:, a:b], ['s_kr'], ['kr_p%d' % pc])
            P.dma(krT[64:128, a:b], s_kr[:, a:b], ['s_kr'], ['kr_p%d' % pc])
        P.dma(qTr[64:128, :, :], qTr[0:64, :, :], ['qTr'], ['qTr2'])
        qblocks = [(0, CTX, 2)] + [(CTX + 512 * i, 512, NKT) for i in range(NLOC // 512)]

        def load_kv(h, pc):
            a, b = pc * KPP * 128, (pc + 1) * KPP * 128
            P.dma(KT[:, a:b], s_KT[h, :, a:b], ['s_KT'], ['KT_p%d' % pc])
            P.dma(Vh[:, pc * KPP:(pc + 1) * KPP, :], s_V[h, :, pc * KPP:(pc + 1) * KPP, :], ['s_V'], ['V_p%d' % pc])
        tiles = []
        qbi = 0
        for h in range(4):
            for qi, (q0, n, nkt) in enumerate(qblocks):
                po, pl = 4 + qbi % 2, 6
                qbi += 1
                for kt in range(nkt):
                    tiles.append((h, qi, q0, n, nkt, kt, po, pl))

        def emit_qk_pair(i0):
            js = [j for j in (i0, i0 + 1) if j < len(tiles)]
            for j in js:
                h, qi, q0, n, nkt, kt, po, pl = tiles[j]
                sb = j % 4
                P.mm(ps[sb][:, 0:n], KT[:, kt * 128:(kt + 1) * 128], qTn[:, h, q0:q0 + n], True, False,
                     ['KT_p%d' % (kt // KPP), 'qTn'], ['ps%d' % sb])
            for j in js:
                h, qi, q0, n, nkt, kt, po, pl = tiles[j]
                sb = j % 4
                r0 = 0 if j % 2 == 0 else 64
                P.mm(ps[sb][:, 0:n], krT[r0:r0 + 64, kt * 128:(kt + 1) * 128], qTr[r0:r0 + 64, h, q0:q0 + n], False, True,
                     ['kr_p%d' % (kt // KPP), 'qTr', 'qTr2'], ['ps%d' % sb], tp=(r0, 0))
        for pc in range(NPC):
            load_kv(0, pc)
        emit_qk_pair(0)
        for i, (h, qi, q0, n, nkt, kt, po, pl) in enumerate(tiles):
            if i % 2 == 0 and i + 2 < len(tiles):
                emit_qk_pair(i + 2)
            sb = i % 4
            pc = kt // KPP
            pt = PT[i % 6]
            ptk = 'PT%d' % (i % 6)
            P.act(pt[:, 0:n], ps[sb][:, 0:n], AF.Exp, ['ps%d' % sb], [ptk], scale=ATTN_SCALE)
            P.mm(ps[po][:, 0:n], Vh[:, kt, :], pt[:, 0:n], kt == 0, kt == nkt - 1, ['V_p%d' % pc, ptk], ['ps%d' % po])
            c3 = kt % 3
            la, lak = lacc[po - 4][c3], 'lacc%d_%d' % (po - 4, c3)
            le = 'pool' if c3 == 2 else 'dve'
            if kt < 3:
                P.op(le, lambda e, la=la, pt=pt, n=n: e.tensor_copy(out=la[:, 0:n], in_=pt[:, 0:n]), [ptk], [lak])
            else:
                P.op(le, lambda e, la=la, pt=pt, n=n: e.tensor_tensor(out=la[:, 0:n], in0=la[:, 0:n], in1=pt[:, 0:n],
                                                                     op=ALU.add), [ptk, lak], [lak])
            if kt == nkt - 1:
                nacc = min(3, nkt)
                for c in range(nacc):
                    P.mm(ps[pl][:, 0:n], onesf, lacc[po - 4][c][:, 0:n], c == 0, c == nacc - 1,
                         ['lacc%d_%d' % (po - 4, c), 'onesf'], ['ps%d' % pl])
                P.op('dve', lambda e, pl=pl, n=n: e.reciprocal(out=rinv[:, 0:n], in_=ps[pl][:, 0:n]),
                     ['ps%d' % pl], ['rinv'])
                P.op('dve', lambda e, po=po, n=n, h=h, q0=q0: e.tensor_tensor(
                    out=mixT[:, 4 + h, q0:q0 + n], in0=ps[po][:, 0:n], in1=rinv[:, 0:n], op=ALU.mult),
                    ['ps%d' % po, 'rinv'], ['mix_c'])
            if qi == len(qblocks) - 1 and kt % KPP == KPP - 1 and h < 3:
                load_kv(h + 1, kt // KPP)
        P.fence()
        A.release(mP)

        wout_bf = A.bf(8, 1024)
        C.load_cast(wout_bf, d_wout, 1024, 8, 'wout', stage=[(A.f32(1024), 'wst5_0'), (A.f32(1024), 'wst5_1')])
        wr = A.f32(8, 16)
        P.dma(wr, d_wr, [], ['wr'])
        xr = A.f32(8, 512)
        x1 = A.f32(8, 512)
        sq5 = A.bf(8, 512)
        rstd5 = A.f32(512)
        tmp5 = [A.f32(512), A.f32(512)]
        h2 = A.f32(8, 512)
        sm = A.f32(4, 4)
        pe_ = A.f32(4, 16)
        for (src, n, w, q0, l0) in lblocks:
            P.dma(xr[:, :, 0:n], src, [], ['xr'])
            for j in range(8):
                pi = C.next_ps()
                pk = 'ps%d' % pi
                for k in range(8):
                    P.mm(ps[pi][:, 0:n], wout_bf[:, k, j * 128:(j + 1) * 128], mixT[:, k, q0:q0 + n], k == 0, k == 7,
                         ['wout', 'mix_a', 'mix_b', 'mix_c'], [pk])
                P.op('dve', lambda e, j=j, pi=pi, n=n, w=w: e.scalar_tensor_tensor(
                    out=x1[:, j, 0:n], in0=ps[pi][:, 0:n], scalar=svc(2, w)[:, j:j + 1], in1=xr[:, j, 0:n],
                    op0=ALU.mult, op1=ALU.add), [pk, 'xr', 'svt'], ['x1'])
            P.dma(o_x1[:, :, q0:q0 + n], x1[:, :, 0:n], ['x1'], ['o_x1'])
            C.sumsq_rstd(x1[:, :, 0:n], 8, n, D, rstd5, ones, sq5, ['x1'], 'n5')
            C.norm_mod(x1, n, rstd5, svc(3, w), svc(4, w), h2, tmp5, ['x1'], ['h2'], 'n5')
            for tt in range(n // 128):
                pi = C.next_ps()
                pk = 'ps%d' % pi
                si = tt % 4
                smk = 'sm%d' % si
                for k in range(8):
                    P.mm(ps[pi][:, 0:16], h2[:, k, tt * 128:(tt + 1) * 128], wr[:, k, :], k == 0, k == 7,
                         ['h2', 'wr'], [pk])
                P.op('dve', lambda e, pi=pi, si=si: e.reduce_max(out=sm[:, si, 0:1], in_=ps[pi][:, 0:16],
                                                                 axis=mybir.AxisListType.X), [pk], [smk])
                P.op('dve', lambda e, si=si: e.tensor_single_scalar(out=sm[:, si, 1:2], in_=sm[:, si, 0:1], scalar=-1.0,
                                                                    op=ALU.mult), [smk], [smk])
                P.act(pe_[:, si, :], ps[pi][:, 0:16], AF.Exp, [pk, smk], ['pe%d' % si], bias=sm[:, si, 1:2],
                      accum=sm[:, si, 2:3])
                P.op('dve', lambda e, si=si: e.reciprocal(out=sm[:, si, 3:4], in_=sm[:, si, 2:3]), ['pe%d' % si, smk],
                     [smk])
                P.op('dve', lambda e, si=si: e.tensor_scalar(out=pe_[:, si, :], in0=pe_[:, si, :],
                                                             scalar1=sm[:, si, 3:4], scalar2=None, op0=ALU.mult),
                     [smk, 'pe%d' % si], ['pe%d' % si])
                P.dma(o_probs[q0 + tt * 128:q0 + (tt + 1) * 128, :], pe_[:, si, :], ['pe%d' % si], ['o_probs'])
        P.finalize()
        return nc, P, A


def _fm(a):
    k = a.shape[0] // 128
    return np.ascontiguousarray(a.reshape(k, 128, -1).transpose(1, 0, 2))


def _fmv(v):
    return np.ascontiguousarray(v.reshape(-1, 128).T)


def _rope_perm():
    p = np.arange(64)
    o = ((p % 32) // 16) * 32 + (p // 32) * 16 + (p % 16)
    osw = o[(p + 32) % 64]
    return o, osw


def _rope_tables():
    rows = SEQ // 64
    r = np.repeat(np.arange(rows), 64).astype(np.float32)
    col = np.tile(np.arange(64), rows).astype(np.float32)
    inv = (np.float32(10000.0) ** (-np.arange(16, dtype=np.float32) / np.float32(16))).astype(np.float32)
    ang = np.concatenate([r[:, None] * inv, col[:, None] * inv], axis=-1).astype(np.float32)
    cos, sin = np.cos(ang).astype(np.float32), np.sin(ang).astype(np.float32)
    C_ = np.concatenate([cos, cos], axis=1).T
    S_ = np.concatenate([-sin, sin], axis=1).T
    return np.ascontiguousarray(C_), np.ascontiguousarray(S_)


def prep_AB(inp, l, xl, xc):
    o, osw = _rope_perm()
    xall = _fm(np.ascontiguousarray(np.concatenate([xc, xl], axis=0).T))
    cvec = np.stack([_fmv(inp['c'][0]), _fmv(inp['c_ctx'])], axis=-1)
    w_in = inp['w_in'][l]
    wA = np.concatenate([w_in[:, 512:768], w_in[:, 1408:1664], w_in[:, 1664 + o], w_in[:, 1664 + osw]], axis=1)
    wB = np.concatenate([w_in[:, 0:512], w_in[:, 768:1024], w_in[:, 1024:1408]], axis=1)
    wuq = inp['w_uq'][l]
    cols = []
    for h in range(4):
        cols += [wuq[:, h * 192:h * 192 + 128], wuq[:, h * 192 + 128 + o], wuq[:, h * 192 + 128 + osw]]
    wuq2 = np.concatenate(cols, axis=1)
    wukv = inp['w_ukv'][l]
    wukv2 = np.concatenate([wukv[:, h * 256:h * 256 + 128] for h in range(4)] +
                           [wukv[:, h * 256 + 128:h * 256 + 256] for h in range(4)], axis=1)
    lruw = np.zeros((128, 8, 128), np.float32)
    vecs = np.zeros((128, 64), np.float32)
    vecs[:, 0:8] = _fmv(inp['norm1_g'][l])
    vecs[:, 8:16] = _fmv(inp['norm2_g'][l])
    for d in range(2):
        for g in range(2):
            W = (inp['lru_wa'] if g == 0 else inp['lru_wx'])[l][d]
            bb = (inp['lru_ba'] if g == 0 else inp['lru_bx'])[l][d]
            for ct in range(2):
                i = d * 4 + g * 2 + ct
                lruw[0:64, i, 0:64] = W[2 * ct]
                lruw[64:128, i, 64:128] = W[2 * ct + 1]
                vecs[:, 16 + i] = bb[ct * 128:(ct + 1) * 128]
        for ct in range(2):
            vecs[:, 24 + d * 2 + ct] = inp['lru_lambda'][l][d][ct * 128:(ct + 1) * 128]
    for ct in range(2):
        for k in range(4):
            vecs[:, 28 + ct * 4 + k] = inp['conv_w'][l][k][ct * 128:(ct + 1) * 128]
        vecs[:, 36 + ct] = inp['conv_b'][l][ct * 128:(ct + 1) * 128]
    vecs[:, 38:41] = _fmv(inp['q_norm_g'][l])
    vecs[:, 41:43] = _fmv(inp['kv_norm_g'][l])
    sguw = np.ascontiguousarray(inp['sgu_w'][l].transpose(2, 0, 1))
    sgub = np.zeros((128, 2, 128), np.float32)
    for g in range(4):
        sgub[(g % 2) * 64:(g % 2) * 64 + 64, g // 2, :] = inp['sgu_b'][l][g][None, :]
    rC, rS = _rope_tables()
    common = dict(xall=xall, cvec=np.ascontiguousarray(cvec), wmod=_fm(np.ascontiguousarray(inp['w_mod'][l][:, :5120])),
                  bmod=_fmv(inp['b_mod'][l][:5120]), wA=_fm(wA), wB=_fm(wB), wuq=_fm(wuq2), wukv=_fm(wukv2),
                  wout=_fm(inp['w_out'][l]), lruw=lruw, sguw=sguw, sgub=sgub, wr=_fm(inp['w_router'][l]),
                  ropeC=rC, ropeS=rS, ident=np.eye(128, dtype=np.float32))
    maps = []
    for c in range(NCORES):
        v = vecs.copy()
        v[:, 43 + c] = 1.0
        m = dict(common)
        m['vecs'] = v
        m['xloc'] = np.ascontiguousarray(xall[:, :, CTX + NLOC * c:CTX + NLOC * (c + 1)])
        m['ropeCl'] = np.ascontiguousarray(rC[:, NLOC * c:NLOC * (c + 1)])
        m['ropeSl'] = np.ascontiguousarray(rS[:, NLOC * c:NLOC * (c + 1)])
        maps.append(m)
    return maps


def _unfm(a):
    return np.ascontiguousarray(a.transpose(1, 0, 2).reshape(-1, a.shape[2]).T)


def gather_AB(res):
    xl1 = np.concatenate([_unfm(r['x1T'][:, :, CTX:]) for r in res], axis=0)
    xc1 = _unfm(res[0]['x1T'][:, :, :CTX])
    pl = np.concatenate([r['probs'][CTX:] for r in res], axis=0)
    pc = res[0]['probs'][:CTX]
    return xl1, xc1, pl, pc


NBIS = 30


def build_C():
    nc = bass.Bass("TRN2", target_bir_lowering=False)

    def din(name, shape, dt=F32):
        return nc.dram_tensor(name, list(shape), dt, kind="ExternalInput").ap()
    d_x1 = din("x1T", [128, 8, NL])
    d_pA = din("probsA", [128, NLOC])
    d_pC = din("probsC", [16, CTX])
    d_gm = din("gmT", [16, NL])
    d_cvec = din("cvec", [128, 8, 2])
    d_wmod = din("wmod", [128, 8, 3072])
    d_bmod = din("bmod", [128, 24])
    d_vecs = din("vecs", [128, 16])
    d_G = din("G", [128, 128])
    d_oh = din("oh16", [16, 16])
    d_wg = din("wg", [NEXP, D, D])
    d_wu = din("wu", [NEXP, D, D])
    d_wd = din("wd", [NEXP, D, D])
    o_x2 = nc.dram_tensor("x2T", [128, 8, NL], F32, kind="ExternalOutput").ap()
    o_on = nc.dram_tensor("outN", [128, 8, NL], F32, kind="ExternalOutput").ap()

    AW = 51 * 1024
    with ExitStack() as es:
        arena_t = es.enter_context(nc.sbuf_tensor("arena", [128, AW], F32))
        ps = [es.enter_context(nc.psum_tensor("ps%d" % i, [128, 512], F32)) for i in range(7)]
        P = Prog(nc)
        A = Arena(arena_t, AW)
        C = Ctx(nc, P, A, ps, None)

        vecs = A.f32(16)
        modT = A.f32(24, 2)
        cs = A.f32(64)
        sv = A.f32(3, 8, 2)
        G = A.f32(128)
        oh = A.f32(16)
        ones16 = A.f32(128)
        ones = A.bf(128)
        ts = A.f32(16)
        m0 = A.mark()
        P.dma(vecs, d_vecs, [], ['vecs'])
        P.dma(G, d_G, [], ['consts'])
        P.dma(oh[0:16, :], d_oh, [], ['consts'])
        P.op('dve', lambda e: e.memset(ones, 1.0), [], ['consts'])
        P.op('dve', lambda e: e.memset(ones16, 1.0), [], ['consts'])
        stage8 = A.f32(8, 1024)
        stage8b = A.f32(8, 1024)
        C.compute_mods(d_cvec, d_wmod, d_bmod, 3, modT, cs, [stage8, stage8b], oh[0:2, 0:2])
        n2g, fng = vecs[:, 0:8], vecs[:, 8:16]
        mv = modT.rearrange("p (b k) w -> p b k w", b=3)
        for w in range(2):
            P.op('dve', lambda e, w=w: e.scalar_tensor_tensor(
                out=sv[:, 0, :, w], in0=mv[:, 1, :, w], scalar=1.0, in1=n2g, op0=ALU.add, op1=ALU.mult),
                ['modv', 'vecs'], ['svt'])
            P.op('dve', lambda e, w=w: e.tensor_copy(out=sv[:, 1, :, w], in_=mv[:, 0, :, w]), ['modv'], ['svt'])
            P.op('dve', lambda e, w=w: e.tensor_copy(out=sv[:, 2, :, w], in_=mv[:, 2, :, w]), ['modv'], ['svt'])
        P.fence()
        A.release(m0)

        h2T = A.bf(8, NL)
        acc = A.f32(8, NL)
        gmT = A.f32(NL)
        mL = A.mark()
        lblocks = [(0, CTX, 1)] + [(CTX + 512 * i, 512, 0) for i in range(NLOC // 512)]

        xs = [A.f32(8, 512), A.f32(8, 512)]
        sq = A.bf(8, 512)
        rstd = A.f32(512)
        tmpf = [A.f32(512), A.f32(512)]
        for bi, (q0, n, w) in enumerate(lblocks):
            x = xs[bi % 2]
            xk = 'xs%d' % (bi % 2)
            P.dma(x[:, :, 0:n], d_x1[:, :, q0:q0 + n], [], [xk])
            C.sumsq_rstd(x[:, :, 0:n], 8, n, D, rstd, ones, sq, [xk], 'n2')
            C.norm_mod(x, n, rstd, sv[:, 0, :, w], sv[:, 1, :, w], h2T[:, :, q0:q0 + n], tmpf, [xk], ['h2T'], 'n2')
        P.fence()
        A.release(mL)

        pA = A.f32(NLOC)
        mk = A.f32(NLOC)
        pC = A.f32(CTX)
        mkc = A.f32(CTX)
        P.dma(pA, d_pA, [], ['pA'])
        P.dma(pC[0:16, :], d_pC, [], ['pC'])
        P.dma(gmT[0:16, :], d_gm, [], ['gmT'])
        lo, hi, mid, tot, gt, dd, Kv = (ts[:, 0:2], ts[:, 2:4], ts[:, 4:6], ts[:, 6:8], ts[:, 8:10], ts[:, 10:12],
                                        ts[:, 12:14])
        P.op('dve', lambda e: e.memset(ts, 0.0), [], ['ts'])
        P.op('dve', lambda e: e.memset(hi, 1.0), ['ts'], ['ts'])
        P.op('dve', lambda e: e.memset(mid, 0.5), ['ts'], ['ts'])
        P.op('dve', lambda e: e.memset(ts[:, 12:13], NLOC - 0.5), ['ts'], ['ts'])
        P.op('dve', lambda e: e.memset(ts[:, 13:14], 2 * CTX // NEXP - 0.5), ['ts'], ['ts'])
        for it in range(NBIS):
            P.op('dve', lambda e: e.tensor_scalar(out=mk, in0=pA, scalar1=ts[:, 4:5], scalar2=None, op0=ALU.is_gt),
                 ['pA', 'ts'], ['mk'])
            P.op('dve', lambda e: e.reduce_sum(out=ts[:, 14:15], in_=mk, axis=mybir.AxisListType.X), ['mk'], ['cc'])
            P.op('dve', lambda e: e.tensor_scalar(out=mkc[0:16, :], in0=pC[0:16, :], scalar1=ts[0:16, 5:6], scalar2=None,
                                                  op0=ALU.is_gt), ['pC', 'ts'], ['mkc'])
            P.op('dve', lambda e: e.reduce_sum(out=ts[0:16, 7:8], in_=mkc[0:16, :], axis=mybir.AxisListType.X),
                 ['mkc', 'ts'], ['ts'])
            P.mm(ps[6][:, 0:1], G, ts[:, 14:15], True, True, ['cc', 'consts'], ['ps6'])
            P.op('dve', lambda e: e.tensor_copy(out=ts[:, 6:7], in_=ps[6][:, 0:1]), ['ps6', 'ts'], ['ts'])
            P.op('dve', lambda e: e.tensor_tensor(out=gt, in0=tot, in1=Kv, op=ALU.is_gt), ['ts'], ['ts'])
            P.op('dve', lambda e: e.tensor_tensor(out=dd, in0=mid, in1=lo, op=ALU.subtract), ['ts'], ['ts'])
            P.op('dve', lambda e: e.tensor_tensor(out=dd, in0=dd, in1=gt, op=ALU.mult), ['ts'], ['ts'])
            P.op('dve', lambda e: e.tensor_tensor(out=lo, in0=lo, in1=dd, op=ALU.add), ['ts'], ['ts'])
            P.op('dve', lambda e: e.tensor_tensor(out=dd, in0=hi, in1=mid, op=ALU.subtract), ['ts'], ['ts'])
            P.op('dve', lambda e: e.tensor_tensor(out=dd, in0=dd, in1=gt, op=ALU.mult), ['ts'], ['ts'])
            P.op('dve', lambda e: e.tensor_tensor(out=hi, in0=mid, in1=dd, op=ALU.add), ['ts'], ['ts'])
            P.op('dve', lambda e: e.tensor_tensor(out=dd, in0=lo, in1=hi, op=ALU.add), ['ts'], ['ts'])
            P.op('dve', lambda e: e.tensor_single_scalar(out=mid, in_=dd, scalar=0.5, op=ALU.mult), ['ts'], ['ts'])
        P.op('dve', lambda e: e.tensor_scalar(out=mk[0:16, 0:NLOC], in0=gmT[0:16, CTX:NL], scalar1=ts[0:16, 2:3],
                                              scalar2=None, op0=ALU.is_gt), ['gmT', 'ts'], ['mk'])
        P.op('dve', lambda e: e.tensor_tensor(out=gmT[0:16, CTX:NL], in0=gmT[0:16, CTX:NL], in1=mk[0:16, 0:NLOC],
                                              op=ALU.mult), ['mk', 'gmT'], ['gmT'])
        P.op('dve', lambda e: e.tensor_scalar(out=mkc[0:16, :], in0=gmT[0:16, 0:CTX], scalar1=ts[0:16, 3:4],
                                              scalar2=None, op0=ALU.is_gt), ['gmT', 'ts'], ['mkc'])
        P.op('dve', lambda e: e.tensor_tensor(out=gmT[0:16, 0:CTX], in0=gmT[0:16, 0:CTX], in1=mkc[0:16, :],
                                              op=ALU.mult), ['mkc', 'gmT'], ['gmT'])
        P.fence()
        A.release(mL)

        HF = 512
        wbuf = [dict(g=A.bf(8, HF), u=A.bf(8, HF), d=A.bf(4, 1024)) for _ in range(2)]
        wst = [(A.f32(1024), 'wst0'), (A.f32(1024), 'wst1')]
        hid = A.bf(4, 512)
        sg = [A.f32(512), A.f32(512)]
        tt_ = [A.f32(512), A.f32(512)]
        gmb = A.f32(512)
        gme = A.f32(512)
        units = [(ex, hf) for ex in range(NEXP) for hf in range(2)]
        stc = [0]
        gmbs = [gmb, A.f32(512)]
        gmes = [gme, A.f32(512)]

        def emit_gm(gi):
            ui2, bi2 = gi // len(lblocks), gi % len(lblocks)
            ex2 = units[ui2][0]
            q02, n2, _ = lblocks[bi2]
            ge, gb_ = gmes[gi % 2], gmbs[gi % 2]
            P.op('dve', lambda e: e.tensor_scalar(out=ge[0:16, 0:n2], in0=gmT[0:16, q02:q02 + n2],
                                                  scalar1=oh[0:16, ex2:ex2 + 1], scalar2=None, op0=ALU.mult),
                 ['gmT', 'consts'], ['gme%d' % (gi % 2)])
            P.mm(ps[6][:, 0:n2], ones16[0:16, :], ge[0:16, 0:n2], True, True, ['gme%d' % (gi % 2), 'consts'], ['ps6'])
            P.act(gb_[:, 0:n2], ps[6][:, 0:n2], AF.Copy, ['ps6'], ['gmb%d' % (gi % 2)])

        def load_steps(ui):
            ex, hf = units[ui]
            wb = wbuf[ui % 2]
            tag = 'w%d' % (ui % 2)
            steps = []
            for nm, dsrc in (('g', d_wg), ('u', d_wu)):
                for k in range(8):
                    def f(nm=nm, dsrc=dsrc, k=k):
                        st, sk = wst[stc[0] % 2]
                        stc[0] += 1
                        P.dma(st[:, 0:HF], dsrc[ex, k * 128:(k + 1) * 128, hf * HF:(hf + 1) * HF], [], [sk])
                        P.op(C.ew(), lambda en, st=st: en.tensor_copy(out=wb[nm][:, k, :], in_=st[:, 0:HF]),
                             [sk], [tag + nm])
                    steps.append(f)
            for f4 in range(4):
                def f(f4=f4):
                    st, sk = wst[stc[0] % 2]
                    stc[0] += 1
                    r0 = (hf * 4 + f4) * 128
                    P.dma(st[:, 0:1024], d_wd[ex, r0:r0 + 128, :], [], [sk])
                    P.op(C.ew(), lambda en, st=st: en.tensor_copy(out=wb['d'][:, f4, :], in_=st[:, 0:1024]),
                         [sk], [tag + 'd'])
                steps.append(f)
            return steps
        for f in load_steps(0):
            f()
        first_acc = True
        for ui, (ex, hf) in enumerate(units):
            wb = wbuf[ui % 2]
            tag = 'w%d' % (ui % 2)
            nxt = load_steps(ui + 1) if ui + 1 < len(units) else []
            per = (len(nxt) + len(lblocks) - 1) // len(lblocks)
            for bi, (q0, n, w) in enumerate(lblocks):
                gi = ui * len(lblocks) + bi
                if gi == 0:
                    emit_gm(0)
                gmb, gmk = gmbs[gi % 2], 'gmb%d' % (gi % 2)
                for f in range(4):
                    pg, pu = f % 2, 2 + f % 2
                    for k in range(8):
                        P.mm(ps[pg][:, 0:n], wb['g'][:, k, f * 128:(f + 1) * 128], h2T[:, k, q0:q0 + n], k == 0, k == 7,
                             [tag + 'g', 'h2T'], ['ps%d' % pg])
                    for k in range(8):
                        P.mm(ps[pu][:, 0:n], wb['u'][:, k, f * 128:(f + 1) * 128], h2T[:, k, q0:q0 + n], k == 0, k == 7,
                             [tag + 'u', 'h2T'], ['ps%d' % pu])
                    s_, t_ = sg[f % 2], tt_[f % 2]
                    P.act(s_[:, 0:n], ps[pg][:, 0:n], AF.Silu, ['ps%d' % pg], ['sg%d' % (f % 2)])
                    P.op('pool', lambda e, s_=s_, t_=t_, n=n, gmb=gmb: e.tensor_tensor(out=t_[:, 0:n], in0=s_[:, 0:n],
                                                                                      in1=gmb[:, 0:n], op=ALU.mult),
                         ['sg%d' % (f % 2), gmk], ['tt%d' % (f % 2)])
                    P.op('dve', lambda e, t_=t_, n=n, f=f, pu=pu: e.tensor_tensor(
                        out=hid[:, f, 0:n], in0=t_[:, 0:n], in1=ps[pu][:, 0:n], op=ALU.mult),
                        ['tt%d' % (f % 2), 'ps%d' % pu], ['hid'])
                if gi + 1 < len(units) * len(lblocks):
                    emit_gm(gi + 1)
                for j in range(8):
                    pd = 4 + j % 2
                    for f in range(4):
                        P.mm(ps[pd][:, 0:n], wb['d'][:, f, j * 128:(j + 1) * 128], hid[:, f, 0:n], f == 0, f == 3,
                             [tag + 'd', 'hid'], ['ps%d' % pd])
                    if ui == 0:
                        P.op('dve', lambda e, j=j, pd=pd, n=n, q0=q0: e.tensor_copy(out=acc[:, j, q0:q0 + n],
                                                                                   in_=ps[pd][:, 0:n]),
                             ['ps%d' % pd], ['acc'])
                    else:
                        P.op('dve', lambda e, j=j, pd=pd, n=n, q0=q0: e.tensor_tensor(
                            out=acc[:, j, q0:q0 + n], in0=acc[:, j, q0:q0 + n], in1=ps[pd][:, 0:n], op=ALU.add),
                            ['ps%d' % pd, 'acc'], ['acc'])
                for f in nxt[bi * per:(bi + 1) * per]:
                    f()
        P.fence()
        A.release(mL)

        xr = [A.f32(8, 512)] * 2
        x2 = [A.f32(8, 512)] * 2
        on = A.f32(8, 512)
        sq2 = A.bf(8, 512)
        rstd2 = A.f32(512)
        tmp2 = [A.f32(512), A.f32(512)]
        for bi, (q0, n, w) in enumerate(lblocks):
            x, xk = xr[0], 'xr'
            y, yk = x2[0], 'x2'
            P.dma(x[:, :, 0:n], d_x1[:, :, q0:q0 + n], [], [xk])
            for j in range(8):
                P.op('dve', lambda e, j=j, n=n, w=w, q0=q0, x=x, y=y: e.scalar_tensor_tensor(
                    out=y[:, j, 0:n], in0=acc[:, j, q0:q0 + n], scalar=sv[:, 2, j:j + 1, w], in1=x[:, j, 0:n],
                    op0=ALU.mult, op1=ALU.add), [xk, 'acc', 'svt'], [yk])
            P.dma(o_x2[:, :, q0:q0 + n], y[:, :, 0:n], [yk], ['o_x2'])
            C.sumsq_rstd(y[:, :, 0:n], 8, n, D, rstd2, ones, sq2, [yk], 'fn')
            for k in range(8):
                e_ = C.ew()
                t = tmp2[k % 2]
                tk = 'fn_tmp%d' % (k % 2)
                P.op(e_, lambda e, k=k, t=t, n=n, y=y: e.tensor_tensor(out=t[:, 0:n], in0=y[:, k, 0:n], in1=rstd2[:, 0:n],
                                                                      op=ALU.mult), [yk, 'fn_rstd'], [tk])
                P.op(e_, lambda e, k=k, t=t, n=n: e.tensor_scalar(out=on[:, k, 0:n], in0=t[:, 0:n],
                                                                  scalar1=fng[:, k:k + 1], scalar2=None, op0=ALU.mult),
                     [tk, 'vecs'], ['on'])
            P.dma(o_on[:, :, q0:q0 + n], on[:, :, 0:n], ['on'], ['o_on'])
        P.finalize()
        return nc, P, A


def build_C2():
    nc = bass.Bass("TRN2", target_bir_lowering=False)

    def din(name, shape, dt=F32):
        return nc.dram_tensor(name, list(shape), dt, kind="ExternalInput").ap()
    d_x1 = din("x1T", [128, 8, NL])
    d_pA = din("probsA", [128, NLOC])
    d_pC = din("probsC", [16, CTX])
    d_gm = din("gmT", [16, NL])
    d_cvec = din("cvec", [128, 8, 2])
    d_wmod = din("wmod", [128, 8, 3072])
    d_bmod = din("bmod", [128, 24])
    d_vecs = din("vecs", [128, 16])
    d_G = din("G", [128, 128])
    d_oh = din("oh16", [16, 16])
    d_ident = din("ident", [128, 128])
    d_iotaf = din("iotaf", [128, 512])
    d_iotap = din("iotap", [128, 4])
    d_wg = din("wg", [NEXP, D, D])
    d_wu = din("wu", [NEXP, D, D])
    d_wd = din("wd", [NEXP, D, D])
    o_x2 = nc.dram_tensor("x2T", [128, 8, NL], F32, kind="ExternalOutput").ap()
    o_on = nc.dram_tensor("outN", [128, 8, NL], F32, kind="ExternalOutput").ap()

    AW = 51 * 1024
    with ExitStack() as es:
        arena_t = es.enter_context(nc.sbuf_tensor("arena", [128, AW], F32))
        ps = [es.enter_context(nc.psum_tensor("ps%d" % i, [128, 512], F32)) for i in range(7)]
        psb = es.enter_context(nc.psum_tensor("psb", [128, 1024], BF16))
        P = Prog(nc)
        A = Arena(arena_t, AW)
        C = Ctx(nc, P, A, ps, psb)

        vecs = A.f32(16)
        modT = A.f32(24, 2)
        cs = A.f32(64)
        sv = A.f32(3, 8, 2)
        G = A.f32(128)
        oh = A.f32(16)
        ones16 = A.f32(128)
        ones = A.bf(128)
        identb = A.bf(128)
        iotaf = A.f32(512)
        iotap = A.f32(4)
        ts = A.f32(16)
        m0 = A.mark()
        P.dma(vecs, d_vecs, [], ['vecs'])
        P.dma(G, d_G, [], ['consts'])
        P.dma(oh[0:16, :], d_oh, [], ['consts'])
        P.dma(iotaf, d_iotaf, [], ['consts'])
        P.dma(iotap, d_iotap, [], ['consts'])
        P.op('dve', lambda e: e.memset(ones, 1.0), [], ['consts'])
        P.op('dve', lambda e: e.memset(ones16, 1.0), [], ['consts'])
        stage8 = A.f32(8, 1024)
        stage8b = A.f32(8, 1024)
        C.compute_mods(d_cvec, d_wmod, d_bmod, 3, modT, cs, [stage8, stage8b], oh[0:2, 0:2])
        P.dma(stage8[:, 0, 0:128], d_ident, [], ['wm_st0a'])
        P.op('dve', lambda e: e.tensor_copy(out=identb, in_=stage8[:, 0, 0:128]), ['wm_st0a'], ['consts'])
        n2g, fng = vecs[:, 0:8], vecs[:, 8:16]
        mv = modT.rearrange("p (b k) w -> p b k w", b=3)
        for w in range(2):
            P.op('dve', lambda e, w=w: e.scalar_tensor_tensor(
                out=sv[:, 0, :, w], in0=mv[:, 1, :, w], scalar=1.0, in1=n2g, op0=ALU.add, op1=ALU.mult),
                ['modv', 'vecs'], ['svt'])
            P.op('dve', lambda e, w=w: e.tensor_copy(out=sv[:, 1, :, w], in_=mv[:, 0, :, w]), ['modv'], ['svt'])
            P.op('dve', lambda e, w=w: e.tensor_copy(out=sv[:, 2, :, w], in_=mv[:, 2, :, w]), ['modv'], ['svt'])
        P.fence()
        A.release(m0)

        NTT = NL // 128
        h2tok = A.bf(NTT, 1024)
        acc = A.f32(8, NL)
        gmT = A.f32(NL)
        ssel = A.f32(NL)
        slotT = A.f32(NTT, 16)
        mL = A.mark()
        lblocks = [(0, CTX, 1)] + [(CTX + 512 * i, 512, 0) for i in range(NLOC // 512)]

        xs = [A.f32(8, 512), A.f32(8, 512)]
        sq = A.bf(8, 512)
        rstd = A.f32(512)
        tmpf = [A.f32(512), A.f32(512)]
        h2b = [A.bf(8, 512), A.bf(8, 512)]
        for bi, (q0, n, w) in enumerate(lblocks):
            x = xs[bi % 2]
            xk = 'xs%d' % (bi % 2)
            P.dma(x[:, :, 0:n], d_x1[:, :, q0:q0 + n], [], [xk])
            C.sumsq_rstd(x[:, :, 0:n], 8, n, D, rstd, ones, sq, [xk], 'n2')
            hb, hk = h2b[bi % 2], 'h2b%d' % (bi % 2)
            C.norm_mod(x, n, rstd, sv[:, 0, :, w], sv[:, 1, :, w], hb, tmpf, [xk], [hk], 'n2')
            for tt in range(n // 128):
                for k in range(8):
                    P.tr(psb[:, k * 128:(k + 1) * 128], hb[:, k, tt * 128:(tt + 1) * 128], identb, [hk, 'consts'], ['psb'])
                P.act(h2tok[:, q0 // 128 + tt, :], psb[:, 0:1024], AF.Copy, ['psb'], ['h2tok'])
        P.fence()
        A.release(mL)

        pA = A.f32(NLOC)
        mk = A.f32(NLOC)
        pC = A.f32(CTX)
        mkc = A.f32(CTX)
        P.dma(pA, d_pA, [], ['pA'])
        P.dma(pC[0:16, :], d_pC, [], ['pC'])
        P.dma(gmT[0:16, :], d_gm, [], ['gmT'])
        lo, hi, mid, tot, gt, dd, Kv = (ts[:, 0:2], ts[:, 2:4], ts[:, 4:6], ts[:, 6:8], ts[:, 8:10], ts[:, 10:12],
                                        ts[:, 12:14])
        P.op('dve', lambda e: e.memset(ts, 0.0), [], ['ts'])
        P.op('dve', lambda e: e.memset(hi, 1.0), ['ts'], ['ts'])
        P.op('dve', lambda e: e.memset(mid, 0.5), ['ts'], ['ts'])
        P.op('dve', lambda e: e.memset(ts[:, 12:13], NLOC - 0.5), ['ts'], ['ts'])
        P.op('dve', lambda e: e.memset(ts[:, 13:14], 2 * CTX // NEXP - 0.5), ['ts'], ['ts'])
        for it in range(NBIS):
            P.op('dve', lambda e: e.tensor_scalar(out=mk, in0=pA, scalar1=ts[:, 4:5], scalar2=None, op0=ALU.is_gt),
                 ['pA', 'ts'], ['mk'])
            P.op('dve', lambda e: e.reduce_sum(out=ts[:, 14:15], in_=mk, axis=mybir.AxisListType.X), ['mk'], ['cc'])
            P.op('dve', lambda e: e.tensor_scalar(out=mkc[0:16, :], in0=pC[0:16, :], scalar1=ts[0:16, 5:6], scalar2=None,
                                                  op0=ALU.is_gt), ['pC', 'ts'], ['mkc'])
            P.op('dve', lambda e: e.reduce_sum(out=ts[0:16, 7:8], in_=mkc[0:16, :], axis=mybir.AxisListType.X),
                 ['mkc', 'ts'], ['ts'])
            P.mm(ps[6][:, 0:1], G, ts[:, 14:15], True, True, ['cc', 'consts'], ['ps6'])
            P.op('dve', lambda e: e.tensor_copy(out=ts[:, 6:7], in_=ps[6][:, 0:1]), ['ps6', 'ts'], ['ts'])
            P.op('dve', lambda e: e.tensor_tensor(out=gt, in0=tot, in1=Kv, op=ALU.is_gt), ['ts'], ['ts'])
            P.op('dve', lambda e: e.tensor_tensor(out=dd, in0=mid, in1=lo, op=ALU.subtract), ['ts'], ['ts'])
            P.op('dve', lambda e: e.tensor_tensor(out=dd, in0=dd, in1=gt, op=ALU.mult), ['ts'], ['ts'])
            P.op('dve', lambda e: e.tensor_tensor(out=lo, in0=lo, in1=dd, op=ALU.add), ['ts'], ['ts'])
            P.op('dve', lambda e: e.tensor_tensor(out=dd, in0=hi, in1=mid, op=ALU.subtract), ['ts'], ['ts'])
            P.op('dve', lambda e: e.tensor_tensor(out=dd, in0=dd, in1=gt, op=ALU.mult), ['ts'], ['ts'])
            P.op('dve', lambda e: e.tensor_tensor(out=hi, in0=mid, in1=dd, op=ALU.add), ['ts'], ['ts'])
            P.op('dve', lambda e: e.tensor_tensor(out=dd, in0=lo, in1=hi, op=ALU.add), ['ts'], ['ts'])
            P.op('dve', lambda e: e.tensor_single_scalar(out=mid, in_=dd, scalar=0.5, op=ALU.mult), ['ts'], ['ts'])
        P.op('dve', lambda e: e.tensor_scalar(out=mk[0:16, 0:NLOC], in0=gmT[0:16, CTX:NL], scalar1=ts[0:16, 2:3],
                                              scalar2=None, op0=ALU.is_gt), ['gmT', 'ts'], ['mk'])
        P.op('dve', lambda e: e.tensor_tensor(out=gmT[0:16, CTX:NL], in0=gmT[0:16, CTX:NL], in1=mk[0:16, 0:NLOC],
                                              op=ALU.mult), ['mk', 'gmT'], ['gmT'])
        P.op('dve', lambda e: e.tensor_scalar(out=mkc[0:16, :], in0=gmT[0:16, 0:CTX], scalar1=ts[0:16, 3:4],
                                              scalar2=None, op0=ALU.is_gt), ['gmT', 'ts'], ['mkc'])
        P.op('dve', lambda e: e.tensor_tensor(out=gmT[0:16, 0:CTX], in0=gmT[0:16, 0:CTX], in1=mkc[0:16, :],
                                              op=ALU.mult), ['mkc', 'gmT'], ['gmT'])
        mrow = A.f32(NL)
        orow = A.f32(NL)
        P.op('dve', lambda e: e.tensor_copy(out=mrow[0:16, CTX:NL], in_=mk[0:16, 0:NLOC]), ['mk'], ['mrow'])
        P.op('dve', lambda e: e.tensor_copy(out=mrow[0:16, 0:CTX], in_=mkc[0:16, :]), ['mkc'], ['mrow'])
        P.op('dve', lambda e: e.memset(orow[0:16, :], 1.0), [], ['orow'])
        P.op('dve', lambda e: e.tensor_tensor_scan(out=ssel[0:16, :], data0=orow[0:16, :], data1=mrow[0:16, :],
                                                   initial=0.0, op0=ALU.mult, op1=ALU.add), ['orow', 'mrow'], ['ssel'])
        P.op('dve', lambda e: e.tensor_tensor(out=ssel[0:16, :], in0=ssel[0:16, :], in1=mrow[0:16, :], op=ALU.mult),
             ['ssel', 'mrow'], ['ssel'])
        for tt in range(NTT):
            P.tr(ps[6][:, 0:16], ssel[0:16, tt * 128:(tt + 1) * 128], oh[0:16, :], ['ssel', 'consts'], ['ps6'])
            P.op('dve', lambda e, tt=tt: e.tensor_copy(out=slotT[:, tt, :], in_=ps[6][:, 0:16]), ['ps6'], ['slotT'])
        P.fence()
        A.release(mL)

        CS = 384
        NST = CS // 128
        QF = 256
        NRING = 3
        wgu = [dict(g=A.bf(8, QF), u=A.bf(8, QF)) for _ in range(NRING)]
        wd = A.bf(8, 1024)
        SS = [A.bf(512) for _ in range(4)]
        SS2 = [A.bf(512) for _ in range(NST)]
        xy = A.bf(8 * CS)
        xsT = xy.rearrange("p (a b) -> p a b", a=8)
        ye = xy.rearrange("p (a b) -> p a b", a=NST)
        hid = A.bf(8, CS)
        sg = [A.bf(CS), A.bf(CS)]
        gmb = A.f32(512)
        gme = A.f32(512)
        sse = A.f32(512)

        def load_gu(ex, q, slot):
            wb, tag = wgu[slot], 'wgu%d' % slot
            for nm, dsrc in (('g', d_wg), ('u', d_wu)):
                P.dma(wb[nm], dsrc[ex].rearrange("(k p) f -> p k f", p=128)[:, :, q * QF:(q + 1) * QF], [], [tag],
                      eng='pool')

        def load_d(ex):
            for hh in range(2):
                P.dma(wd[:, :, hh * 512:(hh + 1) * 512],
                      d_wd[ex].rearrange("(k p) f -> p k f", p=128)[:, :, hh * 512:(hh + 1) * 512], [], ['wd'], eng='pool')
        usl = [0]
        for q0_ in range(NRING):
            load_gu(0, q0_, q0_)
        for ex in range(NEXP):
            si = 0
            for dh in range(2):
                for tt in range(NTT):
                    S_, Sk = SS[si % 4], 'SS%d' % (si % 4)
                    si += 1
                    P.op('dve', lambda e, S_=S_, tt=tt, ex=ex: e.tensor_scalar(
                        out=S_[:, 0:CS], in0=iotaf[:, 0:CS], scalar1=slotT[:, tt, ex:ex + 1], scalar2=None,
                        op0=ALU.is_equal),
                        ['consts', 'slotT'], [Sk])
                    for i in range(4):
                        dci = dh * 4 + i
                        P.mm(ps[i][:, 0:CS], h2tok[:, tt, dci * 128:(dci + 1) * 128], S_[:, 0:CS], tt == 0, tt == NTT - 1,
                             ['h2tok', Sk], ['ps%d' % i])
                for i in range(4):
                    P.act(xsT[:, dh * 4 + i, :], ps[i][:, 0:CS], AF.Copy, ['ps%d' % i], ['xsye'])
            if ex == 0:
                load_d(0)
            for q in range(4):
                slot = (ex * 4 + q) % NRING
                wb, tag = wgu[slot], 'wgu%d' % slot
                for f2 in range(2):
                    f = q * 2 + f2
                    pg, pu = f % 2, 2 + f % 2
                    for k in range(8):
                        P.mm(ps[pg][:, 0:CS], wb['g'][:, k, f2 * 128:(f2 + 1) * 128], xsT[:, k, :], k == 0, k == 7,
                             [tag, 'xsye'], ['ps%d' % pg])
                    for k in range(8):
                        P.mm(ps[pu][:, 0:CS], wb['u'][:, k, f2 * 128:(f2 + 1) * 128], xsT[:, k, :], k == 0, k == 7,
                             [tag, 'xsye'], ['ps%d' % pu])
                    s_ = sg[f % 2]
                    P.act(s_, ps[pg][:, 0:CS], AF.Silu, ['ps%d' % pg], ['sg%d' % (f % 2)])
                    P.op('dve', lambda e, s_=s_, f=f, pu=pu: e.tensor_tensor(out=hid[:, f, :], in0=s_, in1=ps[pu][:, 0:CS],
                                                                          op=ALU.mult),
                         ['sg%d' % (f % 2), 'ps%d' % pu], ['hid'])
                nq = ex * 4 + q + NRING
                if nq < NEXP * 4:
                    load_gu(nq // 4, nq % 4, slot)
            def prep_scatter(bi_, ex=ex):
                q0_, n_, _ = lblocks[bi_]
                gi_ = ex * len(lblocks) + bi_
                par = gi_ % 2
                tiles_ = SS if par == 0 else SS2
                P.op('dve', lambda e: e.tensor_scalar(out=sse[0:16, 0:n_], in0=ssel[0:16, q0_:q0_ + n_],
                                                      scalar1=oh[0:16, ex:ex + 1], scalar2=None, op0=ALU.mult),
                     ['ssel', 'consts'], ['sse'])
                P.op('dve', lambda e: e.tensor_scalar(out=gme[0:16, 0:n_], in0=gmT[0:16, q0_:q0_ + n_],
                                                      scalar1=oh[0:16, ex:ex + 1], scalar2=None, op0=ALU.mult),
                     ['gmT', 'consts'], ['gme'])
                P.mm(ps[5][:, 0:n_], ones16[0:16, :], sse[0:16, 0:n_], True, True, ['sse', 'consts'], ['ps5'])
                P.mm(ps[6][:, 0:n_], ones16[0:16, :], gme[0:16, 0:n_], True, True, ['gme', 'consts'], ['ps6'])
                P.act(gmb[:, 0:n_], ps[6][:, 0:n_], AF.Copy, ['ps6'], ['gmb'])
                for st_ in range(NST):
                    P.op('dve', lambda e, st_=st_: e.scalar_tensor_tensor(
                        out=tiles_[st_][:, 0:n_], in0=ps[5][:, 0:n_], scalar=iotap[:, st_:st_ + 1], in1=gmb[:, 0:n_],
                        op0=ALU.is_equal, op1=ALU.mult), ['ps5', 'gmb', 'consts'],
                        ['SS%d' % st_ if par == 0 else 'SSb%d' % st_])
            prep_scatter(0)
            for st_ in range(NST):
                for hh in range(2):
                    for f in range(8):
                        P.mm(ps[4][:, 0:512], hid[:, f, st_ * 128:(st_ + 1) * 128], wd[:, f, hh * 512:(hh + 1) * 512],
                             f == 0, f == 7, ['hid', 'wd'], ['ps4'])
                    P.act(ye[:, st_, hh * 512:(hh + 1) * 512], ps[4][:, 0:512], AF.Copy, ['ps4'], ['xsye'])
            if ex + 1 < NEXP:
                load_d(ex + 1)
            for bi_, (q0, n, w) in enumerate(lblocks):
                gi_ = ex * len(lblocks) + bi_
                par = gi_ % 2
                tiles_ = SS if par == 0 else SS2
                if bi_ + 1 < len(lblocks):
                    prep_scatter(bi_ + 1)
                for j in range(8):
                    pi = j % 4
                    for st_ in range(NST):
                        P.mm(ps[pi][:, 0:n], ye[:, st_, j * 128:(j + 1) * 128], tiles_[st_][:, 0:n], st_ == 0, st_ == NST - 1,
                             ['xsye', 'SS%d' % st_ if par == 0 else 'SSb%d' % st_], ['ps%d' % pi])
                    if ex == 0:
                        P.op('dve', lambda e, j=j, pi=pi, n=n, q0=q0: e.tensor_copy(out=acc[:, j, q0:q0 + n],
                                                                                   in_=ps[pi][:, 0:n]),
                             ['ps%d' % pi], ['acc'])
                    else:
                        P.op('dve', lambda e, j=j, pi=pi, n=n, q0=q0: e.tensor_tensor(
                            out=acc[:, j, q0:q0 + n], in0=acc[:, j, q0:q0 + n], in1=ps[pi][:, 0:n], op=ALU.add),
                            ['ps%d' % pi, 'acc'], ['acc'])
        P.fence()
        A.release(mL)

        xr = [A.f32(8, 512)] * 2
        x2 = [A.f32(8, 512)] * 2
        on = A.f32(8, 512)
        sq2 = A.bf(8, 512)
        rstd2 = A.f32(512)
        tmp2 = [A.f32(512), A.f32(512)]
        for bi, (q0, n, w) in enumerate(lblocks):
            x, xk = xr[0], 'xr'
            y, yk = x2[0], 'x2'
            P.dma(x[:, :, 0:n], d_x1[:, :, q0:q0 + n], [], [xk])
            for j in range(8):
                P.op('dve', lambda e, j=j, n=n, w=w, q0=q0, x=x, y=y: e.scalar_tensor_tensor(
                    out=y[:, j, 0:n], in0=acc[:, j, q0:q0 + n], scalar=sv[:, 2, j:j + 1, w], in1=x[:, j, 0:n],
                    op0=ALU.mult, op1=ALU.add), [xk, 'acc', 'svt'], [yk])
            P.dma(o_x2[:, :, q0:q0 + n], y[:, :, 0:n], [yk], ['o_x2'])
            C.sumsq_rstd(y[:, :, 0:n], 8, n, D, rstd2, ones, sq2, [yk], 'fn')
            for k in range(8):
                e_ = C.ew()
                t = tmp2[k % 2]
                tk = 'fn_tmp%d' % (k % 2)
                P.op(e_, lambda e, k=k, t=t, n=n, y=y: e.tensor_tensor(out=t[:, 0:n], in0=y[:, k, 0:n], in1=rstd2[:, 0:n],
                                                                      op=ALU.mult), [yk, 'fn_rstd'], [tk])
                P.op(e_, lambda e, k=k, t=t, n=n: e.tensor_scalar(out=on[:, k, 0:n], in0=t[:, 0:n],
                                                                  scalar1=fng[:, k:k + 1], scalar2=None, op0=ALU.mult),
                     [tk, 'vecs'], ['on'])
            P.dma(o_on[:, :, q0:q0 + n], on[:, :, 0:n], ['on'], ['o_on'])
        P.finalize()
        return nc, P, A


def prep_C(inp, l, resAB):
    G = (np.arange(128)[:, None] % 16 == np.arange(128)[None, :] % 16).astype(np.float32)
    cvec = np.ascontiguousarray(np.stack([_fmv(inp['c'][0]), _fmv(inp['c_ctx'])], axis=-1))
    vecs = np.zeros((128, 16), np.float32)
    vecs[:, 0:8] = _fmv(inp['norm2_g'][l])
    vecs[:, 8:16] = _fmv(inp['final_norm_g'])
    pA = np.ascontiguousarray(np.stack([r['probs'][CTX:].T for r in resAB], axis=0).reshape(128, NLOC))
    pC = np.ascontiguousarray(resAB[0]['probs'][:CTX].T)
    iotaf = np.ascontiguousarray(np.broadcast_to(np.arange(1, 513, dtype=np.float32)[None, :], (128, 512)))
    iotap = (np.arange(128, dtype=np.float32)[:, None] + 1 + 128 * np.arange(4, dtype=np.float32)[None, :]).astype(np.float32)
    common = dict(ident=np.eye(128, dtype=np.float32), iotaf=iotaf, iotap=iotap, cvec=cvec, wmod=_fm(np.ascontiguousarray(inp['w_mod'][l][:, 3072:6144])),
                  bmod=_fmv(inp['b_mod'][l][3072:6144]), vecs=vecs, G=G, oh16=np.eye(16, dtype=np.float32),
                  probsA=pA, probsC=pC, wg=inp['w_gate'][l], wu=inp['w_up'][l], wd=inp['w_down'][l])
    maps = []
    for c in range(NCORES):
        m = dict(common)
        m['x1T'] = resAB[c]['x1T']
        m['gmT'] = np.ascontiguousarray(resAB[c]['probs'].T)
        maps.append(m)
    return maps


def kernel(**inp):
    inp = {k: np.asarray(v) for k, v in inp.items()}
    ncAB = build_AB()[0]
    ncC = build_C2()[0]
    xl, xc = inp['x'][0], inp['ctx'][0]
    out = None
    for l in range(2):
        resAB = run_bass_kernel_spmd(ncAB, prep_AB(inp, l, xl, xc), core_ids=list(range(NCORES))).results
        resC = run_bass_kernel_spmd(ncC, prep_C(inp, l, resAB), core_ids=list(range(NCORES))).results
        xl = np.concatenate([_unfm(r['x2T'][:, :, CTX:]) for r in resC], axis=0)
        xc = _unfm(resC[0]['x2T'][:, :, :CTX])
        out = np.concatenate([_unfm(r['outN'][:, :, CTX:]) for r in resC], axis=0)
    return np.ascontiguousarray(out[None].astype(np.float32))
```
